# Optimizing a Trainium2 kernel written in Bass

```python
import math
import jax, jax.numpy as jnp
from jax import lax
import numpy as np


D_MODEL = 1024
BATCH = 4
SEQ = 4096
DEPTH = 4

HEAD_DIM = 64
ROPE_DIM = HEAD_DIM // 4
ROPE_THETA = 500000.0
Q_BLOCK = 128
FOX_HEADS = D_MODEL // (2 * HEAD_DIM)
NSA_HEADS = D_MODEL // (2 * HEAD_DIM)
NSA_KV_GROUPS = 2
NSA_HPG = NSA_HEADS // NSA_KV_GROUPS
CMP_BLOCK = 32
CMP_STRIDE = 16
CMP_HIDDEN = 2 * HEAD_DIM
SLC_BLOCK = 64
SLC_TOPK = 16
WINDOW = 512
DIFF_HEADS = D_MODEL // (2 * HEAD_DIM)
MEM_TOKENS = 256
MEM_HEADS = 4
D_FF = 256 * math.ceil(8 * D_MODEL / 3 / 256)
N_EVEN = (DEPTH + 1) // 2
N_ODD = DEPTH // 2
RMS_EPS = 1e-6

FOX_W = FOX_HEADS * HEAD_DIM
NSA_W = NSA_HEADS * HEAD_DIM
NSA_KV_W = NSA_KV_GROUPS * HEAD_DIM
EVEN_SPLITS = (FOX_W, FOX_W, FOX_W, FOX_HEADS, NSA_W) + (NSA_KV_W,) * 6 + (3 * NSA_HEADS,)
EVEN_IN = sum(EVEN_SPLITS)
MIX_W = FOX_W + NSA_W
DIFF_QK_W = DIFF_HEADS * 2 * HEAD_DIM
DIFF_V_W = DIFF_HEADS * 2 * HEAD_DIM
ODD_IN = 2 * DIFF_QK_W + DIFF_V_W
MEM_W = MEM_HEADS * HEAD_DIM

kernel_name = 'fox_nsa_diff_hybrid_trunk'


def rms_norm(x, g):
    xf = x.astype(jnp.float32)
    y = xf * lax.rsqrt(jnp.mean(xf * xf, axis=-1, keepdims=True) + RMS_EPS)
    return (y * g.astype(jnp.float32)).astype(x.dtype)


def rope_tables(positions):
    inv = ROPE_THETA ** (-jnp.arange(0, ROPE_DIM, 2, dtype=jnp.float32) / ROPE_DIM)
    ang = positions.astype(jnp.float32)[..., None] * inv
    return jnp.cos(ang), jnp.sin(ang)


def apply_partial_rope(x, cos, sin):
    half = ROPE_DIM // 2
    x1, x2, rest = x[..., :half], x[..., half:ROPE_DIM], x[..., ROPE_DIM:]
    c = cos[:, :, None, :].astype(x.dtype)
    s = sin[:, :, None, :].astype(x.dtype)
    return jnp.concatenate([x1 * c - x2 * s, x2 * c + x1 * s, rest], axis=-1)


def masked_softmax(logits, mask):
    z = jnp.where(mask, logits.astype(jnp.float32), -jnp.inf)
    m = jnp.max(z, axis=-1, keepdims=True)
    m = jnp.where(jnp.isfinite(m), m, 0.0)
    p = jnp.exp(z - m)
    return p / jnp.maximum(jnp.sum(p, axis=-1, keepdims=True), 1e-30)


def stack_blocks(out, B, T):
    return jnp.moveaxis(out, 0, 1).reshape((B, T) + out.shape[3:])


def fox_attention(q, k, v, log_f):
    B, T, H, dh = q.shape
    c = jnp.cumsum(log_f, axis=1)
    c_k = jnp.transpose(c, (0, 2, 1))[:, :, None, :]
    kpos = jnp.arange(T)
    scale = dh ** -0.5

    def block(i):
        q0 = i * Q_BLOCK
        qb = lax.dynamic_slice_in_dim(q, q0, Q_BLOCK, 1)
        cq = jnp.transpose(lax.dynamic_slice_in_dim(c, q0, Q_BLOCK, 1), (0, 2, 1))[..., None]
        s = jnp.einsum('bqhd,bkhd->bhqk', qb, k).astype(jnp.float32) * scale + (cq - c_k)
        qpos = q0 + jnp.arange(Q_BLOCK)
        p = masked_softmax(s, kpos[None, :] <= qpos[:, None])
        return jnp.einsum('bhqk,bkhd->bqhd', p.astype(v.dtype), v)

    return stack_blocks(lax.map(block, jnp.arange(T // Q_BLOCK)), B, T)


def compress(x, pos, w1, w2):
    B, T, G, dh = x.shape
    nc = (T - CMP_BLOCK) // CMP_STRIDE + 1
    idx = np.arange(nc)[:, None] * CMP_STRIDE + np.arange(CMP_BLOCK)[None, :]
    blocks = x[:, idx] + pos[None, None, :, None, :].astype(x.dtype)
    flat = jnp.moveaxis(blocks, 3, 2).reshape(B, nc, G, CMP_BLOCK * dh)
    return jax.nn.gelu(flat @ w1) @ w2


def nsa_attention(q, kc, vc, ks, vs, kw, vw, gates):
    B, T, H, dh = q.shape
    G = NSA_KV_GROUPS
    nc = kc.shape[1]
    ns = T // SLC_BLOCK
    n_sel = min(SLC_TOPK, ns)
    scale = dh ** -0.5
    cmp_start = np.arange(nc) * CMP_STRIDE
    slc_start = np.arange(ns) * SLC_BLOCK
    cmp_end = jnp.asarray(cmp_start + CMP_BLOCK - 1)
    ov = np.clip(np.minimum(cmp_start[:, None] + CMP_BLOCK, slc_start[None, :] + SLC_BLOCK)
                 - np.maximum(cmp_start[:, None], slc_start[None, :]), 0, None) / CMP_BLOCK
    overlap = jnp.asarray(ov, dtype=jnp.float32)
    blk = jnp.arange(ns)
    k_blocks = jnp.transpose(ks.reshape(B, ns, SLC_BLOCK, G, dh), (0, 3, 1, 2, 4))
    v_blocks = jnp.transpose(vs.reshape(B, ns, SLC_BLOCK, G, dh), (0, 3, 1, 2, 4))
    kw_pad = jnp.pad(kw, ((0, 0), (WINDOW, 0), (0, 0), (0, 0)))
    vw_pad = jnp.pad(vw, ((0, 0), (WINDOW, 0), (0, 0), (0, 0)))
    bi = jnp.arange(B)[:, None, None, None]
    gi = jnp.arange(G)[None, :, None, None]

    def block(i):
        q0 = i * Q_BLOCK
        qpos = q0 + jnp.arange(Q_BLOCK)
        qg = lax.dynamic_slice_in_dim(q, q0, Q_BLOCK, 1).reshape(B, Q_BLOCK, G, NSA_HPG, dh)
        gb = lax.dynamic_slice_in_dim(gates, q0, Q_BLOCK, 1).reshape(B, Q_BLOCK, G, NSA_HPG, 3)
        s_c = jnp.einsum('bqgnd,bcgd->bgnqc', qg, kc).astype(jnp.float32) * scale
        p_c = masked_softmax(s_c, cmp_end[None, :] <= qpos[:, None])
        o_c = jnp.einsum('bgnqc,bcgd->bqgnd', p_c.astype(vc.dtype), vc)
        imp = jnp.einsum('bgnqc,cs->bgqs', p_c, overlap)
        cur = (qpos // SLC_BLOCK)[:, None]
        valid = blk[None, :] * SLC_BLOCK <= qpos[:, None]
        forced = (blk[None, :] == 0) | (blk[None, :] == cur) | (blk[None, :] == cur - 1)
        score = jnp.where(valid, jnp.where(forced, jnp.inf, imp), -jnp.inf)
        _, sel = lax.top_k(score, n_sel)
        k_sel = k_blocks[bi, gi, sel]
        v_sel = v_blocks[bi, gi, sel].reshape(B, G, Q_BLOCK, n_sel * SLC_BLOCK, dh)
        tok = sel[..., None] * SLC_BLOCK + jnp.arange(SLC_BLOCK)
        smask = (tok <= qpos[None, None, :, None, None]).reshape(B, G, 1, Q_BLOCK, n_sel * SLC_BLOCK)
        s_s = jnp.einsum('bqgnd,bgqkld->bgnqkl', qg, k_sel).astype(jnp.float32) * scale
        p_s = masked_softmax(s_s.reshape(B, G, NSA_HPG, Q_BLOCK, n_sel * SLC_BLOCK), smask)
        o_s = jnp.einsum('bgnqm,bgqmd->bqgnd', p_s.astype(v_sel.dtype), v_sel)
        kwb = lax.dynamic_slice_in_dim(kw_pad, q0, WINDOW + Q_BLOCK, 1)
        vwb = lax.dynamic_slice_in_dim(vw_pad, q0, WINDOW + Q_BLOCK, 1)
        kpos = q0 - WINDOW + jnp.arange(WINDOW + Q_BLOCK)
        wmask = ((kpos[None, :] <= qpos[:, None]) & (kpos[None, :] > qpos[:, None] - WINDOW)
                 & (kpos[None, :] >= 0))
        s_w = jnp.einsum('bqgnd,bkgd->bgnqk', qg, kwb).astype(jnp.float32) * scale
        p_w = masked_softmax(s_w, wmask)
        o_w = jnp.einsum('bgnqk,bkgd->bqgnd', p_w.astype(vwb.dtype), vwb)
        o = gb[..., 0:1] * o_c + gb[..., 1:2] * o_s + gb[..., 2:3] * o_w
        return o.reshape(B, Q_BLOCK, H, dh)

    return stack_blocks(lax.map(block, jnp.arange(T // Q_BLOCK)), B, T)


def diff_attention(q1, q2, k1, k2, v, lam):
    B, T, H, dh = q1.shape
    kpos = jnp.arange(T)
    scale = dh ** -0.5

    def block(i):
        q0 = i * Q_BLOCK
        qpos = q0 + jnp.arange(Q_BLOCK)
        mask = kpos[None, :] <= qpos[:, None]
        qb1 = lax.dynamic_slice_in_dim(q1, q0, Q_BLOCK, 1)
        qb2 = lax.dynamic_slice_in_dim(q2, q0, Q_BLOCK, 1)
        a1 = masked_softmax(jnp.einsum('bqhd,bkhd->bhqk', qb1, k1).astype(jnp.float32) * scale, mask)
        a2 = masked_softmax(jnp.einsum('bqhd,bkhd->bhqk', qb2, k2).astype(jnp.float32) * scale, mask)
        p = a1 - lam * a2
        return jnp.einsum('bhqk,bkhd->bqhd', p.astype(v.dtype), v)

    return stack_blocks(lax.map(block, jnp.arange(T // Q_BLOCK)), B, T)


def even_mixer(h, cos, sin, w_in, f_bias, cpk, c1k, c2k, cpv, c1v, c2v, w_out):
    B, T, _ = h.shape
    G = NSA_KV_GROUPS
    offsets = [int(o) for o in np.cumsum(EVEN_SPLITS)[:-1]]
    fq, fk, fv, fl, nq, kc, vc, ksl, vsl, kwn, vwn, gl = jnp.split(h @ w_in, offsets, axis=-1)
    heads = lambda t, n: t.reshape(B, T, n, HEAD_DIM)
    rope = lambda t: apply_partial_rope(t, cos, sin)
    log_f = jax.nn.log_sigmoid(fl.astype(jnp.float32) + f_bias.astype(jnp.float32))
    o_fox = fox_attention(heads(fq, FOX_HEADS), heads(fk, FOX_HEADS), heads(fv, FOX_HEADS), log_f)
    o_nsa = nsa_attention(
        rope(heads(nq, NSA_HEADS)),
        compress(rope(heads(kc, G)), cpk, c1k, c2k),
        compress(heads(vc, G), cpv, c1v, c2v),
        rope(heads(ksl, G)), heads(vsl, G),
        rope(heads(kwn, G)), heads(vwn, G),
        jax.nn.sigmoid(gl).reshape(B, T, NSA_HEADS, 3))
    o = jnp.concatenate([o_fox.reshape(B, T, FOX_W), o_nsa.reshape(B, T, NSA_W)], axis=-1)
    return o @ w_out


def diff_mixer(h, cos, sin, w_in, lam_p, subln_g, w_out, layer):
    B, T, _ = h.shape
    q, k, v = jnp.split(h @ w_in, [DIFF_QK_W, 2 * DIFF_QK_W], axis=-1)
    q = apply_partial_rope(q.reshape(B, T, 2 * DIFF_HEADS, HEAD_DIM), cos, sin).reshape(B, T, DIFF_HEADS, 2, HEAD_DIM)
    k = apply_partial_rope(k.reshape(B, T, 2 * DIFF_HEADS, HEAD_DIM), cos, sin).reshape(B, T, DIFF_HEADS, 2, HEAD_DIM)
    v = v.reshape(B, T, DIFF_HEADS, 2 * HEAD_DIM)
    lam_init = 0.8 - 0.6 * math.exp(-0.3 * layer)
    lp = lam_p.astype(jnp.float32)
    lam = jnp.exp(jnp.sum(lp[0] * lp[1])) - jnp.exp(jnp.sum(lp[2] * lp[3])) + lam_init
    o = diff_attention(q[..., 0, :], q[..., 1, :], k[..., 0, :], k[..., 1, :], v, lam)
    o = rms_norm(o, subln_g) * (1.0 - lam_init)
    return o.reshape(B, T, DIFF_V_W) @ w_out


def memory_cross_attention(h, mem_n, wq, wk, wv, wo):
    B, T, _ = h.shape
    M = mem_n.shape[1]
    q = (h @ wq).reshape(B, T, MEM_HEADS, HEAD_DIM)
    k = (mem_n @ wk).reshape(B, M, MEM_HEADS, HEAD_DIM)
    v = (mem_n @ wv).reshape(B, M, MEM_HEADS, HEAD_DIM)
    s = jnp.einsum('bqhd,bmhd->bhqm', q, k).astype(jnp.float32) * HEAD_DIM ** -0.5
    p = jax.nn.softmax(s, axis=-1)
    o = jnp.einsum('bhqm,bmhd->bqhd', p.astype(v.dtype), v).reshape(B, T, MEM_W)
    return o @ wo


def swiglu(h, wg, wu, wd):
    return (jax.nn.silu(h @ wg) * (h @ wu)) @ wd


def setup_inputs(seed: int = 0) -> dict:
    key = jax.random.key(seed)
    ks = jax.random.split(key, 32)

    def nrm(k, shape, fan_in):
        return jax.random.normal(k, shape, jnp.float32) * (fan_in ** -0.5)

    def gain(k, shape):
        return 1.0 + 0.02 * jax.random.normal(k, shape, jnp.float32)

    D = D_MODEL
    flat_cmp = CMP_BLOCK * HEAD_DIM
    return {
        'x': jax.random.normal(ks[0], (BATCH, SEQ, D), jnp.float32),
        'mem': jax.random.normal(ks[1], (BATCH, MEM_TOKENS, D), jnp.float32),
        'positions': jnp.broadcast_to(jnp.arange(SEQ, dtype=jnp.int32), (BATCH, SEQ)),
        'sandwich_g': gain(ks[2], (DEPTH, 6, D)),
        'mem_norm_g': gain(ks[3], (DEPTH, D)),
        'ev_w_in': nrm(ks[4], (N_EVEN, D, EVEN_IN), D),
        'ev_fox_fbias': 4.0 + 0.5 * jax.random.normal(ks[5], (N_EVEN, FOX_HEADS), jnp.float32),
        'ev_cmp_pos_k': 0.02 * jax.random.normal(ks[6], (N_EVEN, CMP_BLOCK, HEAD_DIM), jnp.float32),
        'ev_cmp_w1_k': nrm(ks[7], (N_EVEN, flat_cmp, CMP_HIDDEN), flat_cmp),
        'ev_cmp_w2_k': nrm(ks[8], (N_EVEN, CMP_HIDDEN, HEAD_DIM), CMP_HIDDEN),
        'ev_cmp_pos_v': 0.02 * jax.random.normal(ks[9], (N_EVEN, CMP_BLOCK, HEAD_DIM), jnp.float32),
        'ev_cmp_w1_v': nrm(ks[10], (N_EVEN, flat_cmp, CMP_HIDDEN), flat_cmp),
        'ev_cmp_w2_v': nrm(ks[11], (N_EVEN, CMP_HIDDEN, HEAD_DIM), CMP_HIDDEN),
        'ev_w_out': nrm(ks[12], (N_EVEN, MIX_W, D), MIX_W),
        'od_w_in': nrm(ks[13], (N_ODD, D, ODD_IN), D),
        'od_lambda': 0.1 * jax.random.normal(ks[14], (N_ODD, 4, HEAD_DIM), jnp.float32),
        'od_subln_g': gain(ks[15], (N_ODD, 2 * HEAD_DIM)),
        'od_w_out': nrm(ks[16], (N_ODD, DIFF_V_W, D), DIFF_V_W),
        'ca_wq': nrm(ks[17], (DEPTH, D, MEM_W), D),
        'ca_wk': nrm(ks[18], (DEPTH, D, MEM_W), D),
        'ca_wv': nrm(ks[19], (DEPTH, D, MEM_W), D),
        'ca_wo': nrm(ks[20], (DEPTH, MEM_W, D), MEM_W),
        'ffn_wg': nrm(ks[21], (DEPTH, D, D_FF), D),
        'ffn_wu': nrm(ks[22], (DEPTH, D, D_FF), D),
        'ffn_wd': nrm(ks[23], (DEPTH, D_FF, D), D_FF),
    }


def reference(x, mem, positions, sandwich_g, mem_norm_g, ev_w_in, ev_fox_fbias,
              ev_cmp_pos_k, ev_cmp_w1_k, ev_cmp_w2_k, ev_cmp_pos_v, ev_cmp_w1_v, ev_cmp_w2_v,
              ev_w_out, od_w_in, od_lambda, od_subln_g, od_w_out,
              ca_wq, ca_wk, ca_wv, ca_wo, ffn_wg, ffn_wu, ffn_wd):
    cos, sin = rope_tables(positions)
    for layer in range(DEPTH):
        g = sandwich_g[layer]
        h = rms_norm(x, g[0])
        if layer % 2 == 0:
            e = layer // 2
            y = even_mixer(h, cos, sin, ev_w_in[e], ev_fox_fbias[e],
                           ev_cmp_pos_k[e], ev_cmp_w1_k[e], ev_cmp_w2_k[e],
                           ev_cmp_pos_v[e], ev_cmp_w1_v[e], ev_cmp_w2_v[e], ev_w_out[e])
        else:
            o = layer // 2
            y = diff_mixer(h, cos, sin, od_w_in[o], od_lambda[o], od_subln_g[o], od_w_out[o], layer)
        x = x + rms_norm(y, g[1])
        mem_n = rms_norm(mem, mem_norm_g[layer])
        y = memory_cross_attention(rms_norm(x, g[2]), mem_n, ca_wq[layer], ca_wk[layer], ca_wv[layer], ca_wo[layer])
        x = x + rms_norm(y, g[3])
        y = swiglu(rms_norm(x, g[4]), ffn_wg[layer], ffn_wu[layer], ffn_wd[layer])
        x = x + rms_norm(y, g[5])
    return x
```

```python
from contextlib import ExitStack
import numpy as np
import concourse.bass as bass
import concourse.mybir as mybir

F32 = mybir.dt.float32
BF16 = mybir.dt.bfloat16
I32 = mybir.dt.int32
AF = mybir.ActivationFunctionType
ALU = mybir.AluOpType
AX = mybir.AxisListType


class Buf:
    _n = 0

    def __init__(self, t, name):
        self.t = t
        self.name = name
        self.regions = {}
        self.whole = [None, {}]

    def __getitem__(self, idx):
        return self.t[idx]


class Prog:
    ENG = ["pe", "dve", "act", "pool", "sp"]

    def __init__(self, nc, n_dma_sems=10):
        self.nc = nc
        self.es = ExitStack()
        self.eng = {"pe": nc.tensor, "dve": nc.vector, "act": nc.scalar, "pool": nc.gpsimd, "sp": nc.sync}
        self.sem = {e: self.es.enter_context(nc.semaphore("s_" + e)) for e in self.ENG}
        self.cnt = {e: 0 for e in self.ENG}
        self.sem["cc"] = self.es.enter_context(nc.semaphore("s_cc"))
        self.cnt["cc"] = 0
        self.waited = {}
        self.dsem = {}
        self.dval = {}
        self.dnext = {}
        for q in ["sp", "act", "pool"]:
            self.dsem[q] = [self.es.enter_context(nc.semaphore(f"d_{q}{i}")) for i in range(n_dma_sems)]
            self.dval[q] = [0] * n_dma_sems
            self.dnext[q] = 0
        self.dwaited = {}
        self.n_inst = 0
        self.n_wait = 0

    def sbuf(self, name, shape, dtype):
        t = self.es.enter_context(self.nc.sbuf_tensor(name, list(shape), dtype))
        return Buf(t, name)

    def psum(self, name, shape, dtype):
        t = self.es.enter_context(self.nc.psum_tensor(name, list(shape), dtype))
        return Buf(t, name)

    def close(self):
        self.es.close()

    def _states(self, buf, key):
        if key is None:
            return [buf.whole] + list(buf.regions.values())
        if key not in buf.regions:
            buf.regions[key] = [None, {}]
        return [buf.whole, buf.regions[key]]

    def _need(self, deps, tok):
        if tok is not None:
            deps.add(tok)

    def _collect(self, reads, writes):
        deps = set()
        for (b, k) in reads:
            for st in self._states(b, k):
                self._need(deps, st[0])
        for (b, k) in writes:
            for st in self._states(b, k):
                self._need(deps, st[0])
                for tok in st[1].values():
                    deps.add(tok)
        return deps

    def _emit_waits(self, e, deps, skip_same=False):
        engobj = self.eng[e]
        best = {}
        for tok in deps:
            if tok[0] == "e":
                _, f, c = tok
                if f == e and skip_same:
                    continue
                key = ("e", f)
                best[key] = max(best.get(key, 0), c)
            else:
                _, q, i, v = tok
                key = ("d", q, i)
                best[key] = max(best.get(key, 0), v)
        for key, v in best.items():
            wk = (e,) + key
            if self.waited.get(wk, -1) >= v:
                continue
            self.waited[wk] = v
            if key[0] == "e":
                engobj.wait_ge(self.sem[key[1]], v)
            else:
                engobj.wait_ge(self.dsem[key[1]][key[2]], v)
            self.n_wait += 1

    def _record(self, tok, reads, writes):
        for (b, k) in reads:
            if k is None:
                b.whole[1][tok[1] if tok[0] == "e" else ("d",) + tok[1:3]] = tok
            else:
                st = self._states(b, k)[1]
                st[1][tok[1] if tok[0] == "e" else ("d",) + tok[1:3]] = tok
        for (b, k) in writes:
            if k is None:
                b.regions.clear()
                b.whole[0] = tok
                b.whole[1] = {}
            else:
                st = self._states(b, k)[1]
                st[0] = tok
                st[1] = {}

    def alias(self, ap, name):
        return Buf(ap, name)

    def barrier(self):
        alld = set()
        for e in self.ENG + ["cc"]:
            if self.cnt[e] > 0:
                alld.add(("e", e, self.cnt[e]))
        for q in self.dsem:
            for i, v in enumerate(self.dval[q]):
                if v > 0:
                    alld.add(("d", q, i, v))
        for e in self.ENG:
            self._emit_waits(e, alld)

    def op(self, e, fn, reads=(), writes=(), skip_same=False, inc=True):
        deps = self._collect(reads, writes)
        if e == "pe":
            skip_same = True
        if skip_same is False and e in ("dve", "act", "pool"):
            raw = set()
            for (b, k) in reads:
                for st in self._states(b, k):
                    if st[0] is not None:
                        raw.add(st[0])
            deps = {t for t in deps if not (t[0] == "e" and t[1] == e) or t in raw}
        self._emit_waits(e, deps, skip_same=skip_same)
        inst = fn(self.eng[e])
        if inc:
            self.cnt[e] += 1
            inst.then_inc(self.sem[e], 1)
            tok = ("e", e, self.cnt[e])
        else:
            tok = ("e", e, self.cnt[e] + 1)
        self._record(tok, reads, writes)
        self.n_inst += 1
        return inst

    def dma(self, q, out_ap, in_ap, reads=(), writes=(), **kw):
        e = q
        deps = self._collect(reads, writes)
        i = self.dnext[q]
        self.dnext[q] = (i + 1) % len(self.dsem[q])
        if self.dval[q][i] > 0:
            deps.add(("d", q, i, self.dval[q][i]))
        self._emit_waits(e, deps)
        self.dval[q][i] += 16
        inst = self.eng[e].dma_start(out=out_ap, in_=in_ap, **kw)
        inst.then_inc(self.dsem[q][i], 16)
        tok = ("d", q, i, self.dval[q][i])
        self._record(tok, reads, writes)
        self.n_inst += 1
        return inst

    def collective(self, kind, in_ap, out_ap, groups, reads=(), writes=()):
        deps = self._collect(reads, writes)
        self._emit_waits("pool", deps)
        self.cnt["cc"] += 1
        inst = self.nc.gpsimd.collective_compute(kind, mybir.AluOpType.bypass, replica_groups=groups, ins=[in_ap], outs=[out_ap])
        inst.then_inc(self.sem["cc"])
        tok = ("e", "cc", self.cnt["cc"])
        self._record(tok, reads, writes)
        self.n_inst += 1
        return inst

    def finish(self, out_bufs):
        deps = set()
        for b in out_bufs:
            for st in [b.whole] + list(b.regions.values()):
                if st[0] is not None:
                    deps.add(st[0])
        self._emit_waits("sp", deps)
        alld = set()
        for e in self.ENG + ["cc"]:
            if self.cnt[e] > 0:
                alld.add(("e", e, self.cnt[e]))
        for q in self.dsem:
            for i, v in enumerate(self.dval[q]):
                if v > 0:
                    alld.add(("d", q, i, v))
        self._emit_waits("sp", alld)


import math
import numpy as np
import ml_dtypes
from contextlib import ExitStack

D = 1024
T = 4096
TOK = 2048
NT = TOK // 128
TB = 1024
DFF = 2816
NFF = DFF // 128
EPS = 1e-6
MEM = 256
NEG = -30000.0


def np_bf16(a):
    return np.asarray(a, dtype=np.float32).astype(ml_dtypes.bfloat16)


class Ctx:
    pass


def make_ctx(nc, ident_ap):
    cx = Ctx()
    cx.nc = nc
    p = Prog(nc)
    cx.p = p
    cx.uid = 0
    cx.ident = p.sbuf("ident_sb", [128, 128], BF16)
    p.dma("sp", cx.ident[:], ident_ap, writes=[(cx.ident, None)])
    cx.psi = 0
    cx.outb = Buf(None, "dram_out")
    cx.epsb = p.sbuf("epsb", [128, 1], F32)
    p.op("pool", lambda e: e.memset(cx.epsb[:], EPS), writes=[(cx.epsb, None)])
    return cx


def rot(cx, n=7):
    b = cx.ps[cx.psi % n]
    cx.psi += 1
    return b


def mm_group(cx, bank_ap, bank_dep, pairs):
    p = cx.p
    n = len(pairs)
    for i, (lhsT, rhs, reads) in enumerate(pairs):
        p.op("pe", lambda e, lhsT=lhsT, rhs=rhs, i=i: e.matmul(bank_ap, lhsT=lhsT, rhs=rhs, start=(i == 0), stop=(i == n - 1)),
             reads=reads, writes=[bank_dep], inc=(i == n - 1))


def load_w_bf16(cx, dst_ap, dst_dep, w_ap, kc, ncols, stage, q="sp", cast_eng="pool"):
    p = cx.p
    assert kc * ncols <= 2048
    sv = stage[:, 0:kc * ncols].rearrange("p (c n) -> p c n", c=kc)
    p.dma(q, sv, w_ap.rearrange("(c p) n -> p c n", p=128), writes=[(stage, None)])
    p.op(cast_eng, lambda e: e.tensor_copy(out=dst_ap, in_=sv), reads=[(stage, None)], writes=[dst_dep])


def rms_ss(cx, src_ap, src_dep, ncol, stat, junk, key):
    p = cx.p
    p.op("act", lambda e: e.activation(out=junk[:, 0:ncol], in_=src_ap, func=AF.Square, accum_out=stat[:, key:key + 1]),
         reads=[src_dep], writes=[(junk, None), (stat, key)])


def rstd_from_ss(cx, stat, k0, k1, dim):
    p = cx.p
    p.op("act", lambda e: e.activation(out=stat[:, k0:k1], in_=stat[:, k0:k1], func=AF.Sqrt, scale=1.0 / dim, bias=cx.epsb[:, 0:1]),
         reads=[(stat, None), (cx.epsb, None)], writes=[(stat, None)])
    p.op("dve", lambda e: e.reciprocal(out=stat[:, k0:k1], in_=stat[:, k0:k1]), reads=[(stat, None)], writes=[(stat, None)])


def norm_transpose(cx, x, tiles, g_ap, hT, L, q="sp"):
    p = cx.p
    gB, stat, junk, hb = L["gB"], L["stat"], L["junk"], L["hb"]
    p.dma(q, gB[:], g_ap.to_broadcast([128, D]), writes=[(gB, None)])
    n = len(tiles)
    for j, t in enumerate(tiles):
        rms_ss(cx, x[:, t, :], (x, t), D, stat, junk, j)
    rstd_from_ss(cx, stat, 0, n, D)
    for j, t in enumerate(tiles):
        hbt = hb[j % 2]
        p.op("dve", lambda e, t=t, j=j, hbt=hbt: e.scalar_tensor_tensor(out=hbt[:], in0=x[:, t, :], scalar=stat[:, j:j + 1], in1=gB[:],
                                                                         op0=ALU.mult, op1=ALU.mult),
             reads=[(x, t), (stat, None), (gB, None)], writes=[(hbt, None)])
        for c in range(8):
            p.op("pe", lambda e, c=c, hbt=hbt: e.transpose(out=cx.pst[:, c * 128:(c + 1) * 128], in_=hbt[:, c * 128:(c + 1) * 128], identity=cx.ident[:]),
                 reads=[(hbt, None), (cx.ident, None)], writes=[(cx.pst, None)], inc=(c == 7))
        src = cx.pst[:].rearrange("p (c n) -> p c n", c=8)
        if j % 2 == 0:
            p.op("act", lambda e, j=j, src=src: e.activation(out=hT[:, :, j * 128:(j + 1) * 128], in_=src, func=AF.Copy),
                 reads=[(cx.pst, None)], writes=[(hT, j)])
        else:
            p.op("dve", lambda e, j=j, src=src: e.tensor_copy(out=hT[:, :, j * 128:(j + 1) * 128], in_=src),
                 reads=[(cx.pst, None)], writes=[(hT, j)])


def norm_residual(cx, x, t, banks, g_ready_gB, L):
    p = cx.p
    stat2, junk, tmp, gB = L["stat2"], L["junk"], L["tmp"], g_ready_gB
    for nb in range(2):
        rms_ss(cx, banks[nb][:], (banks[nb], None), 512, stat2, junk, nb)
    p.op("dve", lambda e: e.tensor_tensor(out=stat2[:, 2:3], in0=stat2[:, 0:1], in1=stat2[:, 1:2], op=ALU.add),
         reads=[(stat2, None)], writes=[(stat2, None)])
    rstd_from_ss(cx, stat2, 2, 3, D)
    for nb in range(2):
        p.op("dve", lambda e, nb=nb, b=banks[nb]: e.scalar_tensor_tensor(out=tmp[:, nb * 512:(nb + 1) * 512], in0=b[:], scalar=stat2[:, 2:3],
                                                                        in1=gB[:, nb * 512:(nb + 1) * 512], op0=ALU.mult, op1=ALU.mult),
             reads=[(banks[nb], None), (stat2, None), (gB, None)], writes=[(tmp, nb)])
    p.op("pool", lambda e, t=t: e.tensor_tensor(out=x[:, t, :], in0=x[:, t, :], in1=tmp[:], op=ALU.add),
         reads=[(tmp, None), (x, t)], writes=[(x, t)])


def proj_residual(cx, x, tiles, lhs_fn, kc, w, g_ap, L, q="sp"):
    p = cx.p
    gB = L["gB"]
    p.dma(q, gB[:], g_ap.to_broadcast([128, D]), writes=[(gB, None)])
    for j, t in enumerate(tiles):
        banks = [rot(cx), rot(cx)]
        for nb in range(2):
            pairs = []
            for c in range(kc):
                lhsT, ldep = lhs_fn(c, j)
                pairs.append((lhsT, w[:, c, nb * 512:(nb + 1) * 512], [ldep, (w, None)]))
            mm_group(cx, banks[nb][:], (banks[nb], None), pairs)
        norm_residual(cx, x, t, banks, gB, L)


def phase_C(cx, x, oT_ap, W, hT_next_ap, g_next_ap, oT_load=None, hT_store=None):
    p = cx.p
    with ExitStack() as es:
        cx.uid += 1
        uu = cx.uid
        def sb(name, shape, dt):
            return Buf(es.enter_context(cx.nc.sbuf_tensor(f"{name}_{uu}", list(shape), dt)), name)
        cx.ps, cx.pst = psum_set(cx, es, 7, True)
        cx.psi = 0
        L = {}
        L["gB"] = sb("gB", [128, D], F32)
        L["stat"] = sb("stat", [128, 16], F32)
        L["stat2"] = sb("stat2", [128, 4], F32)
        L["junk"] = sb("junk", [128, D], BF16)
        L["tmp"] = sb("tmp", [128, D], F32)
        L["hb"] = [sb(f"hb{i}", [128, D], BF16) for i in range(2)]
        hT = sb("hT", [128, 8, TB], BF16)
        big = sb("big", [128, NFF, TB], BF16)
        stage = [sb(f"stage{i}", [128, 2048], F32) for i in range(2)]
        wsm = sb("wsm", [128, 8, 1024], BF16)
        memT = sb("memT", [128, 8, MEM], BF16)
        kmT = sb("kmT", [128, 2, MEM], BF16)
        vm = sb("vm", [128, 2, 4, 128], BF16)
        pT = [sb(f"pTc{i}", [128, 512], BF16) for i in range(4)]
        rs = sb("rs_c", [128, 512], F32)
        wg = [sb(f"wg{i}", [128, 8, 128], BF16) for i in range(2)]
        wu = [sb(f"wu{i}", [128, 8, 128], BF16) for i in range(2)]
        wd = [sb(f"wd{i}", [128, 1024], BF16) for i in range(3)]
        gsb = [sb(f"gsb{i}", [128, 512], BF16) for i in range(2)]
        bigflat = big.t[:].rearrange("p a b -> p (a b)")
        memx = p.alias(stage[1][:].rearrange("p (t d) -> p t d", t=2), "memx")
        p.dma("sp", memx[:], W["mem"].rearrange("(t p) d -> p t d", p=128), writes=[(memx, None), (stage[1], None)])
        norm_transpose(cx, memx, [0, 1], W["mem_g"], memT, L)
        p.barrier()
        sti = 0
        for (nm, c0) in (("ca_wk", 0), ("ca_wv", 256)):
            load_w_bf16(cx, wsm[:, :, c0:c0 + 256], (wsm, None), W[nm], 8, 256, stage[sti % 2], q="pool")
            sti += 1
        for ht in range(2):
            b = rot(cx)
            mm_group(cx, b[:, 0:MEM], (b, None), [(wsm[:, c, ht * 128:(ht + 1) * 128], memT[:, c, :], [(wsm, None), (memT, None)]) for c in range(8)])
            p.op("act", lambda e, b=b, ht=ht: e.activation(out=kmT[:, ht, :], in_=b[:, 0:MEM], func=AF.Copy), reads=[(b, None)], writes=[(kmT, None)])
        p.op("pool", lambda e: e.memset(vm[:], 1.0), writes=[(vm, None)])
        for mc in range(2):
            b = rot(cx)
            mm_group(cx, b[:, 0:256], (b, None), [(memT[:, c, mc * 128:(mc + 1) * 128], wsm[:, c, 256:512], [(wsm, None), (memT, None)]) for c in range(8)])
            p.op("act", lambda e, b=b, mc=mc: e.activation(out=vm[:, mc, :, 0:64], in_=b[:, 0:256].rearrange("p (h d) -> p h d", h=4), func=AF.Copy),
                 reads=[(b, None)], writes=[(vm, None)])
        for tb in range(TOK // TB):
            tiles = list(range(tb * 8, tb * 8 + 8))
            tsl = slice(tb * TB, (tb + 1) * TB)
            oT = p.alias(bigflat[:, 0:8 * TB].rearrange("p (c n) -> p c n", c=8), "oT")
            if oT_load is None:
                p.dma("sp", oT[:], oT_ap[:, tsl].rearrange("(c p) n -> p c n", p=128), writes=[(oT, None), (big, None)])
            else:
                oT_load(oT, tb, [(oT, None), (big, None)])
            for qd in range(4):
                load_w_bf16(cx, wsm[:, :, qd * 256:(qd + 1) * 256], (wsm, None), W["w_out"][:, qd * 256:(qd + 1) * 256], 8, 256, stage[sti % 2], q="pool")
                sti += 1
            proj_residual(cx, x, tiles, lambda c, j: (oT[:, c, j * 128:(j + 1) * 128], (oT, None)), 8, wsm, W["g"][1:2, :], L)
            p.barrier()
            qcT = p.alias(bigflat[:, 0:2 * TB].rearrange("p (c n) -> p c n", c=2), "qcT")
            ocT = p.alias(bigflat[:, 2 * TB:4 * TB].rearrange("p (c n) -> p c n", c=2), "ocT")
            norm_transpose(cx, x, tiles, W["g"][2:3, :], hT, L)
            load_w_bf16(cx, wsm[:, :, 0:256], (wsm, None), W["ca_wq"], 8, 256, stage[sti % 2], q="pool")
            sti += 1
            for ht in range(2):
                for qb in range(TB // 512):
                    b = rot(cx)
                    mm_group(cx, b[:], (b, None), [(wsm[:, c, ht * 128:(ht + 1) * 128], hT[:, c, qb * 512:(qb + 1) * 512], [(wsm, None), (hT, None)]) for c in range(8)])
                    p.op("act", lambda e, b=b, ht=ht, qb=qb: e.activation(out=qcT[:, ht, qb * 512:(qb + 1) * 512], in_=b[:], func=AF.Copy),
                         reads=[(b, None)], writes=[(qcT, (ht, qb))])
            pi = 0
            for h in range(4):
                ht, r0 = h // 2, (h % 2) * 64
                for qb in range(TB // 512):
                    pts = []
                    for mc in range(2):
                        sbk = rot(cx)
                        p.op("pe", lambda e, sbk=sbk, mc=mc, ht=ht, r0=r0, qb=qb: e.matmul(sbk[:], lhsT=kmT[r0:r0 + 64, ht, mc * 128:(mc + 1) * 128],
                                                                                           rhs=qcT[r0:r0 + 64, ht, qb * 512:(qb + 1) * 512], start=True, stop=True),
                             reads=[(kmT, None), (qcT, (ht, qb))], writes=[(sbk, None)])
                        pt = pT[pi % 4]
                        pi += 1
                        p.op("act", lambda e, sbk=sbk, pt=pt: e.activation(out=pt[:], in_=sbk[:], func=AF.Exp, scale=0.125), reads=[(sbk, None)], writes=[(pt, None)])
                        pts.append(pt)
                    ob = rot(cx)
                    mm_group(cx, ob[:], (ob, None), [(vm[:, mc, h, :], pts[mc][:], [(vm, None), (pts[mc], None)]) for mc in range(2)])
                    p.op("dve", lambda e, ob=ob: e.reciprocal(out=rs[64:128, :], in_=ob[64:128, :]), reads=[(ob, None)], writes=[(rs, None)])
                    p.op("dve", lambda e, ob=ob, ht=ht, r0=r0, qb=qb: e.tensor_tensor(out=ocT[r0:r0 + 64, ht, qb * 512:(qb + 1) * 512], in0=ob[0:64, :], in1=rs[64:128, :], op=ALU.mult),
                         reads=[(ob, None), (rs, None)], writes=[(ocT, (ht, qb))])
            for hf in range(4):
                load_w_bf16(cx, wsm[:, 0:2, hf * 256:(hf + 1) * 256], (wsm, None), W["ca_wo"][:, hf * 256:(hf + 1) * 256], 2, 256, stage[sti % 2], q="pool")
                sti += 1
            proj_residual(cx, x, tiles, lambda c, j: (ocT[:, c, j * 128:(j + 1) * 128], (ocT, None)), 2, wsm, W["g"][3:4, :], L)
            p.barrier()
            norm_transpose(cx, x, tiles, W["g"][4:5, :], hT, L)
            aT = big
            for f in range(NFF):
                k = f % 2
                load_w_bf16(cx, wg[k][:], (wg[k], None), W["ffn_wg"][:, f * 128:(f + 1) * 128], 8, 128, stage[0], q="sp", cast_eng="pool")
                load_w_bf16(cx, wu[k][:], (wu[k], None), W["ffn_wu"][:, f * 128:(f + 1) * 128], 8, 128, stage[1], q="sp", cast_eng="pool")
                for nb in range(TB // 512):
                    bg = rot(cx)
                    bu = rot(cx)
                    for (bank, w) in ((bg, wg[k]), (bu, wu[k])):
                        mm_group(cx, bank[:], (bank, None), [(w[:, c, :], hT[:, c, nb * 512:(nb + 1) * 512], [(w, None), (hT, None)]) for c in range(8)])
                    gs = gsb[nb % 2]
                    p.op("act", lambda e, bg=bg, gs=gs: e.activation(out=gs[:], in_=bg[:], func=AF.Silu), reads=[(bg, None)], writes=[(gs, None)])
                    p.op("dve", lambda e, bu=bu, f=f, nb=nb, gs=gs: e.tensor_tensor(out=aT[:, f, nb * 512:(nb + 1) * 512], in0=bu[:], in1=gs[:], op=ALU.mult),
                         reads=[(bu, None), (gs, None)], writes=[(aT, (f, nb))])
            gB = L["gB"]
            p.dma("sp", gB[:], W["g"][5:6, :].to_broadcast([128, D]), writes=[(gB, None)])
            for grp in ([0, 1, 2], [3, 4, 5], [6, 7]):
                banks = {}
                for i, key in enumerate([(tt, nb) for tt in grp for nb in range(2)]):
                    banks[key] = cx.ps[i]
                for f in range(NFF):
                    k = f % 3
                    st = stage[f % 2]
                    p.dma("sp", st[:, 0:1024], W["ffn_wd"][f * 128:(f + 1) * 128, :], writes=[(st, None)])
                    p.op("pool", lambda e, k=k, st=st: e.tensor_copy(out=wd[k][:], in_=st[:, 0:1024]), reads=[(st, None)], writes=[(wd[k], None)])
                    for tt in grp:
                        for nb in range(2):
                            b = banks[(tt, nb)]
                            p.op("pe", lambda e, b=b, tt=tt, nb=nb, k=k, f=f: e.matmul(b[:], lhsT=aT[:, f, tt * 128:(tt + 1) * 128], rhs=wd[k][:, nb * 512:(nb + 1) * 512],
                                                                                      start=(f == 0), stop=(f == NFF - 1)),
                                 reads=[(aT, (f, tt // 4)), (wd[k], None)], writes=[(b, None)], inc=(tt == grp[-1] and nb == 1))
                for tt in grp:
                    norm_residual(cx, x, tiles[tt], [banks[(tt, 0)], banks[(tt, 1)]], gB, L)
            if hT_store is not None:
                norm_transpose(cx, x, tiles, g_next_ap, hT, L)
                hT_store(hT, tb)
            elif hT_next_ap is not None:
                norm_transpose(cx, x, tiles, g_next_ap, hT, L)
                p.dma("sp", hT_next_ap[:, tsl].rearrange("(c p) n -> p c n", p=128), hT[:], reads=[(hT, None)], writes=[(cx.outb, ("hT", tb))])
            p.barrier()


def psum_set(cx, es, n_f32, with_bf16):
    cx.uid += 1
    u = cx.uid
    ps = [Buf(es.enter_context(cx.nc.psum_tensor(f"ps{i}_{u}", [128, 512], F32)), f"ps{i}") for i in range(n_f32)]
    pst = Buf(es.enter_context(cx.nc.psum_tensor(f"pst_{u}", [128, 1024], BF16)), "pst") if with_bf16 else None
    return ps, pst


def build_rope_tables(cx, pos_ap, ropeinv_ap, Ct, St, scratch_i, scratch_f):
    p = cx.p
    inv = cx.ropeinv
    p.dma("sp", inv[:], ropeinv_ap, writes=[(inv, None)])
    p.dma("sp", scratch_i[:], pos_ap.to_broadcast([128, T]), writes=[(scratch_i, None)])
    p.op("dve", lambda e: e.tensor_copy(out=scratch_f[:], in_=scratch_i[:]), reads=[(scratch_i, None)], writes=[(scratch_f, None)])
    for (dst, col, off) in ((St, 0, 0.0), (Ct, 1, 0.25)):
        p.op("dve", lambda e, dst=dst, col=col, off=off: e.tensor_scalar(out=dst[:], in0=scratch_f[:], scalar1=inv[:, col:col + 1], scalar2=off,
                                                                      op0=ALU.mult, op1=ALU.add),
             reads=[(scratch_f, None), (inv, None)], writes=[(dst, None)])
        p.op("dve", lambda e, dst=dst: e.tensor_copy(out=scratch_i[:], in_=dst[:]), reads=[(dst, None)], writes=[(scratch_i, None)])
        p.op("pool", lambda e, dst=dst: e.tensor_tensor(out=dst[:], in0=dst[:], in1=scratch_i[:], op=ALU.subtract),
             reads=[(dst, None), (scratch_i, None)], writes=[(dst, None)])
        p.op("dve", lambda e, dst=dst: e.scalar_tensor_tensor(out=dst[:], in0=dst[:], scalar=0.5, in1=dst[:], op0=ALU.is_gt, op1=ALU.subtract),
             reads=[(dst, None)], writes=[(dst, None)])
        p.op("dve", lambda e, dst=dst: e.scalar_tensor_tensor(out=dst[:], in0=dst[:], scalar=0.5, in1=dst[:], op0=ALU.is_gt, op1=ALU.subtract),
             reads=[(dst, None)], writes=[(dst, None)])
    for dst in (St, Ct):
        p.op("act", lambda e, dst=dst: e.activation(out=dst[:], in_=dst[:], func=AF.Sin, scale=2.0 * math.pi), reads=[(dst, None)], writes=[(dst, None)])


def proj_rope(cx, hT, wq, wqs, col0, tok0, Ct, St, out_ap, out_dep, L, banks):
    p = cx.p
    bA, bB = banks
    mm_group(cx, bA[:], (bA, None), [(wq[:, c, col0:col0 + 128], hT[:, c, tok0:tok0 + 512], [(wq, None), (hT, None)]) for c in range(8)])
    mm_group(cx, bB[:], (bB, None), [(wqs[:, c, col0:col0 + 128], hT[:, c, tok0:tok0 + 512], [(wqs, None), (hT, None)]) for c in range(8)])
    t1, t2 = L["rt1"], L["rt2"]
    p.op("dve", lambda e: e.tensor_tensor(out=t1[:], in0=bA[:], in1=Ct[:, tok0:tok0 + 512], op=ALU.mult), reads=[(bA, None), (Ct, None)], writes=[(t1, None)])
    p.op("dve", lambda e: e.tensor_tensor(out=t2[:], in0=bB[:], in1=St[:, tok0:tok0 + 512], op=ALU.mult), reads=[(bB, None), (St, None)], writes=[(t2, None)])
    p.op("pool", lambda e: e.tensor_tensor(out=out_ap, in0=t1[:], in1=t2[:], op=ALU.add), reads=[(t1, None), (t2, None)], writes=[out_dep])


def run_streams(cx, streams, kbs, sbanks, pts, L, keep=None):
    p = cx.p
    n = len(kbs)
    st = L.setdefault("_rs", {"sb": 0, "pt": 0})

    def tail(pend, i):
        for (s, bank, kb) in pend:
            pt = pts[st["pt"] % len(pts)]
            st["pt"] += 1
            if keep is not None:
                keep.append((kb, pt))
            if s.get("bias") is not None:
                bap, bdeps = s["bias"](kb)
                p.op("act", lambda e, pt=pt, bank=bank, bap=bap, s=s: e.activation(out=pt[:], in_=bank[:], func=AF.Exp, scale=s["scale"], bias=bap),
                     reads=[(bank, None)] + bdeps, writes=[(pt, None)])
            else:
                p.op("act", lambda e, pt=pt, bank=bank, s=s: e.activation(out=pt[:], in_=bank[:], func=AF.Exp, scale=s["scale"]),
                     reads=[(bank, None)], writes=[(pt, None)])
            for (map_, mdeps) in s["masks"](kb):
                p.op("pool", lambda e, pt=pt, map_=map_: e.tensor_tensor(out=pt[:], in0=pt[:], in1=map_, op=ALU.mult),
                     reads=[(pt, None)] + mdeps, writes=[(pt, None)])
            for (obank, lfn) in s["pv"]:
                lap, ldeps = lfn(kb)
                p.op("pe", lambda e, obank=obank, lap=lap, pt=pt, i=i: e.matmul(obank, lhsT=lap, rhs=pt[:], start=(i == 0), stop=(i == n - 1)),
                     reads=[(pt, None)] + ldeps, writes=[s["pv_dep"]])

    pend = None
    pend_i = -1
    for i, kb in enumerate(kbs):
        cur = []
        for s in streams:
            bank = sbanks[st["sb"] % len(sbanks)]
            st["sb"] += 1
            kap, kdeps = s["k"](kb)
            qap, qdeps = s["q"]
            p.op("pe", lambda e, bank=bank, kap=kap, qap=qap: e.matmul(bank[:], lhsT=kap, rhs=qap, start=True, stop=True),
                 reads=kdeps + qdeps, writes=[(bank, None)])
            cur.append((s, bank, kb))
        if pend is not None:
            tail(pend, pend_i)
        pend, pend_i = cur, i
    tail(pend, pend_i)


def phase_B_odd(cx, load_hT, W, oT_ap):
    p = cx.p
    with ExitStack() as es:
        cx.uid += 1
        uu = cx.uid
        def sb(name, shape, dt):
            return Buf(es.enter_context(cx.nc.sbuf_tensor(f"{name}_{uu}", list(shape), dt)), name)
        ps, _ = psum_set(cx, es, 8, False)
        L = {}
        hT = sb("hT_all", [128, 8, T], BF16)
        load_hT(hT)
        Ct = sb("Ct", [128, T], F32)
        St = sb("St", [128, T], F32)
        cx.ropeinv = sb("ropeinv", [128, 2], F32)
        with ExitStack() as es2:
            sci = Buf(es2.enter_context(cx.nc.sbuf_tensor(f"sci_{uu}", [128, T], I32)), "sci")
            scf = Buf(es2.enter_context(cx.nc.sbuf_tensor(f"scf_{uu}", [128, T], F32)), "scf")
            build_rope_tables(cx, W["pos"], W["ropeinv"], Ct, St, sci, scf)
            p.barrier()
        qT = sb("qT", [128, 2, T], BF16)
        kT = sb("kT", [128, 2, T], BF16)
        vv = sb("vv", [128, 32, 256], BF16)
        stage = [sb(f"stage{i}", [128, 2048], F32) for i in range(2)]
        wq = sb("wq", [128, 8, 256], BF16)
        wqs = sb("wqs", [128, 8, 256], BF16)
        cmask = sb("cmask", [128, 8, 512], BF16)
        ones_b = sb("ones_b", [128, 128], BF16)
        ones_f = sb("ones_f", [128, 128], F32)
        pts = [sb(f"pt{i}", [128, 512], BF16) for i in range(6)]
        L["rt1"] = sb("rt1", [128, 512], F32)
        L["rt2"] = sb("rt2", [128, 512], F32)
        r1 = sb("r1", [128, 512], F32)
        r2 = sb("r2", [128, 512], F32)
        osb = sb("osb", [128, 512], F32)
        osq = sb("osq", [128, 512], F32)
        ob = [sb(f"ob{i}", [128, 512], BF16) for i in range(2)]
        lamt = sb("lamt", [128, 256], F32)
        lam2 = sb("lam2", [128, 8], F32)
        gcol = sb("gcol", [128, 1], F32)
        p.dma("sp", cmask[:], W["cmask"].rearrange("p (j n) -> p j n", j=8), writes=[(cmask, None)])
        p.op("pool", lambda e: e.memset(ones_b[:], 1.0), writes=[(ones_b, None)])
        p.op("pool", lambda e: e.memset(ones_f[:], 1.0), writes=[(ones_f, None)])
        p.dma("sp", lamt[:], W["lam"].to_broadcast([128, 256]), writes=[(lamt, None)])
        p.dma("sp", gcol[:], W["subg"], writes=[(gcol, None)])
        for i in range(2):
            p.op("dve", lambda e, i=i: e.tensor_tensor(out=lamt[:, i * 128:i * 128 + 64], in0=lamt[:, i * 128:i * 128 + 64], in1=lamt[:, i * 128 + 64:i * 128 + 128], op=ALU.mult),
                 reads=[(lamt, None)], writes=[(lamt, None)])
            p.op("dve", lambda e, i=i: e.tensor_reduce(out=lam2[:, i:i + 1], in_=lamt[:, i * 128:i * 128 + 64], axis=AX.X, op=ALU.add),
                 reads=[(lamt, None)], writes=[(lam2, None)])
        p.op("act", lambda e: e.activation(out=lam2[:, 2:4], in_=lam2[:, 0:2], func=AF.Exp), reads=[(lam2, None)], writes=[(lam2, None)])
        lic = sb("lic", [128, 2], F32)
        p.dma("sp", lic[:], W["laminit"].to_broadcast([128, 2]), writes=[(lic, None)])
        p.op("dve", lambda e: e.scalar_tensor_tensor(out=lam2[:, 4:5], in0=lam2[:, 3:4], scalar=lic[:, 0:1], in1=lam2[:, 2:3], op0=ALU.add, op1=ALU.subtract),
             reads=[(lam2, None), (lic, None)], writes=[(lam2, None)])
        p.op("dve", lambda e: e.tensor_scalar(out=gcol[:], in0=gcol[:], scalar1=lic[:, 1:2], scalar2=None, op0=ALU.mult), reads=[(gcol, None), (lic, None)], writes=[(gcol, None)])
        sti = 0
        for hp in range(2):
            for (dst, base) in ((qT, 0), (kT, 1024)):
                c0 = base + hp * 256
                load_w_bf16(cx, wq[:], (wq, None), W["w_in"][:, c0:c0 + 256], 8, 256, stage[sti % 2], q="pool"); sti += 1
                load_w_bf16(cx, wqs[:], (wqs, None), W["w_in"][:, c0 + 512:c0 + 768], 8, 256, stage[sti % 2], q="pool"); sti += 1
                for hh in range(2):
                    for tb in range(8):
                        banks = (ps[(2 * tb) % 8], ps[(2 * tb + 1) % 8])
                        proj_rope(cx, hT, wq, wqs, hh * 128, tb * 512, Ct, St, dst[:, hh, tb * 512:(tb + 1) * 512], (dst, (hh, tb)), L, banks)
            c0 = 2048 + hp * 256
            load_w_bf16(cx, wq[:], (wq, None), W["w_in"][:, c0:c0 + 256], 8, 256, stage[sti % 2], q="pool"); sti += 1
            for tt in range(32):
                b = ps[tt % 8]
                mm_group(cx, b[:, 0:256], (b, None), [(hT[:, c, tt * 128:(tt + 1) * 128], wq[:, c, :], [(wq, None), (hT, None)]) for c in range(8)])
                if tt % 2 == 0:
                    p.op("act", lambda e, b=b, tt=tt: e.activation(out=vv[:, tt, :], in_=b[:, 0:256], func=AF.Copy), reads=[(b, None)], writes=[(vv, tt)])
                else:
                    p.op("dve", lambda e, b=b, tt=tt: e.tensor_copy(out=vv[:, tt, :], in_=b[:, 0:256]), reads=[(b, None)], writes=[(vv, tt)])
            for hh in range(2):
                head = hp * 2 + hh
                for qb in range(8):
                    kbs = list(range(4 * qb + 4))
                    O1, S1, O2, S2 = ps[4], ps[5], ps[6], ps[7]
                    streams = []
                    for comp, (Ob, Sb) in enumerate(((O1, S1), (O2, S2))):
                        r0 = comp * 64
                        streams.append(dict(
                            k=lambda kb, r0=r0: (kT[r0:r0 + 64, hh, kb * 128:(kb + 1) * 128], [(kT, (hh, kb // 4))]),
                            q=(qT[r0:r0 + 64, hh, qb * 512:(qb + 1) * 512], [(qT, (hh, qb))]),
                            scale=0.125, bias=None,
                            masks=lambda kb: ([(cmask[:, kb - 4 * qb, :], [(cmask, None)])] if kb >= 4 * qb else []),
                            pv=[(Ob[:], lambda kb: (vv[:, kb, hh * 128:(hh + 1) * 128], [(vv, kb)])),
                                (Sb[:], lambda kb: (ones_b[:], [(ones_b, None)]))],
                            pv_dep=(Ob, None)))
                    run_streams(cx, streams, kbs, ps[0:4], pts, L)
                    p.op("dve", lambda e: e.reciprocal(out=r1[:], in_=S1[:]), reads=[(O1, None), (S1, None)], writes=[(r1, None)])
                    p.op("dve", lambda e: e.reciprocal(out=r2[:], in_=S2[:]), reads=[(O2, None), (S2, None)], writes=[(r2, None)])
                    p.op("dve", lambda e: e.tensor_tensor(out=r1[:], in0=O1[:], in1=r1[:], op=ALU.mult), reads=[(O1, None), (r1, None)], writes=[(r1, None)])
                    p.op("dve", lambda e: e.tensor_tensor(out=r2[:], in0=O2[:], in1=r2[:], op=ALU.mult), reads=[(O2, None), (r2, None)], writes=[(r2, None), (S1, None), (S2, None)])
                    p.op("dve", lambda e: e.scalar_tensor_tensor(out=osb[:], in0=r2[:], scalar=lam2[:, 4:5], in1=r1[:], op0=ALU.mult, op1=ALU.add),
                         reads=[(r1, None), (r2, None), (lam2, None)], writes=[(osb, None)])
                    p.op("pool", lambda e: e.tensor_tensor(out=osq[:], in0=osb[:], in1=osb[:], op=ALU.mult), reads=[(osb, None)], writes=[(osq, None)])
                    sbk = ps[qb % 4]
                    p.op("pe", lambda e, sbk=sbk: e.matmul(sbk[:], lhsT=ones_f[:], rhs=osq[:], start=True, stop=True), reads=[(osq, None), (ones_f, None)], writes=[(sbk, None)])
                    p.op("act", lambda e, sbk=sbk: e.activation(out=r1[:], in_=sbk[:], func=AF.Sqrt, scale=1.0 / 128, bias=cx.epsb[:, 0:1]),
                         reads=[(sbk, None), (cx.epsb, None)], writes=[(r1, None)])
                    p.op("dve", lambda e: e.reciprocal(out=r1[:], in_=r1[:]), reads=[(r1, None)], writes=[(r1, None)])
                    obt = ob[qb % 2]
                    p.op("dve", lambda e, obt=obt: e.scalar_tensor_tensor(out=obt[:], in0=osb[:], scalar=gcol[:, 0:1], in1=r1[:], op0=ALU.mult, op1=ALU.mult),
                         reads=[(osb, None), (gcol, None), (r1, None)], writes=[(obt, None)])
                    p.dma("sp", oT_ap[head * 128:(head + 1) * 128, qb * 512:(qb + 1) * 512], obt[:], reads=[(obt, None)], writes=[(cx.outb, ("oT", head, qb))])
        p.barrier()


EV = dict(fq=0, fk=256, fv=512, nq=768, nqs=1024, kc=1280, kcs=1344, ks=1408, kss=1472, kw=1536, kws=1600, vc=1664, vs=1728, vw=1792, fl=1856, gl=1860)
EV_NCOL = 1872
TBK = 512


def build_rope_block(cx, pos_ap, tok0, n, Ct, St, sci, scf):
    p = cx.p
    inv = cx.ropeinv
    p.dma("sp", sci[:, 0:n], pos_ap[:, tok0:tok0 + n].to_broadcast([128, n]), writes=[(sci, None)])
    p.op("dve", lambda e: e.tensor_copy(out=scf[:, 0:n], in_=sci[:, 0:n]), reads=[(sci, None)], writes=[(scf, None)])
    for (dst, col, off) in ((St, 0, 0.0), (Ct, 1, 0.25)):
        p.op("dve", lambda e, dst=dst, col=col, off=off: e.tensor_scalar(out=dst[:, 0:n], in0=scf[:, 0:n], scalar1=inv[:, col:col + 1], scalar2=off,
                                                                      op0=ALU.mult, op1=ALU.add),
             reads=[(scf, None), (inv, None)], writes=[(dst, None)])
        p.op("dve", lambda e, dst=dst: e.tensor_copy(out=sci[:, 0:n], in_=dst[:, 0:n]), reads=[(dst, None)], writes=[(sci, None)])
        p.op("pool", lambda e, dst=dst: e.tensor_tensor(out=dst[:, 0:n], in0=dst[:, 0:n], in1=sci[:, 0:n], op=ALU.subtract),
             reads=[(dst, None), (sci, None)], writes=[(dst, None)])
        for _ in range(2):
            p.op("dve", lambda e, dst=dst: e.scalar_tensor_tensor(out=dst[:, 0:n], in0=dst[:, 0:n], scalar=0.5, in1=dst[:, 0:n], op0=ALU.is_gt, op1=ALU.subtract),
                 reads=[(dst, None)], writes=[(dst, None)])
        p.op("act", lambda e, dst=dst: e.activation(out=dst[:, 0:n], in_=dst[:, 0:n], func=AF.Sin, scale=2.0 * math.pi), reads=[(dst, None)], writes=[(dst, None)])


def phase_B_even(cx, hT_src, W, oT_ap):
    p = cx.p
    nc = cx.nc
    with ExitStack() as es:
        cx.uid += 1
        uu = cx.uid

        def mk_sb(stack):
            def sb(name, shape, dt):
                return Buf(stack.enter_context(nc.sbuf_tensor(f"{name}_{uu}", list(shape), dt)), name)
            return sb
        sb = mk_sb(es)
        ps, pst = psum_set(cx, es, 7, True)
        L = {}
        cmask = sb("cmask", [128, 8, 512], BF16)
        p.dma("sp", cmask[:], W["cmask"].rearrange("p (j n) -> p j n", j=8), writes=[(cmask, None)])
        cx.ropeinv = sb("ropeinv", [128, 2], F32)
        p.dma("sp", cx.ropeinv[:], W["ropeinv"], writes=[(cx.ropeinv, None)])
        ones_f = sb("ones_f", [128, 128], F32)
        p.op("pool", lambda e: e.memset(ones_f[:], 1.0), writes=[(ones_f, None)])
        pts = [sb(f"pt{i}", [128, 512], BF16) for i in range(6)]
        rs = sb("rs", [128, 512], F32)
        fac = sb("fac", [128, 512], F32)
        obt = [sb(f"obt{i}", [128, 512], BF16) for i in range(2)]

        def load_hT_block(hTb, tok0):
            for c in range(8):
                hT_src(hTb[:, c, :], c, tok0, TBK, "sp" if c % 2 == 0 else "pool", [(hTb, None)])

        with ExitStack() as esf:
            sbf = mk_sb(esf)
            fqT = sbf("fqT", [128, 2, T], BF16)
            fkT = sbf("fkT", [128, 2, T], BF16)
            fvv = sbf("fvv", [128, 32, 4, 128], BF16)
            FB = sbf("FB", [128, 4, 32, 8], F32)
            ncum = sbf("ncum", [128, 128], F32)
            p.op("pool", lambda e: e.memset(fvv[:], 1.0), writes=[(fvv, None)])
            with ExitStack() as esp:
                sbp = mk_sb(esp)
                hTb = sbp("hTb", [128, 8, TBK], BF16)
                wf = sbp("wf", [128, 8, 768], BF16)
                wfl = sbp("wfl", [128, 8, 4], BF16)
                stage = [sbp(f"stage{i}", [128, 2048], F32) for i in range(2)]
                tri = sbp("tri", [128, 128], F32)
                sel127 = sbp("sel127", [128, 128], F32)
                fbB = sbp("fbB", [128, 128], F32)
                nlf = sbp("nlf", [128, 128], F32)
                tot = sbp("tot", [128, 128], F32)
                inc = sbp("inc", [128, 128], F32)
                refsb = sbp("refsb", [128, 128], F32)
                p.dma("sp", tri[:], W["tri"], writes=[(tri, None)])
                p.dma("sp", sel127[:], W["sel127"], writes=[(sel127, None)])
                p.dma("sp", fbB[:], W["fbias_rep"].to_broadcast([128, 128]), writes=[(fbB, None)])
                for i in range(3):
                    load_w_bf16(cx, wf[:, :, i * 256:(i + 1) * 256], (wf, None), W["w_in"][:, i * 256:(i + 1) * 256], 8, 256, stage[i % 2], q="pool")
                load_w_bf16(cx, wfl[:], (wfl, None), W["w_in"][:, EV["fl"]:EV["fl"] + 4], 8, 4, stage[1], q="pool")
                FLP = ps[6]
                for blk in range(T // TBK):
                    tok0 = blk * TBK
                    load_hT_block(hTb, tok0)
                    for (dst, cb) in ((fqT, 0), (fkT, 256)):
                        for hh in range(2):
                            for tb in range(TBK // 512):
                                b = ps[(hh * 2 + tb) % 4]
                                mm_group(cx, b[:], (b, None), [(wf[:, c, cb + hh * 128:cb + (hh + 1) * 128], hTb[:, c, tb * 512:(tb + 1) * 512], [(wf, None), (hTb, None)]) for c in range(8)])
                                gtb = (tok0 + tb * 512) // 512
                                if tb % 2 == 0:
                                    p.op("act", lambda e, b=b, dst=dst, hh=hh, gtb=gtb: e.activation(out=dst[:, hh, gtb * 512:(gtb + 1) * 512], in_=b[:], func=AF.Copy),
                                         reads=[(b, None)], writes=[(dst, (hh, gtb))])
                                else:
                                    p.op("dve", lambda e, b=b, dst=dst, hh=hh, gtb=gtb: e.tensor_copy(out=dst[:, hh, gtb * 512:(gtb + 1) * 512], in_=b[:]),
                                         reads=[(b, None)], writes=[(dst, (hh, gtb))])
                    for tl in range(TBK // 128):
                        tt = tok0 // 128 + tl
                        b = ps[4 + tl % 2]
                        mm_group(cx, b[:, 0:256], (b, None), [(hTb[:, c, tl * 128:(tl + 1) * 128], wf[:, c, 512:768], [(wf, None), (hTb, None)]) for c in range(8)])
                        p.op("dve", lambda e, b=b, tt=tt: e.tensor_copy(out=fvv[:, tt, :, 0:64], in_=b[:, 0:256].rearrange("p (h d) -> p h d", h=4)),
                             reads=[(b, None)], writes=[(fvv, tt)])
                        mm_group(cx, FLP[:, tt * 4:(tt + 1) * 4], (FLP, None), [(hTb[:, c, tl * 128:(tl + 1) * 128], wfl[:, c, :], [(wfl, None), (hTb, None)]) for c in range(8)])
                p.op("dve", lambda e: e.tensor_tensor(out=nlf[:], in0=FLP[:, 0:128], in1=fbB[:], op=ALU.add), reads=[(FLP, None), (fbB, None)], writes=[(nlf, None)])
                p.op("act", lambda e: e.activation(out=nlf[:], in_=nlf[:], func=AF.Exp, scale=-1.0), reads=[(nlf, None)], writes=[(nlf, None)])
                p.op("act", lambda e: e.activation(out=nlf[:], in_=nlf[:], func=AF.Ln, bias=1.0), reads=[(nlf, None)], writes=[(nlf, None)])
                W1b, TOTb, REFb = ps[0], ps[1], ps[2]
                p.op("pe", lambda e: e.matmul(W1b[:, 0:128], lhsT=tri[:], rhs=nlf[:], start=True, stop=True), reads=[(tri, None), (nlf, None)], writes=[(W1b, None)])
                p.op("pe", lambda e: e.matmul(TOTb[:, 0:128], lhsT=ones_f[:], rhs=nlf[:], start=True, stop=True), reads=[(ones_f, None), (nlf, None)], writes=[(TOTb, None)])
                p.op("dve", lambda e: e.tensor_copy(out=tot[:], in_=TOTb[:, 0:128]), reads=[(TOTb, None)], writes=[(tot, None)])
                for j in range(4):
                    tv = tot[:].rearrange("p (t h) -> p t h", h=4)[:, :, j]
                    iv = inc[:].rearrange("p (t h) -> p t h", h=4)[:, :, j]
                    ov = ones_f[:, 0:32]
                    p.op("dve", lambda e, tv=tv, iv=iv, ov=ov: e.tensor_tensor_scan(out=iv, data0=ov, data1=tv, initial=0.0, op0=ALU.mult, op1=ALU.add),
                         reads=[(tot, None), (ones_f, None)], writes=[(inc, None)])
                p.op("dve", lambda e: e.tensor_tensor(out=inc[:], in0=inc[:], in1=tot[:], op=ALU.subtract), reads=[(inc, None), (tot, None)], writes=[(inc, None)])
                p.op("dve", lambda e: e.tensor_tensor(out=ncum[:], in0=W1b[:, 0:128], in1=inc[:], op=ALU.add), reads=[(W1b, None), (inc, None)], writes=[(ncum, None)])
                p.op("pe", lambda e: e.matmul(REFb[:, 0:128], lhsT=sel127[:], rhs=ncum[:], start=True, stop=True), reads=[(sel127, None), (ncum, None)], writes=[(REFb, None)])
                p.op("dve", lambda e: e.tensor_copy(out=refsb[:], in_=REFb[:, 0:128]), reads=[(REFb, None)], writes=[(refsb, None)])
                ncv = ncum[:].rearrange("p (t h) -> p t h", h=4)
                for j in range(4):
                    for qb in range(8):
                        col = (4 * qb + 3) * 4 + j
                        p.op("dve", lambda e, j=j, qb=qb, col=col: e.tensor_scalar(out=FB[:, j, :, qb], in0=ncv[:, :, j], scalar1=refsb[:, col:col + 1], scalar2=None, op0=ALU.subtract),
                             reads=[(ncum, None), (refsb, None)], writes=[(FB, None)])
                p.barrier()
            for head in range(4):
                hh, r0 = head // 2, (head % 2) * 64
                for qb in range(8):
                    kbs = list(range(4 * qb + 4))
                    Ob = ps[4 + (qb % 2)]
                    stream = dict(
                        k=lambda kb: (fkT[r0:r0 + 64, hh, kb * 128:(kb + 1) * 128], [(fkT, (hh, kb // 4))]),
                        q=(fqT[r0:r0 + 64, hh, qb * 512:(qb + 1) * 512], [(fqT, (hh, qb))]),
                        scale=0.125,
                        bias=lambda kb: (FB[:, head, kb, qb:qb + 1], [(FB, None)]),
                        masks=lambda kb: ([(cmask[:, kb - 4 * qb, :], [(cmask, None)])] if kb >= 4 * qb else []),
                        pv=[(Ob[:], lambda kb: (fvv[:, kb, head, :], [(fvv, kb)]))],
                        pv_dep=(Ob, None))
                    run_streams(cx, [stream], kbs, ps[0:4], pts, L)
                    ot = obt[qb % 2]
                    p.op("dve", lambda e, Ob=Ob: e.reciprocal(out=rs[64:128, :], in_=Ob[64:128, :]), reads=[(Ob, None)], writes=[(rs, None)])
                    p.op("dve", lambda e, Ob=Ob, ot=ot: e.tensor_tensor(out=ot[0:64, :], in0=Ob[0:64, :], in1=rs[64:128, :], op=ALU.mult),
                         reads=[(Ob, None), (rs, None)], writes=[(ot, None)])
                    p.dma("sp", oT_ap[head * 64:(head + 1) * 64, qb * 512:(qb + 1) * 512], ot[0:64, :], reads=[(ot, None)], writes=[(cx.outb, ("oTf", head, qb))])
            p.barrier()
        nsa_part(cx, W, oT_ap, hT_src, load_hT_block, mk_sb, es, ps, pst, pts, cmask, ones_f, rs, fac, obt, L)
        p.barrier()


NWC = dict(nq=0, nqs=256, ks=512, kss=640, kw=768, kws=896, kc=1024, kcs=1152, vc=1280, vsw=1344, gl=1472)
NW = 1484


def gelu_tanh(cx, src_bank_ap, src_dep, bias_col, bias_dep, out_ap, out_dep, tmpa, tmpb, n):
    p = cx.p
    p.op("act", lambda e: e.activation(out=tmpa[:, 0:n], in_=src_bank_ap, func=AF.Identity, bias=bias_col), reads=[src_dep, bias_dep], writes=[(tmpa, None)])
    p.op("dve", lambda e: e.tensor_tensor(out=tmpb[:, 0:n], in0=tmpa[:, 0:n], in1=tmpa[:, 0:n], op=ALU.mult), reads=[(tmpa, None)], writes=[(tmpb, None)])
    p.op("dve", lambda e: e.tensor_scalar(out=tmpb[:, 0:n], in0=tmpb[:, 0:n], scalar1=0.044715, scalar2=1.0, op0=ALU.mult, op1=ALU.add), reads=[(tmpb, None)], writes=[(tmpb, None)])
    p.op("dve", lambda e: e.tensor_tensor(out=tmpb[:, 0:n], in0=tmpb[:, 0:n], in1=tmpa[:, 0:n], op=ALU.mult), reads=[(tmpa, None), (tmpb, None)], writes=[(tmpb, None)])
    p.op("act", lambda e: e.activation(out=tmpb[:, 0:n], in_=tmpb[:, 0:n], func=AF.Tanh, scale=0.7978845608028654), reads=[(tmpb, None)], writes=[(tmpb, None)])
    p.op("dve", lambda e: e.scalar_tensor_tensor(out=tmpb[:, 0:n], in0=tmpb[:, 0:n], scalar=1.0, in1=tmpa[:, 0:n], op0=ALU.add, op1=ALU.mult),
         reads=[(tmpa, None), (tmpb, None)], writes=[(tmpb, None)])
    p.op("act", lambda e: e.activation(out=out_ap, in_=tmpb[:, 0:n], func=AF.Copy, scale=0.5), reads=[(tmpb, None)], writes=[out_dep])


def nsa_part(cx, W, oT_ap, hT_src, load_hT_block, mk_sb, es, ps, pst, pts, cmask, ones_f, rs, fac, obt, L):
    p = cx.p
    sb = mk_sb(es)
    QA = sb("QA", [128, 4, T], BF16)
    KSA = sb("KSA", [128, T], BF16)
    KSB = sb("KSB", [128, T], BF16)
    KW2 = sb("KW2", [128, T], BF16)
    VS = sb("VS", [128, 32, 128], BF16)
    VW = sb("VW", [128, 32, 128], BF16)
    GLT = sb("GLT", [12, T], BF16)
    KCMP = sb("KCMP", [128, 256], BF16)
    VCMP = sb("VCMP", [128, 2, 128], BF16)
    cmpmask = sb("cmpmask", [128, 5, 512], BF16)
    ovl = sb("ovl", [128, 2, 65], BF16)
    sel12 = sb("sel12", [12, 12 * 128], BF16)
    tkadd = sb("tkadd", [128, 128], F32)
    tkmul = sb("tkmul", [128, 128], F32)
    p.dma("sp", cmpmask[:], W["cmpmask"].rearrange("p (j n) -> p j n", j=5), writes=[(cmpmask, None)])
    p.dma("sp", ovl[:], W["ovl"].rearrange("p (c n) -> p c n", c=2), writes=[(ovl, None)])
    p.dma("sp", sel12[:], W["sel12"], writes=[(sel12, None)])
    p.dma("sp", tkadd[:], W["tkadd"], writes=[(tkadd, None)])
    p.dma("sp", tkmul[:], W["tkmul"], writes=[(tkmul, None)])
    for b_ in (VS, VW, VCMP):
        p.op("pool", lambda e, b_=b_: e.memset(b_[:], 1.0), writes=[(b_, None)])
    with ExitStack() as esp:
        sbp = mk_sb(esp)
        hTb = sbp("hTbn", [128, 8, TBK], BF16)
        wn = sbp("wn", [128, 8, NW], BF16)
        stage = [sbp(f"stagen{i}", [128, 2048], F32) for i in range(2)]
        Ct = sbp("Ctb", [128, TBK], F32)
        St = sbp("Stb", [128, TBK], F32)
        sci = sbp("scib", [128, TBK], I32)
        scf = sbp("scfb", [128, TBK], F32)
        L["rt1"] = sbp("rt1n", [128, 512], F32)
        L["rt2"] = sbp("rt2n", [128, 512], F32)
        KC2 = sbp("KC2", [128, T], BF16)
        VC = sbp("VC", [128, T], BF16)
        W1 = sbp("W1c", [64, 32, 128], BF16)
        posT = sbp("posT", [64, 32], BF16)
        posTf = sbp("posTf", [64, 32], F32)
        W2d = sbp("W2d", [128, 128], BF16)
        posb = sbp("posb", [128, 1], F32)
        HID = sbp("HID", [128, 256], BF16)
        ga = sbp("ga", [128, 256], F32)
        gb = sbp("gb", [128, 256], F32)
        sti = [0]

        def stg():
            sti[0] += 1
            return stage[sti[0] % 2]
        E = EV
        load_w_bf16(cx, wn[:, :, 0:256], (wn, None), W["w_in"][:, E["nq"]:E["nq"] + 256], 8, 256, stg(), q="pool")
        load_w_bf16(cx, wn[:, :, 256:512], (wn, None), W["w_in"][:, E["nqs"]:E["nqs"] + 256], 8, 256, stg(), q="pool")
        for (src, dstc, dup) in ((E["ks"], NWC["ks"], True), (E["kss"], NWC["kss"], True), (E["kw"], NWC["kw"], True), (E["kws"], NWC["kws"], True),
                                 (E["kc"], NWC["kc"], True), (E["kcs"], NWC["kcs"], True), (E["vc"], NWC["vc"], False)):
            st = stg()
            sv = st[:, 0:8 * 64].rearrange("p (c n) -> p c n", c=8)
            p.dma("pool", sv, W["w_in"][:, src:src + 64].rearrange("(c p) n -> p c n", p=128), writes=[(st, None)])
            p.op("pool", lambda e, sv=sv, dstc=dstc: e.tensor_copy(out=wn[:, :, dstc:dstc + 64], in_=sv), reads=[(st, None)], writes=[(wn, None)])
            if dup:
                p.op("pool", lambda e, sv=sv, dstc=dstc: e.tensor_copy(out=wn[:, :, dstc + 64:dstc + 128], in_=sv), reads=[(st, None)], writes=[(wn, None)])
        load_w_bf16(cx, wn[:, :, NWC["vsw"]:NWC["vsw"] + 128], (wn, None), W["w_in"][:, E["vs"]:E["vs"] + 128], 8, 128, stg(), q="pool")
        load_w_bf16(cx, wn[:, :, NWC["gl"]:NWC["gl"] + 12], (wn, None), W["w_in"][:, E["gl"]:E["gl"] + 12], 8, 12, stg(), q="pool")
        for blk in range(T // TBK):
            tok0 = blk * TBK
            load_hT_block(hTb, tok0)
            build_rope_block(cx, W["pos"], tok0, TBK, Ct, St, sci, scf)
            for tb in range(TBK // 512):
                g0 = tok0 + tb * 512
                gtb = g0 // 512
                lsl = slice(tb * 512, (tb + 1) * 512)
                for i in range(2):
                    bA, bB = ps[0], ps[1]
                    mm_group(cx, bA[:], (bA, None), [(wn[:, c, i * 128:(i + 1) * 128], hTb[:, c, lsl], [(wn, None), (hTb, None)]) for c in range(8)])
                    mm_group(cx, bB[:], (bB, None), [(wn[:, c, 256 + i * 128:256 + (i + 1) * 128], hTb[:, c, lsl], [(wn, None), (hTb, None)]) for c in range(8)])
                    t1, t2 = L["rt1"], L["rt2"]
                    p.op("dve", lambda e, bA=bA: e.tensor_tensor(out=t1[:], in0=bA[:], in1=Ct[:, lsl], op=ALU.mult), reads=[(bA, None), (Ct, None)], writes=[(t1, None)])
                    p.op("dve", lambda e, bB=bB: e.tensor_tensor(out=t2[:], in0=bB[:], in1=St[:, lsl], op=ALU.mult), reads=[(bB, None), (St, None)], writes=[(t2, None)])
                    p.op("pool", lambda e, i=i, g0=g0: e.tensor_tensor(out=QA[0:64, 2 * i, g0:g0 + 512], in0=t1[0:64, :], in1=t2[0:64, :], op=ALU.add),
                         reads=[(t1, None), (t2, None)], writes=[(QA, (2 * i, gtb))])
                    p.op("pool", lambda e, i=i, g0=g0: e.tensor_tensor(out=QA[64:128, 2 * i + 1, g0:g0 + 512], in0=t1[64:128, :], in1=t2[64:128, :], op=ALU.add),
                         reads=[(t1, None), (t2, None)], writes=[(QA, (2 * i + 1, gtb))])
                for (dst, cb, cbs) in ((KSA, NWC["ks"], NWC["kss"]), (KW2, NWC["kw"], NWC["kws"]), (KC2, NWC["kc"], NWC["kcs"])):
                    bA, bB = ps[2], ps[3]
                    mm_group(cx, bA[:], (bA, None), [(wn[:, c, cb:cb + 128], hTb[:, c, lsl], [(wn, None), (hTb, None)]) for c in range(8)])
                    mm_group(cx, bB[:], (bB, None), [(wn[:, c, cbs:cbs + 128], hTb[:, c, lsl], [(wn, None), (hTb, None)]) for c in range(8)])
                    t1, t2 = L["rt1"], L["rt2"]
                    p.op("dve", lambda e, bA=bA: e.tensor_tensor(out=t1[:], in0=bA[:], in1=Ct[:, lsl], op=ALU.mult), reads=[(bA, None), (Ct, None)], writes=[(t1, None)])
                    p.op("dve", lambda e, bB=bB: e.tensor_tensor(out=t2[:], in0=bB[:], in1=St[:, lsl], op=ALU.mult), reads=[(bB, None), (St, None)], writes=[(t2, None)])
                    p.op("pool", lambda e, dst=dst, g0=g0: e.tensor_tensor(out=dst[:, g0:g0 + 512], in0=t1[:], in1=t2[:], op=ALU.add),
                         reads=[(t1, None), (t2, None)], writes=[(dst, gtb)])
                b = ps[4]
                mm_group(cx, b[0:64, :], (b, None), [(wn[:, c, NWC["vc"]:NWC["vc"] + 64], hTb[:, c, lsl], [(wn, None), (hTb, None)]) for c in range(8)])
                p.op("act", lambda e, b=b, g0=g0: e.activation(out=VC[0:64, g0:g0 + 512], in_=b[0:64, :], func=AF.Copy), reads=[(b, None)], writes=[(VC, gtb)])
                b = ps[5]
                mm_group(cx, b[0:12, :], (b, None), [(wn[:, c, NWC["gl"]:NWC["gl"] + 12], hTb[:, c, lsl], [(wn, None), (hTb, None)]) for c in range(8)])
                p.op("act", lambda e, b=b, g0=g0: e.activation(out=GLT[0:12, g0:g0 + 512], in_=b[0:12, :], func=AF.Sigmoid), reads=[(b, None)], writes=[(GLT, gtb)])
            for tl in range(TBK // 128):
                tt = tok0 // 128 + tl
                b = ps[(tl % 2)]
                mm_group(cx, b[:, 0:128], (b, None), [(hTb[:, c, tl * 128:(tl + 1) * 128], wn[:, c, NWC["vsw"]:NWC["vsw"] + 128], [(wn, None), (hTb, None)]) for c in range(8)])
                p.op("dve", lambda e, b=b, tt=tt: e.tensor_copy(out=VS[:, tt, 0:64], in_=b[:, 0:64]), reads=[(b, None)], writes=[(VS, tt)])
                p.op("act", lambda e, b=b, tt=tt: e.activation(out=VW[:, tt, 0:64], in_=b[:, 64:128], func=AF.Copy), reads=[(b, None)], writes=[(VW, tt)])
        p.op("pool", lambda e: e.tensor_copy(out=KSB[:], in_=KSA[:]), reads=[(KSA, None)], writes=[(KSB, None)])
        p.dma("sp", KSA[64:128, :], W["onehot"], writes=[(KSA, None)])
        p.dma("sp", KSB[0:64, :], W["onehot"], writes=[(KSB, None)])
        for which in ("k", "v"):
            src = KC2 if which == "k" else VC
            for half in range(2):
                st = stg()
                sv = st[0:64, :].rearrange("p (l h) -> p l h", l=16)
                p.dma("sp", sv, W["c1" + which][half * 1024:(half + 1) * 1024, :].rearrange("(l d) h -> d l h", d=64), writes=[(st, None)])
                p.op("pool", lambda e, sv=sv, half=half: e.tensor_copy(out=W1[:, half * 16:(half + 1) * 16, :], in_=sv), reads=[(st, None)], writes=[(W1, None)])
            p.dma("sp", posTf[:], W["cp" + which + "T"], writes=[(posTf, None)])
            p.op("pool", lambda e: e.tensor_copy(out=posT[:], in_=posTf[:]), reads=[(posTf, None)], writes=[(posT, None)])
            st = stg()
            p.dma("sp", st[:, 0:64], W["c2" + which], writes=[(st, None)])
            p.op("pool", lambda e, st=st: e.tensor_copy(out=W2d[:, 0:64], in_=st[:, 0:64]), reads=[(st, None)], writes=[(W2d, None)])
            p.op("pool", lambda e, st=st: e.tensor_copy(out=W2d[:, 64:128], in_=st[:, 0:64]), reads=[(st, None)], writes=[(W2d, None)])
            hb_, pb_ = ps[0], ps[1]
            srcv = src[0:64, :].rearrange("p (c s) -> p c s", s=16)
            mm_group(cx, hb_[:, 0:255], (hb_, None),
                     [(W1[:, l, :], srcv[:, (l // 16):(l // 16) + 255, l % 16], [(W1, None), (src, None)]) for l in range(32)])
            mm_group(cx, pb_[:, 0:1], (pb_, None), [(W1[:, l, :], posT[:, l:l + 1], [(W1, None), (posT, None)]) for l in range(32)])
            p.op("dve", lambda e: e.tensor_copy(out=posb[:], in_=pb_[:, 0:1]), reads=[(pb_, None)], writes=[(posb, None)])
            p.op("pool", lambda e: e.memset(HID[:], 0.0), writes=[(HID, None)])
            gelu_tanh(cx, hb_[:, 0:255], (hb_, None), posb[:, 0:1], (posb, None), HID[:, 0:255], (HID, None), ga, gb, 255)
            if which == "k":
                ob_ = ps[2]
                p.op("pe", lambda e: e.matmul(ob_[:, 0:256], lhsT=W2d[:], rhs=HID[:], start=True, stop=True), reads=[(W2d, None), (HID, None)], writes=[(ob_, None)])
                p.op("act", lambda e: e.activation(out=KCMP[:], in_=ob_[:, 0:256], func=AF.Copy), reads=[(ob_, None)], writes=[(KCMP, None)])
            else:
                for cc in range(2):
                    ob_ = ps[3 + cc]
                    p.op("pe", lambda e, cc=cc, ob_=ob_: e.matmul(ob_[:, 0:64], lhsT=HID[:, cc * 128:(cc + 1) * 128], rhs=W2d[:, 0:64], start=True, stop=True),
                         reads=[(W2d, None), (HID, None)], writes=[(ob_, None)])
                    p.op("act", lambda e, cc=cc, ob_=ob_: e.activation(out=VCMP[:, cc, 0:64], in_=ob_[:, 0:64], func=AF.Copy), reads=[(ob_, None)], writes=[(VCMP, None)])
        p.barrier()
    with ExitStack() as esa:
        sba = mk_sb(esa)
        OC = sba("OC", [128, 4, 512], F32)
        pcs = [sba(f"pc{i}", [128, 512], BF16) for i in range(2)]
        acc = sba("acc", [128, 4, 64], F32)
        sc = sba("sc", [128, 64], F32)
        sc2 = sba("sc2", [128, 64], F32)
        m8 = sba("m8", [128, 8], F32)
        m8b = sba("m8b", [128, 8], F32)
        rsi = sba("rsi", [128, 1], F32)
        NM = sba("NM", [128, 128], BF16)
        tacc = sba("tacc", [128, 512], F32)
        t2 = sba("t2", [128, 512], F32)
        sbanks = ps[0:3]
        Oacc = [ps[3], ps[4]]
        Gb, IMb = ps[5], ps[6]
        oi = [0]

        def next_O():
            oi[0] += 1
            return Oacc[oi[0] % 2]

        def gate_fac(Ob, n, j, qb, clamp):
            jj = 3 * n + j
            p.op("pe", lambda e: e.matmul(Gb[:], lhsT=sel12[0:12, jj * 128:(jj + 1) * 128], rhs=GLT[0:12, qb * 512:(qb + 1) * 512], start=True, stop=True),
                 reads=[(sel12, None), (GLT, qb)], writes=[(Gb, None)])
            if clamp:
                p.op("dve", lambda e: e.tensor_scalar(out=rs[64:128, :], in0=Ob[64:128, :], scalar1=1e-30, scalar2=None, op0=ALU.max), reads=[(Ob, None)], writes=[(rs, None)])
                p.op("dve", lambda e: e.reciprocal(out=rs[64:128, :], in_=rs[64:128, :]), reads=[(rs, None)], writes=[(rs, None)])
            else:
                p.op("dve", lambda e: e.reciprocal(out=rs[64:128, :], in_=Ob[64:128, :]), reads=[(Ob, None)], writes=[(rs, None)])
            p.op("dve", lambda e: e.tensor_tensor(out=fac[64:128, :], in0=Gb[64:128, :], in1=rs[64:128, :], op=ALU.mult), reads=[(Gb, None), (rs, None)], writes=[(fac, None)])

        for qb in range(8):
            chunks = [0] if qb < 4 else [0, 1]
            for n in range(4):
                r0 = (n % 2) * 64
                Ob = next_O()

                def cmask_fn(cc, qb=qb):
                    delta = 2048 * cc - 512 * qb
                    if delta <= -2560:
                        return []
                    return [(cmpmask[:, (delta + 2048) // 512, :], [(cmpmask, None)])]
                stream = dict(
                    k=lambda cc: (KCMP[r0:r0 + 64, cc * 128:(cc + 1) * 128], [(KCMP, None)]),
                    q=(QA[r0:r0 + 64, n, qb * 512:(qb + 1) * 512], [(QA, (n, qb))]),
                    scale=0.125, bias=None, masks=cmask_fn,
                    pv=[(Ob[:], lambda cc: (VCMP[:, cc, :], [(VCMP, None)]))], pv_dep=(Ob, None))
                keep = []
                run_streams(cx, [stream], chunks, sbanks, pcs, L, keep=keep)
                for t4 in range(4):
                    mm_group(cx, IMb[:, 0:65], (IMb, None),
                             [(pt[:, t4 * 128:(t4 + 1) * 128], ovl[:, cc, :], [(pt, None), (ovl, None)]) for (cc, pt) in keep])
                    p.op("dve", lambda e: e.tensor_scalar(out=rsi[:], in0=IMb[:, 64:65], scalar1=1e-30, scalar2=None, op0=ALU.max), reads=[(IMb, None)], writes=[(rsi, None)])
                    p.op("dve", lambda e: e.reciprocal(out=rsi[:], in_=rsi[:]), reads=[(rsi, None)], writes=[(rsi, None)])
                    if n == 0:
                        p.op("dve", lambda e, t4=t4: e.tensor_scalar(out=acc[:, t4, :], in0=IMb[:, 0:64], scalar1=rsi[:, 0:1], scalar2=None, op0=ALU.mult),
                             reads=[(IMb, None), (rsi, None)], writes=[(acc, t4)])
                    else:
                        p.op("dve", lambda e, t4=t4: e.scalar_tensor_tensor(out=acc[:, t4, :], in0=IMb[:, 0:64], scalar=rsi[:, 0:1], in1=acc[:, t4, :], op0=ALU.mult, op1=ALU.add),
                             reads=[(IMb, None), (rsi, None), (acc, t4)], writes=[(acc, t4)])
                gate_fac(Ob, n, 0, qb, True)
                p.op("dve", lambda e, n=n, Ob=Ob: e.tensor_tensor(out=OC[0:64, n, :], in0=Ob[0:64, :], in1=fac[64:128, :], op=ALU.mult),
                     reads=[(Ob, None), (fac, None)], writes=[(OC, n)])
            for t4 in range(4):
                qt = 4 * qb + t4
                o0 = 62 - 2 * qt
                p.op("dve", lambda e, t4=t4, o0=o0: e.tensor_tensor(out=sc[:], in0=acc[:, t4, :], in1=tkmul[:, o0:o0 + 64], op=ALU.mult), reads=[(acc, t4), (tkmul, None)], writes=[(sc, None)])
                p.op("dve", lambda e, o0=o0: e.tensor_tensor(out=sc[:], in0=sc[:], in1=tkadd[:, o0:o0 + 64], op=ALU.add), reads=[(sc, None), (tkadd, None)], writes=[(sc, None)])
                p.op("dve", lambda e: e.memset(sc[:, 0:1], 1e30), reads=[(sc, None)], writes=[(sc, None)])
                p.op("dve", lambda e: e.max(out=m8[:], in_=sc[:]), reads=[(sc, None)], writes=[(m8, None)])
                p.op("dve", lambda e: e.match_replace(out=sc2[:], in_to_replace=m8[:], in_values=sc[:], imm_value=-3.0e38), reads=[(sc, None), (m8, None)], writes=[(sc2, None)])
                p.op("dve", lambda e: e.max(out=m8b[:], in_=sc2[:]), reads=[(sc2, None)], writes=[(m8b, None)])
                p.op("dve", lambda e: e.tensor_scalar(out=sc2[:], in0=sc[:], scalar1=m8b[:, 7:8], scalar2=None, op0=ALU.is_ge), reads=[(sc, None), (m8b, None)], writes=[(sc2, None)])
                for hf in range(2):
                    p.op("dve", lambda e, hf=hf: e.tensor_scalar(out=NM[:, hf * 64:(hf + 1) * 64], in0=sc2[:], scalar1=-1.0, scalar2=-NEG, op0=ALU.add, op1=ALU.mult),
                         reads=[(sc2, None)], writes=[(NM, None)])
                p.op("pe", lambda e: e.transpose(out=pst[:, 0:128], in_=NM[:], identity=cx.ident[:]), reads=[(NM, None), (cx.ident, None)], writes=[(pst, None)])
                cols = slice(qt * 128, (qt + 1) * 128)
                for n in range(4):
                    rr = slice(64, 128) if n % 2 == 0 else slice(0, 64)
                    eng = "act" if n % 2 == 0 else "dve"
                    if eng == "act":
                        p.op("act", lambda e, n=n, rr=rr, cols=cols: e.activation(out=QA[rr, n, cols], in_=pst[rr, 0:128], func=AF.Copy), reads=[(pst, None)], writes=[(QA, (n, qb))])
                    else:
                        p.op("dve", lambda e, n=n, rr=rr, cols=cols: e.tensor_copy(out=QA[rr, n, cols], in_=pst[rr, 0:128]), reads=[(pst, None)], writes=[(QA, (n, qb))])
            for n in range(4):
                r0 = (n % 2) * 64
                KS = KSA if n % 2 == 0 else KSB
                Ob = next_O()
                stream = dict(
                    k=lambda kb: (KS[:, kb * 128:(kb + 1) * 128], [(KS, None)]),
                    q=(QA[:, n, qb * 512:(qb + 1) * 512], [(QA, (n, qb))]),
                    scale=0.125, bias=None,
                    masks=lambda kb: ([(cmask[:, kb - 4 * qb, :], [(cmask, None)])] if kb >= 4 * qb else []),
                    pv=[(Ob[:], lambda kb: (VS[:, kb, :], [(VS, kb)]))], pv_dep=(Ob, None))
                run_streams(cx, [stream], list(range(4 * qb + 4)), sbanks, pts, L)
                gate_fac(Ob, n, 1, qb, False)
                p.op("dve", lambda e, Ob=Ob: e.tensor_tensor(out=tacc[0:64, :], in0=Ob[0:64, :], in1=fac[64:128, :], op=ALU.mult), reads=[(Ob, None), (fac, None)], writes=[(tacc, None)])
                p.op("pool", lambda e, n=n: e.tensor_tensor(out=tacc[0:64, :], in0=tacc[0:64, :], in1=OC[0:64, n, :], op=ALU.add), reads=[(tacc, None), (OC, n)], writes=[(tacc, None)])
                Ob2 = next_O()

                def wmasks(kb, qb=qb):
                    if kb >= 4 * qb:
                        return [(cmask[:, kb - 4 * qb, :], [(cmask, None)])]
                    return [(cmask[:, 4 + kb - (4 * qb - 4), :], [(cmask, None)])]
                stream = dict(
                    k=lambda kb: (KW2[r0:r0 + 64, kb * 128:(kb + 1) * 128], [(KW2, kb // 4)]),
                    q=(QA[r0:r0 + 64, n, qb * 512:(qb + 1) * 512], [(QA, (n, qb))]),
                    scale=0.125, bias=None, masks=wmasks,
                    pv=[(Ob2[:], lambda kb: (VW[:, kb, :], [(VW, kb)]))], pv_dep=(Ob2, None))
                run_streams(cx, [stream], list(range(max(0, 4 * qb - 4), 4 * qb + 4)), sbanks, pts, L)
                gate_fac(Ob2, n, 2, qb, False)
                p.op("dve", lambda e, Ob2=Ob2: e.tensor_tensor(out=t2[0:64, :], in0=Ob2[0:64, :], in1=fac[64:128, :], op=ALU.mult), reads=[(Ob2, None), (fac, None)], writes=[(t2, None)])
                ot = obt[n % 2]
                p.op("pool", lambda e, ot=ot: e.tensor_tensor(out=ot[0:64, :], in0=tacc[0:64, :], in1=t2[0:64, :], op=ALU.add), reads=[(tacc, None), (t2, None)], writes=[(ot, None)])
                p.dma("sp", oT_ap[256 + n * 64:256 + (n + 1) * 64, qb * 512:(qb + 1) * 512], ot[0:64, :], reads=[(ot, None)], writes=[(cx.outb, ("oTn", n, qb))])
        p.barrier()


import numpy as np, math
import ml_dtypes
def np_bf16(a):
    return np.asarray(a, dtype=np.float32).astype(ml_dtypes.bfloat16)

def swap_cols(w):
    w = w.reshape(w.shape[0], -1, 64).copy()
    a = w[:, :, 0:8].copy(); w[:, :, 0:8] = w[:, :, 8:16]; w[:, :, 8:16] = a
    return w.reshape(w.shape[0], -1)

def odd_w_own(w_in, hh):
    q = w_in[:, 512 * hh:512 * hh + 512]; k = w_in[:, 1024 + 512 * hh:1024 + 512 * hh + 512]; v = w_in[:, 2048 + 512 * hh:2048 + 512 * hh + 512]
    return np.ascontiguousarray(np.concatenate([q, swap_cols(q), k, swap_cols(k), v], axis=1))

def even_w_own(w, hh):
    def c(o, n): return w[:, o:o + n]
    fq = c(256 * hh, 256); fk = c(512 + 256 * hh, 256); fv = c(1024 + 256 * hh, 256); fl = c(1536 + 4 * hh, 4)
    nq = c(1544 + 256 * hh, 256); kc = c(2056 + 64 * hh, 64); vc = c(2184 + 64 * hh, 64); ks = c(2312 + 64 * hh, 64)
    vs = c(2440 + 64 * hh, 64); kw = c(2568 + 64 * hh, 64); vw = c(2696 + 64 * hh, 64); gl = c(2824 + 12 * hh, 12)
    return np.ascontiguousarray(np.concatenate([fq, fk, fv, nq, swap_cols(nq), kc, swap_cols(kc), ks, swap_cols(ks), kw, swap_cols(kw), vc, vs, vw, fl, gl], axis=1))

def host_consts():
    c = {}
    c["ident"] = np_bf16(np.eye(128))
    k = np.arange(128)[:, None]; q = np.arange(512)[None, :]
    cm = np.stack([(128 * j + k <= q) for j in range(4)]).astype(np.float32)
    c["cmask"] = np_bf16(np.concatenate([cm, 1.0 - cm], axis=0).transpose(1, 0, 2).reshape(128, 8 * 512))
    inv = (500000.0 ** (-np.arange(0, 16, 2, dtype=np.float64) / 16)) / (2 * np.pi)
    r = np.zeros((128, 2), np.float32)
    for p_ in range(128):
        j = p_ % 64
        if j < 8: r[p_, 0] = -inv[j]; r[p_, 1] = inv[j]
        elif j < 16: r[p_, 0] = inv[j - 8]; r[p_, 1] = inv[j - 8]
    c["ropeinv"] = r
    cmp = np.stack([(16 * k + 31 + (-2048 + 512 * i) <= q) for i in range(5)]).astype(np.float32)
    c["cmpmask"] = np_bf16(cmp.transpose(1, 0, 2).reshape(128, 5 * 512))
    cs = np.arange(256)[:, None] * 16; ss = np.arange(64)[None, :] * 64
    ov = np.clip(np.minimum(cs + 32, ss + 64) - np.maximum(cs, ss), 0, None) / 32.0
    ov[255] = 0
    ovl = np.concatenate([ov, np.ones((256, 1))], axis=1).reshape(2, 128, 65).transpose(1, 0, 2).reshape(128, 130)
    c["ovl"] = np_bf16(ovl)
    sel = np.zeros((12, 12, 128), np.float32)
    for j in range(12): sel[j, j, :] = 1
    c["sel12"] = np_bf16(sel.reshape(12, 12 * 128))
    add = np.zeros((128, 128), np.float32); mul = np.zeros((128, 128), np.float32)
    for p_ in range(128):
        cur = 1 if p_ >= 64 else 0
        for i in range(128):
            s_ = i - 62
            valid = s_ <= cur
            forced = (s_ == cur) or (s_ == cur - 1)
            if not valid: add[p_, i] = -1e30
            elif forced: add[p_, i] = 1e30
            else: mul[p_, i] = 1.0
    c["tkadd"] = add; c["tkmul"] = mul
    c["onehot"] = np_bf16((np.arange(4096)[None, :] // 64 == np.arange(64)[:, None]).astype(np.float32))
    c["tri"] = (np.arange(128)[:, None] <= np.arange(128)[None, :]).astype(np.float32)
    s127 = np.zeros((128, 128), np.float32); s127[127, :] = 1
    c["sel127"] = s127
    return c


from concourse.bass_utils import run_bass_kernel_spmd

CONST_SPECS = {"cmask": ([128, 4096], BF16), "ropeinv": ([128, 2], F32), "cmpmask": ([128, 2560], BF16), "ovl": ([128, 130], BF16), "sel12": ([12, 1536], BF16),
               "tkadd": ([128, 128], F32), "tkmul": ([128, 128], F32), "onehot": ([64, 4096], BF16), "tri": ([128, 128], F32), "sel127": ([128, 128], F32)}
GROUPS = [[0, 1], [2, 3], [4, 5], [6, 7]]
_PROG = {}
DEPTH = 4


class OTMap:
    def __init__(self, ap_a, ap_b):
        self.aps = (ap_a, ap_b)

    def __getitem__(self, idx):
        rows, cols = idx
        qb = cols.start // 512
        half, c0 = qb // 4, (qb % 4) * 512
        k = rows.start // 256
        assert (rows.stop - 1) // 256 == k
        r0, r1 = rows.start - 256 * k, rows.stop - 256 * k
        return self.aps[k][half * 256 + r0:half * 256 + r1, c0:c0 + 512]


def build_fused():
    nc = bass.Bass("TRN2", target_bir_lowering=False)

    def din(name, shape, dt=F32):
        return nc.dram_tensor(name, list(shape), dt, kind="ExternalInput").ap()
    ident = din("ident", [128, 128], BF16)
    x_in = din("x_in", [TOK, D])
    C = {k: din(k, s, dt) for k, (s, dt) in CONST_SPECS.items()}
    pos = din("pos", [1, T], I32)
    mem = din("mem", [MEM, D])
    g_all = din("g_all", [DEPTH * 6, D])
    mem_g = din("mem_g", [DEPTH, D])
    WL = []
    for l in range(DEPTH):
        W = {"g": g_all[l * 6:(l + 1) * 6, :], "mem": mem, "mem_g": mem_g[l:l + 1, :], "pos": pos}
        W.update(C)
        for nm, shp in (("w_out", [D, D]), ("ca_wq", [D, 256]), ("ca_wk", [D, 256]), ("ca_wv", [D, 256]), ("ca_wo", [256, D]),
                        ("ffn_wg", [D, DFF]), ("ffn_wu", [D, DFF]), ("ffn_wd", [DFF, D])):
            W[nm] = din(f"{nm}_{l}", shp)
        if l % 2 == 0:
            for nm, shp in (("w_in", [D, EV_NCOL]), ("fbias_rep", [1, 128]), ("c1k", [2048, 128]), ("c2k", [128, 64]), ("cpkT", [64, 32]),
                            ("c1v", [2048, 128]), ("c2v", [128, 64]), ("cpvT", [64, 32])):
                W[nm] = din(f"{nm}_{l}", shp)
        else:
            for nm, shp in (("w_in", [D, 2560]), ("lam", [1, 256]), ("subg", [128, 1]), ("laminit", [1, 2])):
                W[nm] = din(f"{nm}_{l}", shp)
        WL.append(W)
    x_out = nc.dram_tensor("x_out", [TOK, D], F32, kind="ExternalOutput").ap()
    hT_own_t = [nc.dram_tensor(f"hT_own{k}", [512, TOK], BF16) for k in range(2)]
    hT_g_t = [nc.dram_tensor(f"hT_g{k}", [1024, TOK], BF16) for k in range(2)]
    oT_own_t = [nc.dram_tensor(f"oT_own{k}", [512, TOK], BF16) for k in range(2)]
    oT_g_t = [nc.dram_tensor(f"oT_g{k}", [1024, TOK], BF16) for k in range(2)]
    x_scr_t = nc.dram_tensor("x_scr", [TOK, D], F32)
    x_scr = x_scr_t.ap()
    HTO, HTG, OTO, OTG, XS = Buf(None, "hT_own"), Buf(None, "hT_g"), Buf(None, "oT_own"), Buf(None, "oT_g"), Buf(None, "x_scr")
    cx = make_ctx(nc, ident)
    p = cx.p
    cx.outb = HTO
    pid = nc.sync.partition_id()
    hh256 = (pid % 2) * 256

    def gather_hT():
        for k in range(2):
            p.collective("AllGather", hT_own_t[k].ap().opt(), hT_g_t[k].ap().opt(), GROUPS, reads=[(HTO, None)], writes=[(HTG, k)])

    def gather_oT():
        for k in range(2):
            p.collective("AllGather", oT_own_t[k].ap().opt(), oT_g_t[k].ap().opt(), GROUPS, reads=[(OTO, None)], writes=[(OTG, k)])

    def hT_store(hT, tb):
        for k in range(2):
            p.dma("sp", hT_own_t[k].ap()[:, tb * TB:(tb + 1) * TB].rearrange("(c p) n -> p c n", p=128), hT[:, 4 * k:4 * k + 4, :],
                  reads=[(hT, None)], writes=[(HTO, (k, tb))])

    def hT_chunk(r, c):
        return hT_g_t[c // 4].ap()[r * 512 + (c % 4) * 128:r * 512 + (c % 4 + 1) * 128, :]

    def x_view(ap):
        return ap.rearrange("(t p) d -> p t d", p=128)

    with ExitStack() as es:
        def sb(name, shape, dt):
            return Buf(es.enter_context(nc.sbuf_tensor(name + "_a0", list(shape), dt)), name)
        x = sb("x", [128, NT, D], F32)
        p.dma("sp", x[:], x_view(x_in), writes=[(x, None)])
        cx.ps, cx.pst = psum_set(cx, es, 1, True)
        L = {"gB": sb("gB", [128, D], F32), "stat": sb("stat", [128, 16], F32), "junk": sb("junk", [128, D], BF16),
             "hb": [sb(f"hb{i}", [128, D], BF16) for i in range(2)]}
        hT = sb("hT", [128, 8, TOK], BF16)
        norm_transpose(cx, x, list(range(NT)), g_all[0:1, :], hT, L)
        for k in range(2):
            p.dma("sp", hT_own_t[k].ap().rearrange("(c p) n -> p c n", p=128), hT[:, 4 * k:4 * k + 4, :], reads=[(hT, None)], writes=[(HTO, (k, 0))])
        p.barrier()
    gather_hT()

    for l in range(DEPTH):
        W = WL[l]
        cx.outb = OTO
        if l % 2 == 0:
            def hT_src(dst_ap, c, tok0, n, q, writes):
                r = tok0 // TOK
                p.dma(q, dst_ap, hT_chunk(r, c)[:, tok0 % TOK:tok0 % TOK + n], reads=[(HTG, None)], writes=writes)
            phase_B_even(cx, hT_src, W, OTMap(oT_own_t[0].ap(), oT_own_t[1].ap()))
        else:
            def load_hT(hT):
                for c in range(8):
                    for r in range(2):
                        p.dma("sp" if c % 2 == 0 else "pool", hT[:, c, r * TOK:(r + 1) * TOK], hT_chunk(r, c),
                              reads=[(HTG, None)], writes=[(hT, None)])
            phase_B_odd(cx, load_hT, W, OTMap(oT_own_t[0].ap(), oT_own_t[1].ap()))
        p.barrier()
        gather_oT()
        cx.outb = HTO
        with ExitStack() as es:
            x = Buf(es.enter_context(nc.sbuf_tensor(f"x_l{l}", [128, NT, D], F32)), "x")
            p.dma("sp", x[:], x_view(x_in if l == 0 else x_scr), reads=[(XS, None)], writes=[(x, None)])

            def oT_load(oT, tb, writes):
                for r in range(2):
                    for k in range(2):
                        src = oT_g_t[k].ap()[bass.ds(hh256 + r * 512, 256), tb * TB:(tb + 1) * TB]
                        p.dma("sp", oT[:, 4 * r + 2 * k:4 * r + 2 * k + 2, :], src.rearrange("(c p) n -> p c n", p=128), reads=[(OTG, None)], writes=writes)
            last = (l == DEPTH - 1)
            phase_C(cx, x, None, W, None, None if last else g_all[(l + 1) * 6:(l + 1) * 6 + 1, :], oT_load=oT_load, hT_store=None if last else hT_store)
            if last:
                OUT = Buf(None, "x_out")
                p.dma("sp", x_view(x_out), x[:], reads=[(x, None)], writes=[(OUT, None)])
                p.finish([OUT])
            else:
                p.dma("sp", x_view(x_scr), x[:], reads=[(x, None)], writes=[(XS, None)])
                p.barrier()
        if not last:
            gather_hT()
    print("fused program: n_inst", p.n_inst, "n_wait", p.n_wait)
    p.close()
    return nc


def _ca(a):
    return np.ascontiguousarray(a)


def kernel(x, mem, positions, sandwich_g, mem_norm_g, ev_w_in, ev_fox_fbias,
           ev_cmp_pos_k, ev_cmp_w1_k, ev_cmp_w2_k, ev_cmp_pos_v, ev_cmp_w1_v, ev_cmp_w2_v,
           ev_w_out, od_w_in, od_lambda, od_subln_g, od_w_out,
           ca_wq, ca_wk, ca_wv, ca_wo, ffn_wg, ffn_wu, ffn_wd):
    f32 = lambda a: np.asarray(a, dtype=np.float32)
    x = f32(x); mem = f32(mem); positions = np.asarray(positions).astype(np.int32)
    sandwich_g = f32(sandwich_g); mem_norm_g = f32(mem_norm_g)
    hc = host_consts()
    if "nc" not in _PROG:
        _PROG["nc"] = build_fused()
    nc = _PROG["nc"]
    cores = list(range(8))
    shared = {k: hc[k] for k in CONST_SPECS}
    shared["ident"] = hc["ident"]
    shared["g_all"] = _ca(sandwich_g.reshape(DEPTH * 6, D))
    shared["mem_g"] = _ca(mem_norm_g)
    per_h = [dict(), dict()]
    for l in range(DEPTH):
        for nm, arr in (("ca_wq", ca_wq), ("ca_wk", ca_wk), ("ca_wv", ca_wv), ("ca_wo", ca_wo), ("ffn_wg", ffn_wg), ("ffn_wu", ffn_wu), ("ffn_wd", ffn_wd)):
            shared[f"{nm}_{l}"] = _ca(f32(arr[l]))
        if l % 2 == 0:
            e = l // 2
            wo = f32(ev_w_out[e])
            shared[f"w_out_{l}"] = _ca(np.concatenate([wo[0:256], wo[512:768], wo[256:512], wo[768:1024]], axis=0))
            shared[f"c1k_{l}"] = _ca(f32(ev_cmp_w1_k[e])); shared[f"c2k_{l}"] = _ca(f32(ev_cmp_w2_k[e])); shared[f"cpkT_{l}"] = _ca(f32(ev_cmp_pos_k[e]).T)
            shared[f"c1v_{l}"] = _ca(f32(ev_cmp_w1_v[e])); shared[f"c2v_{l}"] = _ca(f32(ev_cmp_w2_v[e])); shared[f"cpvT_{l}"] = _ca(f32(ev_cmp_pos_v[e]).T)
            for hh in range(2):
                per_h[hh][f"w_in_{l}"] = even_w_own(f32(ev_w_in[e]), hh)
                per_h[hh][f"fbias_rep_{l}"] = _ca(np.tile(f32(ev_fox_fbias[e])[4 * hh:4 * hh + 4], 32)[None, :])
        else:
            o = l // 2
            lam_init = 0.8 - 0.6 * math.exp(-0.3 * l)
            shared[f"w_out_{l}"] = _ca(f32(od_w_out[o]))
            shared[f"lam_{l}"] = _ca(f32(od_lambda[o]).reshape(1, 256))
            shared[f"subg_{l}"] = _ca(f32(od_subln_g[o]).reshape(128, 1))
            shared[f"laminit_{l}"] = np.array([[-lam_init, 1.0 - lam_init]], np.float32)
            for hh in range(2):
                per_h[hh][f"w_in_{l}"] = odd_w_own(f32(od_w_in[o]), hh)
    maps = []
    for c in cores:
        b, hh = c // 2, c % 2
        m = dict(shared)
        m.update(per_h[hh])
        m["x_in"] = _ca(x[b, TOK * hh:TOK * (hh + 1)])
        m["pos"] = _ca(positions[b:b + 1])
        m["mem"] = _ca(mem[b])
        maps.append(m)
    res = run_bass_kernel_spmd(nc, maps, core_ids=cores)
    out = np.zeros((4, T, D), np.float32)
    for c in cores:
        out[c // 2, TOK * (c % 2):TOK * (c % 2 + 1)] = res.results[c]["x_out"]
    return out
```

```python
from contextlib import ExitStack
import numpy as np
import concourse.bass as bass
import concourse.mybir as mybir

F32 = mybir.dt.float32
BF16 = mybir.dt.bfloat16
I32 = mybir.dt.int32
AF = mybir.ActivationFunctionType
ALU = mybir.AluOpType
AX = mybir.AxisListType


class Buf:
    _n = 0

    def __init__(self, t, name):
        self.t = t
        self.name = name
        self.regions = {}
        self.whole = [None, {}]

    def __getitem__(self, idx):
        return self.t[idx]


class Prog:
    ENG = ["pe", "dve", "act", "pool", "sp"]

    def __init__(self, nc, n_dma_sems=10):
        self.nc = nc
        self.es = ExitStack()
        self.eng = {"pe": nc.tensor, "dve": nc.vector, "act": nc.scalar, "pool": nc.gpsimd, "sp": nc.sync}
        self.sem = {e: self.es.enter_context(nc.semaphore("s_" + e)) for e in self.ENG}
        self.cnt = {e: 0 for e in self.ENG}
        self.sem["cc"] = self.es.enter_context(nc.semaphore("s_cc"))
        self.cnt["cc"] = 0
        self.waited = {}
        self.dsem = {}
        self.dval = {}
        self.dnext = {}
        for q in ["sp", "act", "pool"]:
            self.dsem[q] = [self.es.enter_context(nc.semaphore(f"d_{q}{i}")) for i in range(n_dma_sems)]
            self.dval[q] = [0] * n_dma_sems
            self.dnext[q] = 0
        self.dwaited = {}
        self.n_inst = 0
        self.n_wait = 0

    def sbuf(self, name, shape, dtype):
        t = self.es.enter_context(self.nc.sbuf_tensor(name, list(shape), dtype))
        return Buf(t, name)

    def psum(self, name, shape, dtype):
        t = self.es.enter_context(self.nc.psum_tensor(name, list(shape), dtype))
        return Buf(t, name)

    def close(self):
        self.es.close()

    def _states(self, buf, key):
        if key is None:
            return [buf.whole] + list(buf.regions.values())
        if key not in buf.regions:
            buf.regions[key] = [None, {}]
        return [buf.whole, buf.regions[key]]

    def _need(self, deps, tok):
        if tok is not None:
            deps.add(tok)

    def _collect(self, reads, writes):
        deps = set()
        for (b, k) in reads:
            for st in self._states(b, k):
                self._need(deps, st[0])
        for (b, k) in writes:
            for st in self._states(b, k):
                self._need(deps, st[0])
                for tok in st[1].values():
                    deps.add(tok)
        return deps

    def _emit_waits(self, e, deps, skip_same=False):
        engobj = self.eng[e]
        best = {}
        for tok in deps:
            if tok[0] == "e":
                _, f, c = tok
                if f == e and skip_same:
                    continue
                key = ("e", f)
                best[key] = max(best.get(key, 0), c)
            else:
                _, q, i, v = tok
                key = ("d", q, i)
                best[key] = max(best.get(key, 0), v)
        for key, v in best.items():
            wk = (e,) + key
            if self.waited.get(wk, -1) >= v:
                continue
            self.waited[wk] = v
            if key[0] == "e":
                engobj.wait_ge(self.sem[key[1]], v)
            else:
                engobj.wait_ge(self.dsem[key[1]][key[2]], v)
            self.n_wait += 1

    def _record(self, tok, reads, writes):
        for (b, k) in reads:
            if k is None:
                b.whole[1][tok[1] if tok[0] == "e" else ("d",) + tok[1:3]] = tok
            else:
                st = self._states(b, k)[1]
                st[1][tok[1] if tok[0] == "e" else ("d",) + tok[1:3]] = tok
        for (b, k) in writes:
            if k is None:
                b.regions.clear()
                b.whole[0] = tok
                b.whole[1] = {}
            else:
                st = self._states(b, k)[1]
                st[0] = tok
                st[1] = {}

    def alias(self, ap, name):
        return Buf(ap, name)

    def barrier(self):
        alld = set()
        for e in self.ENG + ["cc"]:
            if self.cnt[e] > 0:
                alld.add(("e", e, self.cnt[e]))
        for q in self.dsem:
            for i, v in enumerate(self.dval[q]):
                if v > 0:
                    alld.add(("d", q, i, v))
        for e in self.ENG:
            self._emit_waits(e, alld)

    def op(self, e, fn, reads=(), writes=(), skip_same=False, inc=True):
        deps = self._collect(reads, writes)
        if e == "pe":
            skip_same = True
        if skip_same is False and e in ("dve", "act", "pool"):
            raw = set()
            for (b, k) in reads:
                for st in self._states(b, k):
                    if st[0] is not None:
                        raw.add(st[0])
            deps = {t for t in deps if not (t[0] == "e" and t[1] == e) or t in raw}
        self._emit_waits(e, deps, skip_same=skip_same)
        inst = fn(self.eng[e])
        if inc:
            self.cnt[e] += 1
            inst.then_inc(self.sem[e], 1)
            tok = ("e", e, self.cnt[e])
        else:
            tok = ("e", e, self.cnt[e] + 1)
        self._record(tok, reads, writes)
        self.n_inst += 1
        return inst

    def dma(self, q, out_ap, in_ap, reads=(), writes=(), **kw):
        e = q
        deps = self._collect(reads, writes)
        i = self.dnext[q]
        self.dnext[q] = (i + 1) % len(self.dsem[q])
        if self.dval[q][i] > 0:
            deps.add(("d", q, i, self.dval[q][i]))
        self._emit_waits(e, deps)
        self.dval[q][i] += 16
        inst = self.eng[e].dma_start(out=out_ap, in_=in_ap, **kw)
        inst.then_inc(self.dsem[q][i], 16)
        tok = ("d", q, i, self.dval[q][i])
        self._record(tok, reads, writes)
        self.n_inst += 1
        return inst

    def collective(self, kind, in_ap, out_ap, groups, reads=(), writes=()):
        deps = self._collect(reads, writes)
        self._emit_waits("pool", deps)
        self.cnt["cc"] += 1
        inst = self.nc.gpsimd.collective_compute(kind, mybir.AluOpType.bypass, replica_groups=groups, ins=[in_ap], outs=[out_ap])
        inst.then_inc(self.sem["cc"])
        tok = ("e", "cc", self.cnt["cc"])
        self._record(tok, reads, writes)
        self.n_inst += 1
        return inst

    def finish(self, out_bufs):
        deps = set()
        for b in out_bufs:
            for st in [b.whole] + list(b.regions.values()):
                if st[0] is not None:
                    deps.add(st[0])
        self._emit_waits("sp", deps)
        alld = set()
        for e in self.ENG + ["cc"]:
            if self.cnt[e] > 0:
                alld.add(("e", e, self.cnt[e]))
        for q in self.dsem:
            for i, v in enumerate(self.dval[q]):
                if v > 0:
                    alld.add(("d", q, i, v))
        self._emit_waits("sp", alld)


import math
import numpy as np
import ml_dtypes
from contextlib import ExitStack

D = 1024
T = 4096
TOK = 2048
NT = TOK // 128
TB = 1024
DFF = 2816
NFF = DFF // 128
EPS = 1e-6
MEM = 256
NEG = -30000.0


def np_bf16(a):
    return np.asarray(a, dtype=np.float32).astype(ml_dtypes.bfloat16)


class Ctx:
    pass


def make_ctx(nc, ident_ap):
    cx = Ctx()
    cx.nc = nc
    p = Prog(nc)
    cx.p = p
    cx.uid = 0
    cx.ident = p.sbuf("ident_sb", [128, 128], BF16)
    p.dma("sp", cx.ident[:], ident_ap, writes=[(cx.ident, None)])
    cx.psi = 0
    cx.outb = Buf(None, "dram_out")
    cx.epsb = p.sbuf("epsb", [128, 1], F32)
    p.op("pool", lambda e: e.memset(cx.epsb[:], EPS), writes=[(cx.epsb, None)])
    return cx


def rot(cx, n=7):
    b = cx.ps[cx.psi % n]
    cx.psi += 1
    return b


def mm_group(cx, bank_ap, bank_dep, pairs):
    p = cx.p
    n = len(pairs)
    for i, (lhsT, rhs, reads) in enumerate(pairs):
        p.op("pe", lambda e, lhsT=lhsT, rhs=rhs, i=i: e.matmul(bank_ap, lhsT=lhsT, rhs=rhs, start=(i == 0), stop=(i == n - 1)),
             reads=reads, writes=[bank_dep], inc=(i == n - 1))


def cast_copy(cx, dst_ap, src_ap, reads, writes):
    p = cx.p
    cx.cast_i = getattr(cx, "cast_i", 0) + 1
    if cx.cast_i % 2 == 0:
        p.op("dve", lambda e: e.tensor_copy(out=dst_ap, in_=src_ap), reads=reads, writes=writes)
    else:
        p.op("act", lambda e: e.activation(out=dst_ap, in_=src_ap, func=AF.Copy), reads=reads, writes=writes)


def load_w_bf16(cx, dst_ap, dst_dep, w_ap, kc, ncols, stage, q="sp", cast_eng="pool"):
    p = cx.p
    assert kc * ncols <= 2048
    sv = stage[:, 0:kc * ncols].rearrange("p (c n) -> p c n", c=kc)
    p.dma(q, sv, w_ap.rearrange("(c p) n -> p c n", p=128), writes=[(stage, None)])
    cast_copy(cx, dst_ap, sv, [(stage, None)], [dst_dep])


def rms_ss(cx, src_ap, src_dep, ncol, stat, junk, key):
    p = cx.p
    p.op("act", lambda e: e.activation(out=junk[:, 0:ncol], in_=src_ap, func=AF.Square, accum_out=stat[:, key:key + 1]),
         reads=[src_dep], writes=[(junk, None), (stat, key)])


def rstd_from_ss(cx, stat, k0, k1, dim):
    p = cx.p
    p.op("act", lambda e: e.activation(out=stat[:, k0:k1], in_=stat[:, k0:k1], func=AF.Sqrt, scale=1.0 / dim, bias=cx.epsb[:, 0:1]),
         reads=[(stat, None), (cx.epsb, None)], writes=[(stat, None)])
    p.op("dve", lambda e: e.reciprocal(out=stat[:, k0:k1], in_=stat[:, k0:k1]), reads=[(stat, None)], writes=[(stat, None)])


def norm_transpose(cx, x, tiles, g_ap, hT, L, q="sp"):
    p = cx.p
    gB, stat, junk, hb = L["gB"], L["stat"], L["junk"], L["hb"]
    p.dma(q, gB[:], g_ap.to_broadcast([128, D]), writes=[(gB, None)])
    n = len(tiles)
    for j, t in enumerate(tiles):
        rms_ss(cx, x[:, t, :], (x, t), D, stat, junk, j)
    rstd_from_ss(cx, stat, 0, n, D)
    for j, t in enumerate(tiles):
        hbt = hb[j % 2]
        p.op("dve", lambda e, t=t, j=j, hbt=hbt: e.scalar_tensor_tensor(out=hbt[:], in0=x[:, t, :], scalar=stat[:, j:j + 1], in1=gB[:],
                                                                         op0=ALU.mult, op1=ALU.mult),
             reads=[(x, t), (stat, None), (gB, None)], writes=[(hbt, None)])
        for c in range(8):
            p.op("pe", lambda e, c=c, hbt=hbt: e.transpose(out=cx.pst[:, c * 128:(c + 1) * 128], in_=hbt[:, c * 128:(c + 1) * 128], identity=cx.ident[:]),
                 reads=[(hbt, None), (cx.ident, None)], writes=[(cx.pst, None)], inc=(c == 7))
        src = cx.pst[:].rearrange("p (c n) -> p c n", c=8)
        if j % 2 == 0:
            p.op("act", lambda e, j=j, src=src: e.activation(out=hT[:, :, j * 128:(j + 1) * 128], in_=src, func=AF.Copy),
                 reads=[(cx.pst, None)], writes=[(hT, j)])
        else:
            p.op("dve", lambda e, j=j, src=src: e.tensor_copy(out=hT[:, :, j * 128:(j + 1) * 128], in_=src),
                 reads=[(cx.pst, None)], writes=[(hT, j)])


def norm_residual(cx, x, t, banks, g_ready_gB, L):
    p = cx.p
    stat2, junk, tmp, gB = L["stat2"], L["junk"], L["tmp"], g_ready_gB
    for nb in range(2):
        rms_ss(cx, banks[nb][:], (banks[nb], None), 512, stat2, junk, nb)
    p.op("dve", lambda e: e.tensor_tensor(out=stat2[:, 2:3], in0=stat2[:, 0:1], in1=stat2[:, 1:2], op=ALU.add),
         reads=[(stat2, None)], writes=[(stat2, None)])
    rstd_from_ss(cx, stat2, 2, 3, D)
    for nb in range(2):
        p.op("dve", lambda e, nb=nb, b=banks[nb]: e.scalar_tensor_tensor(out=tmp[:, nb * 512:(nb + 1) * 512], in0=b[:], scalar=stat2[:, 2:3],
                                                                        in1=gB[:, nb * 512:(nb + 1) * 512], op0=ALU.mult, op1=ALU.mult),
             reads=[(banks[nb], None), (stat2, None), (gB, None)], writes=[(tmp, nb)])
    p.op("pool", lambda e, t=t: e.tensor_tensor(out=x[:, t, :], in0=x[:, t, :], in1=tmp[:], op=ALU.add),
         reads=[(tmp, None), (x, t)], writes=[(x, t)])


def proj_residual(cx, x, tiles, lhs_fn, kc, w, g_ap, L, q="sp"):
    p = cx.p
    gB = L["gB"]
    p.dma(q, gB[:], g_ap.to_broadcast([128, D]), writes=[(gB, None)])
    for j, t in enumerate(tiles):
        banks = [rot(cx), rot(cx)]
        for nb in range(2):
            pairs = []
            for c in range(kc):
                lhsT, ldep = lhs_fn(c, j)
                pairs.append((lhsT, w[:, c, nb * 512:(nb + 1) * 512], [ldep, (w, None)]))
            mm_group(cx, banks[nb][:], (banks[nb], None), pairs)
        norm_residual(cx, x, t, banks, gB, L)


def phase_C(cx, x, oT_ap, W, hT_next_ap, g_next_ap, oT_load=None, hT_store=None, wscr=None):
    p = cx.p
    with ExitStack() as es:
        cx.uid += 1
        uu = cx.uid
        def sb(name, shape, dt):
            return Buf(es.enter_context(cx.nc.sbuf_tensor(f"{name}_{uu}", list(shape), dt)), name)
        cx.ps, cx.pst = psum_set(cx, es, 7, True)
        cx.psi = 0
        L = {}
        L["gB"] = sb("gB", [128, D], F32)
        L["stat"] = sb("stat", [128, 16], F32)
        L["stat2"] = sb("stat2", [128, 4], F32)
        L["junk"] = sb("junk", [128, D], BF16)
        L["tmp"] = sb("tmp", [128, D], F32)
        L["hb"] = [sb(f"hb{i}", [128, D], BF16) for i in range(2)]
        hT = sb("hT", [128, 8, TB], BF16)
        big = sb("big", [128, NFF, TB], BF16)
        stage = [sb(f"stage{i}", [128, 2048], F32) for i in range(2)]
        wsm = sb("wsm", [128, 8, 1024], BF16)
        memT = sb("memT", [128, 8, MEM], BF16)
        kmT = sb("kmT", [128, 2, MEM], BF16)
        vm = sb("vm", [128, 2, 4, 128], BF16)
        pT = [sb(f"pTc{i}", [128, 512], BF16) for i in range(4)]
        rs = sb("rs_c", [128, 512], F32)
        wg = [sb(f"wg{i}", [128, 8, 128], BF16) for i in range(2)]
        wu = [sb(f"wu{i}", [128, 8, 128], BF16) for i in range(2)]
        wd = [sb(f"wd{i}", [128, 1024], BF16) for i in range(3)]
        gsb = [sb(f"gsb{i}", [128, 512], BF16) for i in range(2)]
        bigflat = big.t[:].rearrange("p a b -> p (a b)")
        memx = p.alias(stage[1][:].rearrange("p (t d) -> p t d", t=2), "memx")
        p.dma("sp", memx[:], W["mem"].rearrange("(t p) d -> p t d", p=128), writes=[(memx, None), (stage[1], None)])
        norm_transpose(cx, memx, [0, 1], W["mem_g"], memT, L)
        p.barrier()
        sti = 0
        for (nm, c0) in (("ca_wk", 0), ("ca_wv", 256)):
            load_w_bf16(cx, wsm[:, :, c0:c0 + 256], (wsm, None), W[nm], 8, 256, stage[sti % 2], q="pool")
            sti += 1
        for ht in range(2):
            b = rot(cx)
            mm_group(cx, b[:, 0:MEM], (b, None), [(wsm[:, c, ht * 128:(ht + 1) * 128], memT[:, c, :], [(wsm, None), (memT, None)]) for c in range(8)])
            p.op("act", lambda e, b=b, ht=ht: e.activation(out=kmT[:, ht, :], in_=b[:, 0:MEM], func=AF.Copy), reads=[(b, None)], writes=[(kmT, None)])
        p.op("pool", lambda e: e.memset(vm[:], 1.0), writes=[(vm, None)])
        for mc in range(2):
            b = rot(cx)
            mm_group(cx, b[:, 0:256], (b, None), [(memT[:, c, mc * 128:(mc + 1) * 128], wsm[:, c, 256:512], [(wsm, None), (memT, None)]) for c in range(8)])
            p.op("act", lambda e, b=b, mc=mc: e.activation(out=vm[:, mc, :, 0:64], in_=b[:, 0:256].rearrange("p (h d) -> p h d", h=4), func=AF.Copy),
                 reads=[(b, None)], writes=[(vm, None)])
        for tb in range(TOK // TB):
            tiles = list(range(tb * 8, tb * 8 + 8))
            tsl = slice(tb * TB, (tb + 1) * TB)
            oT = p.alias(bigflat[:, 0:8 * TB].rearrange("p (c n) -> p c n", c=8), "oT")
            if oT_load is None:
                p.dma("sp", oT[:], oT_ap[:, tsl].rearrange("(c p) n -> p c n", p=128), writes=[(oT, None), (big, None)])
            else:
                oT_load(oT, tb, [(oT, None), (big, None)])
            for qd in range(4):
                load_w_bf16(cx, wsm[:, :, qd * 256:(qd + 1) * 256], (wsm, None), W["w_out"][:, qd * 256:(qd + 1) * 256], 8, 256, stage[sti % 2], q="pool")
                sti += 1
            proj_residual(cx, x, tiles, lambda c, j: (oT[:, c, j * 128:(j + 1) * 128], (oT, None)), 8, wsm, W["g"][1:2, :], L)
            p.barrier()
            qcT = p.alias(bigflat[:, 0:2 * TB].rearrange("p (c n) -> p c n", c=2), "qcT")
            ocT = p.alias(bigflat[:, 2 * TB:4 * TB].rearrange("p (c n) -> p c n", c=2), "ocT")
            norm_transpose(cx, x, tiles, W["g"][2:3, :], hT, L)
            load_w_bf16(cx, wsm[:, :, 0:256], (wsm, None), W["ca_wq"], 8, 256, stage[sti % 2], q="pool")
            sti += 1
            for ht in range(2):
                for qb in range(TB // 512):
                    b = rot(cx)
                    mm_group(cx, b[:], (b, None), [(wsm[:, c, ht * 128:(ht + 1) * 128], hT[:, c, qb * 512:(qb + 1) * 512], [(wsm, None), (hT, None)]) for c in range(8)])
                    p.op("act", lambda e, b=b, ht=ht, qb=qb: e.activation(out=qcT[:, ht, qb * 512:(qb + 1) * 512], in_=b[:], func=AF.Copy),
                         reads=[(b, None)], writes=[(qcT, (ht, qb))])
            pi = 0
            for h in range(4):
                ht, r0 = h // 2, (h % 2) * 64
                for qb in range(TB // 512):
                    pts = []
                    for mc in range(2):
                        sbk = rot(cx)
                        p.op("pe", lambda e, sbk=sbk, mc=mc, ht=ht, r0=r0, qb=qb: e.matmul(sbk[:], lhsT=kmT[r0:r0 + 64, ht, mc * 128:(mc + 1) * 128],
                                                                                           rhs=qcT[r0:r0 + 64, ht, qb * 512:(qb + 1) * 512], start=True, stop=True),
                             reads=[(kmT, None), (qcT, (ht, qb))], writes=[(sbk, None)])
                        pt = pT[pi % 4]
                        pi += 1
                        p.op("act", lambda e, sbk=sbk, pt=pt: e.activation(out=pt[:], in_=sbk[:], func=AF.Exp, scale=0.125), reads=[(sbk, None)], writes=[(pt, None)])
                        pts.append(pt)
                    ob = rot(cx)
                    mm_group(cx, ob[:], (ob, None), [(vm[:, mc, h, :], pts[mc][:], [(vm, None), (pts[mc], None)]) for mc in range(2)])
                    p.op("dve", lambda e, ob=ob: e.reciprocal(out=rs[64:128, :], in_=ob[64:128, :]), reads=[(ob, None)], writes=[(rs, None)])
                    p.op("dve", lambda e, ob=ob, ht=ht, r0=r0, qb=qb: e.tensor_tensor(out=ocT[r0:r0 + 64, ht, qb * 512:(qb + 1) * 512], in0=ob[0:64, :], in1=rs[64:128, :], op=ALU.mult),
                         reads=[(ob, None), (rs, None)], writes=[(ocT, (ht, qb))])
            for hf in range(4):
                load_w_bf16(cx, wsm[:, 0:2, hf * 256:(hf + 1) * 256], (wsm, None), W["ca_wo"][:, hf * 256:(hf + 1) * 256], 2, 256, stage[sti % 2], q="pool")
                sti += 1
            proj_residual(cx, x, tiles, lambda c, j: (ocT[:, c, j * 128:(j + 1) * 128], (ocT, None)), 2, wsm, W["g"][3:4, :], L)
            p.barrier()
            norm_transpose(cx, x, tiles, W["g"][4:5, :], hT, L)
            aT = big
            stg4 = [stage[0], stage[1]]
            sq = [0]

            def nst():
                sq[0] += 1
                return stg4[sq[0] % len(stg4)]
            for f in range(NFF):
                k = f % 2
                for (nm, wb) in (("ffn_wg", wg[k]), ("ffn_wu", wu[k])):
                    flat = wb[:].rearrange("p c n -> p (c n)")
                    if wscr is None or tb == 0:
                        load_w_bf16(cx, wb[:], (wb, None), W[nm][:, f * 128:(f + 1) * 128], 8, 128, nst(), q="sp")
                        if wscr is not None:
                            p.dma("pool", wscr[nm][f], flat, reads=[(wb, None)], writes=[(cx.wsc, (nm, f))])
                    else:
                        p.dma("sp", flat, wscr[nm][f], reads=[(cx.wsc, (nm, f))], writes=[(wb, None)])
                for nb in range(TB // 512):
                    bg = rot(cx)
                    bu = rot(cx)
                    for (bank, w) in ((bg, wg[k]), (bu, wu[k])):
                        mm_group(cx, bank[:], (bank, None), [(w[:, c, :], hT[:, c, nb * 512:(nb + 1) * 512], [(w, None), (hT, None)]) for c in range(8)])
                    gs = gsb[nb % 2]
                    p.op("act", lambda e, bg=bg, gs=gs: e.activation(out=gs[:], in_=bg[:], func=AF.Silu), reads=[(bg, None)], writes=[(gs, None)])
                    p.op("dve", lambda e, bu=bu, f=f, nb=nb, gs=gs: e.tensor_tensor(out=aT[:, f, nb * 512:(nb + 1) * 512], in0=bu[:], in1=gs[:], op=ALU.mult),
                         reads=[(bu, None), (gs, None)], writes=[(aT, (f, nb))])
            gB = L["gB"]
            p.dma("sp", gB[:], W["g"][5:6, :].to_broadcast([128, D]), writes=[(gB, None)])
            for grp in ([0, 1, 2], [3, 4, 5], [6, 7]):
                banks = {}
                for i, key in enumerate([(tt, nb) for tt in grp for nb in range(2)]):
                    banks[key] = cx.ps[i]
                for f in range(NFF):
                    k = f % 3
                    if wscr is None or (tb == 0 and grp[0] == 0):
                        st = nst()
                        p.dma("sp", st[:, 0:1024], W["ffn_wd"][f * 128:(f + 1) * 128, :], writes=[(st, None)])
                        cast_copy(cx, wd[k][:], st[:, 0:1024], [(st, None)], [(wd[k], None)])
                        if wscr is not None:
                            p.dma("pool", wscr["ffn_wd"][f], wd[k][:], reads=[(wd[k], None)], writes=[(cx.wsc, ("ffn_wd", f))])
                    else:
                        p.dma("sp", wd[k][:], wscr["ffn_wd"][f], reads=[(cx.wsc, ("ffn_wd", f))], writes=[(wd[k], None)])
                    for tt in grp:
                        for nb in range(2):
                            b = banks[(tt, nb)]
                            p.op("pe", lambda e, b=b, tt=tt, nb=nb, k=k, f=f: e.matmul(b[:], lhsT=aT[:, f, tt * 128:(tt + 1) * 128], rhs=wd[k][:, nb * 512:(nb + 1) * 512],
                                                                                      start=(f == 0), stop=(f == NFF - 1)),
                                 reads=[(aT, (f, tt // 4)), (wd[k], None)], writes=[(b, None)], inc=(tt == grp[-1] and nb == 1))
                for tt in grp:
                    norm_residual(cx, x, tiles[tt], [banks[(tt, 0)], banks[(tt, 1)]], gB, L)
            if hT_store is not None:
                norm_transpose(cx, x, tiles, g_next_ap, hT, L)
                hT_store(hT, tb)
            elif hT_next_ap is not None:
                norm_transpose(cx, x, tiles, g_next_ap, hT, L)
                p.dma("sp", hT_next_ap[:, tsl].rearrange("(c p) n -> p c n", p=128), hT[:], reads=[(hT, None)], writes=[(cx.outb, ("hT", tb))])
            p.barrier()


def psum_set(cx, es, n_f32, with_bf16):
    cx.uid += 1
    u = cx.uid
    ps = [Buf(es.enter_context(cx.nc.psum_tensor(f"ps{i}_{u}", [128, 512], F32)), f"ps{i}") for i in range(n_f32)]
    pst = Buf(es.enter_context(cx.nc.psum_tensor(f"pst_{u}", [128, 1024], BF16)), "pst") if with_bf16 else None
    return ps, pst


def build_rope_tables(cx, pos_ap, ropeinv_ap, Ct, St, scratch_i, scratch_f):
    p = cx.p
    inv = cx.ropeinv
    p.dma("sp", inv[:], ropeinv_ap, writes=[(inv, None)])
    p.dma("sp", scratch_i[:], pos_ap.to_broadcast([128, T]), writes=[(scratch_i, None)])
    p.op("dve", lambda e: e.tensor_copy(out=scratch_f[:], in_=scratch_i[:]), reads=[(scratch_i, None)], writes=[(scratch_f, None)])
    for (dst, col, off) in ((St, 0, 0.0), (Ct, 1, 0.25)):
        p.op("dve", lambda e, dst=dst, col=col, off=off: e.tensor_scalar(out=dst[:], in0=scratch_f[:], scalar1=inv[:, col:col + 1], scalar2=off,
                                                                      op0=ALU.mult, op1=ALU.add),
             reads=[(scratch_f, None), (inv, None)], writes=[(dst, None)])
        p.op("dve", lambda e, dst=dst: e.tensor_copy(out=scratch_i[:], in_=dst[:]), reads=[(dst, None)], writes=[(scratch_i, None)])
        p.op("pool", lambda e, dst=dst: e.tensor_tensor(out=dst[:], in0=dst[:], in1=scratch_i[:], op=ALU.subtract),
             reads=[(dst, None), (scratch_i, None)], writes=[(dst, None)])
        p.op("dve", lambda e, dst=dst: e.scalar_tensor_tensor(out=dst[:], in0=dst[:], scalar=0.5, in1=dst[:], op0=ALU.is_gt, op1=ALU.subtract),
             reads=[(dst, None)], writes=[(dst, None)])
        p.op("dve", lambda e, dst=dst: e.scalar_tensor_tensor(out=dst[:], in0=dst[:], scalar=0.5, in1=dst[:], op0=ALU.is_gt, op1=ALU.subtract),
             reads=[(dst, None)], writes=[(dst, None)])
    for dst in (St, Ct):
        p.op("act", lambda e, dst=dst: e.activation(out=dst[:], in_=dst[:], func=AF.Sin, scale=2.0 * math.pi), reads=[(dst, None)], writes=[(dst, None)])


def proj_rope(cx, hT, wq, wqs, col0, tok0, Ct, St, out_ap, out_dep, L, banks):
    p = cx.p
    bA, bB = banks
    mm_group(cx, bA[:], (bA, None), [(wq[:, c, col0:col0 + 128], hT[:, c, tok0:tok0 + 512], [(wq, None), (hT, None)]) for c in range(8)])
    mm_group(cx, bB[:], (bB, None), [(wqs[:, c, col0:col0 + 128], hT[:, c, tok0:tok0 + 512], [(wqs, None), (hT, None)]) for c in range(8)])
    t1, t2 = L["rt1"], L["rt2"]
    p.op("dve", lambda e: e.tensor_tensor(out=t1[:], in0=bA[:], in1=Ct[:, tok0:tok0 + 512], op=ALU.mult), reads=[(bA, None), (Ct, None)], writes=[(t1, None)])
    p.op("dve", lambda e: e.tensor_tensor(out=t2[:], in0=bB[:], in1=St[:, tok0:tok0 + 512], op=ALU.mult), reads=[(bB, None), (St, None)], writes=[(t2, None)])
    p.op("dve", lambda e: e.tensor_tensor(out=out_ap, in0=t1[:], in1=t2[:], op=ALU.add), reads=[(t1, None), (t2, None)], writes=[out_dep])


def run_streams(cx, streams, kbs, sbanks, pts, L, keep=None):
    p = cx.p
    n = len(kbs)
    st = L.setdefault("_rs", {"sb": 0, "pt": 0})

    def tail(pend, i):
        for (s, bank, kb) in pend:
            pt = pts[st["pt"] % len(pts)]
            st["pt"] += 1
            if keep is not None:
                keep.append((kb, pt))
            if s.get("bias") is not None:
                bap, bdeps = s["bias"](kb)
                p.op("act", lambda e, pt=pt, bank=bank, bap=bap, s=s: e.activation(out=pt[:], in_=bank[:], func=AF.Exp, scale=s["scale"], bias=bap),
                     reads=[(bank, None)] + bdeps, writes=[(pt, None)])
            else:
                p.op("act", lambda e, pt=pt, bank=bank, s=s: e.activation(out=pt[:], in_=bank[:], func=AF.Exp, scale=s["scale"]),
                     reads=[(bank, None)], writes=[(pt, None)])
            for (map_, mdeps) in s["masks"](kb):
                p.op("pool", lambda e, pt=pt, map_=map_: e.tensor_tensor(out=pt[:], in0=pt[:], in1=map_, op=ALU.mult),
                     reads=[(pt, None)] + mdeps, writes=[(pt, None)])
            for (obank, lfn) in s["pv"]:
                lap, ldeps = lfn(kb)
                p.op("pe", lambda e, obank=obank, lap=lap, pt=pt, i=i: e.matmul(obank, lhsT=lap, rhs=pt[:], start=(i == 0), stop=(i == n - 1)),
                     reads=[(pt, None)] + ldeps, writes=[s["pv_dep"]])

    depth = 2 if (len(streams) == 1 and len(sbanks) >= 3) else 1
    queue = []
    for i, kb in enumerate(kbs):
        cur = []
        for s in streams:
            bank = sbanks[st["sb"] % len(sbanks)]
            st["sb"] += 1
            kap, kdeps = s["k"](kb)
            qap, qdeps = s["q"]
            p.op("pe", lambda e, bank=bank, kap=kap, qap=qap: e.matmul(bank[:], lhsT=kap, rhs=qap, start=True, stop=True),
                 reads=kdeps + qdeps, writes=[(bank, None)])
            cur.append((s, bank, kb))
        queue.append((cur, i))
        if len(queue) > depth:
            pc, pi_ = queue.pop(0)
            tail(pc, pi_)
    while queue:
        pc, pi_ = queue.pop(0)
        tail(pc, pi_)


def phase_B_odd(cx, load_hT, W, oT_ap):
    p = cx.p
    with ExitStack() as es:
        cx.uid += 1
        uu = cx.uid
        def sb(name, shape, dt):
            return Buf(es.enter_context(cx.nc.sbuf_tensor(f"{name}_{uu}", list(shape), dt)), name)
        ps, _ = psum_set(cx, es, 8, False)
        L = {}
        hT = sb("hT_all", [128, 8, T], BF16)
        load_hT(hT)
        Ct = sb("Ct", [128, T], F32)
        St = sb("St", [128, T], F32)
        cx.ropeinv = sb("ropeinv", [128, 2], F32)
        with ExitStack() as es2:
            sci = Buf(es2.enter_context(cx.nc.sbuf_tensor(f"sci_{uu}", [128, T], I32)), "sci")
            scf = Buf(es2.enter_context(cx.nc.sbuf_tensor(f"scf_{uu}", [128, T], F32)), "scf")
            build_rope_tables(cx, W["pos"], W["ropeinv"], Ct, St, sci, scf)
            p.barrier()
        qT = sb("qT", [128, 2, T], BF16)
        kT = sb("kT", [128, 2, T], BF16)
        vv = sb("vv", [128, 32, 256], BF16)
        stage = [sb(f"stage{i}", [128, 2048], F32) for i in range(2)]
        wq = sb("wq", [128, 8, 256], BF16)
        wqs = sb("wqs", [128, 8, 256], BF16)
        cmask = sb("cmask", [128, 8, 512], BF16)
        ones_b = sb("ones_b", [128, 128], BF16)
        ones_f = sb("ones_f", [128, 128], F32)
        pts = [sb(f"pt{i}", [128, 512], BF16) for i in range(6)]
        L["rt1"] = sb("rt1", [128, 512], F32)
        L["rt2"] = sb("rt2", [128, 512], F32)
        r1 = sb("r1", [128, 512], F32)
        r2 = sb("r2", [128, 512], F32)
        osb = sb("osb", [128, 512], F32)
        osq = sb("osq", [128, 512], F32)
        ob = [sb(f"ob{i}", [128, 512], BF16) for i in range(2)]
        lamt = sb("lamt", [128, 256], F32)
        lam2 = sb("lam2", [128, 8], F32)
        gcol = sb("gcol", [128, 1], F32)
        p.dma("sp", cmask[:], W["cmask"].rearrange("p (j n) -> p j n", j=8), writes=[(cmask, None)])
        p.op("pool", lambda e: e.memset(ones_b[:], 1.0), writes=[(ones_b, None)])
        p.op("pool", lambda e: e.memset(ones_f[:], 1.0), writes=[(ones_f, None)])
        p.dma("sp", lamt[:], W["lam"].to_broadcast([128, 256]), writes=[(lamt, None)])
        p.dma("sp", gcol[:], W["subg"], writes=[(gcol, None)])
        for i in range(2):
            p.op("dve", lambda e, i=i: e.tensor_tensor(out=lamt[:, i * 128:i * 128 + 64], in0=lamt[:, i * 128:i * 128 + 64], in1=lamt[:, i * 128 + 64:i * 128 + 128], op=ALU.mult),
                 reads=[(lamt, None)], writes=[(lamt, None)])
            p.op("dve", lambda e, i=i: e.tensor_reduce(out=lam2[:, i:i + 1], in_=lamt[:, i * 128:i * 128 + 64], axis=AX.X, op=ALU.add),
                 reads=[(lamt, None)], writes=[(lam2, None)])
        p.op("act", lambda e: e.activation(out=lam2[:, 2:4], in_=lam2[:, 0:2], func=AF.Exp), reads=[(lam2, None)], writes=[(lam2, None)])
        lic = sb("lic", [128, 2], F32)
        p.dma("sp", lic[:], W["laminit"].to_broadcast([128, 2]), writes=[(lic, None)])
        p.op("dve", lambda e: e.scalar_tensor_tensor(out=lam2[:, 4:5], in0=lam2[:, 3:4], scalar=lic[:, 0:1], in1=lam2[:, 2:3], op0=ALU.add, op1=ALU.subtract),
             reads=[(lam2, None), (lic, None)], writes=[(lam2, None)])
        p.op("dve", lambda e: e.tensor_scalar(out=gcol[:], in0=gcol[:], scalar1=lic[:, 1:2], scalar2=None, op0=ALU.mult), reads=[(gcol, None), (lic, None)], writes=[(gcol, None)])
        sti = 0
        for hp in range(2):
            for (dst, base) in ((qT, 0), (kT, 1024)):
                c0 = base + hp * 256
                load_w_bf16(cx, wq[:], (wq, None), W["w_in"][:, c0:c0 + 256], 8, 256, stage[sti % 2], q="pool"); sti += 1
                load_w_bf16(cx, wqs[:], (wqs, None), W["w_in"][:, c0 + 512:c0 + 768], 8, 256, stage[sti % 2], q="pool"); sti += 1
                for hh in range(2):
                    for tb in range(8):
                        banks = (ps[(2 * tb) % 8], ps[(2 * tb + 1) % 8])
                        proj_rope(cx, hT, wq, wqs, hh * 128, tb * 512, Ct, St, dst[:, hh, tb * 512:(tb + 1) * 512], (dst, (hh, tb)), L, banks)
            c0 = 2048 + hp * 256
            load_w_bf16(cx, wq[:], (wq, None), W["w_in"][:, c0:c0 + 256], 8, 256, stage[sti % 2], q="pool"); sti += 1
            for tt in range(32):
                b = ps[tt % 8]
                mm_group(cx, b[:, 0:256], (b, None), [(hT[:, c, tt * 128:(tt + 1) * 128], wq[:, c, :], [(wq, None), (hT, None)]) for c in range(8)])
                if tt % 2 == 0:
                    p.op("act", lambda e, b=b, tt=tt: e.activation(out=vv[:, tt, :], in_=b[:, 0:256], func=AF.Copy), reads=[(b, None)], writes=[(vv, tt)])
                else:
                    p.op("dve", lambda e, b=b, tt=tt: e.tensor_copy(out=vv[:, tt, :], in_=b[:, 0:256]), reads=[(b, None)], writes=[(vv, tt)])
            for hh in range(2):
                head = hp * 2 + hh
                for qb in range(8):
                    kbs = list(range(4 * qb + 4))
                    O1, S1, O2, S2 = ps[4], ps[5], ps[6], ps[7]
                    streams = []
                    for comp, (Ob, Sb) in enumerate(((O1, S1), (O2, S2))):
                        r0 = comp * 64
                        streams.append(dict(
                            k=lambda kb, r0=r0: (kT[r0:r0 + 64, hh, kb * 128:(kb + 1) * 128], [(kT, (hh, kb // 4))]),
                            q=(qT[r0:r0 + 64, hh, qb * 512:(qb + 1) * 512], [(qT, (hh, qb))]),
                            scale=0.125, bias=None,
                            masks=lambda kb: ([(cmask[:, kb - 4 * qb, :], [(cmask, None)])] if kb >= 4 * qb else []),
                            pv=[(Ob[:], lambda kb: (vv[:, kb, hh * 128:(hh + 1) * 128], [(vv, kb)])),
                                (Sb[:], lambda kb: (ones_b[:], [(ones_b, None)]))],
                            pv_dep=(Ob, None)))
                    run_streams(cx, streams, kbs, ps[0:4], pts, L)
                    p.op("dve", lambda e: e.reciprocal(out=r1[:], in_=S1[:]), reads=[(O1, None), (S1, None)], writes=[(r1, None)])
                    p.op("dve", lambda e: e.reciprocal(out=r2[:], in_=S2[:]), reads=[(O2, None), (S2, None)], writes=[(r2, None)])
                    p.op("dve", lambda e: e.tensor_tensor(out=r1[:], in0=O1[:], in1=r1[:], op=ALU.mult), reads=[(O1, None), (r1, None)], writes=[(r1, None)])
                    p.op("dve", lambda e: e.tensor_tensor(out=r2[:], in0=O2[:], in1=r2[:], op=ALU.mult), reads=[(O2, None), (r2, None)], writes=[(r2, None), (S1, None), (S2, None)])
                    p.op("dve", lambda e: e.scalar_tensor_tensor(out=osb[:], in0=r2[:], scalar=lam2[:, 4:5], in1=r1[:], op0=ALU.mult, op1=ALU.add),
                         reads=[(r1, None), (r2, None), (lam2, None)], writes=[(osb, None)])
                    p.op("pool", lambda e: e.tensor_tensor(out=osq[:], in0=osb[:], in1=osb[:], op=ALU.mult), reads=[(osb, None)], writes=[(osq, None)])
                    sbk = ps[qb % 4]
                    p.op("pe", lambda e, sbk=sbk: e.matmul(sbk[:], lhsT=ones_f[:], rhs=osq[:], start=True, stop=True), reads=[(osq, None), (ones_f, None)], writes=[(sbk, None)])
                    p.op("act", lambda e, sbk=sbk: e.activation(out=r1[:], in_=sbk[:], func=AF.Sqrt, scale=1.0 / 128, bias=cx.epsb[:, 0:1]),
                         reads=[(sbk, None), (cx.epsb, None)], writes=[(r1, None)])
                    p.op("dve", lambda e: e.reciprocal(out=r1[:], in_=r1[:]), reads=[(r1, None)], writes=[(r1, None)])
                    obt = ob[qb % 2]
                    p.op("dve", lambda e, obt=obt: e.scalar_tensor_tensor(out=obt[:], in0=osb[:], scalar=gcol[:, 0:1], in1=r1[:], op0=ALU.mult, op1=ALU.mult),
                         reads=[(osb, None), (gcol, None), (r1, None)], writes=[(obt, None)])
                    p.dma("sp", oT_ap[head * 128:(head + 1) * 128, qb * 512:(qb + 1) * 512], obt[:], reads=[(obt, None)], writes=[(cx.outb, ("oT", head, qb))])
        p.barrier()


EV = dict(fq=0, fk=256, fv=512, nq=768, nqs=1024, kc=1280, kcs=1344, ks=1408, kss=1472, kw=1536, kws=1600, vc=1664, vs=1728, vw=1792, fl=1856, gl=1860)
EV_NCOL = 1872
TBK = 512


def build_rope_block(cx, pos_ap, tok0, n, Ct, St, sci, scf):
    p = cx.p
    inv = cx.ropeinv
    p.dma("sp", sci[:, 0:n], pos_ap[:, tok0:tok0 + n].to_broadcast([128, n]), writes=[(sci, None)])
    p.op("dve", lambda e: e.tensor_copy(out=scf[:, 0:n], in_=sci[:, 0:n]), reads=[(sci, None)], writes=[(scf, None)])
    for (dst, col, off) in ((St, 0, 0.0), (Ct, 1, 0.25)):
        p.op("dve", lambda e, dst=dst, col=col, off=off: e.tensor_scalar(out=dst[:, 0:n], in0=scf[:, 0:n], scalar1=inv[:, col:col + 1], scalar2=off,
                                                                      op0=ALU.mult, op1=ALU.add),
             reads=[(scf, None), (inv, None)], writes=[(dst, None)])
        p.op("dve", lambda e, dst=dst: e.tensor_copy(out=sci[:, 0:n], in_=dst[:, 0:n]), reads=[(dst, None)], writes=[(sci, None)])
        p.op("pool", lambda e, dst=dst: e.tensor_tensor(out=dst[:, 0:n], in0=dst[:, 0:n], in1=sci[:, 0:n], op=ALU.subtract),
             reads=[(dst, None), (sci, None)], writes=[(dst, None)])
        for _ in range(2):
            p.op("dve", lambda e, dst=dst: e.scalar_tensor_tensor(out=dst[:, 0:n], in0=dst[:, 0:n], scalar=0.5, in1=dst[:, 0:n], op0=ALU.is_gt, op1=ALU.subtract),
                 reads=[(dst, None)], writes=[(dst, None)])
        p.op("act", lambda e, dst=dst: e.activation(out=dst[:, 0:n], in_=dst[:, 0:n], func=AF.Sin, scale=2.0 * math.pi), reads=[(dst, None)], writes=[(dst, None)])


def phase_B_even(cx, hT_src, W, oT_ap):
    p = cx.p
    nc = cx.nc
    with ExitStack() as es:
        cx.uid += 1
        uu = cx.uid

        def mk_sb(stack):
            def sb(name, shape, dt):
                return Buf(stack.enter_context(nc.sbuf_tensor(f"{name}_{uu}", list(shape), dt)), name)
            return sb
        sb = mk_sb(es)
        ps, pst = psum_set(cx, es, 7, True)
        L = {}
        cmask = sb("cmask", [128, 8, 512], BF16)
        p.dma("sp", cmask[:], W["cmask"].rearrange("p (j n) -> p j n", j=8), writes=[(cmask, None)])
        cx.ropeinv = sb("ropeinv", [128, 2], F32)
        p.dma("sp", cx.ropeinv[:], W["ropeinv"], writes=[(cx.ropeinv, None)])
        ones_f = sb("ones_f", [128, 128], F32)
        p.op("pool", lambda e: e.memset(ones_f[:], 1.0), writes=[(ones_f, None)])
        pts = [sb(f"pt{i}", [128, 512], BF16) for i in range(6)]
        rs = sb("rs", [128, 512], F32)
        fac = sb("fac", [128, 512], F32)
        obt = [sb(f"obt{i}", [128, 512], BF16) for i in range(2)]

        def load_hT_block(hTb, tok0):
            for c in range(8):
                hT_src(hTb[:, c, :], c, tok0, TBK, "sp" if c % 2 == 0 else "pool", [(hTb, None)])

        with ExitStack() as esf:
            sbf = mk_sb(esf)
            fqT = sbf("fqT", [128, 2, T], BF16)
            fkT = sbf("fkT", [128, 2, T], BF16)
            fvv = sbf("fvv", [128, 32, 4, 128], BF16)
            FB = sbf("FB", [128, 4, 32, 8], F32)
            ncum = sbf("ncum", [128, 128], F32)
            p.op("pool", lambda e: e.memset(fvv[:], 1.0), writes=[(fvv, None)])
            with ExitStack() as esp:
                sbp = mk_sb(esp)
                hTb = sbp("hTb", [128, 8, TBK], BF16)
                wf = sbp("wf", [128, 8, 768], BF16)
                wfl = sbp("wfl", [128, 8, 4], BF16)
                stage = [sbp(f"stage{i}", [128, 2048], F32) for i in range(2)]
                tri = sbp("tri", [128, 128], F32)
                sel127 = sbp("sel127", [128, 128], F32)
                fbB = sbp("fbB", [128, 128], F32)
                nlf = sbp("nlf", [128, 128], F32)
                tot = sbp("tot", [128, 128], F32)
                inc = sbp("inc", [128, 128], F32)
                refsb = sbp("refsb", [128, 128], F32)
                p.dma("sp", tri[:], W["tri"], writes=[(tri, None)])
                p.dma("sp", sel127[:], W["sel127"], writes=[(sel127, None)])
                p.dma("sp", fbB[:], W["fbias_rep"].to_broadcast([128, 128]), writes=[(fbB, None)])
                for i in range(3):
                    load_w_bf16(cx, wf[:, :, i * 256:(i + 1) * 256], (wf, None), W["w_in"][:, i * 256:(i + 1) * 256], 8, 256, stage[i % 2], q="pool")
                load_w_bf16(cx, wfl[:], (wfl, None), W["w_in"][:, EV["fl"]:EV["fl"] + 4], 8, 4, stage[1], q="pool")
                FLP = ps[6]
                for blk in range(T // TBK):
                    tok0 = blk * TBK
                    load_hT_block(hTb, tok0)
                    for (dst, cb) in ((fqT, 0), (fkT, 256)):
                        for hh in range(2):
                            for tb in range(TBK // 512):
                                b = ps[(hh * 2 + tb) % 4]
                                mm_group(cx, b[:], (b, None), [(wf[:, c, cb + hh * 128:cb + (hh + 1) * 128], hTb[:, c, tb * 512:(tb + 1) * 512], [(wf, None), (hTb, None)]) for c in range(8)])
                                gtb = (tok0 + tb * 512) // 512
                                if tb % 2 == 0:
                                    p.op("act", lambda e, b=b, dst=dst, hh=hh, gtb=gtb: e.activation(out=dst[:, hh, gtb * 512:(gtb + 1) * 512], in_=b[:], func=AF.Copy),
                                         reads=[(b, None)], writes=[(dst, (hh, gtb))])
                                else:
                                    p.op("dve", lambda e, b=b, dst=dst, hh=hh, gtb=gtb: e.tensor_copy(out=dst[:, hh, gtb * 512:(gtb + 1) * 512], in_=b[:]),
                                         reads=[(b, None)], writes=[(dst, (hh, gtb))])
                    for tl in range(TBK // 128):
                        tt = tok0 // 128 + tl
                        b = ps[4 + tl % 2]
                        mm_group(cx, b[:, 0:256], (b, None), [(hTb[:, c, tl * 128:(tl + 1) * 128], wf[:, c, 512:768], [(wf, None), (hTb, None)]) for c in range(8)])
                        p.op("dve", lambda e, b=b, tt=tt: e.tensor_copy(out=fvv[:, tt, :, 0:64], in_=b[:, 0:256].rearrange("p (h d) -> p h d", h=4)),
                             reads=[(b, None)], writes=[(fvv, tt)])
                        mm_group(cx, FLP[:, tt * 4:(tt + 1) * 4], (FLP, None), [(hTb[:, c, tl * 128:(tl + 1) * 128], wfl[:, c, :], [(wfl, None), (hTb, None)]) for c in range(8)])
                p.op("dve", lambda e: e.tensor_tensor(out=nlf[:], in0=FLP[:, 0:128], in1=fbB[:], op=ALU.add), reads=[(FLP, None), (fbB, None)], writes=[(nlf, None)])
                p.op("act", lambda e: e.activation(out=nlf[:], in_=nlf[:], func=AF.Exp, scale=-1.0), reads=[(nlf, None)], writes=[(nlf, None)])
                p.op("act", lambda e: e.activation(out=nlf[:], in_=nlf[:], func=AF.Ln, bias=1.0), reads=[(nlf, None)], writes=[(nlf, None)])
                W1b, TOTb, REFb = ps[0], ps[1], ps[2]
                p.op("pe", lambda e: e.matmul(W1b[:, 0:128], lhsT=tri[:], rhs=nlf[:], start=True, stop=True), reads=[(tri, None), (nlf, None)], writes=[(W1b, None)])
                p.op("pe", lambda e: e.matmul(TOTb[:, 0:128], lhsT=ones_f[:], rhs=nlf[:], start=True, stop=True), reads=[(ones_f, None), (nlf, None)], writes=[(TOTb, None)])
                p.op("dve", lambda e: e.tensor_copy(out=tot[:], in_=TOTb[:, 0:128]), reads=[(TOTb, None)], writes=[(tot, None)])
                for j in range(4):
                    tv = tot[:].rearrange("p (t h) -> p t h", h=4)[:, :, j]
                    iv = inc[:].rearrange("p (t h) -> p t h", h=4)[:, :, j]
                    ov = ones_f[:, 0:32]
                    p.op("dve", lambda e, tv=tv, iv=iv, ov=ov: e.tensor_tensor_scan(out=iv, data0=ov, data1=tv, initial=0.0, op0=ALU.mult, op1=ALU.add),
                         reads=[(tot, None), (ones_f, None)], writes=[(inc, None)])
                p.op("dve", lambda e: e.tensor_tensor(out=inc[:], in0=inc[:], in1=tot[:], op=ALU.subtract), reads=[(inc, None), (tot, None)], writes=[(inc, None)])
                p.op("dve", lambda e: e.tensor_tensor(out=ncum[:], in0=W1b[:, 0:128], in1=inc[:], op=ALU.add), reads=[(W1b, None), (inc, None)], writes=[(ncum, None)])
                p.op("pe", lambda e: e.matmul(REFb[:, 0:128], lhsT=sel127[:], rhs=ncum[:], start=True, stop=True), reads=[(sel127, None), (ncum, None)], writes=[(REFb, None)])
                p.op("dve", lambda e: e.tensor_copy(out=refsb[:], in_=REFb[:, 0:128]), reads=[(REFb, None)], writes=[(refsb, None)])
                ncv = ncum[:].rearrange("p (t h) -> p t h", h=4)
                for j in range(4):
                    for qb in range(8):
                        col = (4 * qb + 3) * 4 + j
                        p.op("dve", lambda e, j=j, qb=qb, col=col: e.tensor_scalar(out=FB[:, j, :, qb], in0=ncv[:, :, j], scalar1=refsb[:, col:col + 1], scalar2=None, op0=ALU.subtract),
                             reads=[(ncum, None), (refsb, None)], writes=[(FB, None)])
                p.barrier()
            for head in range(4):
                hh, r0 = head // 2, (head % 2) * 64
                for qb in range(8):
                    kbs = list(range(4 * qb + 4))
                    Ob = ps[4 + (qb % 2)]
                    stream = dict(
                        k=lambda kb: (fkT[r0:r0 + 64, hh, kb * 128:(kb + 1) * 128], [(fkT, (hh, kb // 4))]),
                        q=(fqT[r0:r0 + 64, hh, qb * 512:(qb + 1) * 512], [(fqT, (hh, qb))]),
                        scale=0.125,
                        bias=lambda kb: (FB[:, head, kb, qb:qb + 1], [(FB, None)]),
                        masks=lambda kb: ([(cmask[:, kb - 4 * qb, :], [(cmask, None)])] if kb >= 4 * qb else []),
                        pv=[(Ob[:], lambda kb: (fvv[:, kb, head, :], [(fvv, kb)]))],
                        pv_dep=(Ob, None))
                    run_streams(cx, [stream], kbs, ps[0:4], pts, L)
                    ot = obt[qb % 2]
                    p.op("dve", lambda e, Ob=Ob: e.reciprocal(out=rs[64:128, :], in_=Ob[64:128, :]), reads=[(Ob, None)], writes=[(rs, None)])
                    p.op("dve", lambda e, Ob=Ob, ot=ot: e.tensor_tensor(out=ot[0:64, :], in0=Ob[0:64, :], in1=rs[64:128, :], op=ALU.mult),
                         reads=[(Ob, None), (rs, None)], writes=[(ot, None)])
                    p.dma("sp", oT_ap[head * 64:(head + 1) * 64, qb * 512:(qb + 1) * 512], ot[0:64, :], reads=[(ot, None)], writes=[(cx.outb, ("oTf", head, qb))])
            p.barrier()
        nsa_part(cx, W, oT_ap, hT_src, load_hT_block, mk_sb, es, ps, pst, pts, cmask, ones_f, rs, fac, obt, L)
        p.barrier()


NWC = dict(nq=0, nqs=256, ks=512, kss=640, kw=768, kws=896, kc=1024, kcs=1152, vc=1280, vsw=1344, gl=1472)
NW = 1484


def gelu_tanh(cx, src_bank_ap, src_dep, bias_col, bias_dep, out_ap, out_dep, tmpa, tmpb, n):
    p = cx.p
    p.op("act", lambda e: e.activation(out=tmpa[:, 0:n], in_=src_bank_ap, func=AF.Identity, bias=bias_col), reads=[src_dep, bias_dep], writes=[(tmpa, None)])
    p.op("dve", lambda e: e.tensor_tensor(out=tmpb[:, 0:n], in0=tmpa[:, 0:n], in1=tmpa[:, 0:n], op=ALU.mult), reads=[(tmpa, None)], writes=[(tmpb, None)])
    p.op("dve", lambda e: e.tensor_scalar(out=tmpb[:, 0:n], in0=tmpb[:, 0:n], scalar1=0.044715, scalar2=1.0, op0=ALU.mult, op1=ALU.add), reads=[(tmpb, None)], writes=[(tmpb, None)])
    p.op("dve", lambda e: e.tensor_tensor(out=tmpb[:, 0:n], in0=tmpb[:, 0:n], in1=tmpa[:, 0:n], op=ALU.mult), reads=[(tmpa, None), (tmpb, None)], writes=[(tmpb, None)])
    p.op("act", lambda e: e.activation(out=tmpb[:, 0:n], in_=tmpb[:, 0:n], func=AF.Tanh, scale=0.7978845608028654), reads=[(tmpb, None)], writes=[(tmpb, None)])
    p.op("dve", lambda e: e.scalar_tensor_tensor(out=tmpb[:, 0:n], in0=tmpb[:, 0:n], scalar=1.0, in1=tmpa[:, 0:n], op0=ALU.add, op1=ALU.mult),
         reads=[(tmpa, None), (tmpb, None)], writes=[(tmpb, None)])
    p.op("act", lambda e: e.activation(out=out_ap, in_=tmpb[:, 0:n], func=AF.Copy, scale=0.5), reads=[(tmpb, None)], writes=[out_dep])


def nsa_part(cx, W, oT_ap, hT_src, load_hT_block, mk_sb, es, ps, pst, pts, cmask, ones_f, rs, fac, obt, L):
    p = cx.p
    sb = mk_sb(es)
    QA = sb("QA", [128, 4, T], BF16)
    KSA = sb("KSA", [128, T], BF16)
    KSB = sb("KSB", [128, T], BF16)
    KW2 = sb("KW2", [128, T], BF16)
    VS = sb("VS", [128, 32, 128], BF16)
    VW = sb("VW", [128, 32, 128], BF16)
    GLT = sb("GLT", [12, T], BF16)
    KCMP = sb("KCMP", [128, 256], BF16)
    VCMP = sb("VCMP", [128, 2, 128], BF16)
    cmpmask = sb("cmpmask", [128, 5, 512], BF16)
    ovl = sb("ovl", [128, 2, 65], BF16)
    sel12 = sb("sel12", [12, 12 * 128], BF16)
    tkadd = sb("tkadd", [128, 128], F32)
    tkmul = sb("tkmul", [128, 128], F32)
    p.dma("sp", cmpmask[:], W["cmpmask"].rearrange("p (j n) -> p j n", j=5), writes=[(cmpmask, None)])
    p.dma("sp", ovl[:], W["ovl"].rearrange("p (c n) -> p c n", c=2), writes=[(ovl, None)])
    p.dma("sp", sel12[:], W["sel12"], writes=[(sel12, None)])
    p.dma("sp", tkadd[:], W["tkadd"], writes=[(tkadd, None)])
    p.dma("sp", tkmul[:], W["tkmul"], writes=[(tkmul, None)])
    for b_ in (VS, VW, VCMP):
        p.op("pool", lambda e, b_=b_: e.memset(b_[:], 1.0), writes=[(b_, None)])
    with ExitStack() as esp:
        sbp = mk_sb(esp)
        hTb = sbp("hTbn", [128, 8, TBK], BF16)
        wn = sbp("wn", [128, 8, NW], BF16)
        stage = [sbp(f"stagen{i}", [128, 2048], F32) for i in range(2)]
        Ct = sbp("Ctb", [128, TBK], F32)
        St = sbp("Stb", [128, TBK], F32)
        sci = sbp("scib", [128, TBK], I32)
        scf = sbp("scfb", [128, TBK], F32)
        L["rt1"] = sbp("rt1n", [128, 512], F32)
        L["rt2"] = sbp("rt2n", [128, 512], F32)
        KC2 = sbp("KC2", [128, T], BF16)
        VC = sbp("VC", [128, T], BF16)
        W1 = sbp("W1c", [64, 32, 128], BF16)
        posT = sbp("posT", [64, 32], BF16)
        posTf = sbp("posTf", [64, 32], F32)
        W2d = sbp("W2d", [128, 128], BF16)
        posb = sbp("posb", [128, 1], F32)
        HID = sbp("HID", [128, 256], BF16)
        ga = sbp("ga", [128, 256], F32)
        gb = sbp("gb", [128, 256], F32)
        sti = [0]

        def stg():
            sti[0] += 1
            return stage[sti[0] % 2]
        E = EV
        load_w_bf16(cx, wn[:, :, 0:256], (wn, None), W["w_in"][:, E["nq"]:E["nq"] + 256], 8, 256, stg(), q="pool")
        load_w_bf16(cx, wn[:, :, 256:512], (wn, None), W["w_in"][:, E["nqs"]:E["nqs"] + 256], 8, 256, stg(), q="pool")
        for (src, dstc, dup) in ((E["ks"], NWC["ks"], True), (E["kss"], NWC["kss"], True), (E["kw"], NWC["kw"], True), (E["kws"], NWC["kws"], True),
                                 (E["kc"], NWC["kc"], True), (E["kcs"], NWC["kcs"], True), (E["vc"], NWC["vc"], False)):
            st = stg()
            sv = st[:, 0:8 * 64].rearrange("p (c n) -> p c n", c=8)
            p.dma("pool", sv, W["w_in"][:, src:src + 64].rearrange("(c p) n -> p c n", p=128), writes=[(st, None)])
            p.op("pool", lambda e, sv=sv, dstc=dstc: e.tensor_copy(out=wn[:, :, dstc:dstc + 64], in_=sv), reads=[(st, None)], writes=[(wn, None)])
            if dup:
                p.op("pool", lambda e, sv=sv, dstc=dstc: e.tensor_copy(out=wn[:, :, dstc + 64:dstc + 128], in_=sv), reads=[(st, None)], writes=[(wn, None)])
        load_w_bf16(cx, wn[:, :, NWC["vsw"]:NWC["vsw"] + 128], (wn, None), W["w_in"][:, E["vs"]:E["vs"] + 128], 8, 128, stg(), q="pool")
        load_w_bf16(cx, wn[:, :, NWC["gl"]:NWC["gl"] + 12], (wn, None), W["w_in"][:, E["gl"]:E["gl"] + 12], 8, 12, stg(), q="pool")
        for blk in range(T // TBK):
            tok0 = blk * TBK
            load_hT_block(hTb, tok0)
            build_rope_block(cx, W["pos"], tok0, TBK, Ct, St, sci, scf)
            for tb in range(TBK // 512):
                g0 = tok0 + tb * 512
                gtb = g0 // 512
                lsl = slice(tb * 512, (tb + 1) * 512)
                for i in range(2):
                    bA, bB = ps[2 * i], ps[2 * i + 1]
                    mm_group(cx, bA[:], (bA, None), [(wn[:, c, i * 128:(i + 1) * 128], hTb[:, c, lsl], [(wn, None), (hTb, None)]) for c in range(8)])
                    mm_group(cx, bB[:], (bB, None), [(wn[:, c, 256 + i * 128:256 + (i + 1) * 128], hTb[:, c, lsl], [(wn, None), (hTb, None)]) for c in range(8)])
                    t1, t2 = L["rt1"], L["rt2"]
                    p.op("dve", lambda e, bA=bA: e.tensor_tensor(out=t1[:], in0=bA[:], in1=Ct[:, lsl], op=ALU.mult), reads=[(bA, None), (Ct, None)], writes=[(t1, None)])
                    p.op("dve", lambda e, bB=bB: e.tensor_tensor(out=t2[:], in0=bB[:], in1=St[:, lsl], op=ALU.mult), reads=[(bB, None), (St, None)], writes=[(t2, None)])
                    p.op("dve", lambda e, i=i, g0=g0: e.tensor_tensor(out=QA[0:64, 2 * i, g0:g0 + 512], in0=t1[0:64, :], in1=t2[0:64, :], op=ALU.add),
                         reads=[(t1, None), (t2, None)], writes=[(QA, (2 * i, gtb))])
                    p.op("dve", lambda e, i=i, g0=g0: e.tensor_tensor(out=QA[64:128, 2 * i + 1, g0:g0 + 512], in0=t1[64:128, :], in1=t2[64:128, :], op=ALU.add),
                         reads=[(t1, None), (t2, None)], writes=[(QA, (2 * i + 1, gtb))])
                for ui, (dst, cb, cbs) in enumerate(((KSA, NWC["ks"], NWC["kss"]), (KW2, NWC["kw"], NWC["kws"]), (KC2, NWC["kc"], NWC["kcs"]))):
                    bA, bB = ps[(4 + 2 * ui) % 6], ps[(5 + 2 * ui) % 6]
                    mm_group(cx, bA[:], (bA, None), [(wn[:, c, cb:cb + 128], hTb[:, c, lsl], [(wn, None), (hTb, None)]) for c in range(8)])
                    mm_group(cx, bB[:], (bB, None), [(wn[:, c, cbs:cbs + 128], hTb[:, c, lsl], [(wn, None), (hTb, None)]) for c in range(8)])
                    t1, t2 = L["rt1"], L["rt2"]
                    p.op("dve", lambda e, bA=bA: e.tensor_tensor(out=t1[:], in0=bA[:], in1=Ct[:, lsl], op=ALU.mult), reads=[(bA, None), (Ct, None)], writes=[(t1, None)])
                    p.op("dve", lambda e, bB=bB: e.tensor_tensor(out=t2[:], in0=bB[:], in1=St[:, lsl], op=ALU.mult), reads=[(bB, None), (St, None)], writes=[(t2, None)])
                    p.op("dve", lambda e, dst=dst, g0=g0: e.tensor_tensor(out=dst[:, g0:g0 + 512], in0=t1[:], in1=t2[:], op=ALU.add),
                         reads=[(t1, None), (t2, None)], writes=[(dst, gtb)])
                b = ps[6]
                mm_group(cx, b[0:64, :], (b, None), [(wn[:, c, NWC["vc"]:NWC["vc"] + 64], hTb[:, c, lsl], [(wn, None), (hTb, None)]) for c in range(8)])
                p.op("act", lambda e, b=b, g0=g0: e.activation(out=VC[0:64, g0:g0 + 512], in_=b[0:64, :], func=AF.Copy), reads=[(b, None)], writes=[(VC, gtb)])
                b = ps[4 + tb % 2]
                mm_group(cx, b[0:12, :], (b, None), [(wn[:, c, NWC["gl"]:NWC["gl"] + 12], hTb[:, c, lsl], [(wn, None), (hTb, None)]) for c in range(8)])
                p.op("act", lambda e, b=b, g0=g0: e.activation(out=GLT[0:12, g0:g0 + 512], in_=b[0:12, :], func=AF.Sigmoid), reads=[(b, None)], writes=[(GLT, gtb)])
            for tl in range(TBK // 128):
                tt = tok0 // 128 + tl
                b = ps[(tl % 2)]
                mm_group(cx, b[:, 0:128], (b, None), [(hTb[:, c, tl * 128:(tl + 1) * 128], wn[:, c, NWC["vsw"]:NWC["vsw"] + 128], [(wn, None), (hTb, None)]) for c in range(8)])
                p.op("dve", lambda e, b=b, tt=tt: e.tensor_copy(out=VS[:, tt, 0:64], in_=b[:, 0:64]), reads=[(b, None)], writes=[(VS, tt)])
                p.op("act", lambda e, b=b, tt=tt: e.activation(out=VW[:, tt, 0:64], in_=b[:, 64:128], func=AF.Copy), reads=[(b, None)], writes=[(VW, tt)])
        p.op("pool", lambda e: e.tensor_copy(out=KSB[:], in_=KSA[:]), reads=[(KSA, None)], writes=[(KSB, None)])
        p.dma("sp", KSA[64:128, :], W["onehot"], writes=[(KSA, None)])
        p.dma("sp", KSB[0:64, :], W["onehot"], writes=[(KSB, None)])
        for which in ("k", "v"):
            src = KC2 if which == "k" else VC
            for half in range(2):
                st = stg()
                sv = st[0:64, :].rearrange("p (l h) -> p l h", l=16)
                p.dma("sp", sv, W["c1" + which][half * 1024:(half + 1) * 1024, :].rearrange("(l d) h -> d l h", d=64), writes=[(st, None)])
                p.op("pool", lambda e, sv=sv, half=half: e.tensor_copy(out=W1[:, half * 16:(half + 1) * 16, :], in_=sv), reads=[(st, None)], writes=[(W1, None)])
            p.dma("sp", posTf[:], W["cp" + which + "T"], writes=[(posTf, None)])
            p.op("pool", lambda e: e.tensor_copy(out=posT[:], in_=posTf[:]), reads=[(posTf, None)], writes=[(posT, None)])
            st = stg()
            p.dma("sp", st[:, 0:64], W["c2" + which], writes=[(st, None)])
            p.op("pool", lambda e, st=st: e.tensor_copy(out=W2d[:, 0:64], in_=st[:, 0:64]), reads=[(st, None)], writes=[(W2d, None)])
            p.op("pool", lambda e, st=st: e.tensor_copy(out=W2d[:, 64:128], in_=st[:, 0:64]), reads=[(st, None)], writes=[(W2d, None)])
            hb_, pb_ = ps[0], ps[1]
            srcv = src[0:64, :].rearrange("p (c s) -> p c s", s=16)
            mm_group(cx, hb_[:, 0:255], (hb_, None),
                     [(W1[:, l, :], srcv[:, (l // 16):(l // 16) + 255, l % 16], [(W1, None), (src, None)]) for l in range(32)])
            mm_group(cx, pb_[:, 0:1], (pb_, None), [(W1[:, l, :], posT[:, l:l + 1], [(W1, None), (posT, None)]) for l in range(32)])
            p.op("dve", lambda e: e.tensor_copy(out=posb[:], in_=pb_[:, 0:1]), reads=[(pb_, None)], writes=[(posb, None)])
            p.op("pool", lambda e: e.memset(HID[:], 0.0), writes=[(HID, None)])
            gelu_tanh(cx, hb_[:, 0:255], (hb_, None), posb[:, 0:1], (posb, None), HID[:, 0:255], (HID, None), ga, gb, 255)
            if which == "k":
                ob_ = ps[2]
                p.op("pe", lambda e: e.matmul(ob_[:, 0:256], lhsT=W2d[:], rhs=HID[:], start=True, stop=True), reads=[(W2d, None), (HID, None)], writes=[(ob_, None)])
                p.op("act", lambda e: e.activation(out=KCMP[:], in_=ob_[:, 0:256], func=AF.Copy), reads=[(ob_, None)], writes=[(KCMP, None)])
            else:
                for cc in range(2):
                    ob_ = ps[3 + cc]
                    p.op("pe", lambda e, cc=cc, ob_=ob_: e.matmul(ob_[:, 0:64], lhsT=HID[:, cc * 128:(cc + 1) * 128], rhs=W2d[:, 0:64], start=True, stop=True),
                         reads=[(W2d, None), (HID, None)], writes=[(ob_, None)])
                    p.op("act", lambda e, cc=cc, ob_=ob_: e.activation(out=VCMP[:, cc, 0:64], in_=ob_[:, 0:64], func=AF.Copy), reads=[(ob_, None)], writes=[(VCMP, None)])
        p.barrier()
    with ExitStack() as esa:
        sba = mk_sb(esa)
        OC = sba("OC", [128, 4, 512], F32)
        pcs = [sba(f"pc{i}", [128, 512], BF16) for i in range(2)]
        acc = sba("acc", [128, 4, 64], F32)
        sc = sba("sc", [128, 64], F32)
        sc2 = sba("sc2", [128, 64], F32)
        m8 = sba("m8", [128, 8], F32)
        m8b = sba("m8b", [128, 8], F32)
        rsi = sba("rsi", [128, 1], F32)
        NM = sba("NM", [128, 128], BF16)
        tacc = sba("tacc", [128, 512], F32)
        t2 = sba("t2", [128, 512], F32)
        sbanks = ps[0:3]
        Oacc = [ps[3], ps[4]]
        Gb, IMb = ps[5], ps[6]
        oi = [0]

        def next_O():
            oi[0] += 1
            return Oacc[oi[0] % 2]

        def gate_fac(Ob, n, j, qb, clamp):
            jj = 3 * n + j
            p.op("pe", lambda e: e.matmul(Gb[:], lhsT=sel12[0:12, jj * 128:(jj + 1) * 128], rhs=GLT[0:12, qb * 512:(qb + 1) * 512], start=True, stop=True),
                 reads=[(sel12, None), (GLT, qb)], writes=[(Gb, None)])
            if clamp:
                p.op("dve", lambda e: e.tensor_scalar(out=rs[64:128, :], in0=Ob[64:128, :], scalar1=1e-30, scalar2=None, op0=ALU.max), reads=[(Ob, None)], writes=[(rs, None)])
                p.op("dve", lambda e: e.reciprocal(out=rs[64:128, :], in_=rs[64:128, :]), reads=[(rs, None)], writes=[(rs, None)])
            else:
                p.op("dve", lambda e: e.reciprocal(out=rs[64:128, :], in_=Ob[64:128, :]), reads=[(Ob, None)], writes=[(rs, None)])
            p.op("dve", lambda e: e.tensor_tensor(out=fac[64:128, :], in0=Gb[64:128, :], in1=rs[64:128, :], op=ALU.mult), reads=[(Gb, None), (rs, None)], writes=[(fac, None)])

        for qb in range(8):
            chunks = [0] if qb < 4 else [0, 1]
            for n in range(4):
                r0 = (n % 2) * 64
                Ob = next_O()

                def cmask_fn(cc, qb=qb):
                    delta = 2048 * cc - 512 * qb
                    if delta <= -2560:
                        return []
                    return [(cmpmask[:, (delta + 2048) // 512, :], [(cmpmask, None)])]
                stream = dict(
                    k=lambda cc: (KCMP[r0:r0 + 64, cc * 128:(cc + 1) * 128], [(KCMP, None)]),
                    q=(QA[r0:r0 + 64, n, qb * 512:(qb + 1) * 512], [(QA, (n, qb))]),
                    scale=0.125, bias=None, masks=cmask_fn,
                    pv=[(Ob[:], lambda cc: (VCMP[:, cc, :], [(VCMP, None)]))], pv_dep=(Ob, None))
                keep = []
                run_streams(cx, [stream], chunks, sbanks, pcs, L, keep=keep)
                for t4 in range(4):
                    mm_group(cx, IMb[:, 0:65], (IMb, None),
                             [(pt[:, t4 * 128:(t4 + 1) * 128], ovl[:, cc, :], [(pt, None), (ovl, None)]) for (cc, pt) in keep])
                    p.op("dve", lambda e: e.tensor_scalar(out=rsi[:], in0=IMb[:, 64:65], scalar1=1e-30, scalar2=None, op0=ALU.max), reads=[(IMb, None)], writes=[(rsi, None)])
                    p.op("dve", lambda e: e.reciprocal(out=rsi[:], in_=rsi[:]), reads=[(rsi, None)], writes=[(rsi, None)])
                    if n == 0:
                        p.op("dve", lambda e, t4=t4: e.tensor_scalar(out=acc[:, t4, :], in0=IMb[:, 0:64], scalar1=rsi[:, 0:1], scalar2=None, op0=ALU.mult),
                             reads=[(IMb, None), (rsi, None)], writes=[(acc, t4)])
                    else:
                        p.op("dve", lambda e, t4=t4: e.scalar_tensor_tensor(out=acc[:, t4, :], in0=IMb[:, 0:64], scalar=rsi[:, 0:1], in1=acc[:, t4, :], op0=ALU.mult, op1=ALU.add),
                             reads=[(IMb, None), (rsi, None), (acc, t4)], writes=[(acc, t4)])
                gate_fac(Ob, n, 0, qb, True)
                p.op("dve", lambda e, n=n, Ob=Ob: e.tensor_tensor(out=OC[0:64, n, :], in0=Ob[0:64, :], in1=fac[64:128, :], op=ALU.mult),
                     reads=[(Ob, None), (fac, None)], writes=[(OC, n)])
            for t4 in range(4):
                qt = 4 * qb + t4
                o0 = 62 - 2 * qt
                p.op("dve", lambda e, t4=t4, o0=o0: e.tensor_tensor(out=sc[:], in0=acc[:, t4, :], in1=tkmul[:, o0:o0 + 64], op=ALU.mult), reads=[(acc, t4), (tkmul, None)], writes=[(sc, None)])
                p.op("dve", lambda e, o0=o0: e.tensor_tensor(out=sc[:], in0=sc[:], in1=tkadd[:, o0:o0 + 64], op=ALU.add), reads=[(sc, None), (tkadd, None)], writes=[(sc, None)])
                p.op("dve", lambda e: e.memset(sc[:, 0:1], 1e30), reads=[(sc, None)], writes=[(sc, None)])
                p.op("dve", lambda e: e.max(out=m8[:], in_=sc[:]), reads=[(sc, None)], writes=[(m8, None)])
                p.op("dve", lambda e: e.match_replace(out=sc2[:], in_to_replace=m8[:], in_values=sc[:], imm_value=-3.0e38), reads=[(sc, None), (m8, None)], writes=[(sc2, None)])
                p.op("dve", lambda e: e.max(out=m8b[:], in_=sc2[:]), reads=[(sc2, None)], writes=[(m8b, None)])
                p.op("dve", lambda e: e.tensor_scalar(out=sc2[:], in0=sc[:], scalar1=m8b[:, 7:8], scalar2=None, op0=ALU.is_ge), reads=[(sc, None), (m8b, None)], writes=[(sc2, None)])
                for hf in range(2):
                    p.op("dve", lambda e, hf=hf: e.tensor_scalar(out=NM[:, hf * 64:(hf + 1) * 64], in0=sc2[:], scalar1=-1.0, scalar2=-NEG, op0=ALU.add, op1=ALU.mult),
                         reads=[(sc2, None)], writes=[(NM, None)])
                p.op("pe", lambda e: e.transpose(out=pst[:, 0:128], in_=NM[:], identity=cx.ident[:]), reads=[(NM, None), (cx.ident, None)], writes=[(pst, None)])
                cols = slice(qt * 128, (qt + 1) * 128)
                for n in range(4):
                    rr = slice(64, 128) if n % 2 == 0 else slice(0, 64)
                    eng = "act" if n % 2 == 0 else "dve"
                    if eng == "act":
                        p.op("act", lambda e, n=n, rr=rr, cols=cols: e.activation(out=QA[rr, n, cols], in_=pst[rr, 0:128], func=AF.Copy), reads=[(pst, None)], writes=[(QA, (n, qb))])
                    else:
                        p.op("dve", lambda e, n=n, rr=rr, cols=cols: e.tensor_copy(out=QA[rr, n, cols], in_=pst[rr, 0:128]), reads=[(pst, None)], writes=[(QA, (n, qb))])
            for n in range(4):
                r0 = (n % 2) * 64
                KS = KSA if n % 2 == 0 else KSB
                Ob = next_O()
                stream = dict(
                    k=lambda kb: (KS[:, kb * 128:(kb + 1) * 128], [(KS, None)]),
                    q=(QA[:, n, qb * 512:(qb + 1) * 512], [(QA, (n, qb))]),
                    scale=0.125, bias=None,
                    masks=lambda kb: ([(cmask[:, kb - 4 * qb, :], [(cmask, None)])] if kb >= 4 * qb else []),
                    pv=[(Ob[:], lambda kb: (VS[:, kb, :], [(VS, kb)]))], pv_dep=(Ob, None))
                run_streams(cx, [stream], list(range(4 * qb + 4)), sbanks, pts, L)
                gate_fac(Ob, n, 1, qb, False)
                p.op("dve", lambda e, Ob=Ob: e.tensor_tensor(out=tacc[0:64, :], in0=Ob[0:64, :], in1=fac[64:128, :], op=ALU.mult), reads=[(Ob, None), (fac, None)], writes=[(tacc, None)])
                p.op("pool", lambda e, n=n: e.tensor_tensor(out=tacc[0:64, :], in0=tacc[0:64, :], in1=OC[0:64, n, :], op=ALU.add), reads=[(tacc, None), (OC, n)], writes=[(tacc, None)])
                Ob2 = next_O()

                def wmasks(kb, qb=qb):
                    if kb >= 4 * qb:
                        return [(cmask[:, kb - 4 * qb, :], [(cmask, None)])]
                    return [(cmask[:, 4 + kb - (4 * qb - 4), :], [(cmask, None)])]
                stream = dict(
                    k=lambda kb: (KW2[r0:r0 + 64, kb * 128:(kb + 1) * 128], [(KW2, kb // 4)]),
                    q=(QA[r0:r0 + 64, n, qb * 512:(qb + 1) * 512], [(QA, (n, qb))]),
                    scale=0.125, bias=None, masks=wmasks,
                    pv=[(Ob2[:], lambda kb: (VW[:, kb, :], [(VW, kb)]))], pv_dep=(Ob2, None))
                run_streams(cx, [stream], list(range(max(0, 4 * qb - 4), 4 * qb + 4)), sbanks, pts, L)
                gate_fac(Ob2, n, 2, qb, False)
                p.op("dve", lambda e, Ob2=Ob2: e.tensor_tensor(out=t2[0:64, :], in0=Ob2[0:64, :], in1=fac[64:128, :], op=ALU.mult), reads=[(Ob2, None), (fac, None)], writes=[(t2, None)])
                ot = obt[n % 2]
                p.op("pool", lambda e, ot=ot: e.tensor_tensor(out=ot[0:64, :], in0=tacc[0:64, :], in1=t2[0:64, :], op=ALU.add), reads=[(tacc, None), (t2, None)], writes=[(ot, None)])
                p.dma("sp", oT_ap[256 + n * 64:256 + (n + 1) * 64, qb * 512:(qb + 1) * 512], ot[0:64, :], reads=[(ot, None)], writes=[(cx.outb, ("oTn", n, qb))])
        p.barrier()


import numpy as np, math
import ml_dtypes
def np_bf16(a):
    return np.asarray(a, dtype=np.float32).astype(ml_dtypes.bfloat16)

def swap_cols(w):
    w = w.reshape(w.shape[0], -1, 64).copy()
    a = w[:, :, 0:8].copy(); w[:, :, 0:8] = w[:, :, 8:16]; w[:, :, 8:16] = a
    return w.reshape(w.shape[0], -1)

def odd_w_own(w_in, hh):
    q = w_in[:, 512 * hh:512 * hh + 512]; k = w_in[:, 1024 + 512 * hh:1024 + 512 * hh + 512]; v = w_in[:, 2048 + 512 * hh:2048 + 512 * hh + 512]
    return np.ascontiguousarray(np.concatenate([q, swap_cols(q), k, swap_cols(k), v], axis=1))

def even_w_own(w, hh):
    def c(o, n): return w[:, o:o + n]
    fq = c(256 * hh, 256); fk = c(512 + 256 * hh, 256); fv = c(1024 + 256 * hh, 256); fl = c(1536 + 4 * hh, 4)
    nq = c(1544 + 256 * hh, 256); kc = c(2056 + 64 * hh, 64); vc = c(2184 + 64 * hh, 64); ks = c(2312 + 64 * hh, 64)
    vs = c(2440 + 64 * hh, 64); kw = c(2568 + 64 * hh, 64); vw = c(2696 + 64 * hh, 64); gl = c(2824 + 12 * hh, 12)
    return np.ascontiguousarray(np.concatenate([fq, fk, fv, nq, swap_cols(nq), kc, swap_cols(kc), ks, swap_cols(ks), kw, swap_cols(kw), vc, vs, vw, fl, gl], axis=1))

def host_consts():
    c = {}
    c["ident"] = np_bf16(np.eye(128))
    k = np.arange(128)[:, None]; q = np.arange(512)[None, :]
    cm = np.stack([(128 * j + k <= q) for j in range(4)]).astype(np.float32)
    c["cmask"] = np_bf16(np.concatenate([cm, 1.0 - cm], axis=0).transpose(1, 0, 2).reshape(128, 8 * 512))
    inv = (500000.0 ** (-np.arange(0, 16, 2, dtype=np.float64) / 16)) / (2 * np.pi)
    r = np.zeros((128, 2), np.float32)
    for p_ in range(128):
        j = p_ % 64
        if j < 8: r[p_, 0] = -inv[j]; r[p_, 1] = inv[j]
        elif j < 16: r[p_, 0] = inv[j - 8]; r[p_, 1] = inv[j - 8]
    c["ropeinv"] = r
    cmp = np.stack([(16 * k + 31 + (-2048 + 512 * i) <= q) for i in range(5)]).astype(np.float32)
    c["cmpmask"] = np_bf16(cmp.transpose(1, 0, 2).reshape(128, 5 * 512))
    cs = np.arange(256)[:, None] * 16; ss = np.arange(64)[None, :] * 64
    ov = np.clip(np.minimum(cs + 32, ss + 64) - np.maximum(cs, ss), 0, None) / 32.0
    ov[255] = 0
    ovl = np.concatenate([ov, np.ones((256, 1))], axis=1).reshape(2, 128, 65).transpose(1, 0, 2).reshape(128, 130)
    c["ovl"] = np_bf16(ovl)
    sel = np.zeros((12, 12, 128), np.float32)
    for j in range(12): sel[j, j, :] = 1
    c["sel12"] = np_bf16(sel.reshape(12, 12 * 128))
    add = np.zeros((128, 128), np.float32); mul = np.zeros((128, 128), np.float32)
    for p_ in range(128):
        cur = 1 if p_ >= 64 else 0
        for i in range(128):
            s_ = i - 62
            valid = s_ <= cur
            forced = (s_ == cur) or (s_ == cur - 1)
            if not valid: add[p_, i] = -1e30
            elif forced: add[p_, i] = 1e30
            else: mul[p_, i] = 1.0
    c["tkadd"] = add; c["tkmul"] = mul
    c["onehot"] = np_bf16((np.arange(4096)[None, :] // 64 == np.arange(64)[:, None]).astype(np.float32))
    c["tri"] = (np.arange(128)[:, None] <= np.arange(128)[None, :]).astype(np.float32)
    s127 = np.zeros((128, 128), np.float32); s127[127, :] = 1
    c["sel127"] = s127
    return c


from concourse.bass_utils import run_bass_kernel_spmd

CONST_SPECS = {"cmask": ([128, 4096], BF16), "ropeinv": ([128, 2], F32), "cmpmask": ([128, 2560], BF16), "ovl": ([128, 130], BF16), "sel12": ([12, 1536], BF16),
               "tkadd": ([128, 128], F32), "tkmul": ([128, 128], F32), "onehot": ([64, 4096], BF16), "tri": ([128, 128], F32), "sel127": ([128, 128], F32)}
GROUPS = [[0, 1], [2, 3], [4, 5], [6, 7]]
_PROG = {}
DEPTH = 4


class OTMap:
    def __init__(self, ap_a, ap_b):
        self.aps = (ap_a, ap_b)

    def __getitem__(self, idx):
        rows, cols = idx
        qb = cols.start // 512
        half, c0 = qb // 4, (qb % 4) * 512
        k = rows.start // 256
        assert (rows.stop - 1) // 256 == k
        r0, r1 = rows.start - 256 * k, rows.stop - 256 * k
        return self.aps[k][half * 256 + r0:half * 256 + r1, c0:c0 + 512]


def build_fused():
    nc = bass.Bass("TRN2", target_bir_lowering=False)

    def din(name, shape, dt=F32):
        return nc.dram_tensor(name, list(shape), dt, kind="ExternalInput").ap()
    ident = din("ident", [128, 128], BF16)
    x_in = din("x_in", [TOK, D])
    C = {k: din(k, s, dt) for k, (s, dt) in CONST_SPECS.items()}
    pos = din("pos", [1, T], I32)
    mem = din("mem", [MEM, D])
    g_all = din("g_all", [DEPTH * 6, D])
    mem_g = din("mem_g", [DEPTH, D])
    WL = []
    for l in range(DEPTH):
        W = {"g": g_all[l * 6:(l + 1) * 6, :], "mem": mem, "mem_g": mem_g[l:l + 1, :], "pos": pos}
        W.update(C)
        for nm, shp in (("w_out", [D, D]), ("ca_wq", [D, 256]), ("ca_wk", [D, 256]), ("ca_wv", [D, 256]), ("ca_wo", [256, D]),
                        ("ffn_wg", [D, DFF]), ("ffn_wu", [D, DFF]), ("ffn_wd", [DFF, D])):
            W[nm] = din(f"{nm}_{l}", shp)
        if l % 2 == 0:
            for nm, shp in (("w_in", [D, EV_NCOL]), ("fbias_rep", [1, 128]), ("c1k", [2048, 128]), ("c2k", [128, 64]), ("cpkT", [64, 32]),
                            ("c1v", [2048, 128]), ("c2v", [128, 64]), ("cpvT", [64, 32])):
                W[nm] = din(f"{nm}_{l}", shp)
        else:
            for nm, shp in (("w_in", [D, 2560]), ("lam", [1, 256]), ("subg", [128, 1]), ("laminit", [1, 2])):
                W[nm] = din(f"{nm}_{l}", shp)
        WL.append(W)
    x_out = nc.dram_tensor("x_out", [TOK, D], F32, kind="ExternalOutput").ap()
    hT_own_t = [nc.dram_tensor(f"hT_own{k}", [512, TOK], BF16) for k in range(2)]
    hT_g_t = [nc.dram_tensor(f"hT_g{k}", [1024, TOK], BF16) for k in range(2)]
    oT_own_t = [nc.dram_tensor(f"oT_own{k}", [512, TOK], BF16) for k in range(2)]
    oT_g_t = [nc.dram_tensor(f"oT_g{k}", [1024, TOK], BF16) for k in range(2)]
    x_scr_t = nc.dram_tensor("x_scr", [TOK, D], F32)
    wscr = {nm: nc.dram_tensor(f"scr_{nm}", [NFF, 128, 1024], BF16).ap() for nm in ("ffn_wg", "ffn_wu", "ffn_wd")}
    x_scr = x_scr_t.ap()
    HTO, HTG, OTO, OTG, XS = Buf(None, "hT_own"), Buf(None, "hT_g"), Buf(None, "oT_own"), Buf(None, "oT_g"), Buf(None, "x_scr")
    cx = make_ctx(nc, ident)
    p = cx.p
    cx.wsc = Buf(None, "wscr")
    cx.outb = HTO
    pid = nc.sync.partition_id()
    hh256 = (pid % 2) * 256

    def gather_hT():
        for k in range(2):
            p.collective("AllGather", hT_own_t[k].ap().opt(), hT_g_t[k].ap().opt(), GROUPS, reads=[(HTO, None)], writes=[(HTG, k)])

    def gather_oT():
        for k in range(2):
            p.collective("AllGather", oT_own_t[k].ap().opt(), oT_g_t[k].ap().opt(), GROUPS, reads=[(OTO, None)], writes=[(OTG, k)])

    def hT_store(hT, tb):
        for k in range(2):
            p.dma("sp", hT_own_t[k].ap()[:, tb * TB:(tb + 1) * TB].rearrange("(c p) n -> p c n", p=128), hT[:, 4 * k:4 * k + 4, :],
                  reads=[(hT, None)], writes=[(HTO, (k, tb))])

    def hT_chunk(r, c):
        return hT_g_t[c // 4].ap()[r * 512 + (c % 4) * 128:r * 512 + (c % 4 + 1) * 128, :]

    def x_view(ap):
        return ap.rearrange("(t p) d -> p t d", p=128)

    with ExitStack() as es:
        def sb(name, shape, dt):
            return Buf(es.enter_context(nc.sbuf_tensor(name + "_a0", list(shape), dt)), name)
        x = sb("x", [128, NT, D], F32)
        p.dma("sp", x[:], x_view(x_in), writes=[(x, None)])
        cx.ps, cx.pst = psum_set(cx, es, 1, True)
        L = {"gB": sb("gB", [128, D], F32), "stat": sb("stat", [128, 16], F32), "junk": sb("junk", [128, D], BF16),
             "hb": [sb(f"hb{i}", [128, D], BF16) for i in range(2)]}
        hT = sb("hT", [128, 8, TOK], BF16)
        norm_transpose(cx, x, list(range(NT)), g_all[0:1, :], hT, L)
        for k in range(2):
            p.dma("sp", hT_own_t[k].ap().rearrange("(c p) n -> p c n", p=128), hT[:, 4 * k:4 * k + 4, :], reads=[(hT, None)], writes=[(HTO, (k, 0))])
        p.barrier()
    gather_hT()

    for l in range(DEPTH):
        W = WL[l]
        cx.outb = OTO
        if l % 2 == 0:
            def hT_src(dst_ap, c, tok0, n, q, writes):
                r = tok0 // TOK
                p.dma(q, dst_ap, hT_chunk(r, c)[:, tok0 % TOK:tok0 % TOK + n], reads=[(HTG, None)], writes=writes)
            phase_B_even(cx, hT_src, W, OTMap(oT_own_t[0].ap(), oT_own_t[1].ap()))
        else:
            def load_hT(hT):
                for c in range(8):
                    for r in range(2):
                        p.dma("sp" if c % 2 == 0 else "pool", hT[:, c, r * TOK:(r + 1) * TOK], hT_chunk(r, c),
                              reads=[(HTG, None)], writes=[(hT, None)])
            phase_B_odd(cx, load_hT, W, OTMap(oT_own_t[0].ap(), oT_own_t[1].ap()))
        p.barrier()
        gather_oT()
        cx.outb = HTO
        with ExitStack() as es:
            x = Buf(es.enter_context(nc.sbuf_tensor(f"x_l{l}", [128, NT, D], F32)), "x")
            p.dma("sp", x[:], x_view(x_in if l == 0 else x_scr), reads=[(XS, None)], writes=[(x, None)])

            def oT_load(oT, tb, writes):
                for r in range(2):
                    for k in range(2):
                        src = oT_g_t[k].ap()[bass.ds(hh256 + r * 512, 256), tb * TB:(tb + 1) * TB]
                        p.dma("sp", oT[:, 4 * r + 2 * k:4 * r + 2 * k + 2, :], src.rearrange("(c p) n -> p c n", p=128), reads=[(OTG, None)], writes=writes)
            last = (l == DEPTH - 1)
            phase_C(cx, x, None, W, None, None if last else g_all[(l + 1) * 6:(l + 1) * 6 + 1, :], oT_load=oT_load, hT_store=None if last else hT_store, wscr=wscr)
            if last:
                OUT = Buf(None, "x_out")
                p.dma("sp", x_view(x_out), x[:], reads=[(x, None)], writes=[(OUT, None)])
                p.finish([OUT])
            else:
                p.dma("sp", x_view(x_scr), x[:], reads=[(x, None)], writes=[(XS, None)])
                p.barrier()
        if not last:
            gather_hT()
    print("fused program: n_inst", p.n_inst, "n_wait", p.n_wait)
    p.close()
    return nc


def _ca(a):
    return np.ascontiguousarray(a)


def kernel(x, mem, positions, sandwich_g, mem_norm_g, ev_w_in, ev_fox_fbias,
           ev_cmp_pos_k, ev_cmp_w1_k, ev_cmp_w2_k, ev_cmp_pos_v, ev_cmp_w1_v, ev_cmp_w2_v,
           ev_w_out, od_w_in, od_lambda, od_subln_g, od_w_out,
           ca_wq, ca_wk, ca_wv, ca_wo, ffn_wg, ffn_wu, ffn_wd):
    f32 = lambda a: np.asarray(a, dtype=np.float32)
    x = f32(x); mem = f32(mem); positions = np.asarray(positions).astype(np.int32)
    sandwich_g = f32(sandwich_g); mem_norm_g = f32(mem_norm_g)
    hc = host_consts()
    if "nc" not in _PROG:
        _PROG["nc"] = build_fused()
    nc = _PROG["nc"]
    cores = list(range(8))
    shared = {k: hc[k] for k in CONST_SPECS}
    shared["ident"] = hc["ident"]
    shared["g_all"] = _ca(sandwich_g.reshape(DEPTH * 6, D))
    shared["mem_g"] = _ca(mem_norm_g)
    per_h = [dict(), dict()]
    for l in range(DEPTH):
        for nm, arr in (("ca_wq", ca_wq), ("ca_wk", ca_wk), ("ca_wv", ca_wv), ("ca_wo", ca_wo), ("ffn_wg", ffn_wg), ("ffn_wu", ffn_wu), ("ffn_wd", ffn_wd)):
            shared[f"{nm}_{l}"] = _ca(f32(arr[l]))
        if l % 2 == 0:
            e = l // 2
            wo = f32(ev_w_out[e])
            shared[f"w_out_{l}"] = _ca(np.concatenate([wo[0:256], wo[512:768], wo[256:512], wo[768:1024]], axis=0))
            shared[f"c1k_{l}"] = _ca(f32(ev_cmp_w1_k[e])); shared[f"c2k_{l}"] = _ca(f32(ev_cmp_w2_k[e])); shared[f"cpkT_{l}"] = _ca(f32(ev_cmp_pos_k[e]).T)
            shared[f"c1v_{l}"] = _ca(f32(ev_cmp_w1_v[e])); shared[f"c2v_{l}"] = _ca(f32(ev_cmp_w2_v[e])); shared[f"cpvT_{l}"] = _ca(f32(ev_cmp_pos_v[e]).T)
            for hh in range(2):
                per_h[hh][f"w_in_{l}"] = even_w_own(f32(ev_w_in[e]), hh)
                per_h[hh][f"fbias_rep_{l}"] = _ca(np.tile(f32(ev_fox_fbias[e])[4 * hh:4 * hh + 4], 32)[None, :])
        else:
            o = l // 2
            lam_init = 0.8 - 0.6 * math.exp(-0.3 * l)
            shared[f"w_out_{l}"] = _ca(f32(od_w_out[o]))
            shared[f"lam_{l}"] = _ca(f32(od_lambda[o]).reshape(1, 256))
            shared[f"subg_{l}"] = _ca(f32(od_subln_g[o]).reshape(128, 1))
            shared[f"laminit_{l}"] = np.array([[-lam_init, 1.0 - lam_init]], np.float32)
            for hh in range(2):
                per_h[hh][f"w_in_{l}"] = odd_w_own(f32(od_w_in[o]), hh)
    maps = []
    for c in cores:
        b, hh = c // 2, c % 2
        m = dict(shared)
        m.update(per_h[hh])
        m["x_in"] = _ca(x[b, TOK * hh:TOK * (hh + 1)])
        m["pos"] = _ca(positions[b:b + 1])
        m["mem"] = _ca(mem[b])
        maps.append(m)
    res = run_bass_kernel_spmd(nc, maps, core_ids=cores)
    out = np.zeros((4, T, D), np.float32)
    for c in cores:
        out[c // 2, TOK * (c % 2):TOK * (c % 2 + 1)] = res.results[c]["x_out"]
    return out
```

```python
from contextlib import ExitStack
import numpy as np
import concourse.bass as bass
import concourse.mybir as mybir

F32 = mybir.dt.float32
BF16 = mybir.dt.bfloat16
I32 = mybir.dt.int32
AF = mybir.ActivationFunctionType
ALU = mybir.AluOpType
AX = mybir.AxisListType


class Buf:
    _n = 0

    def __init__(self, t, name):
        self.t = t
        self.name = name
        self.regions = {}
        self.whole = [None, {}]

    def __getitem__(self, idx):
        return self.t[idx]


class Prog:
    ENG = ["pe", "dve", "act", "pool", "sp"]

    def __init__(self, nc, n_dma_sems=10):
        self.nc = nc
        self.es = ExitStack()
        self.eng = {"pe": nc.tensor, "dve": nc.vector, "act": nc.scalar, "pool": nc.gpsimd, "sp": nc.sync}
        self.sem = {e: self.es.enter_context(nc.semaphore("s_" + e)) for e in self.ENG}
        self.cnt = {e: 0 for e in self.ENG}
        self.sem["cc"] = self.es.enter_context(nc.semaphore("s_cc"))
        self.cnt["cc"] = 0
        self.waited = {}
        self.dsem = {}
        self.dval = {}
        self.dnext = {}
        for q in ["sp", "act", "pool"]:
            self.dsem[q] = [self.es.enter_context(nc.semaphore(f"d_{q}{i}")) for i in range(n_dma_sems)]
            self.dval[q] = [0] * n_dma_sems
            self.dnext[q] = 0
        self.dwaited = {}
        self.n_inst = 0
        self.n_wait = 0

    def sbuf(self, name, shape, dtype):
        t = self.es.enter_context(self.nc.sbuf_tensor(name, list(shape), dtype))
        return Buf(t, name)

    def psum(self, name, shape, dtype):
        t = self.es.enter_context(self.nc.psum_tensor(name, list(shape), dtype))
        return Buf(t, name)

    def close(self):
        self.es.close()

    def _states(self, buf, key):
        if key is None:
            return [buf.whole] + list(buf.regions.values())
        if key not in buf.regions:
            buf.regions[key] = [None, {}]
        return [buf.whole, buf.regions[key]]

    def _need(self, deps, tok):
        if tok is not None:
            deps.add(tok)

    def _collect(self, reads, writes):
        deps = set()
        for (b, k) in reads:
            for st in self._states(b, k):
                self._need(deps, st[0])
        for (b, k) in writes:
            for st in self._states(b, k):
                self._need(deps, st[0])
                for tok in st[1].values():
                    deps.add(tok)
        return deps

    def _emit_waits(self, e, deps, skip_same=False):
        engobj = self.eng[e]
        best = {}
        for tok in deps:
            if tok[0] == "e":
                _, f, c = tok
                if f == e and skip_same:
                    continue
                key = ("e", f)
                best[key] = max(best.get(key, 0), c)
            else:
                _, q, i, v = tok
                key = ("d", q, i)
                best[key] = max(best.get(key, 0), v)
        for key, v in best.items():
            wk = (e,) + key
            if self.waited.get(wk, -1) >= v:
                continue
            self.waited[wk] = v
            if key[0] == "e":
                engobj.wait_ge(self.sem[key[1]], v)
            else:
                engobj.wait_ge(self.dsem[key[1]][key[2]], v)
            self.n_wait += 1

    def _record(self, tok, reads, writes):
        for (b, k) in reads:
            if k is None:
                b.whole[1][tok[1] if tok[0] == "e" else ("d",) + tok[1:3]] = tok
            else:
                st = self._states(b, k)[1]
                st[1][tok[1] if tok[0] == "e" else ("d",) + tok[1:3]] = tok
        for (b, k) in writes:
            if k is None:
                b.regions.clear()
                b.whole[0] = tok
                b.whole[1] = {}
            else:
                st = self._states(b, k)[1]
                st[0] = tok
                st[1] = {}

    def alias(self, ap, name):
        return Buf(ap, name)

    def barrier(self):
        alld = set()
        for e in self.ENG + ["cc"]:
            if self.cnt[e] > 0:
                alld.add(("e", e, self.cnt[e]))
        for q in self.dsem:
            for i, v in enumerate(self.dval[q]):
                if v > 0:
                    alld.add(("d", q, i, v))
        for e in self.ENG:
            self._emit_waits(e, alld)

    def op(self, e, fn, reads=(), writes=(), skip_same=False, inc=True):
        deps = self._collect(reads, writes)
        if e == "pe":
            skip_same = True
        if skip_same is False and e in ("dve", "act", "pool"):
            raw = set()
            for (b, k) in reads:
                for st in self._states(b, k):
                    if st[0] is not None:
                        raw.add(st[0])
            deps = {t for t in deps if not (t[0] == "e" and t[1] == e) or t in raw}
        self._emit_waits(e, deps, skip_same=skip_same)
        inst = fn(self.eng[e])
        if inc:
            self.cnt[e] += 1
            inst.then_inc(self.sem[e], 1)
            tok = ("e", e, self.cnt[e])
        else:
            tok = ("e", e, self.cnt[e] + 1)
        self._record(tok, reads, writes)
        self.n_inst += 1
        return inst

    def dma(self, q, out_ap, in_ap, reads=(), writes=(), **kw):
        e = q
        deps = self._collect(reads, writes)
        i = self.dnext[q]
        self.dnext[q] = (i + 1) % len(self.dsem[q])
        if self.dval[q][i] > 0:
            deps.add(("d", q, i, self.dval[q][i]))
        self._emit_waits(e, deps)
        self.dval[q][i] += 16
        inst = self.eng[e].dma_start(out=out_ap, in_=in_ap, **kw)
        inst.then_inc(self.dsem[q][i], 16)
        tok = ("d", q, i, self.dval[q][i])
        self._record(tok, reads, writes)
        self.n_inst += 1
        return inst

    def collective(self, kind, in_ap, out_ap, groups, reads=(), writes=()):
        deps = self._collect(reads, writes)
        self._emit_waits("pool", deps)
        self.cnt["cc"] += 1
        inst = self.nc.gpsimd.collective_compute(kind, mybir.AluOpType.bypass, replica_groups=groups, ins=[in_ap], outs=[out_ap])
        inst.then_inc(self.sem["cc"])
        tok = ("e", "cc", self.cnt["cc"])
        self._record(tok, reads, writes)
        self.n_inst += 1
        return inst

    def finish(self, out_bufs):
        deps = set()
        for b in out_bufs:
            for st in [b.whole] + list(b.regions.values()):
                if st[0] is not None:
                    deps.add(st[0])
        self._emit_waits("sp", deps)
        alld = set()
        for e in self.ENG + ["cc"]:
            if self.cnt[e] > 0:
                alld.add(("e", e, self.cnt[e]))
        for q in self.dsem:
            for i, v in enumerate(self.dval[q]):
                if v > 0:
                    alld.add(("d", q, i, v))
        self._emit_waits("sp", alld)


import math
import numpy as np
import ml_dtypes
from contextlib import ExitStack

D = 1024
T = 4096
TOK = 2048
NT = TOK // 128
TB = 1024
DFF = 2816
NFF = DFF // 128
EPS = 1e-6
MEM = 256
NEG = -30000.0


def np_bf16(a):
    return np.asarray(a, dtype=np.float32).astype(ml_dtypes.bfloat16)


class Ctx:
    pass


def make_ctx(nc, ident_ap):
    cx = Ctx()
    cx.nc = nc
    p = Prog(nc)
    cx.p = p
    cx.uid = 0
    cx.ident = p.sbuf("ident_sb", [128, 128], BF16)
    p.dma("sp", cx.ident[:], ident_ap, writes=[(cx.ident, None)])
    cx.psi = 0
    cx.outb = Buf(None, "dram_out")
    cx.epsb = p.sbuf("epsb", [128, 1], F32)
    p.op("pool", lambda e: e.memset(cx.epsb[:], EPS), writes=[(cx.epsb, None)])
    return cx


def rot(cx, n=7):
    b = cx.ps[cx.psi % n]
    cx.psi += 1
    return b


def mm_group(cx, bank_ap, bank_dep, pairs):
    p = cx.p
    n = len(pairs)
    for i, (lhsT, rhs, reads) in enumerate(pairs):
        p.op("pe", lambda e, lhsT=lhsT, rhs=rhs, i=i: e.matmul(bank_ap, lhsT=lhsT, rhs=rhs, start=(i == 0), stop=(i == n - 1)),
             reads=reads, writes=[bank_dep], inc=(i == n - 1))


def cast_copy(cx, dst_ap, src_ap, reads, writes):
    p = cx.p
    cx.cast_i = getattr(cx, "cast_i", 0) + 1
    if cx.cast_i % 2 == 0:
        p.op("dve", lambda e: e.tensor_copy(out=dst_ap, in_=src_ap), reads=reads, writes=writes)
    else:
        p.op("act", lambda e: e.activation(out=dst_ap, in_=src_ap, func=AF.Copy), reads=reads, writes=writes)


def load_w_bf16(cx, dst_ap, dst_dep, w_ap, kc, ncols, stage, q="sp", cast_eng="pool"):
    p = cx.p
    assert kc * ncols <= 2048
    sv = stage[:, 0:kc * ncols].rearrange("p (c n) -> p c n", c=kc)
    p.dma(q, sv, w_ap.rearrange("(c p) n -> p c n", p=128), writes=[(stage, None)])
    cast_copy(cx, dst_ap, sv, [(stage, None)], [dst_dep])


def rms_ss(cx, src_ap, src_dep, ncol, stat, junk, key):
    p = cx.p
    p.op("act", lambda e: e.activation(out=junk[:, 0:ncol], in_=src_ap, func=AF.Square, accum_out=stat[:, key:key + 1]),
         reads=[src_dep], writes=[(junk, None), (stat, key)])


def rstd_from_ss(cx, stat, k0, k1, dim):
    p = cx.p
    p.op("act", lambda e: e.activation(out=stat[:, k0:k1], in_=stat[:, k0:k1], func=AF.Sqrt, scale=1.0 / dim, bias=cx.epsb[:, 0:1]),
         reads=[(stat, None), (cx.epsb, None)], writes=[(stat, None)])
    p.op("dve", lambda e: e.reciprocal(out=stat[:, k0:k1], in_=stat[:, k0:k1]), reads=[(stat, None)], writes=[(stat, None)])


def norm_transpose(cx, x, tiles, g_ap, hT, L, q="sp"):
    p = cx.p
    gB, stat, junk, hb = L["gB"], L["stat"], L["junk"], L["hb"]
    p.dma(q, gB[:], g_ap.to_broadcast([128, D]), writes=[(gB, None)])
    n = len(tiles)
    for j, t in enumerate(tiles):
        rms_ss(cx, x[:, t, :], (x, t), D, stat, junk, j)
    rstd_from_ss(cx, stat, 0, n, D)
    for j, t in enumerate(tiles):
        hbt = hb[j % 2]
        p.op("dve", lambda e, t=t, j=j, hbt=hbt: e.scalar_tensor_tensor(out=hbt[:], in0=x[:, t, :], scalar=stat[:, j:j + 1], in1=gB[:],
                                                                         op0=ALU.mult, op1=ALU.mult),
             reads=[(x, t), (stat, None), (gB, None)], writes=[(hbt, None)])
        for c in range(8):
            p.op("pe", lambda e, c=c, hbt=hbt: e.transpose(out=cx.pst[:, c * 128:(c + 1) * 128], in_=hbt[:, c * 128:(c + 1) * 128], identity=cx.ident[:]),
                 reads=[(hbt, None), (cx.ident, None)], writes=[(cx.pst, None)], inc=(c == 7))
        src = cx.pst[:].rearrange("p (c n) -> p c n", c=8)
        if j % 2 == 0:
            p.op("act", lambda e, j=j, src=src: e.activation(out=hT[:, :, j * 128:(j + 1) * 128], in_=src, func=AF.Copy),
                 reads=[(cx.pst, None)], writes=[(hT, j)])
        else:
            p.op("dve", lambda e, j=j, src=src: e.tensor_copy(out=hT[:, :, j * 128:(j + 1) * 128], in_=src),
                 reads=[(cx.pst, None)], writes=[(hT, j)])


def norm_residual(cx, x, t, banks, g_ready_gB, L):
    p = cx.p
    stat2, junk, tmp, gB = L["stat2"], L["junk"], L["tmp"], g_ready_gB
    for nb in range(2):
        rms_ss(cx, banks[nb][:], (banks[nb], None), 512, stat2, junk, nb)
    p.op("dve", lambda e: e.tensor_tensor(out=stat2[:, 2:3], in0=stat2[:, 0:1], in1=stat2[:, 1:2], op=ALU.add),
         reads=[(stat2, None)], writes=[(stat2, None)])
    rstd_from_ss(cx, stat2, 2, 3, D)
    for nb in range(2):
        p.op("dve", lambda e, nb=nb, b=banks[nb]: e.scalar_tensor_tensor(out=tmp[:, nb * 512:(nb + 1) * 512], in0=b[:], scalar=stat2[:, 2:3],
                                                                        in1=gB[:, nb * 512:(nb + 1) * 512], op0=ALU.mult, op1=ALU.mult),
             reads=[(banks[nb], None), (stat2, None), (gB, None)], writes=[(tmp, nb)])
    p.op("pool", lambda e, t=t: e.tensor_tensor(out=x[:, t, :], in0=x[:, t, :], in1=tmp[:], op=ALU.add),
         reads=[(tmp, None), (x, t)], writes=[(x, t)])


def proj_residual(cx, x, tiles, lhs_fn, kc, w, g_ap, L, q="sp"):
    p = cx.p
    gB = L["gB"]
    p.dma(q, gB[:], g_ap.to_broadcast([128, D]), writes=[(gB, None)])
    for j, t in enumerate(tiles):
        banks = [rot(cx), rot(cx)]
        for nb in range(2):
            pairs = []
            for c in range(kc):
                lhsT, ldep = lhs_fn(c, j)
                pairs.append((lhsT, w[:, c, nb * 512:(nb + 1) * 512], [ldep, (w, None)]))
            mm_group(cx, banks[nb][:], (banks[nb], None), pairs)
        norm_residual(cx, x, t, banks, gB, L)


def phase_C(cx, x, oT_ap, W, hT_next_ap, g_next_ap, oT_load=None, hT_store=None, wscr=None):
    p = cx.p
    with ExitStack() as es:
        cx.uid += 1
        uu = cx.uid
        def sb(name, shape, dt):
            return Buf(es.enter_context(cx.nc.sbuf_tensor(f"{name}_{uu}", list(shape), dt)), name)
        cx.ps, cx.pst = psum_set(cx, es, 7, True)
        cx.psi = 0
        L = {}
        L["gB"] = sb("gB", [128, D], F32)
        L["stat"] = sb("stat", [128, 16], F32)
        L["stat2"] = sb("stat2", [128, 4], F32)
        L["junk"] = sb("junk", [128, D], BF16)
        L["tmp"] = sb("tmp", [128, D], F32)
        L["hb"] = [sb(f"hb{i}", [128, D], BF16) for i in range(2)]
        hT = sb("hT", [128, 8, TB], BF16)
        big = sb("big", [128, NFF, TB], BF16)
        stage = [sb(f"stage{i}", [128, 2048], F32) for i in range(2)]
        wsm = sb("wsm", [128, 8, 1024], BF16)
        memT = sb("memT", [128, 8, MEM], BF16)
        kmT = sb("kmT", [128, 2, MEM], BF16)
        vm = sb("vm", [128, 2, 4, 128], BF16)
        pT = [sb(f"pTc{i}", [128, 512], BF16) for i in range(4)]
        rs = sb("rs_c", [128, 512], F32)
        wg = [sb(f"wg{i}", [128, 8, 128], BF16) for i in range(2)]
        wu = [sb(f"wu{i}", [128, 8, 128], BF16) for i in range(2)]
        wd = [sb(f"wd{i}", [128, 1024], BF16) for i in range(3)]
        gsb = [sb(f"gsb{i}", [128, 512], BF16) for i in range(2)]
        bigflat = big.t[:].rearrange("p a b -> p (a b)")
        memx = p.alias(stage[1][:].rearrange("p (t d) -> p t d", t=2), "memx")
        p.dma("sp", memx[:], W["mem"].rearrange("(t p) d -> p t d", p=128), writes=[(memx, None), (stage[1], None)])
        norm_transpose(cx, memx, [0, 1], W["mem_g"], memT, L)
        p.barrier()
        sti = 0
        for (nm, c0) in (("ca_wk", 0), ("ca_wv", 256)):
            load_w_bf16(cx, wsm[:, :, c0:c0 + 256], (wsm, None), W[nm], 8, 256, stage[sti % 2], q="pool")
            sti += 1
        for ht in range(2):
            b = rot(cx)
            mm_group(cx, b[:, 0:MEM], (b, None), [(wsm[:, c, ht * 128:(ht + 1) * 128], memT[:, c, :], [(wsm, None), (memT, None)]) for c in range(8)])
            p.op("act", lambda e, b=b, ht=ht: e.activation(out=kmT[:, ht, :], in_=b[:, 0:MEM], func=AF.Copy), reads=[(b, None)], writes=[(kmT, None)])
        p.op("pool", lambda e: e.memset(vm[:], 1.0), writes=[(vm, None)])
        for mc in range(2):
            b = rot(cx)
            mm_group(cx, b[:, 0:256], (b, None), [(memT[:, c, mc * 128:(mc + 1) * 128], wsm[:, c, 256:512], [(wsm, None), (memT, None)]) for c in range(8)])
            p.op("act", lambda e, b=b, mc=mc: e.activation(out=vm[:, mc, :, 0:64], in_=b[:, 0:256].rearrange("p (h d) -> p h d", h=4), func=AF.Copy),
                 reads=[(b, None)], writes=[(vm, None)])
        for tb in range(TOK // TB):
            tiles = list(range(tb * 8, tb * 8 + 8))
            tsl = slice(tb * TB, (tb + 1) * TB)
            oT = p.alias(bigflat[:, 0:8 * TB].rearrange("p (c n) -> p c n", c=8), "oT")
            if oT_load is None:
                p.dma("sp", oT[:], oT_ap[:, tsl].rearrange("(c p) n -> p c n", p=128), writes=[(oT, None), (big, None)])
            else:
                oT_load(oT, tb, [(oT, None), (big, None)])
            for qd in range(4):
                load_w_bf16(cx, wsm[:, :, qd * 256:(qd + 1) * 256], (wsm, None), W["w_out"][:, qd * 256:(qd + 1) * 256], 8, 256, stage[sti % 2], q="pool")
                sti += 1
            proj_residual(cx, x, tiles, lambda c, j: (oT[:, c, j * 128:(j + 1) * 128], (oT, None)), 8, wsm, W["g"][1:2, :], L)
            p.barrier()
            qcT = p.alias(bigflat[:, 0:2 * TB].rearrange("p (c n) -> p c n", c=2), "qcT")
            ocT = p.alias(bigflat[:, 2 * TB:4 * TB].rearrange("p (c n) -> p c n", c=2), "ocT")
            norm_transpose(cx, x, tiles, W["g"][2:3, :], hT, L)
            load_w_bf16(cx, wsm[:, :, 0:256], (wsm, None), W["ca_wq"], 8, 256, stage[sti % 2], q="pool")
            sti += 1
            for ht in range(2):
                for qb in range(TB // 512):
                    b = rot(cx)
                    mm_group(cx, b[:], (b, None), [(wsm[:, c, ht * 128:(ht + 1) * 128], hT[:, c, qb * 512:(qb + 1) * 512], [(wsm, None), (hT, None)]) for c in range(8)])
                    p.op("act", lambda e, b=b, ht=ht, qb=qb: e.activation(out=qcT[:, ht, qb * 512:(qb + 1) * 512], in_=b[:], func=AF.Copy),
                         reads=[(b, None)], writes=[(qcT, (ht, qb))])
            pi = 0
            for h in range(4):
                ht, r0 = h // 2, (h % 2) * 64
                for qb in range(TB // 512):
                    pts = []
                    for mc in range(2):
                        sbk = rot(cx)
                        p.op("pe", lambda e, sbk=sbk, mc=mc, ht=ht, r0=r0, qb=qb: e.matmul(sbk[:], lhsT=kmT[r0:r0 + 64, ht, mc * 128:(mc + 1) * 128],
                                                                                           rhs=qcT[r0:r0 + 64, ht, qb * 512:(qb + 1) * 512], start=True, stop=True),
                             reads=[(kmT, None), (qcT, (ht, qb))], writes=[(sbk, None)])
                        pt = pT[pi % 4]
                        pi += 1
                        p.op("act", lambda e, sbk=sbk, pt=pt: e.activation(out=pt[:], in_=sbk[:], func=AF.Exp, scale=0.125), reads=[(sbk, None)], writes=[(pt, None)])
                        pts.append(pt)
                    ob = rot(cx)
                    mm_group(cx, ob[:], (ob, None), [(vm[:, mc, h, :], pts[mc][:], [(vm, None), (pts[mc], None)]) for mc in range(2)])
                    p.op("dve", lambda e, ob=ob: e.reciprocal(out=rs[64:128, :], in_=ob[64:128, :]), reads=[(ob, None)], writes=[(rs, None)])
                    p.op("dve", lambda e, ob=ob, ht=ht, r0=r0, qb=qb: e.tensor_tensor(out=ocT[r0:r0 + 64, ht, qb * 512:(qb + 1) * 512], in0=ob[0:64, :], in1=rs[64:128, :], op=ALU.mult),
                         reads=[(ob, None), (rs, None)], writes=[(ocT, (ht, qb))])
            for hf in range(4):
                load_w_bf16(cx, wsm[:, 0:2, hf * 256:(hf + 1) * 256], (wsm, None), W["ca_wo"][:, hf * 256:(hf + 1) * 256], 2, 256, stage[sti % 2], q="pool")
                sti += 1
            proj_residual(cx, x, tiles, lambda c, j: (ocT[:, c, j * 128:(j + 1) * 128], (ocT, None)), 2, wsm, W["g"][3:4, :], L)
            p.barrier()
            norm_transpose(cx, x, tiles, W["g"][4:5, :], hT, L)
            aT = big
            stg4 = [stage[0], stage[1]]
            sq = [0]

            def nst():
                sq[0] += 1
                return stg4[sq[0] % len(stg4)]
            for f in range(NFF):
                k = f % 2
                for (nm, wb) in (("ffn_wg", wg[k]), ("ffn_wu", wu[k])):
                    flat = wb[:].rearrange("p c n -> p (c n)")
                    if wscr is None or tb == 0:
                        load_w_bf16(cx, wb[:], (wb, None), W[nm][:, f * 128:(f + 1) * 128], 8, 128, nst(), q="sp")
                        if wscr is not None:
                            p.dma("pool", wscr[nm][f], flat, reads=[(wb, None)], writes=[(cx.wsc, (nm, f))])
                    else:
                        p.dma("sp", flat, wscr[nm][f], reads=[(cx.wsc, (nm, f))], writes=[(wb, None)])
                for nb in range(TB // 512):
                    bg = rot(cx)
                    bu = rot(cx)
                    for (bank, w) in ((bg, wg[k]), (bu, wu[k])):
                        mm_group(cx, bank[:], (bank, None), [(w[:, c, :], hT[:, c, nb * 512:(nb + 1) * 512], [(w, None), (hT, None)]) for c in range(8)])
                    gs = gsb[nb % 2]
                    p.op("act", lambda e, bg=bg, gs=gs: e.activation(out=gs[:], in_=bg[:], func=AF.Silu), reads=[(bg, None)], writes=[(gs, None)])
                    p.op("dve", lambda e, bu=bu, f=f, nb=nb, gs=gs: e.tensor_tensor(out=aT[:, f, nb * 512:(nb + 1) * 512], in0=bu[:], in1=gs[:], op=ALU.mult),
                         reads=[(bu, None), (gs, None)], writes=[(aT, (f, nb))])
            gB = L["gB"]
            p.dma("sp", gB[:], W["g"][5:6, :].to_broadcast([128, D]), writes=[(gB, None)])
            for grp in ([0, 1, 2], [3, 4, 5], [6, 7]):
                banks = {}
                for i, key in enumerate([(tt, nb) for tt in grp for nb in range(2)]):
                    banks[key] = cx.ps[i]
                for f in range(NFF):
                    k = f % 3
                    if wscr is None or (tb == 0 and grp[0] == 0):
                        st = nst()
                        p.dma("sp", st[:, 0:1024], W["ffn_wd"][f * 128:(f + 1) * 128, :], writes=[(st, None)])
                        cast_copy(cx, wd[k][:], st[:, 0:1024], [(st, None)], [(wd[k], None)])
                        if wscr is not None:
                            p.dma("pool", wscr["ffn_wd"][f], wd[k][:], reads=[(wd[k], None)], writes=[(cx.wsc, ("ffn_wd", f))])
                    else:
                        p.dma("sp", wd[k][:], wscr["ffn_wd"][f], reads=[(cx.wsc, ("ffn_wd", f))], writes=[(wd[k], None)])
                    for tt in grp:
                        for nb in range(2):
                            b = banks[(tt, nb)]
                            p.op("pe", lambda e, b=b, tt=tt, nb=nb, k=k, f=f: e.matmul(b[:], lhsT=aT[:, f, tt * 128:(tt + 1) * 128], rhs=wd[k][:, nb * 512:(nb + 1) * 512],
                                                                                      start=(f == 0), stop=(f == NFF - 1)),
                                 reads=[(aT, (f, tt // 4)), (wd[k], None)], writes=[(b, None)], inc=(tt == grp[-1] and nb == 1))
                for tt in grp:
                    norm_residual(cx, x, tiles[tt], [banks[(tt, 0)], banks[(tt, 1)]], gB, L)
            if hT_store is not None:
                norm_transpose(cx, x, tiles, g_next_ap, hT, L)
                hT_store(hT, tb)
            elif hT_next_ap is not None:
                norm_transpose(cx, x, tiles, g_next_ap, hT, L)
                p.dma("sp", hT_next_ap[:, tsl].rearrange("(c p) n -> p c n", p=128), hT[:], reads=[(hT, None)], writes=[(cx.outb, ("hT", tb))])
            p.barrier()


def psum_set(cx, es, n_f32, with_bf16):
    cx.uid += 1
    u = cx.uid
    ps = [Buf(es.enter_context(cx.nc.psum_tensor(f"ps{i}_{u}", [128, 512], F32)), f"ps{i}") for i in range(n_f32)]
    pst = Buf(es.enter_context(cx.nc.psum_tensor(f"pst_{u}", [128, 1024], BF16)), "pst") if with_bf16 else None
    return ps, pst


def build_rope_tables(cx, pos_ap, ropeinv_ap, Ct, St, scratch_i, scratch_f):
    p = cx.p
    inv = cx.ropeinv
    p.dma("sp", inv[:], ropeinv_ap, writes=[(inv, None)])
    p.dma("sp", scratch_i[:], pos_ap.to_broadcast([128, T]), writes=[(scratch_i, None)])
    p.op("dve", lambda e: e.tensor_copy(out=scratch_f[:], in_=scratch_i[:]), reads=[(scratch_i, None)], writes=[(scratch_f, None)])
    for (dst, col, off) in ((St, 0, 0.0), (Ct, 1, 0.25)):
        p.op("dve", lambda e, dst=dst, col=col, off=off: e.tensor_scalar(out=dst[:], in0=scratch_f[:], scalar1=inv[:, col:col + 1], scalar2=off,
                                                                      op0=ALU.mult, op1=ALU.add),
             reads=[(scratch_f, None), (inv, None)], writes=[(dst, None)])
        p.op("dve", lambda e, dst=dst: e.tensor_copy(out=scratch_i[:], in_=dst[:]), reads=[(dst, None)], writes=[(scratch_i, None)])
        p.op("pool", lambda e, dst=dst: e.tensor_tensor(out=dst[:], in0=dst[:], in1=scratch_i[:], op=ALU.subtract),
             reads=[(dst, None), (scratch_i, None)], writes=[(dst, None)])
        p.op("dve", lambda e, dst=dst: e.scalar_tensor_tensor(out=dst[:], in0=dst[:], scalar=0.5, in1=dst[:], op0=ALU.is_gt, op1=ALU.subtract),
             reads=[(dst, None)], writes=[(dst, None)])
        p.op("dve", lambda e, dst=dst: e.scalar_tensor_tensor(out=dst[:], in0=dst[:], scalar=0.5, in1=dst[:], op0=ALU.is_gt, op1=ALU.subtract),
             reads=[(dst, None)], writes=[(dst, None)])
    for dst in (St, Ct):
        p.op("act", lambda e, dst=dst: e.activation(out=dst[:], in_=dst[:], func=AF.Sin, scale=2.0 * math.pi), reads=[(dst, None)], writes=[(dst, None)])


def proj_rope(cx, hT, wq, wqs, col0, tok0, Ct, St, out_ap, out_dep, L, banks):
    p = cx.p
    bA, bB = banks
    mm_group(cx, bA[:], (bA, None), [(wq[:, c, col0:col0 + 128], hT[:, c, tok0:tok0 + 512], [(wq, None), (hT, None)]) for c in range(8)])
    mm_group(cx, bB[:], (bB, None), [(wqs[:, c, col0:col0 + 128], hT[:, c, tok0:tok0 + 512], [(wqs, None), (hT, None)]) for c in range(8)])
    t1, t2 = L["rt1"], L["rt2"]
    p.op("dve", lambda e: e.tensor_tensor(out=t1[:], in0=bA[:], in1=Ct[:, tok0:tok0 + 512], op=ALU.mult), reads=[(bA, None), (Ct, None)], writes=[(t1, None)])
    p.op("dve", lambda e: e.tensor_tensor(out=t2[:], in0=bB[:], in1=St[:, tok0:tok0 + 512], op=ALU.mult), reads=[(bB, None), (St, None)], writes=[(t2, None)])
    p.op("dve", lambda e: e.tensor_tensor(out=out_ap, in0=t1[:], in1=t2[:], op=ALU.add), reads=[(t1, None), (t2, None)], writes=[out_dep])


def run_streams(cx, streams, kbs, sbanks, pts, L, keep=None):
    p = cx.p
    n = len(kbs)
    st = L.setdefault("_rs", {"sb": 0, "pt": 0})

    def tail(pend, i):
        for (s, bank, kb) in pend:
            pt = pts[st["pt"] % len(pts)]
            st["pt"] += 1
            if keep is not None:
                keep.append((kb, pt))
            if s.get("bias") is not None:
                bap, bdeps = s["bias"](kb)
                p.op("act", lambda e, pt=pt, bank=bank, bap=bap, s=s: e.activation(out=pt[:], in_=bank[:], func=AF.Exp, scale=s["scale"], bias=bap),
                     reads=[(bank, None)] + bdeps, writes=[(pt, None)])
            else:
                p.op("act", lambda e, pt=pt, bank=bank, s=s: e.activation(out=pt[:], in_=bank[:], func=AF.Exp, scale=s["scale"]),
                     reads=[(bank, None)], writes=[(pt, None)])
            for (map_, mdeps) in s["masks"](kb):
                p.op("dve", lambda e, pt=pt, map_=map_: e.tensor_tensor(out=pt[:], in0=pt[:], in1=map_, op=ALU.mult),
                     reads=[(pt, None)] + mdeps, writes=[(pt, None)])
            for (obank, lfn) in s["pv"]:
                lap, ldeps = lfn(kb)
                p.op("pe", lambda e, obank=obank, lap=lap, pt=pt, i=i: e.matmul(obank, lhsT=lap, rhs=pt[:], start=(i == 0), stop=(i == n - 1)),
                     reads=[(pt, None)] + ldeps, writes=[s["pv_dep"]])

    depth = 2 if (len(streams) == 1 and len(sbanks) >= 3) else 1
    queue = []
    for i, kb in enumerate(kbs):
        cur = []
        for s in streams:
            bank = sbanks[st["sb"] % len(sbanks)]
            st["sb"] += 1
            kap, kdeps = s["k"](kb)
            qap, qdeps = s["q"]
            p.op("pe", lambda e, bank=bank, kap=kap, qap=qap: e.matmul(bank[:], lhsT=kap, rhs=qap, start=True, stop=True),
                 reads=kdeps + qdeps, writes=[(bank, None)])
            cur.append((s, bank, kb))
        queue.append((cur, i))
        if len(queue) > depth:
            pc, pi_ = queue.pop(0)
            tail(pc, pi_)
    while queue:
        pc, pi_ = queue.pop(0)
        tail(pc, pi_)


def phase_B_odd(cx, load_hT, W, oT_ap):
    p = cx.p
    with ExitStack() as es:
        cx.uid += 1
        uu = cx.uid
        def sb(name, shape, dt):
            return Buf(es.enter_context(cx.nc.sbuf_tensor(f"{name}_{uu}", list(shape), dt)), name)
        ps, _ = psum_set(cx, es, 8, False)
        L = {}
        hT = sb("hT_all", [128, 8, T], BF16)
        load_hT(hT)
        Ct = sb("Ct", [128, T], F32)
        St = sb("St", [128, T], F32)
        cx.ropeinv = sb("ropeinv", [128, 2], F32)
        with ExitStack() as es2:
            sci = Buf(es2.enter_context(cx.nc.sbuf_tensor(f"sci_{uu}", [128, T], I32)), "sci")
            scf = Buf(es2.enter_context(cx.nc.sbuf_tensor(f"scf_{uu}", [128, T], F32)), "scf")
            build_rope_tables(cx, W["pos"], W["ropeinv"], Ct, St, sci, scf)
            p.barrier()
        qT = sb("qT", [128, 2, T], BF16)
        kT = sb("kT", [128, 2, T], BF16)
        vv = sb("vv", [128, 32, 256], BF16)
        stage = [sb(f"stage{i}", [128, 2048], F32) for i in range(2)]
        wq = sb("wq", [128, 8, 256], BF16)
        wqs = sb("wqs", [128, 8, 256], BF16)
        cmask = sb("cmask", [128, 8, 512], BF16)
        ones_b = sb("ones_b", [128, 128], BF16)
        ones_f = sb("ones_f", [128, 128], F32)
        pts = [sb(f"pt{i}", [128, 512], BF16) for i in range(6)]
        L["rt1"] = sb("rt1", [128, 512], F32)
        L["rt2"] = sb("rt2", [128, 512], F32)
        r1 = sb("r1", [128, 512], F32)
        r2 = sb("r2", [128, 512], F32)
        osb = sb("osb", [128, 512], F32)
        osq = sb("osq", [128, 512], F32)
        ob = [sb(f"ob{i}", [128, 512], BF16) for i in range(2)]
        lamt = sb("lamt", [128, 256], F32)
        lam2 = sb("lam2", [128, 8], F32)
        gcol = sb("gcol", [128, 1], F32)
        p.dma("sp", cmask[:], W["cmask"].rearrange("p (j n) -> p j n", j=8), writes=[(cmask, None)])
        p.op("pool", lambda e: e.memset(ones_b[:], 1.0), writes=[(ones_b, None)])
        p.op("pool", lambda e: e.memset(ones_f[:], 1.0), writes=[(ones_f, None)])
        p.dma("sp", lamt[:], W["lam"].to_broadcast([128, 256]), writes=[(lamt, None)])
        p.dma("sp", gcol[:], W["subg"], writes=[(gcol, None)])
        for i in range(2):
            p.op("dve", lambda e, i=i: e.tensor_tensor(out=lamt[:, i * 128:i * 128 + 64], in0=lamt[:, i * 128:i * 128 + 64], in1=lamt[:, i * 128 + 64:i * 128 + 128], op=ALU.mult),
                 reads=[(lamt, None)], writes=[(lamt, None)])
            p.op("dve", lambda e, i=i: e.tensor_reduce(out=lam2[:, i:i + 1], in_=lamt[:, i * 128:i * 128 + 64], axis=AX.X, op=ALU.add),
                 reads=[(lamt, None)], writes=[(lam2, None)])
        p.op("act", lambda e: e.activation(out=lam2[:, 2:4], in_=lam2[:, 0:2], func=AF.Exp), reads=[(lam2, None)], writes=[(lam2, None)])
        lic = sb("lic", [128, 2], F32)
        p.dma("sp", lic[:], W["laminit"].to_broadcast([128, 2]), writes=[(lic, None)])
        p.op("dve", lambda e: e.scalar_tensor_tensor(out=lam2[:, 4:5], in0=lam2[:, 3:4], scalar=lic[:, 0:1], in1=lam2[:, 2:3], op0=ALU.add, op1=ALU.subtract),
             reads=[(lam2, None), (lic, None)], writes=[(lam2, None)])
        p.op("dve", lambda e: e.tensor_scalar(out=gcol[:], in0=gcol[:], scalar1=lic[:, 1:2], scalar2=None, op0=ALU.mult), reads=[(gcol, None), (lic, None)], writes=[(gcol, None)])
        sti = 0
        for hp in range(2):
            for (dst, base) in ((qT, 0), (kT, 1024)):
                c0 = base + hp * 256
                load_w_bf16(cx, wq[:], (wq, None), W["w_in"][:, c0:c0 + 256], 8, 256, stage[sti % 2], q="pool"); sti += 1
                load_w_bf16(cx, wqs[:], (wqs, None), W["w_in"][:, c0 + 512:c0 + 768], 8, 256, stage[sti % 2], q="pool"); sti += 1
                for hh in range(2):
                    for tb in range(8):
                        banks = (ps[(2 * tb) % 8], ps[(2 * tb + 1) % 8])
                        proj_rope(cx, hT, wq, wqs, hh * 128, tb * 512, Ct, St, dst[:, hh, tb * 512:(tb + 1) * 512], (dst, (hh, tb)), L, banks)
            c0 = 2048 + hp * 256
            load_w_bf16(cx, wq[:], (wq, None), W["w_in"][:, c0:c0 + 256], 8, 256, stage[sti % 2], q="pool"); sti += 1
            for tt in range(32):
                b = ps[tt % 8]
                mm_group(cx, b[:, 0:256], (b, None), [(hT[:, c, tt * 128:(tt + 1) * 128], wq[:, c, :], [(wq, None), (hT, None)]) for c in range(8)])
                if tt % 2 == 0:
                    p.op("act", lambda e, b=b, tt=tt: e.activation(out=vv[:, tt, :], in_=b[:, 0:256], func=AF.Copy), reads=[(b, None)], writes=[(vv, tt)])
                else:
                    p.op("dve", lambda e, b=b, tt=tt: e.tensor_copy(out=vv[:, tt, :], in_=b[:, 0:256]), reads=[(b, None)], writes=[(vv, tt)])
            for hh in range(2):
                head = hp * 2 + hh
                for qb in range(8):
                    kbs = list(range(4 * qb + 4))
                    O1, S1, O2, S2 = ps[4], ps[5], ps[6], ps[7]
                    streams = []
                    for comp, (Ob, Sb) in enumerate(((O1, S1), (O2, S2))):
                        r0 = comp * 64
                        streams.append(dict(
                            k=lambda kb, r0=r0: (kT[r0:r0 + 64, hh, kb * 128:(kb + 1) * 128], [(kT, (hh, kb // 4))]),
                            q=(qT[r0:r0 + 64, hh, qb * 512:(qb + 1) * 512], [(qT, (hh, qb))]),
                            scale=0.125, bias=None,
                            masks=lambda kb: ([(cmask[:, kb - 4 * qb, :], [(cmask, None)])] if kb >= 4 * qb else []),
                            pv=[(Ob[:], lambda kb: (vv[:, kb, hh * 128:(hh + 1) * 128], [(vv, kb)])),
                                (Sb[:], lambda kb: (ones_b[:], [(ones_b, None)]))],
                            pv_dep=(Ob, None)))
                    run_streams(cx, streams, kbs, ps[0:4], pts, L)
                    p.op("dve", lambda e: e.reciprocal(out=r1[:], in_=S1[:]), reads=[(O1, None), (S1, None)], writes=[(r1, None)])
                    p.op("dve", lambda e: e.reciprocal(out=r2[:], in_=S2[:]), reads=[(O2, None), (S2, None)], writes=[(r2, None)])
                    p.op("dve", lambda e: e.tensor_tensor(out=r1[:], in0=O1[:], in1=r1[:], op=ALU.mult), reads=[(O1, None), (r1, None)], writes=[(r1, None)])
                    p.op("dve", lambda e: e.tensor_tensor(out=r2[:], in0=O2[:], in1=r2[:], op=ALU.mult), reads=[(O2, None), (r2, None)], writes=[(r2, None), (S1, None), (S2, None)])
                    p.op("dve", lambda e: e.scalar_tensor_tensor(out=osb[:], in0=r2[:], scalar=lam2[:, 4:5], in1=r1[:], op0=ALU.mult, op1=ALU.add),
                         reads=[(r1, None), (r2, None), (lam2, None)], writes=[(osb, None)])
                    p.op("pool", lambda e: e.tensor_tensor(out=osq[:], in0=osb[:], in1=osb[:], op=ALU.mult), reads=[(osb, None)], writes=[(osq, None)])
                    sbk = ps[qb % 4]
                    p.op("pe", lambda e, sbk=sbk: e.matmul(sbk[:], lhsT=ones_f[:], rhs=osq[:], start=True, stop=True), reads=[(osq, None), (ones_f, None)], writes=[(sbk, None)])
                    p.op("act", lambda e, sbk=sbk: e.activation(out=r1[:], in_=sbk[:], func=AF.Sqrt, scale=1.0 / 128, bias=cx.epsb[:, 0:1]),
                         reads=[(sbk, None), (cx.epsb, None)], writes=[(r1, None)])
                    p.op("dve", lambda e: e.reciprocal(out=r1[:], in_=r1[:]), reads=[(r1, None)], writes=[(r1, None)])
                    obt = ob[qb % 2]
                    p.op("dve", lambda e, obt=obt: e.scalar_tensor_tensor(out=obt[:], in0=osb[:], scalar=gcol[:, 0:1], in1=r1[:], op0=ALU.mult, op1=ALU.mult),
                         reads=[(osb, None), (gcol, None), (r1, None)], writes=[(obt, None)])
                    p.dma("sp", oT_ap[head * 128:(head + 1) * 128, qb * 512:(qb + 1) * 512], obt[:], reads=[(obt, None)], writes=[(cx.outb, ("oT", head, qb))])
        p.barrier()


EV = dict(fq=0, fk=256, fv=512, nq=768, nqs=1024, kc=1280, kcs=1344, ks=1408, kss=1472, kw=1536, kws=1600, vc=1664, vs=1728, vw=1792, fl=1856, gl=1860)
EV_NCOL = 1872
TBK = 512


def build_rope_block(cx, pos_ap, tok0, n, Ct, St, sci, scf):
    p = cx.p
    inv = cx.ropeinv
    p.dma("sp", sci[:, 0:n], pos_ap[:, tok0:tok0 + n].to_broadcast([128, n]), writes=[(sci, None)])
    p.op("dve", lambda e: e.tensor_copy(out=scf[:, 0:n], in_=sci[:, 0:n]), reads=[(sci, None)], writes=[(scf, None)])
    for (dst, col, off) in ((St, 0, 0.0), (Ct, 1, 0.25)):
        p.op("dve", lambda e, dst=dst, col=col, off=off: e.tensor_scalar(out=dst[:, 0:n], in0=scf[:, 0:n], scalar1=inv[:, col:col + 1], scalar2=off,
                                                                      op0=ALU.mult, op1=ALU.add),
             reads=[(scf, None), (inv, None)], writes=[(dst, None)])
        p.op("dve", lambda e, dst=dst: e.tensor_copy(out=sci[:, 0:n], in_=dst[:, 0:n]), reads=[(dst, None)], writes=[(sci, None)])
        p.op("pool", lambda e, dst=dst: e.tensor_tensor(out=dst[:, 0:n], in0=dst[:, 0:n], in1=sci[:, 0:n], op=ALU.subtract),
             reads=[(dst, None), (sci, None)], writes=[(dst, None)])
        for _ in range(2):
            p.op("dve", lambda e, dst=dst: e.scalar_tensor_tensor(out=dst[:, 0:n], in0=dst[:, 0:n], scalar=0.5, in1=dst[:, 0:n], op0=ALU.is_gt, op1=ALU.subtract),
                 reads=[(dst, None)], writes=[(dst, None)])
        p.op("act", lambda e, dst=dst: e.activation(out=dst[:, 0:n], in_=dst[:, 0:n], func=AF.Sin, scale=2.0 * math.pi), reads=[(dst, None)], writes=[(dst, None)])


def phase_B_even(cx, hT_src, W, oT_ap):
    p = cx.p
    nc = cx.nc
    with ExitStack() as es:
        cx.uid += 1
        uu = cx.uid

        def mk_sb(stack):
            def sb(name, shape, dt):
                return Buf(stack.enter_context(nc.sbuf_tensor(f"{name}_{uu}", list(shape), dt)), name)
            return sb
        sb = mk_sb(es)
        ps, pst = psum_set(cx, es, 7, True)
        L = {}
        cmask = sb("cmask", [128, 8, 512], BF16)
        p.dma("sp", cmask[:], W["cmask"].rearrange("p (j n) -> p j n", j=8), writes=[(cmask, None)])
        cx.ropeinv = sb("ropeinv", [128, 2], F32)
        p.dma("sp", cx.ropeinv[:], W["ropeinv"], writes=[(cx.ropeinv, None)])
        ones_f = sb("ones_f", [128, 128], F32)
        p.op("pool", lambda e: e.memset(ones_f[:], 1.0), writes=[(ones_f, None)])
        pts = [sb(f"pt{i}", [128, 512], BF16) for i in range(6)]
        rs = sb("rs", [128, 512], F32)
        fac = sb("fac", [128, 512], F32)
        obt = [sb(f"obt{i}", [128, 512], BF16) for i in range(2)]

        def load_hT_block(hTb, tok0):
            for c in range(8):
                hT_src(hTb[:, c, :], c, tok0, TBK, "sp" if c % 2 == 0 else "pool", [(hTb, None)])

        with ExitStack() as esf:
            sbf = mk_sb(esf)
            fqT = sbf("fqT", [128, 2, T], BF16)
            fkT = sbf("fkT", [128, 2, T], BF16)
            fvv = sbf("fvv", [128, 32, 4, 128], BF16)
            FB = sbf("FB", [128, 4, 32, 8], F32)
            ncum = sbf("ncum", [128, 128], F32)
            p.op("pool", lambda e: e.memset(fvv[:], 1.0), writes=[(fvv, None)])
            with ExitStack() as esp:
                sbp = mk_sb(esp)
                hTb = sbp("hTb", [128, 8, TBK], BF16)
                wf = sbp("wf", [128, 8, 768], BF16)
                wfl = sbp("wfl", [128, 8, 4], BF16)
                stage = [sbp(f"stage{i}", [128, 2048], F32) for i in range(2)]
                tri = sbp("tri", [128, 128], F32)
                sel127 = sbp("sel127", [128, 128], F32)
                fbB = sbp("fbB", [128, 128], F32)
                nlf = sbp("nlf", [128, 128], F32)
                tot = sbp("tot", [128, 128], F32)
                inc = sbp("inc", [128, 128], F32)
                refsb = sbp("refsb", [128, 128], F32)
                p.dma("sp", tri[:], W["tri"], writes=[(tri, None)])
                p.dma("sp", sel127[:], W["sel127"], writes=[(sel127, None)])
                p.dma("sp", fbB[:], W["fbias_rep"].to_broadcast([128, 128]), writes=[(fbB, None)])
                for i in range(3):
                    load_w_bf16(cx, wf[:, :, i * 256:(i + 1) * 256], (wf, None), W["w_in"][:, i * 256:(i + 1) * 256], 8, 256, stage[i % 2], q="pool")
                load_w_bf16(cx, wfl[:], (wfl, None), W["w_in"][:, EV["fl"]:EV["fl"] + 4], 8, 4, stage[1], q="pool")
                FLP = ps[6]
                for blk in range(T // TBK):
                    tok0 = blk * TBK
                    load_hT_block(hTb, tok0)
                    for (dst, cb) in ((fqT, 0), (fkT, 256)):
                        for hh in range(2):
                            for tb in range(TBK // 512):
                                b = ps[(hh * 2 + tb) % 4]
                                mm_group(cx, b[:], (b, None), [(wf[:, c, cb + hh * 128:cb + (hh + 1) * 128], hTb[:, c, tb * 512:(tb + 1) * 512], [(wf, None), (hTb, None)]) for c in range(8)])
                                gtb = (tok0 + tb * 512) // 512
                                if tb % 2 == 0:
                                    p.op("act", lambda e, b=b, dst=dst, hh=hh, gtb=gtb: e.activation(out=dst[:, hh, gtb * 512:(gtb + 1) * 512], in_=b[:], func=AF.Copy),
                                         reads=[(b, None)], writes=[(dst, (hh, gtb))])
                                else:
                                    p.op("dve", lambda e, b=b, dst=dst, hh=hh, gtb=gtb: e.tensor_copy(out=dst[:, hh, gtb * 512:(gtb + 1) * 512], in_=b[:]),
                                         reads=[(b, None)], writes=[(dst, (hh, gtb))])
                    for tl in range(TBK // 128):
                        tt = tok0 // 128 + tl
                        b = ps[4 + tl % 2]
                        mm_group(cx, b[:, 0:256], (b, None), [(hTb[:, c, tl * 128:(tl + 1) * 128], wf[:, c, 512:768], [(wf, None), (hTb, None)]) for c in range(8)])
                        p.op("dve", lambda e, b=b, tt=tt: e.tensor_copy(out=fvv[:, tt, :, 0:64], in_=b[:, 0:256].rearrange("p (h d) -> p h d", h=4)),
                             reads=[(b, None)], writes=[(fvv, tt)])
                        mm_group(cx, FLP[:, tt * 4:(tt + 1) * 4], (FLP, None), [(hTb[:, c, tl * 128:(tl + 1) * 128], wfl[:, c, :], [(wfl, None), (hTb, None)]) for c in range(8)])
                p.op("dve", lambda e: e.tensor_tensor(out=nlf[:], in0=FLP[:, 0:128], in1=fbB[:], op=ALU.add), reads=[(FLP, None), (fbB, None)], writes=[(nlf, None)])
                p.op("act", lambda e: e.activation(out=nlf[:], in_=nlf[:], func=AF.Exp, scale=-1.0), reads=[(nlf, None)], writes=[(nlf, None)])
                p.op("act", lambda e: e.activation(out=nlf[:], in_=nlf[:], func=AF.Ln, bias=1.0), reads=[(nlf, None)], writes=[(nlf, None)])
                W1b, TOTb, REFb = ps[0], ps[1], ps[2]
                p.op("pe", lambda e: e.matmul(W1b[:, 0:128], lhsT=tri[:], rhs=nlf[:], start=True, stop=True), reads=[(tri, None), (nlf, None)], writes=[(W1b, None)])
                p.op("pe", lambda e: e.matmul(TOTb[:, 0:128], lhsT=ones_f[:], rhs=nlf[:], start=True, stop=True), reads=[(ones_f, None), (nlf, None)], writes=[(TOTb, None)])
                p.op("dve", lambda e: e.tensor_copy(out=tot[:], in_=TOTb[:, 0:128]), reads=[(TOTb, None)], writes=[(tot, None)])
                for j in range(4):
                    tv = tot[:].rearrange("p (t h) -> p t h", h=4)[:, :, j]
                    iv = inc[:].rearrange("p (t h) -> p t h", h=4)[:, :, j]
                    ov = ones_f[:, 0:32]
                    p.op("dve", lambda e, tv=tv, iv=iv, ov=ov: e.tensor_tensor_scan(out=iv, data0=ov, data1=tv, initial=0.0, op0=ALU.mult, op1=ALU.add),
                         reads=[(tot, None), (ones_f, None)], writes=[(inc, None)])
                p.op("dve", lambda e: e.tensor_tensor(out=inc[:], in0=inc[:], in1=tot[:], op=ALU.subtract), reads=[(inc, None), (tot, None)], writes=[(inc, None)])
                p.op("dve", lambda e: e.tensor_tensor(out=ncum[:], in0=W1b[:, 0:128], in1=inc[:], op=ALU.add), reads=[(W1b, None), (inc, None)], writes=[(ncum, None)])
                p.op("pe", lambda e: e.matmul(REFb[:, 0:128], lhsT=sel127[:], rhs=ncum[:], start=True, stop=True), reads=[(sel127, None), (ncum, None)], writes=[(REFb, None)])
                p.op("dve", lambda e: e.tensor_copy(out=refsb[:], in_=REFb[:, 0:128]), reads=[(REFb, None)], writes=[(refsb, None)])
                ncv = ncum[:].rearrange("p (t h) -> p t h", h=4)
                for j in range(4):
                    for qb in range(8):
                        col = (4 * qb + 3) * 4 + j
                        p.op("dve", lambda e, j=j, qb=qb, col=col: e.tensor_scalar(out=FB[:, j, :, qb], in0=ncv[:, :, j], scalar1=refsb[:, col:col + 1], scalar2=None, op0=ALU.subtract),
                             reads=[(ncum, None), (refsb, None)], writes=[(FB, None)])
                p.barrier()
            for head in range(4):
                hh, r0 = head // 2, (head % 2) * 64
                for qb in range(8):
                    kbs = list(range(4 * qb + 4))
                    Ob = ps[4 + (qb % 2)]
                    stream = dict(
                        k=lambda kb: (fkT[r0:r0 + 64, hh, kb * 128:(kb + 1) * 128], [(fkT, (hh, kb // 4))]),
                        q=(fqT[r0:r0 + 64, hh, qb * 512:(qb + 1) * 512], [(fqT, (hh, qb))]),
                        scale=0.125,
                        bias=lambda kb: (FB[:, head, kb, qb:qb + 1], [(FB, None)]),
                        masks=lambda kb: ([(cmask[:, kb - 4 * qb, :], [(cmask, None)])] if kb >= 4 * qb else []),
                        pv=[(Ob[:], lambda kb: (fvv[:, kb, head, :], [(fvv, kb)]))],
                        pv_dep=(Ob, None))
                    run_streams(cx, [stream], kbs, ps[0:4], pts, L)
                    ot = obt[qb % 2]
                    p.op("dve", lambda e, Ob=Ob: e.reciprocal(out=rs[64:128, :], in_=Ob[64:128, :]), reads=[(Ob, None)], writes=[(rs, None)])
                    p.op("dve", lambda e, Ob=Ob, ot=ot: e.tensor_tensor(out=ot[0:64, :], in0=Ob[0:64, :], in1=rs[64:128, :], op=ALU.mult),
                         reads=[(Ob, None), (rs, None)], writes=[(ot, None)])
                    p.dma("sp", oT_ap[head * 64:(head + 1) * 64, qb * 512:(qb + 1) * 512], ot[0:64, :], reads=[(ot, None)], writes=[(cx.outb, ("oTf", head, qb))])
            p.barrier()
        nsa_part(cx, W, oT_ap, hT_src, load_hT_block, mk_sb, es, ps, pst, pts, cmask, ones_f, rs, fac, obt, L)
        p.barrier()


NWC = dict(nq=0, nqs=256, ks=512, kss=640, kw=768, kws=896, kc=1024, kcs=1152, vc=1280, vsw=1344, gl=1472)
NW = 1484


def gelu_tanh(cx, src_bank_ap, src_dep, bias_col, bias_dep, out_ap, out_dep, tmpa, tmpb, n):
    p = cx.p
    p.op("act", lambda e: e.activation(out=tmpa[:, 0:n], in_=src_bank_ap, func=AF.Identity, bias=bias_col), reads=[src_dep, bias_dep], writes=[(tmpa, None)])
    p.op("dve", lambda e: e.tensor_tensor(out=tmpb[:, 0:n], in0=tmpa[:, 0:n], in1=tmpa[:, 0:n], op=ALU.mult), reads=[(tmpa, None)], writes=[(tmpb, None)])
    p.op("dve", lambda e: e.tensor_scalar(out=tmpb[:, 0:n], in0=tmpb[:, 0:n], scalar1=0.044715, scalar2=1.0, op0=ALU.mult, op1=ALU.add), reads=[(tmpb, None)], writes=[(tmpb, None)])
    p.op("dve", lambda e: e.tensor_tensor(out=tmpb[:, 0:n], in0=tmpb[:, 0:n], in1=tmpa[:, 0:n], op=ALU.mult), reads=[(tmpa, None), (tmpb, None)], writes=[(tmpb, None)])
    p.op("act", lambda e: e.activation(out=tmpb[:, 0:n], in_=tmpb[:, 0:n], func=AF.Tanh, scale=0.7978845608028654), reads=[(tmpb, None)], writes=[(tmpb, None)])
    p.op("dve", lambda e: e.scalar_tensor_tensor(out=tmpb[:, 0:n], in0=tmpb[:, 0:n], scalar=1.0, in1=tmpa[:, 0:n], op0=ALU.add, op1=ALU.mult),
         reads=[(tmpa, None), (tmpb, None)], writes=[(tmpb, None)])
    p.op("act", lambda e: e.activation(out=out_ap, in_=tmpb[:, 0:n], func=AF.Copy, scale=0.5), reads=[(tmpb, None)], writes=[out_dep])


def nsa_part(cx, W, oT_ap, hT_src, load_hT_block, mk_sb, es, ps, pst, pts, cmask, ones_f, rs, fac, obt, L):
    p = cx.p
    sb = mk_sb(es)
    QA = sb("QA", [128, 4, T], BF16)
    KSA = sb("KSA", [128, T], BF16)
    KSB = sb("KSB", [128, T], BF16)
    KW2 = sb("KW2", [128, T], BF16)
    VS = sb("VS", [128, 32, 128], BF16)
    VW = sb("VW", [128, 32, 128], BF16)
    GLT = sb("GLT", [12, T], BF16)
    KCMP = sb("KCMP", [128, 256], BF16)
    VCMP = sb("VCMP", [128, 2, 128], BF16)
    cmpmask = sb("cmpmask", [128, 5, 512], BF16)
    ovl = sb("ovl", [128, 2, 65], BF16)
    sel12 = sb("sel12", [12, 12 * 128], BF16)
    tkadd = sb("tkadd", [128, 128], F32)
    tkmul = sb("tkmul", [128, 128], F32)
    p.dma("sp", cmpmask[:], W["cmpmask"].rearrange("p (j n) -> p j n", j=5), writes=[(cmpmask, None)])
    p.dma("sp", ovl[:], W["ovl"].rearrange("p (c n) -> p c n", c=2), writes=[(ovl, None)])
    p.dma("sp", sel12[:], W["sel12"], writes=[(sel12, None)])
    p.dma("sp", tkadd[:], W["tkadd"], writes=[(tkadd, None)])
    p.dma("sp", tkmul[:], W["tkmul"], writes=[(tkmul, None)])
    for b_ in (VS, VW, VCMP):
        p.op("pool", lambda e, b_=b_: e.memset(b_[:], 1.0), writes=[(b_, None)])
    with ExitStack() as esp:
        sbp = mk_sb(esp)
        hTb = sbp("hTbn", [128, 8, TBK], BF16)
        wn = sbp("wn", [128, 8, NW], BF16)
        stage = [sbp(f"stagen{i}", [128, 2048], F32) for i in range(2)]
        Ct = sbp("Ctb", [128, TBK], F32)
        St = sbp("Stb", [128, TBK], F32)
        sci = sbp("scib", [128, TBK], I32)
        scf = sbp("scfb", [128, TBK], F32)
        L["rt1"] = sbp("rt1n", [128, 512], F32)
        L["rt2"] = sbp("rt2n", [128, 512], F32)
        KC2 = sbp("KC2", [128, T], BF16)
        VC = sbp("VC", [128, T], BF16)
        W1 = sbp("W1c", [64, 32, 128], BF16)
        posT = sbp("posT", [64, 32], BF16)
        posTf = sbp("posTf", [64, 32], F32)
        W2d = sbp("W2d", [128, 128], BF16)
        posb = sbp("posb", [128, 1], F32)
        HID = sbp("HID", [128, 256], BF16)
        ga = sbp("ga", [128, 256], F32)
        gb = sbp("gb", [128, 256], F32)
        sti = [0]

        def stg():
            sti[0] += 1
            return stage[sti[0] % 2]
        E = EV
        load_w_bf16(cx, wn[:, :, 0:256], (wn, None), W["w_in"][:, E["nq"]:E["nq"] + 256], 8, 256, stg(), q="pool")
        load_w_bf16(cx, wn[:, :, 256:512], (wn, None), W["w_in"][:, E["nqs"]:E["nqs"] + 256], 8, 256, stg(), q="pool")
        for (src, dstc, dup) in ((E["ks"], NWC["ks"], True), (E["kss"], NWC["kss"], True), (E["kw"], NWC["kw"], True), (E["kws"], NWC["kws"], True),
                                 (E["kc"], NWC["kc"], True), (E["kcs"], NWC["kcs"], True), (E["vc"], NWC["vc"], False)):
            st = stg()
            sv = st[:, 0:8 * 64].rearrange("p (c n) -> p c n", c=8)
            p.dma("pool", sv, W["w_in"][:, src:src + 64].rearrange("(c p) n -> p c n", p=128), writes=[(st, None)])
            p.op("pool", lambda e, sv=sv, dstc=dstc: e.tensor_copy(out=wn[:, :, dstc:dstc + 64], in_=sv), reads=[(st, None)], writes=[(wn, None)])
            if dup:
                p.op("pool", lambda e, sv=sv, dstc=dstc: e.tensor_copy(out=wn[:, :, dstc + 64:dstc + 128], in_=sv), reads=[(st, None)], writes=[(wn, None)])
        load_w_bf16(cx, wn[:, :, NWC["vsw"]:NWC["vsw"] + 128], (wn, None), W["w_in"][:, E["vs"]:E["vs"] + 128], 8, 128, stg(), q="pool")
        load_w_bf16(cx, wn[:, :, NWC["gl"]:NWC["gl"] + 12], (wn, None), W["w_in"][:, E["gl"]:E["gl"] + 12], 8, 12, stg(), q="pool")
        for blk in range(T // TBK):
            tok0 = blk * TBK
            load_hT_block(hTb, tok0)
            build_rope_block(cx, W["pos"], tok0, TBK, Ct, St, sci, scf)
            for tb in range(TBK // 512):
                g0 = tok0 + tb * 512
                gtb = g0 // 512
                lsl = slice(tb * 512, (tb + 1) * 512)
                for i in range(2):
                    bA, bB = ps[2 * i], ps[2 * i + 1]
                    mm_group(cx, bA[:], (bA, None), [(wn[:, c, i * 128:(i + 1) * 128], hTb[:, c, lsl], [(wn, None), (hTb, None)]) for c in range(8)])
                    mm_group(cx, bB[:], (bB, None), [(wn[:, c, 256 + i * 128:256 + (i + 1) * 128], hTb[:, c, lsl], [(wn, None), (hTb, None)]) for c in range(8)])
                    t1, t2 = L["rt1"], L["rt2"]
                    p.op("dve", lambda e, bA=bA: e.tensor_tensor(out=t1[:], in0=bA[:], in1=Ct[:, lsl], op=ALU.mult), reads=[(bA, None), (Ct, None)], writes=[(t1, None)])
                    p.op("dve", lambda e, bB=bB: e.tensor_tensor(out=t2[:], in0=bB[:], in1=St[:, lsl], op=ALU.mult), reads=[(bB, None), (St, None)], writes=[(t2, None)])
                    p.op("dve", lambda e, i=i, g0=g0: e.tensor_tensor(out=QA[0:64, 2 * i, g0:g0 + 512], in0=t1[0:64, :], in1=t2[0:64, :], op=ALU.add),
                         reads=[(t1, None), (t2, None)], writes=[(QA, (2 * i, gtb))])
                    p.op("dve", lambda e, i=i, g0=g0: e.tensor_tensor(out=QA[64:128, 2 * i + 1, g0:g0 + 512], in0=t1[64:128, :], in1=t2[64:128, :], op=ALU.add),
                         reads=[(t1, None), (t2, None)], writes=[(QA, (2 * i + 1, gtb))])
                for ui, (dst, cb, cbs) in enumerate(((KSA, NWC["ks"], NWC["kss"]), (KW2, NWC["kw"], NWC["kws"]), (KC2, NWC["kc"], NWC["kcs"]))):
                    bA, bB = ps[(4 + 2 * ui) % 6], ps[(5 + 2 * ui) % 6]
                    mm_group(cx, bA[:], (bA, None), [(wn[:, c, cb:cb + 128], hTb[:, c, lsl], [(wn, None), (hTb, None)]) for c in range(8)])
                    mm_group(cx, bB[:], (bB, None), [(wn[:, c, cbs:cbs + 128], hTb[:, c, lsl], [(wn, None), (hTb, None)]) for c in range(8)])
                    t1, t2 = L["rt1"], L["rt2"]
                    p.op("dve", lambda e, bA=bA: e.tensor_tensor(out=t1[:], in0=bA[:], in1=Ct[:, lsl], op=ALU.mult), reads=[(bA, None), (Ct, None)], writes=[(t1, None)])
                    p.op("dve", lambda e, bB=bB: e.tensor_tensor(out=t2[:], in0=bB[:], in1=St[:, lsl], op=ALU.mult), reads=[(bB, None), (St, None)], writes=[(t2, None)])
                    p.op("dve", lambda e, dst=dst, g0=g0: e.tensor_tensor(out=dst[:, g0:g0 + 512], in0=t1[:], in1=t2[:], op=ALU.add),
                         reads=[(t1, None), (t2, None)], writes=[(dst, gtb)])
                b = ps[6]
                mm_group(cx, b[0:64, :], (b, None), [(wn[:, c, NWC["vc"]:NWC["vc"] + 64], hTb[:, c, lsl], [(wn, None), (hTb, None)]) for c in range(8)])
                p.op("act", lambda e, b=b, g0=g0: e.activation(out=VC[0:64, g0:g0 + 512], in_=b[0:64, :], func=AF.Copy), reads=[(b, None)], writes=[(VC, gtb)])
                b = ps[4 + tb % 2]
                mm_group(cx, b[0:12, :], (b, None), [(wn[:, c, NWC["gl"]:NWC["gl"] + 12], hTb[:, c, lsl], [(wn, None), (hTb, None)]) for c in range(8)])
                p.op("act", lambda e, b=b, g0=g0: e.activation(out=GLT[0:12, g0:g0 + 512], in_=b[0:12, :], func=AF.Sigmoid), reads=[(b, None)], writes=[(GLT, gtb)])
            for tl in range(TBK // 128):
                tt = tok0 // 128 + tl
                b = ps[(tl % 2)]
                mm_group(cx, b[:, 0:128], (b, None), [(hTb[:, c, tl * 128:(tl + 1) * 128], wn[:, c, NWC["vsw"]:NWC["vsw"] + 128], [(wn, None), (hTb, None)]) for c in range(8)])
                p.op("dve", lambda e, b=b, tt=tt: e.tensor_copy(out=VS[:, tt, 0:64], in_=b[:, 0:64]), reads=[(b, None)], writes=[(VS, tt)])
                p.op("act", lambda e, b=b, tt=tt: e.activation(out=VW[:, tt, 0:64], in_=b[:, 64:128], func=AF.Copy), reads=[(b, None)], writes=[(VW, tt)])
        p.op("pool", lambda e: e.tensor_copy(out=KSB[:], in_=KSA[:]), reads=[(KSA, None)], writes=[(KSB, None)])
        p.dma("sp", KSA[64:128, :], W["onehot"], writes=[(KSA, None)])
        p.dma("sp", KSB[0:64, :], W["onehot"], writes=[(KSB, None)])
        for which in ("k", "v"):
            src = KC2 if which == "k" else VC
            for half in range(2):
                st = stg()
                sv = st[0:64, :].rearrange("p (l h) -> p l h", l=16)
                p.dma("sp", sv, W["c1" + which][half * 1024:(half + 1) * 1024, :].rearrange("(l d) h -> d l h", d=64), writes=[(st, None)])
                p.op("pool", lambda e, sv=sv, half=half: e.tensor_copy(out=W1[:, half * 16:(half + 1) * 16, :], in_=sv), reads=[(st, None)], writes=[(W1, None)])
            p.dma("sp", posTf[:], W["cp" + which + "T"], writes=[(posTf, None)])
            p.op("pool", lambda e: e.tensor_copy(out=posT[:], in_=posTf[:]), reads=[(posTf, None)], writes=[(posT, None)])
            st = stg()
            p.dma("sp", st[:, 0:64], W["c2" + which], writes=[(st, None)])
            p.op("pool", lambda e, st=st: e.tensor_copy(out=W2d[:, 0:64], in_=st[:, 0:64]), reads=[(st, None)], writes=[(W2d, None)])
            p.op("pool", lambda e, st=st: e.tensor_copy(out=W2d[:, 64:128], in_=st[:, 0:64]), reads=[(st, None)], writes=[(W2d, None)])
            hb_, pb_ = ps[0], ps[1]
            srcv = src[0:64, :].rearrange("p (c s) -> p c s", s=16)
            mm_group(cx, hb_[:, 0:255], (hb_, None),
                     [(W1[:, l, :], srcv[:, (l // 16):(l // 16) + 255, l % 16], [(W1, None), (src, None)]) for l in range(32)])
            mm_group(cx, pb_[:, 0:1], (pb_, None), [(W1[:, l, :], posT[:, l:l + 1], [(W1, None), (posT, None)]) for l in range(32)])
            p.op("dve", lambda e: e.tensor_copy(out=posb[:], in_=pb_[:, 0:1]), reads=[(pb_, None)], writes=[(posb, None)])
            p.op("pool", lambda e: e.memset(HID[:], 0.0), writes=[(HID, None)])
            gelu_tanh(cx, hb_[:, 0:255], (hb_, None), posb[:, 0:1], (posb, None), HID[:, 0:255], (HID, None), ga, gb, 255)
            if which == "k":
                ob_ = ps[2]
                p.op("pe", lambda e: e.matmul(ob_[:, 0:256], lhsT=W2d[:], rhs=HID[:], start=True, stop=True), reads=[(W2d, None), (HID, None)], writes=[(ob_, None)])
                p.op("act", lambda e: e.activation(out=KCMP[:], in_=ob_[:, 0:256], func=AF.Copy), reads=[(ob_, None)], writes=[(KCMP, None)])
            else:
                for cc in range(2):
                    ob_ = ps[3 + cc]
                    p.op("pe", lambda e, cc=cc, ob_=ob_: e.matmul(ob_[:, 0:64], lhsT=HID[:, cc * 128:(cc + 1) * 128], rhs=W2d[:, 0:64], start=True, stop=True),
                         reads=[(W2d, None), (HID, None)], writes=[(ob_, None)])
                    p.op("act", lambda e, cc=cc, ob_=ob_: e.activation(out=VCMP[:, cc, 0:64], in_=ob_[:, 0:64], func=AF.Copy), reads=[(ob_, None)], writes=[(VCMP, None)])
        p.barrier()
    with ExitStack() as esa:
        sba = mk_sb(esa)
        OC = sba("OC", [128, 4, 512], F32)
        pcs = [sba(f"pc{i}", [128, 512], BF16) for i in range(2)]
        acc = sba("acc", [128, 4, 64], F32)
        sc = sba("sc", [128, 64], F32)
        sc2 = sba("sc2", [128, 64], F32)
        m8 = sba("m8", [128, 8], F32)
        m8b = sba("m8b", [128, 8], F32)
        rsi = sba("rsi", [128, 1], F32)
        NM = sba("NM", [128, 128], BF16)
        tacc = sba("tacc", [128, 512], F32)
        t2 = sba("t2", [128, 512], F32)
        sbanks = ps[0:3]
        Oacc = [ps[3], ps[4]]
        Gb, IMb = ps[5], ps[6]
        oi = [0]

        def next_O():
            oi[0] += 1
            return Oacc[oi[0] % 2]

        def gate_fac(Ob, n, j, qb, clamp):
            jj = 3 * n + j
            p.op("pe", lambda e: e.matmul(Gb[:], lhsT=sel12[0:12, jj * 128:(jj + 1) * 128], rhs=GLT[0:12, qb * 512:(qb + 1) * 512], start=True, stop=True),
                 reads=[(sel12, None), (GLT, qb)], writes=[(Gb, None)])
            if clamp:
                p.op("dve", lambda e: e.tensor_scalar(out=rs[64:128, :], in0=Ob[64:128, :], scalar1=1e-30, scalar2=None, op0=ALU.max), reads=[(Ob, None)], writes=[(rs, None)])
                p.op("dve", lambda e: e.reciprocal(out=rs[64:128, :], in_=rs[64:128, :]), reads=[(rs, None)], writes=[(rs, None)])
            else:
                p.op("dve", lambda e: e.reciprocal(out=rs[64:128, :], in_=Ob[64:128, :]), reads=[(Ob, None)], writes=[(rs, None)])
            p.op("dve", lambda e: e.tensor_tensor(out=fac[64:128, :], in0=Gb[64:128, :], in1=rs[64:128, :], op=ALU.mult), reads=[(Gb, None), (rs, None)], writes=[(fac, None)])

        for qb in range(8):
            chunks = [0] if qb < 4 else [0, 1]
            for n in range(4):
                r0 = (n % 2) * 64
                Ob = next_O()

                def cmask_fn(cc, qb=qb):
                    delta = 2048 * cc - 512 * qb
                    if delta <= -2560:
                        return []
                    return [(cmpmask[:, (delta + 2048) // 512, :], [(cmpmask, None)])]
                stream = dict(
                    k=lambda cc: (KCMP[r0:r0 + 64, cc * 128:(cc + 1) * 128], [(KCMP, None)]),
                    q=(QA[r0:r0 + 64, n, qb * 512:(qb + 1) * 512], [(QA, (n, qb))]),
                    scale=0.125, bias=None, masks=cmask_fn,
                    pv=[(Ob[:], lambda cc: (VCMP[:, cc, :], [(VCMP, None)]))], pv_dep=(Ob, None))
                keep = []
                run_streams(cx, [stream], chunks, sbanks, pcs, L, keep=keep)
                for t4 in range(4):
                    mm_group(cx, IMb[:, 0:65], (IMb, None),
                             [(pt[:, t4 * 128:(t4 + 1) * 128], ovl[:, cc, :], [(pt, None), (ovl, None)]) for (cc, pt) in keep])
                    p.op("dve", lambda e: e.tensor_scalar(out=rsi[:], in0=IMb[:, 64:65], scalar1=1e-30, scalar2=None, op0=ALU.max), reads=[(IMb, None)], writes=[(rsi, None)])
                    p.op("dve", lambda e: e.reciprocal(out=rsi[:], in_=rsi[:]), reads=[(rsi, None)], writes=[(rsi, None)])
                    if n == 0:
                        p.op("dve", lambda e, t4=t4: e.tensor_scalar(out=acc[:, t4, :], in0=IMb[:, 0:64], scalar1=rsi[:, 0:1], scalar2=None, op0=ALU.mult),
                             reads=[(IMb, None), (rsi, None)], writes=[(acc, t4)])
                    else:
                        p.op("dve", lambda e, t4=t4: e.scalar_tensor_tensor(out=acc[:, t4, :], in0=IMb[:, 0:64], scalar=rsi[:, 0:1], in1=acc[:, t4, :], op0=ALU.mult, op1=ALU.add),
                             reads=[(IMb, None), (rsi, None), (acc, t4)], writes=[(acc, t4)])
                gate_fac(Ob, n, 0, qb, True)
                p.op("dve", lambda e, n=n, Ob=Ob: e.tensor_tensor(out=OC[0:64, n, :], in0=Ob[0:64, :], in1=fac[64:128, :], op=ALU.mult),
                     reads=[(Ob, None), (fac, None)], writes=[(OC, n)])
            for t4 in range(4):
                qt = 4 * qb + t4
                o0 = 62 - 2 * qt
                p.op("dve", lambda e, t4=t4, o0=o0: e.tensor_tensor(out=sc[:], in0=acc[:, t4, :], in1=tkmul[:, o0:o0 + 64], op=ALU.mult), reads=[(acc, t4), (tkmul, None)], writes=[(sc, None)])
                p.op("dve", lambda e, o0=o0: e.tensor_tensor(out=sc[:], in0=sc[:], in1=tkadd[:, o0:o0 + 64], op=ALU.add), reads=[(sc, None), (tkadd, None)], writes=[(sc, None)])
                p.op("dve", lambda e: e.memset(sc[:, 0:1], 1e30), reads=[(sc, None)], writes=[(sc, None)])
                p.op("dve", lambda e: e.max(out=m8[:], in_=sc[:]), reads=[(sc, None)], writes=[(m8, None)])
                p.op("dve", lambda e: e.match_replace(out=sc2[:], in_to_replace=m8[:], in_values=sc[:], imm_value=-3.0e38), reads=[(sc, None), (m8, None)], writes=[(sc2, None)])
                p.op("dve", lambda e: e.max(out=m8b[:], in_=sc2[:]), reads=[(sc2, None)], writes=[(m8b, None)])
                p.op("dve", lambda e: e.tensor_scalar(out=sc2[:], in0=sc[:], scalar1=m8b[:, 7:8], scalar2=None, op0=ALU.is_ge), reads=[(sc, None), (m8b, None)], writes=[(sc2, None)])
                for hf in range(2):
                    p.op("dve", lambda e, hf=hf: e.tensor_scalar(out=NM[:, hf * 64:(hf + 1) * 64], in0=sc2[:], scalar1=-1.0, scalar2=-NEG, op0=ALU.add, op1=ALU.mult),
                         reads=[(sc2, None)], writes=[(NM, None)])
                p.op("pe", lambda e: e.transpose(out=pst[:, 0:128], in_=NM[:], identity=cx.ident[:]), reads=[(NM, None), (cx.ident, None)], writes=[(pst, None)])
                cols = slice(qt * 128, (qt + 1) * 128)
                for n in range(4):
                    rr = slice(64, 128) if n % 2 == 0 else slice(0, 64)
                    eng = "act" if n % 2 == 0 else "dve"
                    if eng == "act":
                        p.op("act", lambda e, n=n, rr=rr, cols=cols: e.activation(out=QA[rr, n, cols], in_=pst[rr, 0:128], func=AF.Copy), reads=[(pst, None)], writes=[(QA, (n, qb))])
                    else:
                        p.op("dve", lambda e, n=n, rr=rr, cols=cols: e.tensor_copy(out=QA[rr, n, cols], in_=pst[rr, 0:128]), reads=[(pst, None)], writes=[(QA, (n, qb))])
            for n in range(4):
                r0 = (n % 2) * 64
                KS = KSA if n % 2 == 0 else KSB
                Ob = next_O()
                stream = dict(
                    k=lambda kb: (KS[:, kb * 128:(kb + 1) * 128], [(KS, None)]),
                    q=(QA[:, n, qb * 512:(qb + 1) * 512], [(QA, (n, qb))]),
                    scale=0.125, bias=None,
                    masks=lambda kb: ([(cmask[:, kb - 4 * qb, :], [(cmask, None)])] if kb >= 4 * qb else []),
                    pv=[(Ob[:], lambda kb: (VS[:, kb, :], [(VS, kb)]))], pv_dep=(Ob, None))
                run_streams(cx, [stream], list(range(4 * qb + 4)), sbanks, pts, L)
                gate_fac(Ob, n, 1, qb, False)
                p.op("dve", lambda e, Ob=Ob: e.tensor_tensor(out=tacc[0:64, :], in0=Ob[0:64, :], in1=fac[64:128, :], op=ALU.mult), reads=[(Ob, None), (fac, None)], writes=[(tacc, None)])
                p.op("pool", lambda e, n=n: e.tensor_tensor(out=tacc[0:64, :], in0=tacc[0:64, :], in1=OC[0:64, n, :], op=ALU.add), reads=[(tacc, None), (OC, n)], writes=[(tacc, None)])
                Ob2 = next_O()

                def wmasks(kb, qb=qb):
                    if kb >= 4 * qb:
                        return [(cmask[:, kb - 4 * qb, :], [(cmask, None)])]
                    return [(cmask[:, 4 + kb - (4 * qb - 4), :], [(cmask, None)])]
                stream = dict(
                    k=lambda kb: (KW2[r0:r0 + 64, kb * 128:(kb + 1) * 128], [(KW2, kb // 4)]),
                    q=(QA[r0:r0 + 64, n, qb * 512:(qb + 1) * 512], [(QA, (n, qb))]),
                    scale=0.125, bias=None, masks=wmasks,
                    pv=[(Ob2[:], lambda kb: (VW[:, kb, :], [(VW, kb)]))], pv_dep=(Ob2, None))
                run_streams(cx, [stream], list(range(max(0, 4 * qb - 4), 4 * qb + 4)), sbanks, pts, L)
                gate_fac(Ob2, n, 2, qb, False)
                p.op("dve", lambda e, Ob2=Ob2: e.tensor_tensor(out=t2[0:64, :], in0=Ob2[0:64, :], in1=fac[64:128, :], op=ALU.mult), reads=[(Ob2, None), (fac, None)], writes=[(t2, None)])
                ot = obt[n % 2]
                p.op("pool", lambda e, ot=ot: e.tensor_tensor(out=ot[0:64, :], in0=tacc[0:64, :], in1=t2[0:64, :], op=ALU.add), reads=[(tacc, None), (t2, None)], writes=[(ot, None)])
                p.dma("sp", oT_ap[256 + n * 64:256 + (n + 1) * 64, qb * 512:(qb + 1) * 512], ot[0:64, :], reads=[(ot, None)], writes=[(cx.outb, ("oTn", n, qb))])
        p.barrier()


import numpy as np, math
import ml_dtypes
def np_bf16(a):
    return np.asarray(a, dtype=np.float32).astype(ml_dtypes.bfloat16)

def swap_cols(w):
    w = w.reshape(w.shape[0], -1, 64).copy()
    a = w[:, :, 0:8].copy(); w[:, :, 0:8] = w[:, :, 8:16]; w[:, :, 8:16] = a
    return w.reshape(w.shape[0], -1)

def odd_w_own(w_in, hh):
    q = w_in[:, 512 * hh:512 * hh + 512]; k = w_in[:, 1024 + 512 * hh:1024 + 512 * hh + 512]; v = w_in[:, 2048 + 512 * hh:2048 + 512 * hh + 512]
    return np.ascontiguousarray(np.concatenate([q, swap_cols(q), k, swap_cols(k), v], axis=1))

def even_w_own(w, hh):
    def c(o, n): return w[:, o:o + n]
    fq = c(256 * hh, 256); fk = c(512 + 256 * hh, 256); fv = c(1024 + 256 * hh, 256); fl = c(1536 + 4 * hh, 4)
    nq = c(1544 + 256 * hh, 256); kc = c(2056 + 64 * hh, 64); vc = c(2184 + 64 * hh, 64); ks = c(2312 + 64 * hh, 64)
    vs = c(2440 + 64 * hh, 64); kw = c(2568 + 64 * hh, 64); vw = c(2696 + 64 * hh, 64); gl = c(2824 + 12 * hh, 12)
    return np.ascontiguousarray(np.concatenate([fq, fk, fv, nq, swap_cols(nq), kc, swap_cols(kc), ks, swap_cols(ks), kw, swap_cols(kw), vc, vs, vw, fl, gl], axis=1))

def host_consts():
    c = {}
    c["ident"] = np_bf16(np.eye(128))
    k = np.arange(128)[:, None]; q = np.arange(512)[None, :]
    cm = np.stack([(128 * j + k <= q) for j in range(4)]).astype(np.float32)
    c["cmask"] = np_bf16(np.concatenate([cm, 1.0 - cm], axis=0).transpose(1, 0, 2).reshape(128, 8 * 512))
    inv = (500000.0 ** (-np.arange(0, 16, 2, dtype=np.float64) / 16)) / (2 * np.pi)
    r = np.zeros((128, 2), np.float32)
    for p_ in range(128):
        j = p_ % 64
        if j < 8: r[p_, 0] = -inv[j]; r[p_, 1] = inv[j]
        elif j < 16: r[p_, 0] = inv[j - 8]; r[p_, 1] = inv[j - 8]
    c["ropeinv"] = r
    cmp = np.stack([(16 * k + 31 + (-2048 + 512 * i) <= q) for i in range(5)]).astype(np.float32)
    c["cmpmask"] = np_bf16(cmp.transpose(1, 0, 2).reshape(128, 5 * 512))
    cs = np.arange(256)[:, None] * 16; ss = np.arange(64)[None, :] * 64
    ov = np.clip(np.minimum(cs + 32, ss + 64) - np.maximum(cs, ss), 0, None) / 32.0
    ov[255] = 0
    ovl = np.concatenate([ov, np.ones((256, 1))], axis=1).reshape(2, 128, 65).transpose(1, 0, 2).reshape(128, 130)
    c["ovl"] = np_bf16(ovl)
    sel = np.zeros((12, 12, 128), np.float32)
    for j in range(12): sel[j, j, :] = 1
    c["sel12"] = np_bf16(sel.reshape(12, 12 * 128))
    add = np.zeros((128, 128), np.float32); mul = np.zeros((128, 128), np.float32)
    for p_ in range(128):
        cur = 1 if p_ >= 64 else 0
        for i in range(128):
            s_ = i - 62
            valid = s_ <= cur
            forced = (s_ == cur) or (s_ == cur - 1)
            if not valid: add[p_, i] = -1e30
            elif forced: add[p_, i] = 1e30
            else: mul[p_, i] = 1.0
    c["tkadd"] = add; c["tkmul"] = mul
    c["onehot"] = np_bf16((np.arange(4096)[None, :] // 64 == np.arange(64)[:, None]).astype(np.float32))
    c["tri"] = (np.arange(128)[:, None] <= np.arange(128)[None, :]).astype(np.float32)
    s127 = np.zeros((128, 128), np.float32); s127[127, :] = 1
    c["sel127"] = s127
    return c


from concourse.bass_utils import run_bass_kernel_spmd

CONST_SPECS = {"cmask": ([128, 4096], BF16), "ropeinv": ([128, 2], F32), "cmpmask": ([128, 2560], BF16), "ovl": ([128, 130], BF16), "sel12": ([12, 1536], BF16),
               "tkadd": ([128, 128], F32), "tkmul": ([128, 128], F32), "onehot": ([64, 4096], BF16), "tri": ([128, 128], F32), "sel127": ([128, 128], F32)}
GROUPS = [[0, 1], [2, 3], [4, 5], [6, 7]]
_PROG = {}
DEPTH = 4


class OTMap:
    def __init__(self, ap_a, ap_b):
        self.aps = (ap_a, ap_b)

    def __getitem__(self, idx):
        rows, cols = idx
        qb = cols.start // 512
        half, c0 = qb // 4, (qb % 4) * 512
        k = rows.start // 256
        assert (rows.stop - 1) // 256 == k
        r0, r1 = rows.start - 256 * k, rows.stop - 256 * k
        return self.aps[k][half * 256 + r0:half * 256 + r1, c0:c0 + 512]


def build_fused():
    nc = bass.Bass("TRN2", target_bir_lowering=False)

    def din(name, shape, dt=F32):
        return nc.dram_tensor(name, list(shape), dt, kind="ExternalInput").ap()
    ident = din("ident", [128, 128], BF16)
    x_in = din("x_in", [TOK, D])
    C = {k: din(k, s, dt) for k, (s, dt) in CONST_SPECS.items()}
    pos = din("pos", [1, T], I32)
    mem = din("mem", [MEM, D])
    g_all = din("g_all", [DEPTH * 6, D])
    mem_g = din("mem_g", [DEPTH, D])
    WL = []
    for l in range(DEPTH):
        W = {"g": g_all[l * 6:(l + 1) * 6, :], "mem": mem, "mem_g": mem_g[l:l + 1, :], "pos": pos}
        W.update(C)
        for nm, shp in (("w_out", [D, D]), ("ca_wq", [D, 256]), ("ca_wk", [D, 256]), ("ca_wv", [D, 256]), ("ca_wo", [256, D]),
                        ("ffn_wg", [D, DFF]), ("ffn_wu", [D, DFF]), ("ffn_wd", [DFF, D])):
            W[nm] = din(f"{nm}_{l}", shp)
        if l % 2 == 0:
            for nm, shp in (("w_in", [D, EV_NCOL]), ("fbias_rep", [1, 128]), ("c1k", [2048, 128]), ("c2k", [128, 64]), ("cpkT", [64, 32]),
                            ("c1v", [2048, 128]), ("c2v", [128, 64]), ("cpvT", [64, 32])):
                W[nm] = din(f"{nm}_{l}", shp)
        else:
            for nm, shp in (("w_in", [D, 2560]), ("lam", [1, 256]), ("subg", [128, 1]), ("laminit", [1, 2])):
                W[nm] = din(f"{nm}_{l}", shp)
        WL.append(W)
    x_out = nc.dram_tensor("x_out", [TOK, D], F32, kind="ExternalOutput").ap()
    hT_own_t = [nc.dram_tensor(f"hT_own{k}", [512, TOK], BF16) for k in range(2)]
    hT_g_t = [nc.dram_tensor(f"hT_g{k}", [1024, TOK], BF16) for k in range(2)]
    oT_own_t = [nc.dram_tensor(f"oT_own{k}", [512, TOK], BF16) for k in range(2)]
    oT_g_t = [nc.dram_tensor(f"oT_g{k}", [1024, TOK], BF16) for k in range(2)]
    x_scr_t = nc.dram_tensor("x_scr", [TOK, D], F32)
    wscr = {nm: nc.dram_tensor(f"scr_{nm}", [NFF, 128, 1024], BF16).ap() for nm in ("ffn_wg", "ffn_wu", "ffn_wd")}
    x_scr = x_scr_t.ap()
    HTO, HTG, OTO, OTG, XS = Buf(None, "hT_own"), Buf(None, "hT_g"), Buf(None, "oT_own"), Buf(None, "oT_g"), Buf(None, "x_scr")
    cx = make_ctx(nc, ident)
    p = cx.p
    cx.wsc = Buf(None, "wscr")
    cx.outb = HTO
    pid = nc.sync.partition_id()
    hh256 = (pid % 2) * 256

    def gather_hT():
        for k in range(2):
            p.collective("AllGather", hT_own_t[k].ap().opt(), hT_g_t[k].ap().opt(), GROUPS, reads=[(HTO, None)], writes=[(HTG, k)])

    def gather_oT():
        for k in range(2):
            p.collective("AllGather", oT_own_t[k].ap().opt(), oT_g_t[k].ap().opt(), GROUPS, reads=[(OTO, None)], writes=[(OTG, k)])

    def hT_store(hT, tb):
        for k in range(2):
            p.dma("sp", hT_own_t[k].ap()[:, tb * TB:(tb + 1) * TB].rearrange("(c p) n -> p c n", p=128), hT[:, 4 * k:4 * k + 4, :],
                  reads=[(hT, None)], writes=[(HTO, (k, tb))])

    def hT_chunk(r, c):
        return hT_g_t[c // 4].ap()[r * 512 + (c % 4) * 128:r * 512 + (c % 4 + 1) * 128, :]

    def x_view(ap):
        return ap.rearrange("(t p) d -> p t d", p=128)

    with ExitStack() as es:
        def sb(name, shape, dt):
            return Buf(es.enter_context(nc.sbuf_tensor(name + "_a0", list(shape), dt)), name)
        x = sb("x", [128, NT, D], F32)
        p.dma("sp", x[:], x_view(x_in), writes=[(x, None)])
        cx.ps, cx.pst = psum_set(cx, es, 1, True)
        L = {"gB": sb("gB", [128, D], F32), "stat": sb("stat", [128, 16], F32), "junk": sb("junk", [128, D], BF16),
             "hb": [sb(f"hb{i}", [128, D], BF16) for i in range(2)]}
        hT = sb("hT", [128, 8, TOK], BF16)
        norm_transpose(cx, x, list(range(NT)), g_all[0:1, :], hT, L)
        for k in range(2):
            p.dma("sp", hT_own_t[k].ap().rearrange("(c p) n -> p c n", p=128), hT[:, 4 * k:4 * k + 4, :], reads=[(hT, None)], writes=[(HTO, (k, 0))])
        p.barrier()
    gather_hT()

    for l in range(DEPTH):
        W = WL[l]
        cx.outb = OTO
        if l % 2 == 0:
            def hT_src(dst_ap, c, tok0, n, q, writes):
                r = tok0 // TOK
                p.dma(q, dst_ap, hT_chunk(r, c)[:, tok0 % TOK:tok0 % TOK + n], reads=[(HTG, None)], writes=writes)
            phase_B_even(cx, hT_src, W, OTMap(oT_own_t[0].ap(), oT_own_t[1].ap()))
        else:
            def load_hT(hT):
                for c in range(8):
                    for r in range(2):
                        p.dma("sp" if c % 2 == 0 else "pool", hT[:, c, r * TOK:(r + 1) * TOK], hT_chunk(r, c),
                              reads=[(HTG, None)], writes=[(hT, None)])
            phase_B_odd(cx, load_hT, W, OTMap(oT_own_t[0].ap(), oT_own_t[1].ap()))
        p.barrier()
        gather_oT()
        cx.outb = HTO
        with ExitStack() as es:
            x = Buf(es.enter_context(nc.sbuf_tensor(f"x_l{l}", [128, NT, D], F32)), "x")
            p.dma("sp", x[:], x_view(x_in if l == 0 else x_scr), reads=[(XS, None)], writes=[(x, None)])

            def oT_load(oT, tb, writes):
                for r in range(2):
                    for k in range(2):
                        src = oT_g_t[k].ap()[bass.ds(hh256 + r * 512, 256), tb * TB:(tb + 1) * TB]
                        p.dma("sp", oT[:, 4 * r + 2 * k:4 * r + 2 * k + 2, :], src.rearrange("(c p) n -> p c n", p=128), reads=[(OTG, None)], writes=writes)
            last = (l == DEPTH - 1)
            phase_C(cx, x, None, W, None, None if last else g_all[(l + 1) * 6:(l + 1) * 6 + 1, :], oT_load=oT_load, hT_store=None if last else hT_store, wscr=wscr)
            if last:
                OUT = Buf(None, "x_out")
                p.dma("sp", x_view(x_out), x[:], reads=[(x, None)], writes=[(OUT, None)])
                p.finish([OUT])
            else:
                p.dma("sp", x_view(x_scr), x[:], reads=[(x, None)], writes=[(XS, None)])
                p.barrier()
        if not last:
            gather_hT()
    print("fused program: n_inst", p.n_inst, "n_wait", p.n_wait)
    p.close()
    return nc


def _ca(a):
    return np.ascontiguousarray(a)


def kernel(x, mem, positions, sandwich_g, mem_norm_g, ev_w_in, ev_fox_fbias,
           ev_cmp_pos_k, ev_cmp_w1_k, ev_cmp_w2_k, ev_cmp_pos_v, ev_cmp_w1_v, ev_cmp_w2_v,
           ev_w_out, od_w_in, od_lambda, od_subln_g, od_w_out,
           ca_wq, ca_wk, ca_wv, ca_wo, ffn_wg, ffn_wu, ffn_wd):
    f32 = lambda a: np.asarray(a, dtype=np.float32)
    x = f32(x); mem = f32(mem); positions = np.asarray(positions).astype(np.int32)
    sandwich_g = f32(sandwich_g); mem_norm_g = f32(mem_norm_g)
    hc = host_consts()
    if "nc" not in _PROG:
        _PROG["nc"] = build_fused()
    nc = _PROG["nc"]
    cores = list(range(8))
    shared = {k: hc[k] for k in CONST_SPECS}
    shared["ident"] = hc["ident"]
    shared["g_all"] = _ca(sandwich_g.reshape(DEPTH * 6, D))
    shared["mem_g"] = _ca(mem_norm_g)
    per_h = [dict(), dict()]
    for l in range(DEPTH):
        for nm, arr in (("ca_wq", ca_wq), ("ca_wk", ca_wk), ("ca_wv", ca_wv), ("ca_wo", ca_wo), ("ffn_wg", ffn_wg), ("ffn_wu", ffn_wu), ("ffn_wd", ffn_wd)):
            shared[f"{nm}_{l}"] = _ca(f32(arr[l]))
        if l % 2 == 0:
            e = l // 2
            wo = f32(ev_w_out[e])
            shared[f"w_out_{l}"] = _ca(np.concatenate([wo[0:256], wo[512:768], wo[256:512], wo[768:1024]], axis=0))
            shared[f"c1k_{l}"] = _ca(f32(ev_cmp_w1_k[e])); shared[f"c2k_{l}"] = _ca(f32(ev_cmp_w2_k[e])); shared[f"cpkT_{l}"] = _ca(f32(ev_cmp_pos_k[e]).T)
            shared[f"c1v_{l}"] = _ca(f32(ev_cmp_w1_v[e])); shared[f"c2v_{l}"] = _ca(f32(ev_cmp_w2_v[e])); shared[f"cpvT_{l}"] = _ca(f32(ev_cmp_pos_v[e]).T)
            for hh in range(2):
                per_h[hh][f"w_in_{l}"] = even_w_own(f32(ev_w_in[e]), hh)
                per_h[hh][f"fbias_rep_{l}"] = _ca(np.tile(f32(ev_fox_fbias[e])[4 * hh:4 * hh + 4], 32)[None, :])
        else:
            o = l // 2
            lam_init = 0.8 - 0.6 * math.exp(-0.3 * l)
            shared[f"w_out_{l}"] = _ca(f32(od_w_out[o]))
            shared[f"lam_{l}"] = _ca(f32(od_lambda[o]).reshape(1, 256))
            shared[f"subg_{l}"] = _ca(f32(od_subln_g[o]).reshape(128, 1))
            shared[f"laminit_{l}"] = np.array([[-lam_init, 1.0 - lam_init]], np.float32)
            for hh in range(2):
                per_h[hh][f"w_in_{l}"] = odd_w_own(f32(od_w_in[o]), hh)
    maps = []
    for c in cores:
        b, hh = c // 2, c % 2
        m = dict(shared)
        m.update(per_h[hh])
        m["x_in"] = _ca(x[b, TOK * hh:TOK * (hh + 1)])
        m["pos"] = _ca(positions[b:b + 1])
        m["mem"] = _ca(mem[b])
        maps.append(m)
    res = run_bass_kernel_spmd(nc, maps, core_ids=cores)
    out = np.zeros((4, T, D), np.float32)
    for c in cores:
        out[c // 2, TOK * (c % 2):TOK * (c % 2 + 1)] = res.results[c]["x_out"]
    return out
```

```python
from contextlib import ExitStack
import numpy as np
import concourse.bass as bass
import concourse.mybir as mybir

F32 = mybir.dt.float32
BF16 = mybir.dt.bfloat16
I32 = mybir.dt.int32
AF = mybir.ActivationFunctionType
ALU = mybir.AluOpType
AX = mybir.AxisListType


class Buf:
    _n = 0

    def __init__(self, t, name):
        self.t = t
        self.name = name
        self.regions = {}
        self.whole = [None, {}]

    def __getitem__(self, idx):
        return self.t[idx]


class Prog:
    ENG = ["pe", "dve", "act", "pool", "sp"]

    def __init__(self, nc, n_dma_sems=10):
        self.nc = nc
        self.es = ExitStack()
        self.eng = {"pe": nc.tensor, "dve": nc.vector, "act": nc.scalar, "pool": nc.gpsimd, "sp": nc.sync}
        self.sem = {e: self.es.enter_context(nc.semaphore("s_" + e)) for e in self.ENG}
        self.cnt = {e: 0 for e in self.ENG}
        self.sem["cc"] = self.es.enter_context(nc.semaphore("s_cc"))
        self.cnt["cc"] = 0
        self.waited = {}
        self.dsem = {}
        self.dval = {}
        self.dnext = {}
        for q in ["sp", "act", "pool"]:
            self.dsem[q] = [self.es.enter_context(nc.semaphore(f"d_{q}{i}")) for i in range(n_dma_sems)]
            self.dval[q] = [0] * n_dma_sems
            self.dnext[q] = 0
        self.dwaited = {}
        self.n_inst = 0
        self.n_wait = 0

    def sbuf(self, name, shape, dtype):
        t = self.es.enter_context(self.nc.sbuf_tensor(name, list(shape), dtype))
        return Buf(t, name)

    def psum(self, name, shape, dtype):
        t = self.es.enter_context(self.nc.psum_tensor(name, list(shape), dtype))
        return Buf(t, name)

    def close(self):
        self.es.close()

    def _states(self, buf, key):
        if key is None:
            return [buf.whole] + list(buf.regions.values())
        if key not in buf.regions:
            buf.regions[key] = [None, {}]
        return [buf.whole, buf.regions[key]]

    def _need(self, deps, tok):
        if tok is not None:
            deps.add(tok)

    def _collect(self, reads, writes):
        deps = set()
        for (b, k) in reads:
            for st in self._states(b, k):
                self._need(deps, st[0])
        for (b, k) in writes:
            for st in self._states(b, k):
                self._need(deps, st[0])
                for tok in st[1].values():
                    deps.add(tok)
        return deps

    def _emit_waits(self, e, deps, skip_same=False):
        engobj = self.eng[e]
        best = {}
        for tok in deps:
            if tok[0] == "e":
                _, f, c = tok
                if f == e and skip_same:
                    continue
                key = ("e", f)
                best[key] = max(best.get(key, 0), c)
            else:
                _, q, i, v = tok
                key = ("d", q, i)
                best[key] = max(best.get(key, 0), v)
        for key, v in best.items():
            wk = (e,) + key
            if self.waited.get(wk, -1) >= v:
                continue
            self.waited[wk] = v
            if key[0] == "e":
                engobj.wait_ge(self.sem[key[1]], v)
            else:
                engobj.wait_ge(self.dsem[key[1]][key[2]], v)
            self.n_wait += 1

    def _record(self, tok, reads, writes):
        for (b, k) in reads:
            if k is None:
                b.whole[1][tok[1] if tok[0] == "e" else ("d",) + tok[1:3]] = tok
            else:
                st = self._states(b, k)[1]
                st[1][tok[1] if tok[0] == "e" else ("d",) + tok[1:3]] = tok
        for (b, k) in writes:
            if k is None:
                b.regions.clear()
                b.whole[0] = tok
                b.whole[1] = {}
            else:
                st = self._states(b, k)[1]
                st[0] = tok
                st[1] = {}

    def alias(self, ap, name):
        return Buf(ap, name)

    def barrier(self):
        alld = set()
        for e in self.ENG + ["cc"]:
            if self.cnt[e] > 0:
                alld.add(("e", e, self.cnt[e]))
        for q in self.dsem:
            for i, v in enumerate(self.dval[q]):
                if v > 0:
                    alld.add(("d", q, i, v))
        for e in self.ENG:
            self._emit_waits(e, alld)

    def op(self, e, fn, reads=(), writes=(), skip_same=False, inc=True):
        deps = self._collect(reads, writes)
        if e == "pe":
            skip_same = True
        if skip_same is False and e in ("dve", "act", "pool"):
            raw = set()
            for (b, k) in reads:
                for st in self._states(b, k):
                    if st[0] is not None:
                        raw.add(st[0])
            deps = {t for t in deps if not (t[0] == "e" and t[1] == e) or t in raw}
        self._emit_waits(e, deps, skip_same=skip_same)
        inst = fn(self.eng[e])
        if inc:
            self.cnt[e] += 1
            inst.then_inc(self.sem[e], 1)
            tok = ("e", e, self.cnt[e])
        else:
            tok = ("e", e, self.cnt[e] + 1)
        self._record(tok, reads, writes)
        self.n_inst += 1
        return inst

    def dma(self, q, out_ap, in_ap, reads=(), writes=(), **kw):
        e = q
        deps = self._collect(reads, writes)
        i = self.dnext[q]
        self.dnext[q] = (i + 1) % len(self.dsem[q])
        if self.dval[q][i] > 0:
            deps.add(("d", q, i, self.dval[q][i]))
        self._emit_waits(e, deps)
        self.dval[q][i] += 16
        inst = self.eng[e].dma_start(out=out_ap, in_=in_ap, **kw)
        inst.then_inc(self.dsem[q][i], 16)
        tok = ("d", q, i, self.dval[q][i])
        self._record(tok, reads, writes)
        self.n_inst += 1
        return inst

    def collective(self, kind, in_ap, out_ap, groups, reads=(), writes=()):
        deps = self._collect(reads, writes)
        self._emit_waits("pool", deps)
        self.cnt["cc"] += 1
        inst = self.nc.gpsimd.collective_compute(kind, mybir.AluOpType.bypass, replica_groups=groups, ins=[in_ap], outs=[out_ap])
        inst.then_inc(self.sem["cc"])
        tok = ("e", "cc", self.cnt["cc"])
        self._record(tok, reads, writes)
        self.n_inst += 1
        return inst

    def finish(self, out_bufs):
        deps = set()
        for b in out_bufs:
            for st in [b.whole] + list(b.regions.values()):
                if st[0] is not None:
                    deps.add(st[0])
        self._emit_waits("sp", deps)
        alld = set()
        for e in self.ENG + ["cc"]:
            if self.cnt[e] > 0:
                alld.add(("e", e, self.cnt[e]))
        for q in self.dsem:
            for i, v in enumerate(self.dval[q]):
                if v > 0:
                    alld.add(("d", q, i, v))
        self._emit_waits("sp", alld)


import math
import numpy as np
import ml_dtypes
from contextlib import ExitStack

D = 1024
T = 4096
TOK = 2048
NT = TOK // 128
TB = 1024
DFF = 2816
NFF = DFF // 128
EPS = 1e-6
MEM = 256
NEG = -30000.0


def np_bf16(a):
    return np.asarray(a, dtype=np.float32).astype(ml_dtypes.bfloat16)


class Ctx:
    pass


def make_ctx(nc, ident_ap):
    cx = Ctx()
    cx.nc = nc
    p = Prog(nc)
    cx.p = p
    cx.uid = 0
    cx.ident = p.sbuf("ident_sb", [128, 128], BF16)
    p.dma("sp", cx.ident[:], ident_ap, writes=[(cx.ident, None)])
    cx.psi = 0
    cx.outb = Buf(None, "dram_out")
    cx.epsb = p.sbuf("epsb", [128, 1], F32)
    p.op("pool", lambda e: e.memset(cx.epsb[:], EPS), writes=[(cx.epsb, None)])
    return cx


def rot(cx, n=7):
    b = cx.ps[cx.psi % n]
    cx.psi += 1
    return b


def mm_group(cx, bank_ap, bank_dep, pairs):
    p = cx.p
    n = len(pairs)
    for i, (lhsT, rhs, reads) in enumerate(pairs):
        p.op("pe", lambda e, lhsT=lhsT, rhs=rhs, i=i: e.matmul(bank_ap, lhsT=lhsT, rhs=rhs, start=(i == 0), stop=(i == n - 1)),
             reads=reads, writes=[bank_dep], inc=(i == n - 1))


def cast_copy(cx, dst_ap, src_ap, reads, writes):
    p = cx.p
    cx.cast_i = getattr(cx, "cast_i", 0) + 1
    if cx.cast_i % 2 == 0:
        p.op("dve", lambda e: e.tensor_copy(out=dst_ap, in_=src_ap), reads=reads, writes=writes)
    else:
        p.op("act", lambda e: e.activation(out=dst_ap, in_=src_ap, func=AF.Copy), reads=reads, writes=writes)


def load_w_bf16(cx, dst_ap, dst_dep, w_ap, kc, ncols, stage, q="sp", cast_eng="pool"):
    p = cx.p
    assert kc * ncols <= 2048
    sv = stage[:, 0:kc * ncols].rearrange("p (c n) -> p c n", c=kc)
    p.dma(q, sv, w_ap.rearrange("(c p) n -> p c n", p=128), writes=[(stage, None)])
    cast_copy(cx, dst_ap, sv, [(stage, None)], [dst_dep])


def rms_ss(cx, src_ap, src_dep, ncol, stat, junk, key):
    p = cx.p
    p.op("act", lambda e: e.activation(out=junk[:, 0:ncol], in_=src_ap, func=AF.Square, accum_out=stat[:, key:key + 1]),
         reads=[src_dep], writes=[(junk, None), (stat, key)])


def rstd_from_ss(cx, stat, k0, k1, dim):
    p = cx.p
    p.op("act", lambda e: e.activation(out=stat[:, k0:k1], in_=stat[:, k0:k1], func=AF.Sqrt, scale=1.0 / dim, bias=cx.epsb[:, 0:1]),
         reads=[(stat, None), (cx.epsb, None)], writes=[(stat, None)])
    p.op("dve", lambda e: e.reciprocal(out=stat[:, k0:k1], in_=stat[:, k0:k1]), reads=[(stat, None)], writes=[(stat, None)])


def norm_transpose(cx, x, tiles, g_ap, hT, L, q="sp"):
    p = cx.p
    gB, stat, junk, hb = L["gB"], L["stat"], L["junk"], L["hb"]
    p.dma(q, gB[:], g_ap.to_broadcast([128, D]), writes=[(gB, None)])
    n = len(tiles)
    for j, t in enumerate(tiles):
        rms_ss(cx, x[:, t, :], (x, t), D, stat, junk, j)
    rstd_from_ss(cx, stat, 0, n, D)
    for j, t in enumerate(tiles):
        hbt = hb[j % 2]
        p.op("dve", lambda e, t=t, j=j, hbt=hbt: e.scalar_tensor_tensor(out=hbt[:], in0=x[:, t, :], scalar=stat[:, j:j + 1], in1=gB[:],
                                                                         op0=ALU.mult, op1=ALU.mult),
             reads=[(x, t), (stat, None), (gB, None)], writes=[(hbt, None)])
        for c in range(8):
            p.op("pe", lambda e, c=c, hbt=hbt: e.transpose(out=cx.pst[:, c * 128:(c + 1) * 128], in_=hbt[:, c * 128:(c + 1) * 128], identity=cx.ident[:]),
                 reads=[(hbt, None), (cx.ident, None)], writes=[(cx.pst, None)], inc=(c == 7))
        src = cx.pst[:].rearrange("p (c n) -> p c n", c=8)
        if j % 2 == 0:
            p.op("act", lambda e, j=j, src=src: e.activation(out=hT[:, :, j * 128:(j + 1) * 128], in_=src, func=AF.Copy),
                 reads=[(cx.pst, None)], writes=[(hT, j)])
        else:
            p.op("dve", lambda e, j=j, src=src: e.tensor_copy(out=hT[:, :, j * 128:(j + 1) * 128], in_=src),
                 reads=[(cx.pst, None)], writes=[(hT, j)])


def norm_residual(cx, x, t, banks, g_ready_gB, L):
    p = cx.p
    stat2, junk, tmp, gB = L["stat2"], L["junk"], L["tmp"], g_ready_gB
    for nb in range(2):
        rms_ss(cx, banks[nb][:], (banks[nb], None), 512, stat2, junk, nb)
    p.op("dve", lambda e: e.tensor_tensor(out=stat2[:, 2:3], in0=stat2[:, 0:1], in1=stat2[:, 1:2], op=ALU.add),
         reads=[(stat2, None)], writes=[(stat2, None)])
    rstd_from_ss(cx, stat2, 2, 3, D)
    for nb in range(2):
        p.op("dve", lambda e, nb=nb, b=banks[nb]: e.scalar_tensor_tensor(out=tmp[:, nb * 512:(nb + 1) * 512], in0=b[:], scalar=stat2[:, 2:3],
                                                                        in1=gB[:, nb * 512:(nb + 1) * 512], op0=ALU.mult, op1=ALU.mult),
             reads=[(banks[nb], None), (stat2, None), (gB, None)], writes=[(tmp, nb)])
    p.op("dve", lambda e, t=t: e.tensor_tensor(out=x[:, t, :], in0=x[:, t, :], in1=tmp[:], op=ALU.add),
         reads=[(tmp, None), (x, t)], writes=[(x, t)])


def proj_residual(cx, x, tiles, lhs_fn, kc, w, g_ap, L, q="sp"):
    p = cx.p
    gB = L["gB"]
    p.dma(q, gB[:], g_ap.to_broadcast([128, D]), writes=[(gB, None)])
    for j, t in enumerate(tiles):
        banks = [rot(cx), rot(cx)]
        for nb in range(2):
            pairs = []
            for c in range(kc):
                lhsT, ldep = lhs_fn(c, j)
                pairs.append((lhsT, w[:, c, nb * 512:(nb + 1) * 512], [ldep, (w, None)]))
            mm_group(cx, banks[nb][:], (banks[nb], None), pairs)
        norm_residual(cx, x, t, banks, gB, L)


def phase_C(cx, x, oT_ap, W, hT_next_ap, g_next_ap, oT_load=None, hT_store=None, wscr=None):
    p = cx.p
    with ExitStack() as es:
        cx.uid += 1
        uu = cx.uid
        def sb(name, shape, dt):
            return Buf(es.enter_context(cx.nc.sbuf_tensor(f"{name}_{uu}", list(shape), dt)), name)
        cx.ps, cx.pst = psum_set(cx, es, 7, True)
        cx.psi = 0
        L = {}
        L["gB"] = sb("gB", [128, D], F32)
        L["stat"] = sb("stat", [128, 16], F32)
        L["stat2"] = sb("stat2", [128, 4], F32)
        L["junk"] = sb("junk", [128, D], BF16)
        L["tmp"] = sb("tmp", [128, D], F32)
        L["hb"] = [sb(f"hb{i}", [128, D], BF16) for i in range(2)]
        hT = sb("hT", [128, 8, TB], BF16)
        big = sb("big", [128, NFF, TB], BF16)
        stage = [sb(f"stage{i}", [128, 2048], F32) for i in range(2)]
        wsm = sb("wsm", [128, 8, 1024], BF16)
        memT = sb("memT", [128, 8, MEM], BF16)
        kmT = sb("kmT", [128, 2, MEM], BF16)
        vm = sb("vm", [128, 2, 4, 128], BF16)
        pT = [sb(f"pTc{i}", [128, 512], BF16) for i in range(4)]
        rs = sb("rs_c", [128, 512], F32)
        wg = [sb(f"wg{i}", [128, 8, 128], BF16) for i in range(2)]
        wu = [sb(f"wu{i}", [128, 8, 128], BF16) for i in range(2)]
        wd = [sb(f"wd{i}", [128, 1024], BF16) for i in range(3)]
        gsb = [sb(f"gsb{i}", [128, 512], BF16) for i in range(2)]
        bigflat = big.t[:].rearrange("p a b -> p (a b)")
        memx = p.alias(stage[1][:].rearrange("p (t d) -> p t d", t=2), "memx")
        p.dma("sp", memx[:], W["mem"].rearrange("(t p) d -> p t d", p=128), writes=[(memx, None), (stage[1], None)])
        norm_transpose(cx, memx, [0, 1], W["mem_g"], memT, L)
        p.barrier()
        sti = 0
        for (nm, c0) in (("ca_wk", 0), ("ca_wv", 256)):
            load_w_bf16(cx, wsm[:, :, c0:c0 + 256], (wsm, None), W[nm], 8, 256, stage[sti % 2], q="pool")
            sti += 1
        for ht in range(2):
            b = rot(cx)
            mm_group(cx, b[:, 0:MEM], (b, None), [(wsm[:, c, ht * 128:(ht + 1) * 128], memT[:, c, :], [(wsm, None), (memT, None)]) for c in range(8)])
            p.op("act", lambda e, b=b, ht=ht: e.activation(out=kmT[:, ht, :], in_=b[:, 0:MEM], func=AF.Copy), reads=[(b, None)], writes=[(kmT, None)])
        p.op("pool", lambda e: e.memset(vm[:], 1.0), writes=[(vm, None)])
        for mc in range(2):
            b = rot(cx)
            mm_group(cx, b[:, 0:256], (b, None), [(memT[:, c, mc * 128:(mc + 1) * 128], wsm[:, c, 256:512], [(wsm, None), (memT, None)]) for c in range(8)])
            p.op("act", lambda e, b=b, mc=mc: e.activation(out=vm[:, mc, :, 0:64], in_=b[:, 0:256].rearrange("p (h d) -> p h d", h=4), func=AF.Copy),
                 reads=[(b, None)], writes=[(vm, None)])
        for tb in range(TOK // TB):
            tiles = list(range(tb * 8, tb * 8 + 8))
            tsl = slice(tb * TB, (tb + 1) * TB)
            oT = p.alias(bigflat[:, 0:8 * TB].rearrange("p (c n) -> p c n", c=8), "oT")
            if oT_load is None:
                p.dma("sp", oT[:], oT_ap[:, tsl].rearrange("(c p) n -> p c n", p=128), writes=[(oT, None), (big, None)])
            else:
                oT_load(oT, tb, [(oT, None), (big, None)])
            for qd in range(4):
                load_w_bf16(cx, wsm[:, :, qd * 256:(qd + 1) * 256], (wsm, None), W["w_out"][:, qd * 256:(qd + 1) * 256], 8, 256, stage[sti % 2], q="pool")
                sti += 1
            proj_residual(cx, x, tiles, lambda c, j: (oT[:, c, j * 128:(j + 1) * 128], (oT, None)), 8, wsm, W["g"][1:2, :], L)
            p.barrier()
            qcT = p.alias(bigflat[:, 0:2 * TB].rearrange("p (c n) -> p c n", c=2), "qcT")
            ocT = p.alias(bigflat[:, 2 * TB:4 * TB].rearrange("p (c n) -> p c n", c=2), "ocT")
            norm_transpose(cx, x, tiles, W["g"][2:3, :], hT, L)
            load_w_bf16(cx, wsm[:, :, 0:256], (wsm, None), W["ca_wq"], 8, 256, stage[sti % 2], q="pool")
            sti += 1
            for ht in range(2):
                for qb in range(TB // 512):
                    b = rot(cx)
                    mm_group(cx, b[:], (b, None), [(wsm[:, c, ht * 128:(ht + 1) * 128], hT[:, c, qb * 512:(qb + 1) * 512], [(wsm, None), (hT, None)]) for c in range(8)])
                    p.op("act", lambda e, b=b, ht=ht, qb=qb: e.activation(out=qcT[:, ht, qb * 512:(qb + 1) * 512], in_=b[:], func=AF.Copy),
                         reads=[(b, None)], writes=[(qcT, (ht, qb))])
            pi = 0
            for h in range(4):
                ht, r0 = h // 2, (h % 2) * 64
                for qb in range(TB // 512):
                    pts = []
                    for mc in range(2):
                        sbk = rot(cx)
                        p.op("pe", lambda e, sbk=sbk, mc=mc, ht=ht, r0=r0, qb=qb: e.matmul(sbk[:], lhsT=kmT[r0:r0 + 64, ht, mc * 128:(mc + 1) * 128],
                                                                                           rhs=qcT[r0:r0 + 64, ht, qb * 512:(qb + 1) * 512], start=True, stop=True),
                             reads=[(kmT, None), (qcT, (ht, qb))], writes=[(sbk, None)])
                        pt = pT[pi % 4]
                        pi += 1
                        p.op("act", lambda e, sbk=sbk, pt=pt: e.activation(out=pt[:], in_=sbk[:], func=AF.Exp, scale=0.125), reads=[(sbk, None)], writes=[(pt, None)])
                        pts.append(pt)
                    ob = rot(cx)
                    mm_group(cx, ob[:], (ob, None), [(vm[:, mc, h, :], pts[mc][:], [(vm, None), (pts[mc], None)]) for mc in range(2)])
                    p.op("dve", lambda e, ob=ob: e.reciprocal(out=rs[64:128, :], in_=ob[64:128, :]), reads=[(ob, None)], writes=[(rs, None)])
                    p.op("dve", lambda e, ob=ob, ht=ht, r0=r0, qb=qb: e.tensor_tensor(out=ocT[r0:r0 + 64, ht, qb * 512:(qb + 1) * 512], in0=ob[0:64, :], in1=rs[64:128, :], op=ALU.mult),
                         reads=[(ob, None), (rs, None)], writes=[(ocT, (ht, qb))])
            for hf in range(4):
                load_w_bf16(cx, wsm[:, 0:2, hf * 256:(hf + 1) * 256], (wsm, None), W["ca_wo"][:, hf * 256:(hf + 1) * 256], 2, 256, stage[sti % 2], q="pool")
                sti += 1
            proj_residual(cx, x, tiles, lambda c, j: (ocT[:, c, j * 128:(j + 1) * 128], (ocT, None)), 2, wsm, W["g"][3:4, :], L)
            p.barrier()
            norm_transpose(cx, x, tiles, W["g"][4:5, :], hT, L)
            aT = big
            stg4 = [stage[0], stage[1]]
            sq = [0]

            def nst():
                sq[0] += 1
                return stg4[sq[0] % len(stg4)]
            for f in range(NFF):
                k = f % 2
                for (nm, wb) in (("ffn_wg", wg[k]), ("ffn_wu", wu[k])):
                    flat = wb[:].rearrange("p c n -> p (c n)")
                    if wscr is None or tb == 0:
                        load_w_bf16(cx, wb[:], (wb, None), W[nm][:, f * 128:(f + 1) * 128], 8, 128, nst(), q="sp")
                        if wscr is not None:
                            p.dma("pool", wscr[nm][f], flat, reads=[(wb, None)], writes=[(cx.wsc, (nm, f))])
                    else:
                        p.dma("sp", flat, wscr[nm][f], reads=[(cx.wsc, (nm, f))], writes=[(wb, None)])
                for nb in range(TB // 512):
                    bg = rot(cx)
                    bu = rot(cx)
                    for (bank, w) in ((bg, wg[k]), (bu, wu[k])):
                        mm_group(cx, bank[:], (bank, None), [(w[:, c, :], hT[:, c, nb * 512:(nb + 1) * 512], [(w, None), (hT, None)]) for c in range(8)])
                    gs = gsb[nb % 2]
                    p.op("act", lambda e, bg=bg, gs=gs: e.activation(out=gs[:], in_=bg[:], func=AF.Silu), reads=[(bg, None)], writes=[(gs, None)])
                    p.op("dve", lambda e, bu=bu, f=f, nb=nb, gs=gs: e.tensor_tensor(out=aT[:, f, nb * 512:(nb + 1) * 512], in0=bu[:], in1=gs[:], op=ALU.mult),
                         reads=[(bu, None), (gs, None)], writes=[(aT, (f, nb))])
            gB = L["gB"]
            p.dma("sp", gB[:], W["g"][5:6, :].to_broadcast([128, D]), writes=[(gB, None)])
            for grp in ([0, 1, 2], [3, 4, 5], [6, 7]):
                banks = {}
                for i, key in enumerate([(tt, nb) for tt in grp for nb in range(2)]):
                    banks[key] = cx.ps[i]
                for f in range(NFF):
                    k = f % 3
                    if wscr is None or (tb == 0 and grp[0] == 0):
                        st = nst()
                        p.dma("sp", st[:, 0:1024], W["ffn_wd"][f * 128:(f + 1) * 128, :], writes=[(st, None)])
                        cast_copy(cx, wd[k][:], st[:, 0:1024], [(st, None)], [(wd[k], None)])
                        if wscr is not None:
                            p.dma("pool", wscr["ffn_wd"][f], wd[k][:], reads=[(wd[k], None)], writes=[(cx.wsc, ("ffn_wd", f))])
                    else:
                        p.dma("sp", wd[k][:], wscr["ffn_wd"][f], reads=[(cx.wsc, ("ffn_wd", f))], writes=[(wd[k], None)])
                    for tt in grp:
                        for nb in range(2):
                            b = banks[(tt, nb)]
                            p.op("pe", lambda e, b=b, tt=tt, nb=nb, k=k, f=f: e.matmul(b[:], lhsT=aT[:, f, tt * 128:(tt + 1) * 128], rhs=wd[k][:, nb * 512:(nb + 1) * 512],
                                                                                      start=(f == 0), stop=(f == NFF - 1)),
                                 reads=[(aT, (f, tt // 4)), (wd[k], None)], writes=[(b, None)], inc=(tt == grp[-1] and nb == 1))
                for tt in grp:
                    norm_residual(cx, x, tiles[tt], [banks[(tt, 0)], banks[(tt, 1)]], gB, L)
            if hT_store is not None:
                norm_transpose(cx, x, tiles, g_next_ap, hT, L)
                hT_store(hT, tb)
            elif hT_next_ap is not None:
                norm_transpose(cx, x, tiles, g_next_ap, hT, L)
                p.dma("sp", hT_next_ap[:, tsl].rearrange("(c p) n -> p c n", p=128), hT[:], reads=[(hT, None)], writes=[(cx.outb, ("hT", tb))])
            p.barrier()


def psum_set(cx, es, n_f32, with_bf16):
    cx.uid += 1
    u = cx.uid
    ps = [Buf(es.enter_context(cx.nc.psum_tensor(f"ps{i}_{u}", [128, 512], F32)), f"ps{i}") for i in range(n_f32)]
    pst = Buf(es.enter_context(cx.nc.psum_tensor(f"pst_{u}", [128, 1024], BF16)), "pst") if with_bf16 else None
    return ps, pst


def build_rope_tables(cx, pos_ap, ropeinv_ap, Ct, St, scratch_i, scratch_f):
    p = cx.p
    inv = cx.ropeinv
    p.dma("sp", inv[:], ropeinv_ap, writes=[(inv, None)])
    p.dma("sp", scratch_i[:], pos_ap.to_broadcast([128, T]), writes=[(scratch_i, None)])
    p.op("dve", lambda e: e.tensor_copy(out=scratch_f[:], in_=scratch_i[:]), reads=[(scratch_i, None)], writes=[(scratch_f, None)])
    for (dst, col, off) in ((St, 0, 0.0), (Ct, 1, 0.25)):
        p.op("dve", lambda e, dst=dst, col=col, off=off: e.tensor_scalar(out=dst[:], in0=scratch_f[:], scalar1=inv[:, col:col + 1], scalar2=off,
                                                                      op0=ALU.mult, op1=ALU.add),
             reads=[(scratch_f, None), (inv, None)], writes=[(dst, None)])
        p.op("dve", lambda e, dst=dst: e.tensor_copy(out=scratch_i[:], in_=dst[:]), reads=[(dst, None)], writes=[(scratch_i, None)])
        p.op("pool", lambda e, dst=dst: e.tensor_tensor(out=dst[:], in0=dst[:], in1=scratch_i[:], op=ALU.subtract),
             reads=[(dst, None), (scratch_i, None)], writes=[(dst, None)])
        p.op("dve", lambda e, dst=dst: e.scalar_tensor_tensor(out=dst[:], in0=dst[:], scalar=0.5, in1=dst[:], op0=ALU.is_gt, op1=ALU.subtract),
             reads=[(dst, None)], writes=[(dst, None)])
        p.op("dve", lambda e, dst=dst: e.scalar_tensor_tensor(out=dst[:], in0=dst[:], scalar=0.5, in1=dst[:], op0=ALU.is_gt, op1=ALU.subtract),
             reads=[(dst, None)], writes=[(dst, None)])
    for dst in (St, Ct):
        p.op("act", lambda e, dst=dst: e.activation(out=dst[:], in_=dst[:], func=AF.Sin, scale=2.0 * math.pi), reads=[(dst, None)], writes=[(dst, None)])


def proj_rope(cx, hT, wq, wqs, col0, tok0, Ct, St, out_ap, out_dep, L, banks):
    p = cx.p
    bA, bB = banks
    mm_group(cx, bA[:], (bA, None), [(wq[:, c, col0:col0 + 128], hT[:, c, tok0:tok0 + 512], [(wq, None), (hT, None)]) for c in range(8)])
    mm_group(cx, bB[:], (bB, None), [(wqs[:, c, col0:col0 + 128], hT[:, c, tok0:tok0 + 512], [(wqs, None), (hT, None)]) for c in range(8)])
    t1, t2 = L["rt1"], L["rt2"]
    p.op("dve", lambda e: e.tensor_tensor(out=t1[:], in0=bA[:], in1=Ct[:, tok0:tok0 + 512], op=ALU.mult), reads=[(bA, None), (Ct, None)], writes=[(t1, None)])
    p.op("dve", lambda e: e.tensor_tensor(out=t2[:], in0=bB[:], in1=St[:, tok0:tok0 + 512], op=ALU.mult), reads=[(bB, None), (St, None)], writes=[(t2, None)])
    p.op("dve", lambda e: e.tensor_tensor(out=out_ap, in0=t1[:], in1=t2[:], op=ALU.add), reads=[(t1, None), (t2, None)], writes=[out_dep])


def run_streams(cx, streams, kbs, sbanks, pts, L, keep=None):
    p = cx.p
    n = len(kbs)
    st = L.setdefault("_rs", {"sb": 0, "pt": 0})

    def tail(pend, i):
        for (s, bank, kb) in pend:
            pt = pts[st["pt"] % len(pts)]
            st["pt"] += 1
            if keep is not None:
                keep.append((kb, pt))
            if s.get("bias") is not None:
                bap, bdeps = s["bias"](kb)
                p.op("act", lambda e, pt=pt, bank=bank, bap=bap, s=s: e.activation(out=pt[:], in_=bank[:], func=AF.Exp, scale=s["scale"], bias=bap),
                     reads=[(bank, None)] + bdeps, writes=[(pt, None)])
            else:
                p.op("act", lambda e, pt=pt, bank=bank, s=s: e.activation(out=pt[:], in_=bank[:], func=AF.Exp, scale=s["scale"]),
                     reads=[(bank, None)], writes=[(pt, None)])
            for (map_, mdeps) in s["masks"](kb):
                p.op("dve", lambda e, pt=pt, map_=map_: e.tensor_tensor(out=pt[:], in0=pt[:], in1=map_, op=ALU.mult),
                     reads=[(pt, None)] + mdeps, writes=[(pt, None)])
            for (obank, lfn) in s["pv"]:
                lap, ldeps = lfn(kb)
                p.op("pe", lambda e, obank=obank, lap=lap, pt=pt, i=i: e.matmul(obank, lhsT=lap, rhs=pt[:], start=(i == 0), stop=(i == n - 1)),
                     reads=[(pt, None)] + ldeps, writes=[s["pv_dep"]])

    depth = 2 if (len(streams) == 1 and len(sbanks) >= 3) else 1
    queue = []
    for i, kb in enumerate(kbs):
        cur = []
        for s in streams:
            bank = sbanks[st["sb"] % len(sbanks)]
            st["sb"] += 1
            kap, kdeps = s["k"](kb)
            qap, qdeps = s["q"]
            p.op("pe", lambda e, bank=bank, kap=kap, qap=qap: e.matmul(bank[:], lhsT=kap, rhs=qap, start=True, stop=True),
                 reads=kdeps + qdeps, writes=[(bank, None)])
            cur.append((s, bank, kb))
        queue.append((cur, i))
        if len(queue) > depth:
            pc, pi_ = queue.pop(0)
            tail(pc, pi_)
    while queue:
        pc, pi_ = queue.pop(0)
        tail(pc, pi_)


def phase_B_odd(cx, load_hT, W, oT_ap):
    p = cx.p
    with ExitStack() as es:
        cx.uid += 1
        uu = cx.uid
        def sb(name, shape, dt):
            return Buf(es.enter_context(cx.nc.sbuf_tensor(f"{name}_{uu}", list(shape), dt)), name)
        ps, _ = psum_set(cx, es, 8, False)
        L = {}
        hT = sb("hT_all", [128, 8, T], BF16)
        load_hT(hT)
        Ct = sb("Ct", [128, T], F32)
        St = sb("St", [128, T], F32)
        cx.ropeinv = sb("ropeinv", [128, 2], F32)
        with ExitStack() as es2:
            sci = Buf(es2.enter_context(cx.nc.sbuf_tensor(f"sci_{uu}", [128, T], I32)), "sci")
            scf = Buf(es2.enter_context(cx.nc.sbuf_tensor(f"scf_{uu}", [128, T], F32)), "scf")
            build_rope_tables(cx, W["pos"], W["ropeinv"], Ct, St, sci, scf)
            p.barrier()
        qT = sb("qT", [128, 2, T], BF16)
        kT = sb("kT", [128, 2, T], BF16)
        vv = sb("vv", [128, 32, 256], BF16)
        stage = [sb(f"stage{i}", [128, 2048], F32) for i in range(2)]
        wq = sb("wq", [128, 8, 256], BF16)
        wqs = sb("wqs", [128, 8, 256], BF16)
        cmask = sb("cmask", [128, 8, 512], BF16)
        ones_b = sb("ones_b", [128, 128], BF16)
        ones_f = sb("ones_f", [128, 128], F32)
        pts = [sb(f"pt{i}", [128, 512], BF16) for i in range(6)]
        L["rt1"] = sb("rt1", [128, 512], F32)
        L["rt2"] = sb("rt2", [128, 512], F32)
        r1 = sb("r1", [128, 512], F32)
        r2 = sb("r2", [128, 512], F32)
        osb = sb("osb", [128, 512], F32)
        osq = sb("osq", [128, 512], F32)
        ob = [sb(f"ob{i}", [128, 512], BF16) for i in range(2)]
        lamt = sb("lamt", [128, 256], F32)
        lam2 = sb("lam2", [128, 8], F32)
        gcol = sb("gcol", [128, 1], F32)
        p.dma("sp", cmask[:], W["cmask"].rearrange("p (j n) -> p j n", j=8), writes=[(cmask, None)])
        p.op("pool", lambda e: e.memset(ones_b[:], 1.0), writes=[(ones_b, None)])
        p.op("pool", lambda e: e.memset(ones_f[:], 1.0), writes=[(ones_f, None)])
        p.dma("sp", lamt[:], W["lam"].to_broadcast([128, 256]), writes=[(lamt, None)])
        p.dma("sp", gcol[:], W["subg"], writes=[(gcol, None)])
        for i in range(2):
            p.op("dve", lambda e, i=i: e.tensor_tensor(out=lamt[:, i * 128:i * 128 + 64], in0=lamt[:, i * 128:i * 128 + 64], in1=lamt[:, i * 128 + 64:i * 128 + 128], op=ALU.mult),
                 reads=[(lamt, None)], writes=[(lamt, None)])
            p.op("dve", lambda e, i=i: e.tensor_reduce(out=lam2[:, i:i + 1], in_=lamt[:, i * 128:i * 128 + 64], axis=AX.X, op=ALU.add),
                 reads=[(lamt, None)], writes=[(lam2, None)])
        p.op("act", lambda e: e.activation(out=lam2[:, 2:4], in_=lam2[:, 0:2], func=AF.Exp), reads=[(lam2, None)], writes=[(lam2, None)])
        lic = sb("lic", [128, 2], F32)
        p.dma("sp", lic[:], W["laminit"].to_broadcast([128, 2]), writes=[(lic, None)])
        p.op("dve", lambda e: e.scalar_tensor_tensor(out=lam2[:, 4:5], in0=lam2[:, 3:4], scalar=lic[:, 0:1], in1=lam2[:, 2:3], op0=ALU.add, op1=ALU.subtract),
             reads=[(lam2, None), (lic, None)], writes=[(lam2, None)])
        p.op("dve", lambda e: e.tensor_scalar(out=gcol[:], in0=gcol[:], scalar1=lic[:, 1:2], scalar2=None, op0=ALU.mult), reads=[(gcol, None), (lic, None)], writes=[(gcol, None)])
        sti = 0
        for hp in range(2):
            for (dst, base) in ((qT, 0), (kT, 1024)):
                c0 = base + hp * 256
                load_w_bf16(cx, wq[:], (wq, None), W["w_in"][:, c0:c0 + 256], 8, 256, stage[sti % 2], q="pool"); sti += 1
                load_w_bf16(cx, wqs[:], (wqs, None), W["w_in"][:, c0 + 512:c0 + 768], 8, 256, stage[sti % 2], q="pool"); sti += 1
                for hh in range(2):
                    for tb in range(8):
                        banks = (ps[(2 * tb) % 8], ps[(2 * tb + 1) % 8])
                        proj_rope(cx, hT, wq, wqs, hh * 128, tb * 512, Ct, St, dst[:, hh, tb * 512:(tb + 1) * 512], (dst, (hh, tb)), L, banks)
            c0 = 2048 + hp * 256
            load_w_bf16(cx, wq[:], (wq, None), W["w_in"][:, c0:c0 + 256], 8, 256, stage[sti % 2], q="pool"); sti += 1
            for tt in range(32):
                b = ps[tt % 8]
                mm_group(cx, b[:, 0:256], (b, None), [(hT[:, c, tt * 128:(tt + 1) * 128], wq[:, c, :], [(wq, None), (hT, None)]) for c in range(8)])
                if tt % 2 == 0:
                    p.op("act", lambda e, b=b, tt=tt: e.activation(out=vv[:, tt, :], in_=b[:, 0:256], func=AF.Copy), reads=[(b, None)], writes=[(vv, tt)])
                else:
                    p.op("dve", lambda e, b=b, tt=tt: e.tensor_copy(out=vv[:, tt, :], in_=b[:, 0:256]), reads=[(b, None)], writes=[(vv, tt)])
            for hh in range(2):
                head = hp * 2 + hh
                for qb in range(8):
                    kbs = list(range(4 * qb + 4))
                    O1, S1, O2, S2 = ps[4], ps[5], ps[6], ps[7]
                    streams = []
                    for comp, (Ob, Sb) in enumerate(((O1, S1), (O2, S2))):
                        r0 = comp * 64
                        streams.append(dict(
                            k=lambda kb, r0=r0: (kT[r0:r0 + 64, hh, kb * 128:(kb + 1) * 128], [(kT, (hh, kb // 4))]),
                            q=(qT[r0:r0 + 64, hh, qb * 512:(qb + 1) * 512], [(qT, (hh, qb))]),
                            scale=0.125, bias=None,
                            masks=lambda kb: ([(cmask[:, kb - 4 * qb, :], [(cmask, None)])] if kb >= 4 * qb else []),
                            pv=[(Ob[:], lambda kb: (vv[:, kb, hh * 128:(hh + 1) * 128], [(vv, kb)])),
                                (Sb[:], lambda kb: (ones_b[:], [(ones_b, None)]))],
                            pv_dep=(Ob, None)))
                    run_streams(cx, streams, kbs, ps[0:4], pts, L)
                    p.op("dve", lambda e: e.reciprocal(out=r1[:], in_=S1[:]), reads=[(O1, None), (S1, None)], writes=[(r1, None)])
                    p.op("dve", lambda e: e.reciprocal(out=r2[:], in_=S2[:]), reads=[(O2, None), (S2, None)], writes=[(r2, None)])
                    p.op("dve", lambda e: e.tensor_tensor(out=r1[:], in0=O1[:], in1=r1[:], op=ALU.mult), reads=[(O1, None), (r1, None)], writes=[(r1, None)])
                    p.op("dve", lambda e: e.tensor_tensor(out=r2[:], in0=O2[:], in1=r2[:], op=ALU.mult), reads=[(O2, None), (r2, None)], writes=[(r2, None), (S1, None), (S2, None)])
                    p.op("dve", lambda e: e.scalar_tensor_tensor(out=osb[:], in0=r2[:], scalar=lam2[:, 4:5], in1=r1[:], op0=ALU.mult, op1=ALU.add),
                         reads=[(r1, None), (r2, None), (lam2, None)], writes=[(osb, None)])
                    p.op("pool", lambda e: e.tensor_tensor(out=osq[:], in0=osb[:], in1=osb[:], op=ALU.mult), reads=[(osb, None)], writes=[(osq, None)])
                    sbk = ps[qb % 4]
                    p.op("pe", lambda e, sbk=sbk: e.matmul(sbk[:], lhsT=ones_f[:], rhs=osq[:], start=True, stop=True), reads=[(osq, None), (ones_f, None)], writes=[(sbk, None)])
                    p.op("act", lambda e, sbk=sbk: e.activation(out=r1[:], in_=sbk[:], func=AF.Sqrt, scale=1.0 / 128, bias=cx.epsb[:, 0:1]),
                         reads=[(sbk, None), (cx.epsb, None)], writes=[(r1, None)])
                    p.op("dve", lambda e: e.reciprocal(out=r1[:], in_=r1[:]), reads=[(r1, None)], writes=[(r1, None)])
                    obt = ob[qb % 2]
                    p.op("dve", lambda e, obt=obt: e.scalar_tensor_tensor(out=obt[:], in0=osb[:], scalar=gcol[:, 0:1], in1=r1[:], op0=ALU.mult, op1=ALU.mult),
                         reads=[(osb, None), (gcol, None), (r1, None)], writes=[(obt, None)])
                    p.dma("sp", oT_ap[head * 128:(head + 1) * 128, qb * 512:(qb + 1) * 512], obt[:], reads=[(obt, None)], writes=[(cx.outb, ("oT", head, qb))])
        p.barrier()


EV = dict(fq=0, fk=256, fv=512, nq=768, nqs=1024, kc=1280, kcs=1344, ks=1408, kss=1472, kw=1536, kws=1600, vc=1664, vs=1728, vw=1792, fl=1856, gl=1860)
EV_NCOL = 1872
TBK = 512


def build_rope_block(cx, pos_ap, tok0, n, Ct, St, sci, scf):
    p = cx.p
    inv = cx.ropeinv
    p.dma("sp", sci[:, 0:n], pos_ap[:, tok0:tok0 + n].to_broadcast([128, n]), writes=[(sci, None)])
    p.op("dve", lambda e: e.tensor_copy(out=scf[:, 0:n], in_=sci[:, 0:n]), reads=[(sci, None)], writes=[(scf, None)])
    for (dst, col, off) in ((St, 0, 0.0), (Ct, 1, 0.25)):
        p.op("dve", lambda e, dst=dst, col=col, off=off: e.tensor_scalar(out=dst[:, 0:n], in0=scf[:, 0:n], scalar1=inv[:, col:col + 1], scalar2=off,
                                                                      op0=ALU.mult, op1=ALU.add),
             reads=[(scf, None), (inv, None)], writes=[(dst, None)])
        p.op("dve", lambda e, dst=dst: e.tensor_copy(out=sci[:, 0:n], in_=dst[:, 0:n]), reads=[(dst, None)], writes=[(sci, None)])
        p.op("pool", lambda e, dst=dst: e.tensor_tensor(out=dst[:, 0:n], in0=dst[:, 0:n], in1=sci[:, 0:n], op=ALU.subtract),
             reads=[(dst, None), (sci, None)], writes=[(dst, None)])
        for _ in range(2):
            p.op("dve", lambda e, dst=dst: e.scalar_tensor_tensor(out=dst[:, 0:n], in0=dst[:, 0:n], scalar=0.5, in1=dst[:, 0:n], op0=ALU.is_gt, op1=ALU.subtract),
                 reads=[(dst, None)], writes=[(dst, None)])
        p.op("act", lambda e, dst=dst: e.activation(out=dst[:, 0:n], in_=dst[:, 0:n], func=AF.Sin, scale=2.0 * math.pi), reads=[(dst, None)], writes=[(dst, None)])


def phase_B_even(cx, hT_src, W, oT_ap):
    p = cx.p
    nc = cx.nc
    with ExitStack() as es:
        cx.uid += 1
        uu = cx.uid

        def mk_sb(stack):
            def sb(name, shape, dt):
                return Buf(stack.enter_context(nc.sbuf_tensor(f"{name}_{uu}", list(shape), dt)), name)
            return sb
        sb = mk_sb(es)
        ps, pst = psum_set(cx, es, 7, True)
        L = {}
        cmask = sb("cmask", [128, 8, 512], BF16)
        p.dma("sp", cmask[:], W["cmask"].rearrange("p (j n) -> p j n", j=8), writes=[(cmask, None)])
        cx.ropeinv = sb("ropeinv", [128, 2], F32)
        p.dma("sp", cx.ropeinv[:], W["ropeinv"], writes=[(cx.ropeinv, None)])
        ones_f = sb("ones_f", [128, 128], F32)
        p.op("pool", lambda e: e.memset(ones_f[:], 1.0), writes=[(ones_f, None)])
        pts = [sb(f"pt{i}", [128, 512], BF16) for i in range(6)]
        rs = sb("rs", [128, 512], F32)
        fac = sb("fac", [128, 512], F32)
        obt = [sb(f"obt{i}", [128, 512], BF16) for i in range(2)]

        def load_hT_block(hTb, tok0):
            for c in range(8):
                hT_src(hTb[:, c, :], c, tok0, TBK, "sp" if c % 2 == 0 else "pool", [(hTb, None)])

        with ExitStack() as esf:
            sbf = mk_sb(esf)
            fqT = sbf("fqT", [128, 2, T], BF16)
            fkT = sbf("fkT", [128, 2, T], BF16)
            fvv = sbf("fvv", [128, 32, 4, 128], BF16)
            FB = sbf("FB", [128, 4, 32, 8], F32)
            ncum = sbf("ncum", [128, 128], F32)
            p.op("dve", lambda e: e.memset(fvv[:], 1.0), writes=[(fvv, None)])
            with ExitStack() as esp:
                sbp = mk_sb(esp)
                hTb = sbp("hTb", [128, 8, TBK], BF16)
                wf = sbp("wf", [128, 8, 768], BF16)
                wfl = sbp("wfl", [128, 8, 4], BF16)
                stage = [sbp(f"stage{i}", [128, 2048], F32) for i in range(2)]
                tri = sbp("tri", [128, 128], F32)
                sel127 = sbp("sel127", [128, 128], F32)
                fbB = sbp("fbB", [128, 128], F32)
                nlf = sbp("nlf", [128, 128], F32)
                tot = sbp("tot", [128, 128], F32)
                inc = sbp("inc", [128, 128], F32)
                refsb = sbp("refsb", [128, 128], F32)
                p.dma("sp", tri[:], W["tri"], writes=[(tri, None)])
                p.dma("sp", sel127[:], W["sel127"], writes=[(sel127, None)])
                p.dma("sp", fbB[:], W["fbias_rep"].to_broadcast([128, 128]), writes=[(fbB, None)])
                for i in range(3):
                    load_w_bf16(cx, wf[:, :, i * 256:(i + 1) * 256], (wf, None), W["w_in"][:, i * 256:(i + 1) * 256], 8, 256, stage[i % 2], q="pool")
                load_w_bf16(cx, wfl[:], (wfl, None), W["w_in"][:, EV["fl"]:EV["fl"] + 4], 8, 4, stage[1], q="pool")
                FLP = ps[6]
                for blk in range(T // TBK):
                    tok0 = blk * TBK
                    load_hT_block(hTb, tok0)
                    for (dst, cb) in ((fqT, 0), (fkT, 256)):
                        for hh in range(2):
                            for tb in range(TBK // 512):
                                b = ps[(hh * 2 + tb) % 4]
                                mm_group(cx, b[:], (b, None), [(wf[:, c, cb + hh * 128:cb + (hh + 1) * 128], hTb[:, c, tb * 512:(tb + 1) * 512], [(wf, None), (hTb, None)]) for c in range(8)])
                                gtb = (tok0 + tb * 512) // 512
                                if tb % 2 == 0:
                                    p.op("act", lambda e, b=b, dst=dst, hh=hh, gtb=gtb: e.activation(out=dst[:, hh, gtb * 512:(gtb + 1) * 512], in_=b[:], func=AF.Copy),
                                         reads=[(b, None)], writes=[(dst, (hh, gtb))])
                                else:
                                    p.op("dve", lambda e, b=b, dst=dst, hh=hh, gtb=gtb: e.tensor_copy(out=dst[:, hh, gtb * 512:(gtb + 1) * 512], in_=b[:]),
                                         reads=[(b, None)], writes=[(dst, (hh, gtb))])
                    for tl in range(TBK // 128):
                        tt = tok0 // 128 + tl
                        b = ps[4 + tl % 2]
                        mm_group(cx, b[:, 0:256], (b, None), [(hTb[:, c, tl * 128:(tl + 1) * 128], wf[:, c, 512:768], [(wf, None), (hTb, None)]) for c in range(8)])
                        p.op("dve", lambda e, b=b, tt=tt: e.tensor_copy(out=fvv[:, tt, :, 0:64], in_=b[:, 0:256].rearrange("p (h d) -> p h d", h=4)),
                             reads=[(b, None)], writes=[(fvv, tt)])
                        mm_group(cx, FLP[:, tt * 4:(tt + 1) * 4], (FLP, None), [(hTb[:, c, tl * 128:(tl + 1) * 128], wfl[:, c, :], [(wfl, None), (hTb, None)]) for c in range(8)])
                p.op("dve", lambda e: e.tensor_tensor(out=nlf[:], in0=FLP[:, 0:128], in1=fbB[:], op=ALU.add), reads=[(FLP, None), (fbB, None)], writes=[(nlf, None)])
                p.op("act", lambda e: e.activation(out=nlf[:], in_=nlf[:], func=AF.Exp, scale=-1.0), reads=[(nlf, None)], writes=[(nlf, None)])
                p.op("act", lambda e: e.activation(out=nlf[:], in_=nlf[:], func=AF.Ln, bias=1.0), reads=[(nlf, None)], writes=[(nlf, None)])
                W1b, TOTb, REFb = ps[0], ps[1], ps[2]
                p.op("pe", lambda e: e.matmul(W1b[:, 0:128], lhsT=tri[:], rhs=nlf[:], start=True, stop=True), reads=[(tri, None), (nlf, None)], writes=[(W1b, None)])
                p.op("pe", lambda e: e.matmul(TOTb[:, 0:128], lhsT=ones_f[:], rhs=nlf[:], start=True, stop=True), reads=[(ones_f, None), (nlf, None)], writes=[(TOTb, None)])
                p.op("dve", lambda e: e.tensor_copy(out=tot[:], in_=TOTb[:, 0:128]), reads=[(TOTb, None)], writes=[(tot, None)])
                for j in range(4):
                    tv = tot[:].rearrange("p (t h) -> p t h", h=4)[:, :, j]
                    iv = inc[:].rearrange("p (t h) -> p t h", h=4)[:, :, j]
                    ov = ones_f[:, 0:32]
                    p.op("dve", lambda e, tv=tv, iv=iv, ov=ov: e.tensor_tensor_scan(out=iv, data0=ov, data1=tv, initial=0.0, op0=ALU.mult, op1=ALU.add),
                         reads=[(tot, None), (ones_f, None)], writes=[(inc, None)])
                p.op("dve", lambda e: e.tensor_tensor(out=inc[:], in0=inc[:], in1=tot[:], op=ALU.subtract), reads=[(inc, None), (tot, None)], writes=[(inc, None)])
                p.op("dve", lambda e: e.tensor_tensor(out=ncum[:], in0=W1b[:, 0:128], in1=inc[:], op=ALU.add), reads=[(W1b, None), (inc, None)], writes=[(ncum, None)])
                p.op("pe", lambda e: e.matmul(REFb[:, 0:128], lhsT=sel127[:], rhs=ncum[:], start=True, stop=True), reads=[(sel127, None), (ncum, None)], writes=[(REFb, None)])
                p.op("dve", lambda e: e.tensor_copy(out=refsb[:], in_=REFb[:, 0:128]), reads=[(REFb, None)], writes=[(refsb, None)])
                ncv = ncum[:].rearrange("p (t h) -> p t h", h=4)
                for j in range(4):
                    for qb in range(8):
                        col = (4 * qb + 3) * 4 + j
                        p.op("dve", lambda e, j=j, qb=qb, col=col: e.tensor_scalar(out=FB[:, j, :, qb], in0=ncv[:, :, j], scalar1=refsb[:, col:col + 1], scalar2=None, op0=ALU.subtract),
                             reads=[(ncum, None), (refsb, None)], writes=[(FB, None)])
                p.barrier()
            for head in range(4):
                hh, r0 = head // 2, (head % 2) * 64
                for qb in range(8):
                    kbs = list(range(4 * qb + 4))
                    Ob = ps[4 + (qb % 2)]
                    stream = dict(
                        k=lambda kb: (fkT[r0:r0 + 64, hh, kb * 128:(kb + 1) * 128], [(fkT, (hh, kb // 4))]),
                        q=(fqT[r0:r0 + 64, hh, qb * 512:(qb + 1) * 512], [(fqT, (hh, qb))]),
                        scale=0.125,
                        bias=lambda kb: (FB[:, head, kb, qb:qb + 1], [(FB, None)]),
                        masks=lambda kb: ([(cmask[:, kb - 4 * qb, :], [(cmask, None)])] if kb >= 4 * qb else []),
                        pv=[(Ob[:], lambda kb: (fvv[:, kb, head, :], [(fvv, kb)]))],
                        pv_dep=(Ob, None))
                    run_streams(cx, [stream], kbs, ps[0:4], pts, L)
                    ot = obt[qb % 2]
                    p.op("dve", lambda e, Ob=Ob: e.reciprocal(out=rs[64:128, :], in_=Ob[64:128, :]), reads=[(Ob, None)], writes=[(rs, None)])
                    p.op("dve", lambda e, Ob=Ob, ot=ot: e.tensor_tensor(out=ot[0:64, :], in0=Ob[0:64, :], in1=rs[64:128, :], op=ALU.mult),
                         reads=[(Ob, None), (rs, None)], writes=[(ot, None)])
                    p.dma("sp", oT_ap[head * 64:(head + 1) * 64, qb * 512:(qb + 1) * 512], ot[0:64, :], reads=[(ot, None)], writes=[(cx.outb, ("oTf", head, qb))])
            p.barrier()
        nsa_part(cx, W, oT_ap, hT_src, load_hT_block, mk_sb, es, ps, pst, pts, cmask, ones_f, rs, fac, obt, L)
        p.barrier()


NWC = dict(nq=0, nqs=256, ks=512, kss=640, kw=768, kws=896, kc=1024, kcs=1152, vc=1280, vsw=1344, gl=1472)
NW = 1484


def gelu_tanh(cx, src_bank_ap, src_dep, bias_col, bias_dep, out_ap, out_dep, tmpa, tmpb, n):
    p = cx.p
    p.op("act", lambda e: e.activation(out=tmpa[:, 0:n], in_=src_bank_ap, func=AF.Identity, bias=bias_col), reads=[src_dep, bias_dep], writes=[(tmpa, None)])
    p.op("dve", lambda e: e.tensor_tensor(out=tmpb[:, 0:n], in0=tmpa[:, 0:n], in1=tmpa[:, 0:n], op=ALU.mult), reads=[(tmpa, None)], writes=[(tmpb, None)])
    p.op("dve", lambda e: e.tensor_scalar(out=tmpb[:, 0:n], in0=tmpb[:, 0:n], scalar1=0.044715, scalar2=1.0, op0=ALU.mult, op1=ALU.add), reads=[(tmpb, None)], writes=[(tmpb, None)])
    p.op("dve", lambda e: e.tensor_tensor(out=tmpb[:, 0:n], in0=tmpb[:, 0:n], in1=tmpa[:, 0:n], op=ALU.mult), reads=[(tmpa, None), (tmpb, None)], writes=[(tmpb, None)])
    p.op("act", lambda e: e.activation(out=tmpb[:, 0:n], in_=tmpb[:, 0:n], func=AF.Tanh, scale=0.7978845608028654), reads=[(tmpb, None)], writes=[(tmpb, None)])
    p.op("dve", lambda e: e.scalar_tensor_tensor(out=tmpb[:, 0:n], in0=tmpb[:, 0:n], scalar=1.0, in1=tmpa[:, 0:n], op0=ALU.add, op1=ALU.mult),
         reads=[(tmpa, None), (tmpb, None)], writes=[(tmpb, None)])
    p.op("act", lambda e: e.activation(out=out_ap, in_=tmpb[:, 0:n], func=AF.Copy, scale=0.5), reads=[(tmpb, None)], writes=[out_dep])


def nsa_part(cx, W, oT_ap, hT_src, load_hT_block, mk_sb, es, ps, pst, pts, cmask, ones_f, rs, fac, obt, L):
    p = cx.p
    sb = mk_sb(es)
    QA = sb("QA", [128, 4, T], BF16)
    KSA = sb("KSA", [128, T], BF16)
    KSB = sb("KSB", [128, T], BF16)
    KW2 = sb("KW2", [128, T], BF16)
    VS = sb("VS", [128, 32, 128], BF16)
    VW = sb("VW", [128, 32, 128], BF16)
    GLT = sb("GLT", [12, T], BF16)
    KCMP = sb("KCMP", [128, 256], BF16)
    VCMP = sb("VCMP", [128, 2, 128], BF16)
    cmpmask = sb("cmpmask", [128, 5, 512], BF16)
    ovl = sb("ovl", [128, 2, 65], BF16)
    sel12 = sb("sel12", [12, 12 * 128], BF16)
    tkadd = sb("tkadd", [128, 128], F32)
    tkmul = sb("tkmul", [128, 128], F32)
    p.dma("sp", cmpmask[:], W["cmpmask"].rearrange("p (j n) -> p j n", j=5), writes=[(cmpmask, None)])
    p.dma("sp", ovl[:], W["ovl"].rearrange("p (c n) -> p c n", c=2), writes=[(ovl, None)])
    p.dma("sp", sel12[:], W["sel12"], writes=[(sel12, None)])
    p.dma("sp", tkadd[:], W["tkadd"], writes=[(tkadd, None)])
    p.dma("sp", tkmul[:], W["tkmul"], writes=[(tkmul, None)])
    for b_ in (VS, VW, VCMP):
        p.op("dve", lambda e, b_=b_: e.memset(b_[:], 1.0), writes=[(b_, None)])
    with ExitStack() as esp:
        sbp = mk_sb(esp)
        hTb = sbp("hTbn", [128, 8, TBK], BF16)
        wn = sbp("wn", [128, 8, NW], BF16)
        stage = [sbp(f"stagen{i}", [128, 2048], F32) for i in range(2)]
        Ct = sbp("Ctb", [128, TBK], F32)
        St = sbp("Stb", [128, TBK], F32)
        sci = sbp("scib", [128, TBK], I32)
        scf = sbp("scfb", [128, TBK], F32)
        L["rt1"] = sbp("rt1n", [128, 512], F32)
        L["rt2"] = sbp("rt2n", [128, 512], F32)
        KC2 = sbp("KC2", [128, T], BF16)
        VC = sbp("VC", [128, T], BF16)
        W1 = sbp("W1c", [64, 32, 128], BF16)
        posT = sbp("posT", [64, 32], BF16)
        posTf = sbp("posTf", [64, 32], F32)
        W2d = sbp("W2d", [128, 128], BF16)
        posb = sbp("posb", [128, 1], F32)
        HID = sbp("HID", [128, 256], BF16)
        ga = sbp("ga", [128, 256], F32)
        gb = sbp("gb", [128, 256], F32)
        sti = [0]

        def stg():
            sti[0] += 1
            return stage[sti[0] % 2]
        E = EV
        load_w_bf16(cx, wn[:, :, 0:256], (wn, None), W["w_in"][:, E["nq"]:E["nq"] + 256], 8, 256, stg(), q="pool")
        load_w_bf16(cx, wn[:, :, 256:512], (wn, None), W["w_in"][:, E["nqs"]:E["nqs"] + 256], 8, 256, stg(), q="pool")
        for (src, dstc, dup) in ((E["ks"], NWC["ks"], True), (E["kss"], NWC["kss"], True), (E["kw"], NWC["kw"], True), (E["kws"], NWC["kws"], True),
                                 (E["kc"], NWC["kc"], True), (E["kcs"], NWC["kcs"], True), (E["vc"], NWC["vc"], False)):
            st = stg()
            sv = st[:, 0:8 * 64].rearrange("p (c n) -> p c n", c=8)
            p.dma("pool", sv, W["w_in"][:, src:src + 64].rearrange("(c p) n -> p c n", p=128), writes=[(st, None)])
            p.op("pool", lambda e, sv=sv, dstc=dstc: e.tensor_copy(out=wn[:, :, dstc:dstc + 64], in_=sv), reads=[(st, None)], writes=[(wn, None)])
            if dup:
                p.op("pool", lambda e, sv=sv, dstc=dstc: e.tensor_copy(out=wn[:, :, dstc + 64:dstc + 128], in_=sv), reads=[(st, None)], writes=[(wn, None)])
        load_w_bf16(cx, wn[:, :, NWC["vsw"]:NWC["vsw"] + 128], (wn, None), W["w_in"][:, E["vs"]:E["vs"] + 128], 8, 128, stg(), q="pool")
        load_w_bf16(cx, wn[:, :, NWC["gl"]:NWC["gl"] + 12], (wn, None), W["w_in"][:, E["gl"]:E["gl"] + 12], 8, 12, stg(), q="pool")
        for blk in range(T // TBK):
            tok0 = blk * TBK
            load_hT_block(hTb, tok0)
            build_rope_block(cx, W["pos"], tok0, TBK, Ct, St, sci, scf)
            for tb in range(TBK // 512):
                g0 = tok0 + tb * 512
                gtb = g0 // 512
                lsl = slice(tb * 512, (tb + 1) * 512)
                for i in range(2):
                    bA, bB = ps[2 * i], ps[2 * i + 1]
                    mm_group(cx, bA[:], (bA, None), [(wn[:, c, i * 128:(i + 1) * 128], hTb[:, c, lsl], [(wn, None), (hTb, None)]) for c in range(8)])
                    mm_group(cx, bB[:], (bB, None), [(wn[:, c, 256 + i * 128:256 + (i + 1) * 128], hTb[:, c, lsl], [(wn, None), (hTb, None)]) for c in range(8)])
                    t1, t2 = L["rt1"], L["rt2"]
                    p.op("dve", lambda e, bA=bA: e.tensor_tensor(out=t1[:], in0=bA[:], in1=Ct[:, lsl], op=ALU.mult), reads=[(bA, None), (Ct, None)], writes=[(t1, None)])
                    p.op("dve", lambda e, bB=bB: e.tensor_tensor(out=t2[:], in0=bB[:], in1=St[:, lsl], op=ALU.mult), reads=[(bB, None), (St, None)], writes=[(t2, None)])
                    p.op("dve", lambda e, i=i, g0=g0: e.tensor_tensor(out=QA[0:64, 2 * i, g0:g0 + 512], in0=t1[0:64, :], in1=t2[0:64, :], op=ALU.add),
                         reads=[(t1, None), (t2, None)], writes=[(QA, (2 * i, gtb))])
                    p.op("dve", lambda e, i=i, g0=g0: e.tensor_tensor(out=QA[64:128, 2 * i + 1, g0:g0 + 512], in0=t1[64:128, :], in1=t2[64:128, :], op=ALU.add),
                         reads=[(t1, None), (t2, None)], writes=[(QA, (2 * i + 1, gtb))])
                for ui, (dst, cb, cbs) in enumerate(((KSA, NWC["ks"], NWC["kss"]), (KW2, NWC["kw"], NWC["kws"]), (KC2, NWC["kc"], NWC["kcs"]))):
                    bA, bB = ps[(4 + 2 * ui) % 6], ps[(5 + 2 * ui) % 6]
                    mm_group(cx, bA[:], (bA, None), [(wn[:, c, cb:cb + 128], hTb[:, c, lsl], [(wn, None), (hTb, None)]) for c in range(8)])
                    mm_group(cx, bB[:], (bB, None), [(wn[:, c, cbs:cbs + 128], hTb[:, c, lsl], [(wn, None), (hTb, None)]) for c in range(8)])
                    t1, t2 = L["rt1"], L["rt2"]
                    p.op("dve", lambda e, bA=bA: e.tensor_tensor(out=t1[:], in0=bA[:], in1=Ct[:, lsl], op=ALU.mult), reads=[(bA, None), (Ct, None)], writes=[(t1, None)])
                    p.op("dve", lambda e, bB=bB: e.tensor_tensor(out=t2[:], in0=bB[:], in1=St[:, lsl], op=ALU.mult), reads=[(bB, None), (St, None)], writes=[(t2, None)])
                    p.op("dve", lambda e, dst=dst, g0=g0: e.tensor_tensor(out=dst[:, g0:g0 + 512], in0=t1[:], in1=t2[:], op=ALU.add),
                         reads=[(t1, None), (t2, None)], writes=[(dst, gtb)])
                b = ps[6]
                mm_group(cx, b[0:64, :], (b, None), [(wn[:, c, NWC["vc"]:NWC["vc"] + 64], hTb[:, c, lsl], [(wn, None), (hTb, None)]) for c in range(8)])
                p.op("act", lambda e, b=b, g0=g0: e.activation(out=VC[0:64, g0:g0 + 512], in_=b[0:64, :], func=AF.Copy), reads=[(b, None)], writes=[(VC, gtb)])
                b = ps[4 + tb % 2]
                mm_group(cx, b[0:12, :], (b, None), [(wn[:, c, NWC["gl"]:NWC["gl"] + 12], hTb[:, c, lsl], [(wn, None), (hTb, None)]) for c in range(8)])
                p.op("act", lambda e, b=b, g0=g0: e.activation(out=GLT[0:12, g0:g0 + 512], in_=b[0:12, :], func=AF.Sigmoid), reads=[(b, None)], writes=[(GLT, gtb)])
            for tl in range(TBK // 128):
                tt = tok0 // 128 + tl
                b = ps[(tl % 2)]
                mm_group(cx, b[:, 0:128], (b, None), [(hTb[:, c, tl * 128:(tl + 1) * 128], wn[:, c, NWC["vsw"]:NWC["vsw"] + 128], [(wn, None), (hTb, None)]) for c in range(8)])
                p.op("dve", lambda e, b=b, tt=tt: e.tensor_copy(out=VS[:, tt, 0:64], in_=b[:, 0:64]), reads=[(b, None)], writes=[(VS, tt)])
                p.op("act", lambda e, b=b, tt=tt: e.activation(out=VW[:, tt, 0:64], in_=b[:, 64:128], func=AF.Copy), reads=[(b, None)], writes=[(VW, tt)])
        p.op("pool", lambda e: e.tensor_copy(out=KSB[:], in_=KSA[:]), reads=[(KSA, None)], writes=[(KSB, None)])
        p.dma("sp", KSA[64:128, :], W["onehot"], writes=[(KSA, None)])
        p.dma("sp", KSB[0:64, :], W["onehot"], writes=[(KSB, None)])
        for which in ("k", "v"):
            src = KC2 if which == "k" else VC
            for half in range(2):
                st = stg()
                sv = st[0:64, :].rearrange("p (l h) -> p l h", l=16)
                p.dma("sp", sv, W["c1" + which][half * 1024:(half + 1) * 1024, :].rearrange("(l d) h -> d l h", d=64), writes=[(st, None)])
                p.op("pool", lambda e, sv=sv, half=half: e.tensor_copy(out=W1[:, half * 16:(half + 1) * 16, :], in_=sv), reads=[(st, None)], writes=[(W1, None)])
            p.dma("sp", posTf[:], W["cp" + which + "T"], writes=[(posTf, None)])
            p.op("pool", lambda e: e.tensor_copy(out=posT[:], in_=posTf[:]), reads=[(posTf, None)], writes=[(posT, None)])
            st = stg()
            p.dma("sp", st[:, 0:64], W["c2" + which], writes=[(st, None)])
            p.op("pool", lambda e, st=st: e.tensor_copy(out=W2d[:, 0:64], in_=st[:, 0:64]), reads=[(st, None)], writes=[(W2d, None)])
            p.op("pool", lambda e, st=st: e.tensor_copy(out=W2d[:, 64:128], in_=st[:, 0:64]), reads=[(st, None)], writes=[(W2d, None)])
            hb_, pb_ = ps[0], ps[1]
            srcv = src[0:64, :].rearrange("p (c s) -> p c s", s=16)
            mm_group(cx, hb_[:, 0:255], (hb_, None),
                     [(W1[:, l, :], srcv[:, (l // 16):(l // 16) + 255, l % 16], [(W1, None), (src, None)]) for l in range(32)])
            mm_group(cx, pb_[:, 0:1], (pb_, None), [(W1[:, l, :], posT[:, l:l + 1], [(W1, None), (posT, None)]) for l in range(32)])
            p.op("dve", lambda e: e.tensor_copy(out=posb[:], in_=pb_[:, 0:1]), reads=[(pb_, None)], writes=[(posb, None)])
            p.op("pool", lambda e: e.memset(HID[:], 0.0), writes=[(HID, None)])
            gelu_tanh(cx, hb_[:, 0:255], (hb_, None), posb[:, 0:1], (posb, None), HID[:, 0:255], (HID, None), ga, gb, 255)
            if which == "k":
                ob_ = ps[2]
                p.op("pe", lambda e: e.matmul(ob_[:, 0:256], lhsT=W2d[:], rhs=HID[:], start=True, stop=True), reads=[(W2d, None), (HID, None)], writes=[(ob_, None)])
                p.op("act", lambda e: e.activation(out=KCMP[:], in_=ob_[:, 0:256], func=AF.Copy), reads=[(ob_, None)], writes=[(KCMP, None)])
            else:
                for cc in range(2):
                    ob_ = ps[3 + cc]
                    p.op("pe", lambda e, cc=cc, ob_=ob_: e.matmul(ob_[:, 0:64], lhsT=HID[:, cc * 128:(cc + 1) * 128], rhs=W2d[:, 0:64], start=True, stop=True),
                         reads=[(W2d, None), (HID, None)], writes=[(ob_, None)])
                    p.op("act", lambda e, cc=cc, ob_=ob_: e.activation(out=VCMP[:, cc, 0:64], in_=ob_[:, 0:64], func=AF.Copy), reads=[(ob_, None)], writes=[(VCMP, None)])
        p.barrier()
    with ExitStack() as esa:
        sba = mk_sb(esa)
        OC = sba("OC", [128, 4, 512], F32)
        pcs = [sba(f"pc{i}", [128, 512], BF16) for i in range(2)]
        acc = sba("acc", [128, 4, 64], F32)
        sc = sba("sc", [128, 64], F32)
        sc2 = sba("sc2", [128, 64], F32)
        m8 = sba("m8", [128, 8], F32)
        m8b = sba("m8b", [128, 8], F32)
        rsi = sba("rsi", [128, 1], F32)
        NM = sba("NM", [128, 128], BF16)
        tacc = sba("tacc", [128, 512], F32)
        t2 = sba("t2", [128, 512], F32)
        sbanks = ps[0:3]
        Oacc = [ps[3], ps[4]]
        Gb, IMb = ps[5], ps[6]
        oi = [0]

        def next_O():
            oi[0] += 1
            return Oacc[oi[0] % 2]

        def gate_fac(Ob, n, j, qb, clamp):
            jj = 3 * n + j
            p.op("pe", lambda e: e.matmul(Gb[:], lhsT=sel12[0:12, jj * 128:(jj + 1) * 128], rhs=GLT[0:12, qb * 512:(qb + 1) * 512], start=True, stop=True),
                 reads=[(sel12, None), (GLT, qb)], writes=[(Gb, None)])
            if clamp:
                p.op("dve", lambda e: e.tensor_scalar(out=rs[64:128, :], in0=Ob[64:128, :], scalar1=1e-30, scalar2=None, op0=ALU.max), reads=[(Ob, None)], writes=[(rs, None)])
                p.op("dve", lambda e: e.reciprocal(out=rs[64:128, :], in_=rs[64:128, :]), reads=[(rs, None)], writes=[(rs, None)])
            else:
                p.op("dve", lambda e: e.reciprocal(out=rs[64:128, :], in_=Ob[64:128, :]), reads=[(Ob, None)], writes=[(rs, None)])
            p.op("dve", lambda e: e.tensor_tensor(out=fac[64:128, :], in0=Gb[64:128, :], in1=rs[64:128, :], op=ALU.mult), reads=[(Gb, None), (rs, None)], writes=[(fac, None)])

        for qb in range(8):
            chunks = [0] if qb < 4 else [0, 1]
            for n in range(4):
                r0 = (n % 2) * 64
                Ob = next_O()

                def cmask_fn(cc, qb=qb):
                    delta = 2048 * cc - 512 * qb
                    if delta <= -2560:
                        return []
                    return [(cmpmask[:, (delta + 2048) // 512, :], [(cmpmask, None)])]
                stream = dict(
                    k=lambda cc: (KCMP[r0:r0 + 64, cc * 128:(cc + 1) * 128], [(KCMP, None)]),
                    q=(QA[r0:r0 + 64, n, qb * 512:(qb + 1) * 512], [(QA, (n, qb))]),
                    scale=0.125, bias=None, masks=cmask_fn,
                    pv=[(Ob[:], lambda cc: (VCMP[:, cc, :], [(VCMP, None)]))], pv_dep=(Ob, None))
                keep = []
                run_streams(cx, [stream], chunks, sbanks, pcs, L, keep=keep)
                for t4 in range(4):
                    mm_group(cx, IMb[:, 0:65], (IMb, None),
                             [(pt[:, t4 * 128:(t4 + 1) * 128], ovl[:, cc, :], [(pt, None), (ovl, None)]) for (cc, pt) in keep])
                    p.op("dve", lambda e: e.tensor_scalar(out=rsi[:], in0=IMb[:, 64:65], scalar1=1e-30, scalar2=None, op0=ALU.max), reads=[(IMb, None)], writes=[(rsi, None)])
                    p.op("dve", lambda e: e.reciprocal(out=rsi[:], in_=rsi[:]), reads=[(rsi, None)], writes=[(rsi, None)])
                    if n == 0:
                        p.op("dve", lambda e, t4=t4: e.tensor_scalar(out=acc[:, t4, :], in0=IMb[:, 0:64], scalar1=rsi[:, 0:1], scalar2=None, op0=ALU.mult),
                             reads=[(IMb, None), (rsi, None)], writes=[(acc, t4)])
                    else:
                        p.op("dve", lambda e, t4=t4: e.scalar_tensor_tensor(out=acc[:, t4, :], in0=IMb[:, 0:64], scalar=rsi[:, 0:1], in1=acc[:, t4, :], op0=ALU.mult, op1=ALU.add),
                             reads=[(IMb, None), (rsi, None), (acc, t4)], writes=[(acc, t4)])
                gate_fac(Ob, n, 0, qb, True)
                p.op("dve", lambda e, n=n, Ob=Ob: e.tensor_tensor(out=OC[0:64, n, :], in0=Ob[0:64, :], in1=fac[64:128, :], op=ALU.mult),
                     reads=[(Ob, None), (fac, None)], writes=[(OC, n)])
            for t4 in range(4):
                qt = 4 * qb + t4
                o0 = 62 - 2 * qt
                p.op("dve", lambda e, t4=t4, o0=o0: e.tensor_tensor(out=sc[:], in0=acc[:, t4, :], in1=tkmul[:, o0:o0 + 64], op=ALU.mult), reads=[(acc, t4), (tkmul, None)], writes=[(sc, None)])
                p.op("dve", lambda e, o0=o0: e.tensor_tensor(out=sc[:], in0=sc[:], in1=tkadd[:, o0:o0 + 64], op=ALU.add), reads=[(sc, None), (tkadd, None)], writes=[(sc, None)])
                p.op("dve", lambda e: e.memset(sc[:, 0:1], 1e30), reads=[(sc, None)], writes=[(sc, None)])
                p.op("dve", lambda e: e.max(out=m8[:], in_=sc[:]), reads=[(sc, None)], writes=[(m8, None)])
                p.op("dve", lambda e: e.match_replace(out=sc2[:], in_to_replace=m8[:], in_values=sc[:], imm_value=-3.0e38), reads=[(sc, None), (m8, None)], writes=[(sc2, None)])
                p.op("dve", lambda e: e.max(out=m8b[:], in_=sc2[:]), reads=[(sc2, None)], writes=[(m8b, None)])
                p.op("dve", lambda e: e.tensor_scalar(out=sc2[:], in0=sc[:], scalar1=m8b[:, 7:8], scalar2=None, op0=ALU.is_ge), reads=[(sc, None), (m8b, None)], writes=[(sc2, None)])
                for hf in range(2):
                    p.op("dve", lambda e, hf=hf: e.tensor_scalar(out=NM[:, hf * 64:(hf + 1) * 64], in0=sc2[:], scalar1=-1.0, scalar2=-NEG, op0=ALU.add, op1=ALU.mult),
                         reads=[(sc2, None)], writes=[(NM, None)])
                p.op("pe", lambda e: e.transpose(out=pst[:, 0:128], in_=NM[:], identity=cx.ident[:]), reads=[(NM, None), (cx.ident, None)], writes=[(pst, None)])
                cols = slice(qt * 128, (qt + 1) * 128)
                for n in range(4):
                    rr = slice(64, 128) if n % 2 == 0 else slice(0, 64)
                    eng = "act" if n % 2 == 0 else "dve"
                    if eng == "act":
                        p.op("act", lambda e, n=n, rr=rr, cols=cols: e.activation(out=QA[rr, n, cols], in_=pst[rr, 0:128], func=AF.Copy), reads=[(pst, None)], writes=[(QA, (n, qb))])
                    else:
                        p.op("dve", lambda e, n=n, rr=rr, cols=cols: e.tensor_copy(out=QA[rr, n, cols], in_=pst[rr, 0:128]), reads=[(pst, None)], writes=[(QA, (n, qb))])
            for n in range(4):
                r0 = (n % 2) * 64
                KS = KSA if n % 2 == 0 else KSB
                Ob = next_O()
                stream = dict(
                    k=lambda kb: (KS[:, kb * 128:(kb + 1) * 128], [(KS, None)]),
                    q=(QA[:, n, qb * 512:(qb + 1) * 512], [(QA, (n, qb))]),
                    scale=0.125, bias=None,
                    masks=lambda kb: ([(cmask[:, kb - 4 * qb, :], [(cmask, None)])] if kb >= 4 * qb else []),
                    pv=[(Ob[:], lambda kb: (VS[:, kb, :], [(VS, kb)]))], pv_dep=(Ob, None))
                run_streams(cx, [stream], list(range(4 * qb + 4)), sbanks, pts, L)
                gate_fac(Ob, n, 1, qb, False)
                p.op("dve", lambda e, Ob=Ob: e.tensor_tensor(out=tacc[0:64, :], in0=Ob[0:64, :], in1=fac[64:128, :], op=ALU.mult), reads=[(Ob, None), (fac, None)], writes=[(tacc, None)])
                p.op("dve", lambda e, n=n: e.tensor_tensor(out=tacc[0:64, :], in0=tacc[0:64, :], in1=OC[0:64, n, :], op=ALU.add), reads=[(tacc, None), (OC, n)], writes=[(tacc, None)])
                Ob2 = next_O()

                def wmasks(kb, qb=qb):
                    if kb >= 4 * qb:
                        return [(cmask[:, kb - 4 * qb, :], [(cmask, None)])]
                    return [(cmask[:, 4 + kb - (4 * qb - 4), :], [(cmask, None)])]
                stream = dict(
                    k=lambda kb: (KW2[r0:r0 + 64, kb * 128:(kb + 1) * 128], [(KW2, kb // 4)]),
                    q=(QA[r0:r0 + 64, n, qb * 512:(qb + 1) * 512], [(QA, (n, qb))]),
                    scale=0.125, bias=None, masks=wmasks,
                    pv=[(Ob2[:], lambda kb: (VW[:, kb, :], [(VW, kb)]))], pv_dep=(Ob2, None))
                run_streams(cx, [stream], list(range(max(0, 4 * qb - 4), 4 * qb + 4)), sbanks, pts, L)
                gate_fac(Ob2, n, 2, qb, False)
                p.op("dve", lambda e, Ob2=Ob2: e.tensor_tensor(out=t2[0:64, :], in0=Ob2[0:64, :], in1=fac[64:128, :], op=ALU.mult), reads=[(Ob2, None), (fac, None)], writes=[(t2, None)])
                ot = obt[n % 2]
                p.op("dve", lambda e, ot=ot: e.tensor_tensor(out=ot[0:64, :], in0=tacc[0:64, :], in1=t2[0:64, :], op=ALU.add), reads=[(tacc, None), (t2, None)], writes=[(ot, None)])
                p.dma("sp", oT_ap[256 + n * 64:256 + (n + 1) * 64, qb * 512:(qb + 1) * 512], ot[0:64, :], reads=[(ot, None)], writes=[(cx.outb, ("oTn", n, qb))])
        p.barrier()


import numpy as np, math
import ml_dtypes
def np_bf16(a):
    return np.asarray(a, dtype=np.float32).astype(ml_dtypes.bfloat16)

def swap_cols(w):
    w = w.reshape(w.shape[0], -1, 64).copy()
    a = w[:, :, 0:8].copy(); w[:, :, 0:8] = w[:, :, 8:16]; w[:, :, 8:16] = a
    return w.reshape(w.shape[0], -1)

def odd_w_own(w_in, hh):
    q = w_in[:, 512 * hh:512 * hh + 512]; k = w_in[:, 1024 + 512 * hh:1024 + 512 * hh + 512]; v = w_in[:, 2048 + 512 * hh:2048 + 512 * hh + 512]
    return np.ascontiguousarray(np.concatenate([q, swap_cols(q), k, swap_cols(k), v], axis=1))

def even_w_own(w, hh):
    def c(o, n): return w[:, o:o + n]
    fq = c(256 * hh, 256); fk = c(512 + 256 * hh, 256); fv = c(1024 + 256 * hh, 256); fl = c(1536 + 4 * hh, 4)
    nq = c(1544 + 256 * hh, 256); kc = c(2056 + 64 * hh, 64); vc = c(2184 + 64 * hh, 64); ks = c(2312 + 64 * hh, 64)
    vs = c(2440 + 64 * hh, 64); kw = c(2568 + 64 * hh, 64); vw = c(2696 + 64 * hh, 64); gl = c(2824 + 12 * hh, 12)
    return np.ascontiguousarray(np.concatenate([fq, fk, fv, nq, swap_cols(nq), kc, swap_cols(kc), ks, swap_cols(ks), kw, swap_cols(kw), vc, vs, vw, fl, gl], axis=1))

def host_consts():
    c = {}
    c["ident"] = np_bf16(np.eye(128))
    k = np.arange(128)[:, None]; q = np.arange(512)[None, :]
    cm = np.stack([(128 * j + k <= q) for j in range(4)]).astype(np.float32)
    c["cmask"] = np_bf16(np.concatenate([cm, 1.0 - cm], axis=0).transpose(1, 0, 2).reshape(128, 8 * 512))
    inv = (500000.0 ** (-np.arange(0, 16, 2, dtype=np.float64) / 16)) / (2 * np.pi)
    r = np.zeros((128, 2), np.float32)
    for p_ in range(128):
        j = p_ % 64
        if j < 8: r[p_, 0] = -inv[j]; r[p_, 1] = inv[j]
        elif j < 16: r[p_, 0] = inv[j - 8]; r[p_, 1] = inv[j - 8]
    c["ropeinv"] = r
    cmp = np.stack([(16 * k + 31 + (-2048 + 512 * i) <= q) for i in range(5)]).astype(np.float32)
    c["cmpmask"] = np_bf16(cmp.transpose(1, 0, 2).reshape(128, 5 * 512))
    cs = np.arange(256)[:, None] * 16; ss = np.arange(64)[None, :] * 64
    ov = np.clip(np.minimum(cs + 32, ss + 64) - np.maximum(cs, ss), 0, None) / 32.0
    ov[255] = 0
    ovl = np.concatenate([ov, np.ones((256, 1))], axis=1).reshape(2, 128, 65).transpose(1, 0, 2).reshape(128, 130)
    c["ovl"] = np_bf16(ovl)
    sel = np.zeros((12, 12, 128), np.float32)
    for j in range(12): sel[j, j, :] = 1
    c["sel12"] = np_bf16(sel.reshape(12, 12 * 128))
    add = np.zeros((128, 128), np.float32); mul = np.zeros((128, 128), np.float32)
    for p_ in range(128):
        cur = 1 if p_ >= 64 else 0
        for i in range(128):
            s_ = i - 62
            valid = s_ <= cur
            forced = (s_ == cur) or (s_ == cur - 1)
            if not valid: add[p_, i] = -1e30
            elif forced: add[p_, i] = 1e30
            else: mul[p_, i] = 1.0
    c["tkadd"] = add; c["tkmul"] = mul
    c["onehot"] = np_bf16((np.arange(4096)[None, :] // 64 == np.arange(64)[:, None]).astype(np.float32))
    c["tri"] = (np.arange(128)[:, None] <= np.arange(128)[None, :]).astype(np.float32)
    s127 = np.zeros((128, 128), np.float32); s127[127, :] = 1
    c["sel127"] = s127
    return c


from concourse.bass_utils import run_bass_kernel_spmd

CONST_SPECS = {"cmask": ([128, 4096], BF16), "ropeinv": ([128, 2], F32), "cmpmask": ([128, 2560], BF16), "ovl": ([128, 130], BF16), "sel12": ([12, 1536], BF16),
               "tkadd": ([128, 128], F32), "tkmul": ([128, 128], F32), "onehot": ([64, 4096], BF16), "tri": ([128, 128], F32), "sel127": ([128, 128], F32)}
GROUPS = [[0, 1], [2, 3], [4, 5], [6, 7]]
_PROG = {}
DEPTH = 4


class OTMap:
    def __init__(self, ap_a, ap_b):
        self.aps = (ap_a, ap_b)

    def __getitem__(self, idx):
        rows, cols = idx
        qb = cols.start // 512
        half, c0 = qb // 4, (qb % 4) * 512
        k = rows.start // 256
        assert (rows.stop - 1) // 256 == k
        r0, r1 = rows.start - 256 * k, rows.stop - 256 * k
        return self.aps[k][half * 256 + r0:half * 256 + r1, c0:c0 + 512]


def build_fused():
    nc = bass.Bass("TRN2", target_bir_lowering=False)

    def din(name, shape, dt=F32):
        return nc.dram_tensor(name, list(shape), dt, kind="ExternalInput").ap()
    ident = din("ident", [128, 128], BF16)
    x_in = din("x_in", [TOK, D])
    C = {k: din(k, s, dt) for k, (s, dt) in CONST_SPECS.items()}
    pos = din("pos", [1, T], I32)
    mem = din("mem", [MEM, D])
    g_all = din("g_all", [DEPTH * 6, D])
    mem_g = din("mem_g", [DEPTH, D])
    WL = []
    for l in range(DEPTH):
        W = {"g": g_all[l * 6:(l + 1) * 6, :], "mem": mem, "mem_g": mem_g[l:l + 1, :], "pos": pos}
        W.update(C)
        for nm, shp in (("w_out", [D, D]), ("ca_wq", [D, 256]), ("ca_wk", [D, 256]), ("ca_wv", [D, 256]), ("ca_wo", [256, D]),
                        ("ffn_wg", [D, DFF]), ("ffn_wu", [D, DFF]), ("ffn_wd", [DFF, D])):
            W[nm] = din(f"{nm}_{l}", shp)
        if l % 2 == 0:
            for nm, shp in (("w_in", [D, EV_NCOL]), ("fbias_rep", [1, 128]), ("c1k", [2048, 128]), ("c2k", [128, 64]), ("cpkT", [64, 32]),
                            ("c1v", [2048, 128]), ("c2v", [128, 64]), ("cpvT", [64, 32])):
                W[nm] = din(f"{nm}_{l}", shp)
        else:
            for nm, shp in (("w_in", [D, 2560]), ("lam", [1, 256]), ("subg", [128, 1]), ("laminit", [1, 2])):
                W[nm] = din(f"{nm}_{l}", shp)
        WL.append(W)
    x_out = nc.dram_tensor("x_out", [TOK, D], F32, kind="ExternalOutput").ap()
    hT_own_t = [nc.dram_tensor(f"hT_own{k}", [512, TOK], BF16) for k in range(2)]
    hT_g_t = [nc.dram_tensor(f"hT_g{k}", [1024, TOK], BF16) for k in range(2)]
    oT_own_t = [nc.dram_tensor(f"oT_own{k}", [512, TOK], BF16) for k in range(2)]
    oT_g_t = [nc.dram_tensor(f"oT_g{k}", [1024, TOK], BF16) for k in range(2)]
    x_scr_t = nc.dram_tensor("x_scr", [TOK, D], F32)
    wscr = {nm: nc.dram_tensor(f"scr_{nm}", [NFF, 128, 1024], BF16).ap() for nm in ("ffn_wg", "ffn_wu", "ffn_wd")}
    x_scr = x_scr_t.ap()
    HTO, HTG, OTO, OTG, XS = Buf(None, "hT_own"), Buf(None, "hT_g"), Buf(None, "oT_own"), Buf(None, "oT_g"), Buf(None, "x_scr")
    cx = make_ctx(nc, ident)
    p = cx.p
    cx.wsc = Buf(None, "wscr")
    cx.outb = HTO
    pid = nc.sync.partition_id()
    hh256 = (pid % 2) * 256

    def gather_hT():
        for k in range(2):
            p.collective("AllGather", hT_own_t[k].ap().opt(), hT_g_t[k].ap().opt(), GROUPS, reads=[(HTO, None)], writes=[(HTG, k)])

    def gather_oT():
        for k in range(2):
            p.collective("AllGather", oT_own_t[k].ap().opt(), oT_g_t[k].ap().opt(), GROUPS, reads=[(OTO, None)], writes=[(OTG, k)])

    def hT_store(hT, tb):
        for k in range(2):
            p.dma("sp", hT_own_t[k].ap()[:, tb * TB:(tb + 1) * TB].rearrange("(c p) n -> p c n", p=128), hT[:, 4 * k:4 * k + 4, :],
                  reads=[(hT, None)], writes=[(HTO, (k, tb))])

    def hT_chunk(r, c):
        return hT_g_t[c // 4].ap()[r * 512 + (c % 4) * 128:r * 512 + (c % 4 + 1) * 128, :]

    def x_view(ap):
        return ap.rearrange("(t p) d -> p t d", p=128)

    with ExitStack() as es:
        def sb(name, shape, dt):
            return Buf(es.enter_context(nc.sbuf_tensor(name + "_a0", list(shape), dt)), name)
        x = sb("x", [128, NT, D], F32)
        p.dma("sp", x[:], x_view(x_in), writes=[(x, None)])
        cx.ps, cx.pst = psum_set(cx, es, 1, True)
        L = {"gB": sb("gB", [128, D], F32), "stat": sb("stat", [128, 16], F32), "junk": sb("junk", [128, D], BF16),
             "hb": [sb(f"hb{i}", [128, D], BF16) for i in range(2)]}
        hT = sb("hT", [128, 8, TOK], BF16)
        norm_transpose(cx, x, list(range(NT)), g_all[0:1, :], hT, L)
        for k in range(2):
            p.dma("sp", hT_own_t[k].ap().rearrange("(c p) n -> p c n", p=128), hT[:, 4 * k:4 * k + 4, :], reads=[(hT, None)], writes=[(HTO, (k, 0))])
        p.barrier()
    gather_hT()

    for l in range(DEPTH):
        W = WL[l]
        cx.outb = OTO
        if l % 2 == 0:
            def hT_src(dst_ap, c, tok0, n, q, writes):
                r = tok0 // TOK
                p.dma(q, dst_ap, hT_chunk(r, c)[:, tok0 % TOK:tok0 % TOK + n], reads=[(HTG, None)], writes=writes)
            phase_B_even(cx, hT_src, W, OTMap(oT_own_t[0].ap(), oT_own_t[1].ap()))
        else:
            def load_hT(hT):
                for c in range(8):
                    for r in range(2):
                        p.dma("sp" if c % 2 == 0 else "pool", hT[:, c, r * TOK:(r + 1) * TOK], hT_chunk(r, c),
                              reads=[(HTG, None)], writes=[(hT, None)])
            phase_B_odd(cx, load_hT, W, OTMap(oT_own_t[0].ap(), oT_own_t[1].ap()))
        p.barrier()
        gather_oT()
        cx.outb = HTO
        with ExitStack() as es:
            x = Buf(es.enter_context(nc.sbuf_tensor(f"x_l{l}", [128, NT, D], F32)), "x")
            p.dma("sp", x[:], x_view(x_in if l == 0 else x_scr), reads=[(XS, None)], writes=[(x, None)])

            def oT_load(oT, tb, writes):
                for r in range(2):
                    for k in range(2):
                        src = oT_g_t[k].ap()[bass.ds(hh256 + r * 512, 256), tb * TB:(tb + 1) * TB]
                        p.dma("sp", oT[:, 4 * r + 2 * k:4 * r + 2 * k + 2, :], src.rearrange("(c p) n -> p c n", p=128), reads=[(OTG, None)], writes=writes)
            last = (l == DEPTH - 1)
            phase_C(cx, x, None, W, None, None if last else g_all[(l + 1) * 6:(l + 1) * 6 + 1, :], oT_load=oT_load, hT_store=None if last else hT_store, wscr=wscr)
            if last:
                OUT = Buf(None, "x_out")
                p.dma("sp", x_view(x_out), x[:], reads=[(x, None)], writes=[(OUT, None)])
                p.finish([OUT])
            else:
                p.dma("sp", x_view(x_scr), x[:], reads=[(x, None)], writes=[(XS, None)])
                p.barrier()
        if not last:
            gather_hT()
    print("fused program: n_inst", p.n_inst, "n_wait", p.n_wait)
    p.close()
    return nc


def _ca(a):
    return np.ascontiguousarray(a)


def kernel(x, mem, positions, sandwich_g, mem_norm_g, ev_w_in, ev_fox_fbias,
           ev_cmp_pos_k, ev_cmp_w1_k, ev_cmp_w2_k, ev_cmp_pos_v, ev_cmp_w1_v, ev_cmp_w2_v,
           ev_w_out, od_w_in, od_lambda, od_subln_g, od_w_out,
           ca_wq, ca_wk, ca_wv, ca_wo, ffn_wg, ffn_wu, ffn_wd):
    f32 = lambda a: np.asarray(a, dtype=np.float32)
    x = f32(x); mem = f32(mem); positions = np.asarray(positions).astype(np.int32)
    sandwich_g = f32(sandwich_g); mem_norm_g = f32(mem_norm_g)
    hc = host_consts()
    if "nc" not in _PROG:
        _PROG["nc"] = build_fused()
    nc = _PROG["nc"]
    cores = list(range(8))
    shared = {k: hc[k] for k in CONST_SPECS}
    shared["ident"] = hc["ident"]
    shared["g_all"] = _ca(sandwich_g.reshape(DEPTH * 6, D))
    shared["mem_g"] = _ca(mem_norm_g)
    per_h = [dict(), dict()]
    for l in range(DEPTH):
        for nm, arr in (("ca_wq", ca_wq), ("ca_wk", ca_wk), ("ca_wv", ca_wv), ("ca_wo", ca_wo), ("ffn_wg", ffn_wg), ("ffn_wu", ffn_wu), ("ffn_wd", ffn_wd)):
            shared[f"{nm}_{l}"] = _ca(f32(arr[l]))
        if l % 2 == 0:
            e = l // 2
            wo = f32(ev_w_out[e])
            shared[f"w_out_{l}"] = _ca(np.concatenate([wo[0:256], wo[512:768], wo[256:512], wo[768:1024]], axis=0))
            shared[f"c1k_{l}"] = _ca(f32(ev_cmp_w1_k[e])); shared[f"c2k_{l}"] = _ca(f32(ev_cmp_w2_k[e])); shared[f"cpkT_{l}"] = _ca(f32(ev_cmp_pos_k[e]).T)
            shared[f"c1v_{l}"] = _ca(f32(ev_cmp_w1_v[e])); shared[f"c2v_{l}"] = _ca(f32(ev_cmp_w2_v[e])); shared[f"cpvT_{l}"] = _ca(f32(ev_cmp_pos_v[e]).T)
            for hh in range(2):
                per_h[hh][f"w_in_{l}"] = even_w_own(f32(ev_w_in[e]), hh)
                per_h[hh][f"fbias_rep_{l}"] = _ca(np.tile(f32(ev_fox_fbias[e])[4 * hh:4 * hh + 4], 32)[None, :])
        else:
            o = l // 2
            lam_init = 0.8 - 0.6 * math.exp(-0.3 * l)
            shared[f"w_out_{l}"] = _ca(f32(od_w_out[o]))
            shared[f"lam_{l}"] = _ca(f32(od_lambda[o]).reshape(1, 256))
            shared[f"subg_{l}"] = _ca(f32(od_subln_g[o]).reshape(128, 1))
            shared[f"laminit_{l}"] = np.array([[-lam_init, 1.0 - lam_init]], np.float32)
            for hh in range(2):
                per_h[hh][f"w_in_{l}"] = odd_w_own(f32(od_w_in[o]), hh)
    maps = []
    for c in cores:
        b, hh = c // 2, c % 2
        m = dict(shared)
        m.update(per_h[hh])
        m["x_in"] = _ca(x[b, TOK * hh:TOK * (hh + 1)])
        m["pos"] = _ca(positions[b:b + 1])
        m["mem"] = _ca(mem[b])
        maps.append(m)
    res = run_bass_kernel_spmd(nc, maps, core_ids=cores)
    out = np.zeros((4, T, D), np.float32)
    for c in cores:
        out[c // 2, TOK * (c % 2):TOK * (c % 2 + 1)] = res.results[c]["x_out"]
    return out
```
